# Optimizing a Trainium2 kernel written in Bass

```python
import math
import jax, jax.numpy as jnp
from jax import lax
import numpy as np

D_MODEL = 1024
BATCH = 4
SEQ = 8192
DEPTH = 4

CONV_CH = D_MODEL // 2
CONV_K = 3
SSM_WIDTH = D_MODEL // 2
SSM_GROUP = 16
SSM_GROUPS = SSM_WIDTH // SSM_GROUP
SSM_STATE = 64
DT_MIN = 1e-3
DT_MAX = 1e-1
N_HEADS = 16
HEAD_DIM = D_MODEL // N_HEADS
ROPE_THETA = 10000.0
DILATED_PATTERNS = ((128, 1), (512, 4), (2048, 16))
ATT_BLOCK = 128
N_EXPERTS = 32
TOP_K = 4
D_EXPERT = D_MODEL
SWIGLU_LIMIT = 7.0
SWIGLU_ALPHA = 1.702
MOE_BLOCK = 256
DN_ALPHA = (2 * DEPTH) ** 0.25
DN_BETA = (8 * DEPTH) ** -0.25
LN_EPS = 1e-5
N_EVEN = (DEPTH + 1) // 2
N_ODD = DEPTH // 2

kernel_name = "hybrid_conv_s5_dilated_moe_deepnorm"


def layer_norm(x, g, b):
    xf = x.astype(jnp.float32)
    mu = jnp.mean(xf, axis=-1, keepdims=True)
    xc = xf - mu
    var = jnp.mean(xc * xc, axis=-1, keepdims=True)
    return (xc * lax.rsqrt(var + LN_EPS) * g.astype(jnp.float32) + b.astype(jnp.float32)).astype(x.dtype)


def causal_depthwise_conv(v, w):
    c = v.shape[-1]
    return lax.conv_general_dilated(v, w[:, None, :].astype(v.dtype), window_strides=(1,),
                                    padding=[(CONV_K - 1, 0)],
                                    dimension_numbers=("NWC", "WIO", "NWC"),
                                    feature_group_count=c)


def s5_layer(u, a_re, a_im, log_dt, b_re, b_im, c_re, c_im, d_skip, w_glu, b_glu):
    bsz, L, _ = u.shape
    f32 = jnp.float32
    uf = u.astype(f32).reshape(bsz, L, SSM_GROUPS, SSM_GROUP)
    lam_re = jnp.minimum(a_re.astype(f32), -1e-4)
    lam_im = a_im.astype(f32)
    dt = jnp.exp(log_dt.astype(f32))[:, None]
    mag = jnp.exp(lam_re * dt)
    ab_re = mag * jnp.cos(lam_im * dt)
    ab_im = mag * jnp.sin(lam_im * dt)
    nr, ni = ab_re - 1.0, ab_im
    den = lam_re * lam_re + lam_im * lam_im
    coef_re = ((nr * lam_re + ni * lam_im) / den)[..., None]
    coef_im = ((ni * lam_re - nr * lam_im) / den)[..., None]
    br, bi = b_re.astype(f32), b_im.astype(f32)
    bb_re = coef_re * br - coef_im * bi
    bb_im = coef_re * bi + coef_im * br
    bu_re = jnp.einsum("blgh,gph->blgp", uf, bb_re)
    bu_im = jnp.einsum("blgh,gph->blgp", uf, bb_im)
    a_re_t = jnp.broadcast_to(ab_re, bu_re.shape)
    a_im_t = jnp.broadcast_to(ab_im, bu_re.shape)

    def combine(e1, e2):
        a1r, a1i, b1r, b1i = e1
        a2r, a2i, b2r, b2i = e2
        return (a2r * a1r - a2i * a1i, a2r * a1i + a2i * a1r,
                a2r * b1r - a2i * b1i + b2r, a2r * b1i + a2i * b1r + b2i)

    _, _, xr, xi = lax.associative_scan(combine, (a_re_t, a_im_t, bu_re, bu_im), axis=1)
    y = (jnp.einsum("blgp,ghp->blgh", xr, c_re.astype(f32))
         - jnp.einsum("blgp,ghp->blgh", xi, c_im.astype(f32))
         + d_skip.astype(f32) * uf)
    y = y.reshape(bsz, L, SSM_WIDTH)
    z = jax.nn.gelu(y)
    z = z * jax.nn.sigmoid(z @ w_glu.astype(f32) + b_glu.astype(f32))
    return z.astype(u.dtype)


def conv_ssm_mixer(x, w_in, conv_w, a_re, a_im, log_dt, b_re, b_im, c_re, c_im, d_skip,
                   w_glu, b_glu, w_out):
    proj = x @ w_in
    gate_b = proj[..., :CONV_CH]
    gate_c = proj[..., CONV_CH:2 * CONV_CH]
    h = proj[..., 2 * CONV_CH:3 * CONV_CH]
    u = proj[..., 3 * CONV_CH:]
    y_conv = gate_b * causal_depthwise_conv(gate_c * h, conv_w)
    y_ssm = s5_layer(u, a_re, a_im, log_dt, b_re, b_im, c_re, c_im, d_skip, w_glu, b_glu)
    return jnp.concatenate([y_conv, y_ssm], axis=-1) @ w_out


def rope(x):
    L = x.shape[1]
    half = HEAD_DIM // 2
    inv = ROPE_THETA ** (-jnp.arange(half, dtype=jnp.float32) / half)
    ang = jnp.arange(L, dtype=jnp.float32)[:, None] * inv[None, :]
    cos = jnp.cos(ang)[None, :, None, :]
    sin = jnp.sin(ang)[None, :, None, :]
    xf = x.astype(jnp.float32)
    x1, x2 = xf[..., :half], xf[..., half:]
    return jnp.concatenate([x1 * cos - x2 * sin, x2 * cos + x1 * sin], axis=-1).astype(x.dtype)


def dilated_window_attention(q, k, v, window, dilation):
    bsz, L, H, Dh = q.shape
    span = dilation * ATT_BLOCK
    Lp = -(-L // span) * span
    M = Lp // dilation
    nb = M // ATT_BLOCK
    reach = window // dilation

    def to_blocks(a):
        a = jnp.pad(a, ((0, 0), (0, Lp - L), (0, 0), (0, 0)))
        a = a.reshape(bsz, M, dilation, H, Dh).transpose(0, 2, 1, 3, 4)
        return a.reshape(bsz, dilation, nb, ATT_BLOCK, H, Dh)

    def with_prev(a):
        prev = jnp.pad(a, ((0, 0), (0, 0), (1, 0), (0, 0), (0, 0), (0, 0)))[:, :, :-1]
        return jnp.concatenate([prev, a], axis=3)

    qb = to_blocks(q)
    kk = with_prev(to_blocks(k))
    vv = with_prev(to_blocks(v))
    s = jnp.einsum("brnqhe,brnkhe->brnhqk", qb, kk, preferred_element_type=jnp.float32)
    qi = jnp.arange(ATT_BLOCK)[:, None]
    kj = jnp.arange(2 * ATT_BLOCK)[None, :]
    dist = ATT_BLOCK + qi - kj
    band = (dist >= 0) & (dist <= reach)
    first = (jnp.arange(nb) == 0)[:, None, None]
    valid = band[None] & ~(first & (kj < ATT_BLOCK)[None])
    s = jnp.where(valid[:, None], s, -jnp.inf)
    m = jnp.max(s, axis=-1, keepdims=True)
    p = jnp.exp(s - m)
    den = jnp.sum(p, axis=-1, keepdims=True)
    o = jnp.einsum("brnhqk,brnkhe->brnhqe", p, vv.astype(jnp.float32)) / den
    lse = (m + jnp.log(den))[..., 0]
    o = o.transpose(0, 1, 2, 4, 3, 5).reshape(bsz, dilation, M, H, Dh)
    o = o.transpose(0, 2, 1, 3, 4).reshape(bsz, Lp, H, Dh)[:, :L]
    lse = lse.transpose(0, 1, 2, 4, 3).reshape(bsz, dilation, M, H)
    lse = lse.transpose(0, 2, 1, 3).reshape(bsz, Lp, H)[:, :L]
    return o, lse


def dilated_attention_mixer(x, w_qkv, w_o):
    bsz, L, _ = x.shape
    qkv = (x @ w_qkv).reshape(bsz, L, 3, N_HEADS, HEAD_DIM)
    q = rope(qkv[:, :, 0]) * (HEAD_DIM ** -0.5)
    k = rope(qkv[:, :, 1])
    v = qkv[:, :, 2]
    outs, lses = [], []
    for window, dilation in DILATED_PATTERNS:
        o, lse = dilated_window_attention(q, k, v, window, dilation)
        outs.append(o)
        lses.append(lse)
    wts = jax.nn.softmax(jnp.stack(lses, axis=0), axis=0)
    o = jnp.einsum("pblh,pblhe->blhe", wts, jnp.stack(outs, axis=0))
    return o.reshape(bsz, L, D_MODEL).astype(x.dtype) @ w_o


def moe_ffn(x, router_w, router_b, w_gu, b_gu, w_down, b_down):
    bsz, L, D = x.shape
    T = bsz * L
    xt = x.reshape(T, D)
    logits = (xt @ router_w).astype(jnp.float32) + router_b.astype(jnp.float32)
    top_val, top_idx = lax.top_k(logits, TOP_K)
    gates = jax.nn.softmax(top_val, axis=-1)
    n_assign = T * TOP_K
    flat_e = top_idx.reshape(-1)
    flat_tok = jnp.arange(n_assign, dtype=jnp.int32) // TOP_K
    flat_gate = gates.reshape(-1)
    order = jnp.argsort(flat_e)
    se = flat_e[order]
    counts = jnp.bincount(flat_e, length=N_EXPERTS)
    padded = (counts + MOE_BLOCK - 1) // MOE_BLOCK * MOE_BLOCK
    pad_end = jnp.cumsum(padded)
    pad_start = pad_end - padded
    start = jnp.cumsum(counts) - counts
    rank = jnp.arange(n_assign, dtype=jnp.int32) - start[se]
    dest = pad_start[se] + rank
    n_blocks = -(-n_assign // MOE_BLOCK) + N_EXPERTS
    n_rows = n_blocks * MOE_BLOCK
    row_tok = jnp.full((n_rows,), T, jnp.int32).at[dest].set(flat_tok[order])
    row_gate = jnp.zeros((n_rows,), jnp.float32).at[dest].set(flat_gate[order])
    block_start = jnp.arange(n_blocks, dtype=pad_end.dtype) * MOE_BLOCK
    block_e = jnp.minimum(jnp.searchsorted(pad_end, block_start, side="right"), N_EXPERTS - 1)
    x_pad = jnp.concatenate([xt, jnp.zeros((1, D), xt.dtype)], axis=0)

    def expert_block(args):
        tok, gate, e = args
        h = x_pad[tok] @ w_gu[e] + b_gu[e]
        g = jnp.minimum(h[:, :D_EXPERT], SWIGLU_LIMIT)
        lin = jnp.clip(h[:, D_EXPERT:], -SWIGLU_LIMIT, SWIGLU_LIMIT)
        act = (lin + 1.0) * (g * jax.nn.sigmoid(SWIGLU_ALPHA * g))
        y = act @ w_down[e] + b_down[e]
        return y * gate[:, None].astype(y.dtype)

    ys = lax.map(expert_block, (row_tok.reshape(n_blocks, MOE_BLOCK),
                                row_gate.reshape(n_blocks, MOE_BLOCK), block_e))
    out = jnp.zeros((T + 1, D), ys.dtype).at[row_tok].add(ys.reshape(n_rows, D))[:T]
    return out.reshape(bsz, L, D).astype(x.dtype)


def setup_inputs(seed: int = 0) -> dict:
    key = jax.random.key(seed)
    ks = jax.random.split(key, 32)
    f32 = jnp.float32

    def nrm(k, shape, scale):
        return jax.random.normal(k, shape, f32) * scale

    NE, NO = N_EVEN, N_ODD
    G, P, H = SSM_GROUPS, SSM_STATE, SSM_GROUP
    mix_in = 3 * CONV_CH + SSM_WIDTH
    mix_out = CONV_CH + SSM_WIDTH
    return {
        "x": nrm(ks[0], (BATCH, SEQ, D_MODEL), 1.0),
        "hy_w_in": nrm(ks[1], (NE, D_MODEL, mix_in), D_MODEL ** -0.5),
        "conv_w": nrm(ks[2], (NE, CONV_K, CONV_CH), CONV_K ** -0.5),
        "ssm_a_re": -0.5 + nrm(ks[3], (NE, G, P), 0.01),
        "ssm_a_im": jnp.pi * jnp.arange(P, dtype=f32) + nrm(ks[4], (NE, G, P), 0.01),
        "ssm_log_dt": jax.random.uniform(ks[5], (NE, G), f32, math.log(DT_MIN), math.log(DT_MAX)),
        "ssm_b_re": nrm(ks[6], (NE, G, P, H), (2 * H) ** -0.5),
        "ssm_b_im": nrm(ks[7], (NE, G, P, H), (2 * H) ** -0.5),
        "ssm_c_re": nrm(ks[8], (NE, G, H, P), (2 * P) ** -0.5),
        "ssm_c_im": nrm(ks[9], (NE, G, H, P), (2 * P) ** -0.5),
        "ssm_d": nrm(ks[10], (NE, G, H), 1.0),
        "ssm_w_glu": nrm(ks[11], (NE, SSM_WIDTH, SSM_WIDTH), SSM_WIDTH ** -0.5),
        "ssm_b_glu": nrm(ks[12], (NE, SSM_WIDTH), 0.01),
        "hy_w_out": nrm(ks[13], (NE, mix_out, D_MODEL), DN_BETA * mix_out ** -0.5),
        "att_w_qkv": nrm(ks[14], (NO, D_MODEL, 3 * D_MODEL), D_MODEL ** -0.5),
        "att_w_o": nrm(ks[15], (NO, D_MODEL, D_MODEL), DN_BETA * D_MODEL ** -0.5),
        "ln_mix_g": 1.0 + nrm(ks[16], (DEPTH, D_MODEL), 0.01),
        "ln_mix_b": nrm(ks[17], (DEPTH, D_MODEL), 0.01),
        "ln_ffn_g": 1.0 + nrm(ks[18], (DEPTH, D_MODEL), 0.01),
        "ln_ffn_b": nrm(ks[19], (DEPTH, D_MODEL), 0.01),
        "router_w": nrm(ks[20], (DEPTH, D_MODEL, N_EXPERTS), D_MODEL ** -0.5),
        "router_b": nrm(ks[21], (DEPTH, N_EXPERTS), 0.01),
        "expert_w_gu": nrm(ks[22], (DEPTH, N_EXPERTS, D_MODEL, 2 * D_EXPERT), D_MODEL ** -0.5),
        "expert_b_gu": nrm(ks[23], (DEPTH, N_EXPERTS, 2 * D_EXPERT), 0.01),
        "expert_w_down": nrm(ks[24], (DEPTH, N_EXPERTS, D_EXPERT, D_MODEL), DN_BETA * D_EXPERT ** -0.5),
        "expert_b_down": nrm(ks[25], (DEPTH, N_EXPERTS, D_MODEL), 0.01),
    }


def reference(x, hy_w_in, conv_w, ssm_a_re, ssm_a_im, ssm_log_dt, ssm_b_re, ssm_b_im,
              ssm_c_re, ssm_c_im, ssm_d, ssm_w_glu, ssm_b_glu, hy_w_out, att_w_qkv, att_w_o,
              ln_mix_g, ln_mix_b, ln_ffn_g, ln_ffn_b, router_w, router_b, expert_w_gu,
              expert_b_gu, expert_w_down, expert_b_down):
    h = x
    for layer in range(DEPTH):
        i = layer // 2
        if layer % 2 == 0:
            mix = conv_ssm_mixer(h, hy_w_in[i], conv_w[i], ssm_a_re[i], ssm_a_im[i], ssm_log_dt[i],
                                 ssm_b_re[i], ssm_b_im[i], ssm_c_re[i], ssm_c_im[i], ssm_d[i],
                                 ssm_w_glu[i], ssm_b_glu[i], hy_w_out[i])
        else:
            mix = dilated_attention_mixer(h, att_w_qkv[i], att_w_o[i])
        h = layer_norm(DN_ALPHA * h + mix, ln_mix_g[layer], ln_mix_b[layer])
        ffn = moe_ffn(h, router_w[layer], router_b[layer], expert_w_gu[layer], expert_b_gu[layer],
                      expert_w_down[layer], expert_b_down[layer])
        h = layer_norm(DN_ALPHA * h + ffn, ln_ffn_g[layer], ln_ffn_b[layer])
    return h
```

```python
import contextlib
import numpy as np
import ml_dtypes
import concourse.bass as bass
import concourse.mybir as mybir
from concourse.bass_utils import run_bass_kernel_spmd

F32 = mybir.dt.float32
BF16 = mybir.dt.bfloat16
I32 = mybir.dt.int32
U32 = mybir.dt.uint32
AF = mybir.ActivationFunctionType
ALU = mybir.AluOpType
AX = mybir.AxisListType

D = 1024
NE = 32
TOPK = 4
DEPTH = 4
DN_ALPHA = (2 * DEPTH) ** 0.25
LN_EPS = 1e-5
NH = 16
HD = 64
PATTERNS = ((128, 1), (512, 4), (2048, 16))

ENGS = ("pe", "dve", "act", "pool", "sp")
RING = {"sp": 12, "act": 6, "pool": 12}


class Ctx:
    def __init__(self, nc):
        self.nc = nc
        self.stack = contextlib.ExitStack()
        self.prog = {e: [] for e in ENGS}
        self.cnt = {e: 0 for e in ENGS}
        self.semobj = {}
        for e in ENGS:
            self.semobj[("e", e)] = self.stack.enter_context(nc.semaphore("s_" + e))
        for q, n in RING.items():
            for i in range(n):
                self.semobj[("r", q, i)] = self.stack.enter_context(nc.semaphore("r_%s%d" % (q, i)))
        self.dcnt = {q: 0 for q in RING}
        self.seen = {e: {} for e in ENGS}
        self.last_w = {}
        self.readers = {}
        self.n_instr = 0
        self.pstack = None

    def sb(self, name, shape, dt=F32):
        st = self.pstack if self.pstack is not None else self.stack
        self.uid = getattr(self, "uid", 0) + 1
        return st.enter_context(self.nc.sbuf_tensor("%s_u%d" % (name, self.uid), list(shape), dt))

    def reg(self, e, val):
        if not hasattr(self, "_regs"):
            self._regs = {}
        if val not in self._regs:
            self._regs[val] = e.to_reg(val)
        return self._regs[val]

    def ps(self, name, shape, dt=F32):
        return self.stack.enter_context(self.nc.psum_tensor(name, list(shape), dt))

    @contextlib.contextmanager
    def phase(self):
        self.barrier()
        self.pstack = contextlib.ExitStack()
        try:
            yield
        finally:
            self.barrier()
            self.pstack.close()
            self.pstack = None

    def cur_events(self):
        evs = []
        for e in ENGS:
            if self.cnt[e]:
                evs.append((("e", e), self.cnt[e]))
        for q, n in RING.items():
            i = self.dcnt[q]
            for slot in range(n):
                if i > slot:
                    last = ((i - 1 - slot) // n) * n + slot
                    evs.append((("r", q, slot), 16 * (last // n + 1)))
        return evs

    def barrier(self):
        evs = self.cur_events()
        for eng in ENGS:
            for k, v in evs:
                if self.seen[eng].get(k, 0) < v:
                    self.seen[eng][k] = v
                    self.prog[eng].append(("wait", k, v))
        self.last_w = {}
        self.readers = {}

    def _need(self, eng, reads, writes, cowrites=()):
        need = {}

        def add(ev):
            k, v = ev
            if need.get(k, 0) < v:
                need[k] = v
        for k in reads:
            for ev in self.last_w.get(k, ()):
                add(ev)
        for k in writes:
            for ev in self.last_w.get(k, ()):
                add(ev)
            for ev in self.readers.get(k, ()):
                add(ev)
        for k in cowrites:
            for ev in self.readers.get(k, ()):
                add(ev)
        for k, v in need.items():
            if eng == "pe" and k == ("e", "pe"):
                continue
            if self.seen[eng].get(k, 0) >= v:
                continue
            self.seen[eng][k] = v
            self.prog[eng].append(("wait", k, v))

    def _commit(self, ev, reads, writes, cowrites=()):
        for k in reads:
            self.readers.setdefault(k, []).append(ev)
        for k in writes:
            self.last_w[k] = [ev]
            self.readers[k] = []
        for k in cowrites:
            self.last_w.setdefault(k, []).append(ev)

    def op(self, eng, fn, reads=(), writes=()):
        self._need(eng, reads, writes)
        self.cnt[eng] += 1
        ev = (("e", eng), self.cnt[eng])
        self.prog[eng].append(("ins", fn, ("e", eng), 1))
        self._commit(ev, reads, writes)
        self.n_instr += 1
        return ev

    def dma(self, q, fn, reads=(), writes=(), cowrites=()):
        self._need(q, reads, writes, cowrites)
        i = self.dcnt[q]
        n = RING[q]
        slot = i % n
        if i >= n:
            k = ("r", q, slot)
            v = 16 * (i // n)
            if self.seen[q].get(k, 0) < v:
                self.seen[q][k] = v
                self.prog[q].append(("wait", k, v))
        self.dcnt[q] += 1
        ev = (("r", q, slot), 16 * (i // n + 1))
        self.prog[q].append(("ins", fn, ("r", q, slot), 16))
        self._commit(ev, reads, writes, cowrites)
        self.n_instr += 1
        return ev

    def emit(self):
        nc = self.nc
        self.barrier()
        with nc.Block() as block:
            def body(engname):
                def f(e):
                    for it in self.prog[engname]:
                        if it[0] == "wait":
                            e.wait_ge(self.semobj[it[1]], it[2])
                        else:
                            it[1](e).then_inc(self.semobj[it[2]], it[3])
                return f
            block.tensor(body("pe"))
            block.vector(body("dve"))
            block.scalar(body("act"))
            block.gpsimd(body("pool"))
            block.sync(body("sp"))
        self.stack.close()


class G:
    pass


def setup_globals(c, g, W):
    g.bank = [c.ps("bank%d" % i, [128, 512], F32) for i in range(8)]
    g.ident = c.sb("ident", [128, 128], F32)
    g.identb = c.sb("identb", [128, 128], BF16)
    g.onesrow = c.sb("onesrow", [1, 128], BF16)
    c.dma("sp", lambda e: e.dma_start(out=g.ident[:], in_=W["c_ident"][:, :]), writes=["ident"])
    c.dma("pool", lambda e: e.dma_start(out=g.identb[:], in_=W["c_ident"][:, :]), writes=["identb"])
    c.dma("pool", lambda e: e.dma_start(out=g.onesrow[:], in_=W["c_ones"][0:1, :]), writes=["onesrow"])


def ecopy(e, eng, out, in_):
    if eng == "act":
        return e.activation(out=out, in_=in_, func=AF.Copy)
    return e.tensor_copy(out, in_)


def load_bcast(c, tile, key, src_row_ap, q="sp"):
    c.dma(q, lambda e: e.dma_start(out=tile[:], in_=src_row_ap.partition_broadcast(128)), writes=[key])


def layer_norm_tile(c, g, acc, acck, gam, bet, gbk, out_tile, outk, tag):
    st = g.ln_stats
    mv = g.ln_mv
    sk = "ln_small"
    c.op("dve", lambda e: e.bn_stats(st[:, 0, :], acc[:, 0:512]), reads=[acck], writes=[sk])
    c.op("dve", lambda e: e.bn_stats(st[:, 1, :], acc[:, 512:1024]), reads=[acck, sk], writes=[sk])
    c.op("dve", lambda e: e.bn_aggr(mv[:, 0:2], st[:].rearrange("p a b -> p (a b)")), reads=[sk], writes=[sk])
    c.op("act", lambda e: e.activation(out=mv[:, 2:3], in_=mv[:, 1:2], func=AF.Sqrt, bias=g.eps_t[:, 0:1], scale=1.0),
         reads=[sk, "eps"], writes=[sk])
    c.op("dve", lambda e: e.reciprocal(mv[:, 3:4], mv[:, 2:3]), reads=[sk], writes=[sk])
    c.op("dve", lambda e: e.tensor_scalar(mv[:, 4:5], mv[:, 0:1], mv[:, 3:4], -1.0, ALU.mult, ALU.mult),
         reads=[sk], writes=[sk])
    c.op("act", lambda e: e.activation(out=acc[:], in_=acc[:], func=AF.Identity, bias=mv[:, 4:5], scale=mv[:, 3:4]),
         reads=[sk, acck], writes=[acck])
    c.op("dve", lambda e: e.tensor_tensor(acc[:], acc[:], gam[:], ALU.mult), reads=[acck, gbk], writes=[acck])
    c.op("dve", lambda e: e.tensor_tensor(out_tile[:], acc[:], bet[:], ALU.add), reads=[acck, gbk], writes=[outk])


def transpose_rows_to_T(c, g, rows, rowsk, xT, xTk, bank, bankk, nchunks=8, fp32_in=True, evac="dve"):
    if fp32_in:
        for half in range(nchunks // 4):
            for j in range(4):
                kc = half * 4 + j
                c.op("pe", (lambda kc, j: lambda e: e.transpose(bank[:, j * 128:(j + 1) * 128], rows[:, kc * 128:(kc + 1) * 128], g.ident[:]))(kc, j),
                     reads=[rowsk, "ident"], writes=[bankk])
            c.op(evac, (lambda half: lambda e: ecopy(e, evac, xT[:, half * 4:(half + 1) * 4, :].rearrange("p a b -> p (a b)"), bank[:, :]))(half),
                 reads=[bankk], writes=[xTk])
    else:
        bb = bank[:].bitcast(BF16)
        for kc in range(nchunks):
            c.op("pe", (lambda kc: lambda e: e.transpose(bb[:, kc * 128:(kc + 1) * 128], rows[:, kc * 128:(kc + 1) * 128], g.identb[:]))(kc),
                 reads=[rowsk, "identb"], writes=[bankk])
        c.op(evac, lambda e: ecopy(e, evac, xT[:].rearrange("p a b -> p (a b)"), bb[:, 0:nchunks * 128]),
             reads=[bankk], writes=[xTk])


def moe_phase(c, g, W, layer, TT, hsrc, hdst, C):
    NT = TT // 128
    NSL = C // 128
    bank = g.bank
    with c.phase():
        hrow = [c.sb("m_hrow%d" % i, [128, D], F32) for i in range(2)]
        hT = c.sb("m_hT", [128, 8, 128], F32)
        rw = c.sb("m_rw", [128, 8, NE], F32)
        rb = c.sb("m_rb", [128, NE], F32)
        ltri = c.sb("m_ltri", [128, 128], F32)
        ones = c.sb("m_ones", [128, 128], F32)
        iota = c.sb("m_iota", [128, NE], F32)
        base = c.sb("m_base", [128, NE], F32)
        logit = c.sb("m_logit", [128, NE], F32)
        top8 = c.sb("m_top8", [128, 8], F32)
        idx8 = c.sb("m_idx8", [128, 8], U32)
        idxf = c.sb("m_idxf", [128, 4], F32)
        sm = c.sb("m_sm", [128, 16], F32)
        oh = c.sb("m_oh", [128, 4, NE], F32)
        Mt = c.sb("m_M", [128, NE], F32)
        rank = c.sb("m_rank", [128, NE], F32)
        tmp = c.sb("m_tmp", [128, NE], F32)
        pos = c.sb("m_pos", [128, 8], F32)
        c.dma("sp", lambda e: e.dma_start(out=rw[:], in_=W["router_w"][layer].rearrange("(kc p) e -> p kc e", p=128)), writes=["rw"])
        load_bcast(c, rb, "rb", W["router_b"][layer:layer + 1, :])
        c.dma("sp", lambda e: e.dma_start(out=ltri[:], in_=W["c_ltri"][:, :]), writes=["ltri"])
        c.dma("sp", lambda e: e.dma_start(out=ones[:], in_=W["c_onesq"][:, :]), writes=["ones"])
        c.dma("sp", lambda e: e.dma_start(out=iota[:], in_=W["c_iota"][:, :]), writes=["iota"])
        c.op("dve", lambda e: e.memset(base[:], 0.0), writes=["base"])
        S = "m_small"
        for i in range(NT):
            hr = hrow[i % 2]
            hk = "m_hrow%d" % (i % 2)
            c.dma("sp", (lambda i, hr: lambda e: e.dma_start(out=hr[:], in_=hsrc[i * 128:(i + 1) * 128, :]))(i, hr), writes=[hk])
            transpose_rows_to_T(c, g, hr, hk, hT, "m_hT", bank[0], "bank0", fp32_in=True, evac="act")
            for kc in range(8):
                c.op("pe", (lambda kc: lambda e: e.matmul(bank[1][:, 0:NE], hT[:, kc, :], rw[:, kc, :], start=(kc == 0), stop=(kc == 7)))(kc),
                     reads=["m_hT", "rw"], writes=["bank1"])
            c.op("dve", lambda e: e.tensor_tensor(logit[:], bank[1][:, 0:NE], rb[:], ALU.add), reads=["bank1", "rb"], writes=[S])
            c.op("dve", lambda e: e.max(top8[:], logit[:]), reads=[S], writes=[S])
            c.op("dve", lambda e: e.max_index(idx8[:], top8[:], logit[:]), reads=[S], writes=[S])
            c.op("dve", lambda e: e.tensor_copy(idxf[:], idx8[:, 0:4]), reads=[S], writes=[S])
            c.op("dve", lambda e: e.tensor_scalar_mul(sm[:, 0:1], top8[:, 0:1], -1.0), reads=[S], writes=[S])
            c.op("act", lambda e: e.activation(out=sm[:, 4:8], in_=top8[:, 0:4], func=AF.Exp, bias=sm[:, 0:1], scale=1.0, accum_out=sm[:, 1:2]),
                 reads=[S], writes=[S])
            c.op("dve", lambda e: e.reciprocal(sm[:, 2:3], sm[:, 1:2]), reads=[S], writes=[S])
            c.op("dve", (lambda i: lambda e: e.tensor_scalar_mul(g.gates_all[:, i, :], sm[:, 4:8], sm[:, 2:3]))(i), reads=[S], writes=["gates_all"])
            for k in range(4):
                c.op("dve", (lambda k: lambda e: e.tensor_scalar(oh[:, k, :], iota[:], idxf[:, k:k + 1], None, ALU.is_equal))(k),
                     reads=[S, "iota"], writes=[S])
            c.op("dve", lambda e: e.tensor_tensor(Mt[:], oh[:, 0, :], oh[:, 1, :], ALU.add), reads=[S], writes=["m_M"])
            c.op("dve", lambda e: e.tensor_tensor(Mt[:], Mt[:], oh[:, 2, :], ALU.add), reads=[S, "m_M"], writes=["m_M"])
            c.op("dve", lambda e: e.tensor_tensor(Mt[:], Mt[:], oh[:, 3, :], ALU.add), reads=[S, "m_M"], writes=["m_M"])
            c.op("pe", lambda e: e.matmul(bank[2][:, 0:NE], ltri[:], Mt[:], start=True, stop=True), reads=["m_M", "ltri"], writes=["bank2"])
            c.op("pe", lambda e: e.matmul(bank[3][:, 0:NE], ones[:], Mt[:], start=True, stop=True), reads=["m_M", "ones"], writes=["bank3"])
            c.op("dve", lambda e: e.tensor_tensor(rank[:], bank[2][:, 0:NE], base[:], ALU.add), reads=["bank2", "base"], writes=[S])
            c.op("dve", lambda e: e.tensor_tensor(base[:], bank[3][:, 0:NE], base[:], ALU.add), reads=["bank3", S], writes=["base"])
            for k in range(4):
                c.op("dve", (lambda k: lambda e: e.scalar_tensor_tensor(tmp[:], oh[:, k, :], 1.0, rank[:], ALU.mult, ALU.mult, accum_out=pos[:, k:k + 1]))(k),
                     reads=[S], writes=[S])
            c.op("dve", lambda e: e.tensor_scalar(pos[:, 4:8], pos[:, 0:4], float(C), 1.0e6, ALU.is_ge, ALU.mult), reads=[S], writes=[S])
            c.op("dve", lambda e: e.tensor_tensor(pos[:, 0:4], pos[:, 0:4], pos[:, 4:8], ALU.add), reads=[S], writes=[S])
            c.op("dve", lambda e: e.scalar_tensor_tensor(pos[:, 4:8], idxf[:], float(C), pos[:, 0:4], ALU.mult, ALU.add), reads=[S], writes=[S])
            c.op("dve", (lambda i: lambda e: e.tensor_copy(g.drow_all[:, i, :], pos[:, 4:8]))(i), reads=[S], writes=["drow_all"])
            for k in range(4):
                c.dma("pool", (lambda i, k, hr: lambda e: e.indirect_dma_start(
                    out=g.Xd[:, :], out_offset=bass.IndirectOffsetOnAxis(ap=g.drow_all[:, i, k:k + 1], axis=0),
                    in_=hr[:, :], in_offset=None, bounds_check=c.reg(e, NE * C - 1), oob_is_err=False))(i, k, hr),
                    reads=[hk, "drow_all"], cowrites=["Xd"])

    with c.phase():
        wgu = [c.sb("m_wgu%d" % i, [128, 8, 2048], BF16) for i in range(2)]
        wdn = [c.sb("m_wdn%d" % i, [128, 8, 1024], BF16) for i in range(2)]
        bgu = [c.sb("m_bgu%d" % i, [1, 2048], BF16) for i in range(2)]
        bdn = [c.sb("m_bdn%d" % i, [1, 1024], BF16) for i in range(2)]
        xrow = [c.sb("m_xrow%d" % i, [128, D], BF16) for i in range(2)]
        xT = [c.sb("m_xT%d" % i, [128, 8, 128], BF16) for i in range(2)]
        gt = [c.sb("m_g%d" % i, [128, 1024], F32) for i in range(2)]
        sg = [c.sb("m_sg%d" % i, [128, 1024], F32) for i in range(2)]
        lin = [c.sb("m_lin%d" % i, [128, 1024], F32) for i in range(2)]
        act = [c.sb("m_act%d" % i, [128, 1024], BF16) for i in range(2)]
        actT = [c.sb("m_actT%d" % i, [128, 8, 128], BF16) for i in range(2)]
        yt = [c.sb("m_y%d" % i, [128, 1024], F32) for i in range(2)]

        def load_w(ex):
            p = ex % 2
            wg_src = W["w_gu"][layer, ex].rearrange("(kc p) n -> p kc n", p=128)
            wd_src = W["w_dn"][layer, ex].rearrange("(kc p) n -> p kc n", p=128)
            for kc in range(8):
                c.dma("pool", (lambda kc: lambda e: e.dma_start(out=wgu[p][:, kc, :], in_=wg_src[:, kc, :], max_dma_last_dim=8192))(kc),
                      cowrites=["wgu%d" % p], reads=[], writes=[])
            for kc in range(8):
                c.dma("pool", (lambda kc: lambda e: e.dma_start(out=wdn[p][:, kc, :], in_=wd_src[:, kc, :], max_dma_last_dim=4096))(kc),
                      cowrites=["wdn%d" % p])
            c.dma("pool", lambda e: e.dma_start(out=bgu[p][:], in_=W["b_gu"][layer, ex:ex + 1, :], max_dma_last_dim=8192), cowrites=["wgu%d" % p])
            c.dma("pool", lambda e: e.dma_start(out=bdn[p][:], in_=W["b_dn"][layer, ex:ex + 1, :], max_dma_last_dim=4096), cowrites=["wdn%d" % p])

        units = [(ex, i) for ex in range(NE) for i in range(NSL)]

        def st_load(u):
            ex, i = units[u]
            b = u % 2
            r0 = ex * C + i * 128
            c.dma("sp", lambda e: e.dma_start(out=xrow[b][:], in_=g.Xd[r0:r0 + 128, :]), reads=["Xd"], writes=["xrow%d" % b])

        def st_tx(u):
            b = u % 2
            transpose_rows_to_T(c, g, xrow[b], "xrow%d" % b, xT[b], "xT%d" % b, bank[0], "bank0", fp32_in=False, evac="dve")

        def st_gu(u):
            ex, i = units[u]
            b = u % 2
            p = ex % 2
            for n in range(4):
                for kc in range(8):
                    c.op("pe", (lambda n, kc: lambda e: e.matmul(bank[2 + n][:, :], xT[b][:, kc, :], wgu[p][:, kc, n * 512:(n + 1) * 512], start=(kc == 0), stop=False))(n, kc),
                         reads=["xT%d" % b, "wgu%d" % p], writes=["gu%d" % n])
                c.op("pe", (lambda n: lambda e: e.matmul(bank[2 + n][:, :], g.onesrow[0:1, :], bgu[p][0:1, n * 512:(n + 1) * 512], start=False, stop=True))(n),
                     reads=["onesrow", "wgu%d" % p], writes=["gu%d" % n])
            for n in range(2):
                sl = slice(n * 512, (n + 1) * 512)
                c.op("dve", (lambda n, sl: lambda e: e.tensor_scalar_min(gt[b][:, sl], bank[2 + n][:, :], 7.0))(n, sl), reads=["gu%d" % n], writes=["g%d" % b])
                c.op("act", (lambda sl: lambda e: e.activation(out=sg[b][:, sl], in_=gt[b][:, sl], func=AF.Sigmoid, scale=1.702))(sl), reads=["g%d" % b], writes=["sg%d" % b])
                c.op("dve", (lambda n, sl: lambda e: e.tensor_scalar(lin[b][:, sl], bank[4 + n][:, :], 7.0, -7.0, ALU.min, ALU.max))(n, sl), reads=["gu%d" % (2 + n)], writes=["lin%d" % b])
                c.op("pool", (lambda sl: lambda e: e.tensor_tensor(sg[b][:, sl], sg[b][:, sl], gt[b][:, sl], ALU.mult))(sl), reads=["g%d" % b, "sg%d" % b], writes=["sg%d" % b])
                c.op("dve", (lambda sl: lambda e: e.scalar_tensor_tensor(act[b][:, sl], lin[b][:, sl], 1.0, sg[b][:, sl], ALU.add, ALU.mult))(sl),
                     reads=["lin%d" % b, "sg%d" % b], writes=["act%d" % b])

        def st_down(u):
            ex, i = units[u]
            b = u % 2
            p = ex % 2
            transpose_rows_to_T(c, g, act[b], "act%d" % b, actT[b], "actT%d" % b, bank[1], "bank1", fp32_in=False, evac="dve")
            for n in range(2):
                for kc in range(8):
                    c.op("pe", (lambda n, kc: lambda e: e.matmul(bank[6 + n][:, :], actT[b][:, kc, :], wdn[p][:, kc, n * 512:(n + 1) * 512], start=(kc == 0), stop=False))(n, kc),
                         reads=["actT%d" % b, "wdn%d" % p], writes=["dn%d" % n])
                c.op("pe", (lambda n: lambda e: e.matmul(bank[6 + n][:, :], g.onesrow[0:1, :], bdn[p][0:1, n * 512:(n + 1) * 512], start=False, stop=True))(n),
                     reads=["onesrow", "wdn%d" % p], writes=["dn%d" % n])
                c.op("act", (lambda n: lambda e: e.activation(out=yt[b][:, n * 512:(n + 1) * 512], in_=bank[6 + n][:, :], func=AF.Copy))(n),
                     reads=["dn%d" % n], writes=["y%d" % b])
            r0 = ex * C + i * 128
            c.dma("sp", lambda e: e.dma_start(out=g.Yb[r0:r0 + 128, :], in_=yt[b][:]), reads=["y%d" % b], cowrites=["Yb"])

        NU = len(units)
        load_w(0)
        st_load(0)
        st_tx(0)
        for u in range(NU):
            ex, i = units[u]
            if i == 0 and ex + 1 < NE:
                pass
            st_gu(u)
            if u + 1 < NU:
                st_load(u + 1)
            if u >= 1:
                st_down(u - 1)
            if i == 0 and ex + 1 < NE:
                load_w(ex + 1)
            if u + 1 < NU:
                st_tx(u + 1)
        st_down(NU - 1)

    with c.phase():
        hrow = [c.sb("c_hrow%d" % i, [128, D], F32) for i in range(2)]
        yk = [[c.sb("c_y%d_%d" % (i, k), [128, D], F32) for k in range(4)] for i in range(2)]
        outt = [c.sb("c_out%d" % i, [128, D], F32) for i in range(2)]
        gam = c.sb("c_gam", [128, D], F32)
        bet = c.sb("c_bet", [128, D], F32)
        load_bcast(c, gam, "c_gb", W["ln_ffn_g"][layer:layer + 1, :])
        c.dma("sp", lambda e: e.dma_start(out=bet[:], in_=W["ln_ffn_b"][layer:layer + 1, :].partition_broadcast(128)), cowrites=["c_gb"])
        for i in range(NT):
            b = i % 2
            hr = hrow[b]
            hk = "c_hrow%d" % b
            c.dma("sp", (lambda i, hr: lambda e: e.dma_start(out=hr[:], in_=hsrc[i * 128:(i + 1) * 128, :]))(i, hr), writes=[hk])
            for k in range(4):
                c.dma("pool", (lambda i, k, b: lambda e: e.indirect_dma_start(
                    out=yk[b][k][:, :], out_offset=None, in_=g.Yb[:, :],
                    in_offset=bass.IndirectOffsetOnAxis(ap=g.drow_all[:, i, k:k + 1], axis=0),
                    bounds_check=c.reg(e, NE * C - 1), oob_is_err=False))(i, k, b),
                    reads=["Yb", "drow_all"], writes=["c_y%d_%d" % (b, k)])
            c.op("act", (lambda hr: lambda e: e.mul(hr[:], hr[:], float(DN_ALPHA)))(hr), reads=[hk], writes=[hk])
            for k in range(4):
                c.op("dve", (lambda i, k, b, hr: lambda e: e.scalar_tensor_tensor(hr[:], yk[b][k][:], g.gates_all[:, i, k:k + 1], hr[:], ALU.mult, ALU.add))(i, k, b, hr),
                     reads=[hk, "c_y%d_%d" % (b, k), "gates_all"], writes=[hk])
            layer_norm_tile(c, g, hr, hk, gam, bet, "c_gb", outt[b], "c_out%d" % b, "c")
            c.dma("sp", (lambda i, b: lambda e: e.dma_start(out=hdst[i * 128:(i + 1) * 128, :], in_=outt[b][:]))(i, b),
                  reads=["c_out%d" % b], cowrites=["hdst"])


def host_constants(SEQ):
    cst = {}
    cst["c_ident"] = np.eye(128, dtype=np.float32)
    cst["c_ones"] = np.ones((1, 128), np.float32)
    cst["c_onesq"] = np.ones((128, 128), np.float32)
    k = np.arange(128)
    cst["c_ltri"] = (k[:, None] < k[None, :]).astype(np.float32)
    cst["c_iota"] = np.tile(np.arange(NE, dtype=np.float32)[None, :], (128, 1))
    mc = (k[:, None] <= k[None, :]).astype(np.float32)
    mp = (k[:, None] >= k[None, :]).astype(np.float32)
    cst["c_mcur"] = np.tile(mc, (1, 4)).astype(np.float32)
    cst["c_mprev"] = np.tile(mp, (1, 4)).astype(np.float32)
    par = np.zeros((32, 2), np.float32)
    par[0::2, 0] = 1.0
    par[1::2, 1] = 1.0
    cst["c_par"] = par
    half = HD // 2
    inv = (10000.0 ** (-np.arange(half, dtype=np.float32) / half)).astype(np.float32)
    ang = (np.arange(SEQ, dtype=np.float32)[:, None] * inv[None, :]).astype(np.float32)
    cst["c_cos16"] = np.tile(np.cos(ang).astype(np.float32), (1, NH))
    cst["c_sin16"] = np.tile(np.sin(ang).astype(np.float32), (1, NH))
    return cst


WEIGHT_SHAPES = {
    "hy_w_in": (D, 2048), "conv_w": (3, 512), "ssm_a_re": (32, 64), "ssm_a_im": (32, 64), "ssm_log_dt": (32,),
    "ssm_b_re": (32, 64, 16), "ssm_b_im": (32, 64, 16), "ssm_c_re": (32, 16, 64), "ssm_c_im": (32, 16, 64),
    "ssm_d": (32, 16), "ssm_w_glu": (512, 512), "ssm_b_glu": (512,), "hy_w_out": (D, D),
    "att_w_qkv": (D, 3 * D), "att_w_o": (D, D),
    "ln_mix_g": (D,), "ln_mix_b": (D,), "ln_ffn_g": (D,), "ln_ffn_b": (D,),
    "router_w": (D, NE), "router_b": (NE,), "w_gu": (NE, D, 2 * D), "b_gu": (NE, 2 * D),
    "w_dn": (NE, D, D), "b_dn": (NE, D),
}


def build_program(cfg):
    SEQ, NSEQ, C = cfg["SEQ"], cfg["NSEQ"], cfg["C"]
    TT = SEQ * NSEQ
    nc = bass.Bass("TRN2", target_bir_lowering=False)
    W = {}
    W["x"] = nc.dram_tensor("x", [TT, D], F32, kind="ExternalInput").ap()
    for name, shp in WEIGHT_SHAPES.items():
        n0 = cfg["nl"].get(name, 0)
        if n0 == 0:
            continue
        W[name] = nc.dram_tensor(name, [n0] + list(shp), F32, kind="ExternalInput").ap()
    cst = host_constants(SEQ)
    for name, arr in cst.items():
        W[name] = nc.dram_tensor(name, list(arr.shape), F32, kind="ExternalInput").ap()
    out = nc.dram_tensor("out", [TT, D], F32, kind="ExternalOutput").ap()
    c = Ctx(nc)
    g = G()
    setup_globals(c, g, W)
    NT = TT // 128
    g.gates_all = c.sb("gates_all", [128, NT, 4], F32)
    g.drow_all = c.sb("drow_all", [128, NT, 4], U32)
    g.ln_stats = c.sb("ln_stats", [128, 2, 6], F32)
    g.ln_mv = c.sb("ln_mv", [128, 8], F32)
    g.eps_t = c.sb("eps_t", [128, 1], F32)
    c.op("dve", lambda e: e.memset(g.eps_t[:], LN_EPS), writes=["eps"])
    g.Pr = c.sb("ssm_Pr", [128, 16, NRND], F32)
    g.Pi = c.sb("ssm_Pi", [128, 16, NRND], F32)
    g.nPi = c.sb("ssm_nPi", [128, 16, NRND], F32)
    g.lBre = c.sb("ssm_lBre", [128, 16, 128], BF16)
    g.lBim = c.sb("ssm_lBim", [128, 16, 128], BF16)
    g.lCre = c.sb("ssm_lCre", [128, 16, 128], BF16)
    g.lCim = c.sb("ssm_lCim", [128, 16, 128], BF16)
    g.Xd = nc.dram_tensor("Xd", [NE * C, D], BF16).ap()
    g.Yb = nc.dram_tensor("Yb", [NE * C, D], F32).ap()
    hb = [nc.dram_tensor("hbuf%d" % i, [TT, D], F32).ap() for i in range(2)]
    g.qkv_d = nc.dram_tensor("qkv_d", [TT, RW], BF16).ap()
    g.O_d = nc.dram_tensor("O_d", [3, TT, VW], F32).ap()
    g.cfg = cfg
    cur = W["x"]
    plan = cfg["plan"]
    for pi, (kind, layer, mi) in enumerate(plan):
        last = pi == len(plan) - 1
        dst = out if last else hb[pi % 2]
        if kind == "moe":
            moe_phase(c, g, W, layer, TT, cur, dst, C)
        elif kind == "even":
            even_phase(c, g, W, layer, mi, SEQ, NSEQ, cur, dst)
        elif kind == "odd":
            odd_phase(c, g, W, layer, mi, SEQ, NSEQ, cur, dst)
        cur = dst
    c.emit()
    return nc, cst, c


TB = 512
NRND = 9
TWO_PI = 6.283185307179586


def sin_reduced(c, out, src, shift, tmpf, tmpi, S):
    c.op("dve", lambda e: e.tensor_scalar(tmpf[:, 0, :], src[:], float(shift), 1.0 / TWO_PI, ALU.add, ALU.mult), reads=[S], writes=[S])
    c.op("dve", lambda e: e.tensor_copy(tmpi[:], tmpf[:, 0, :]), reads=[S], writes=[S])
    c.op("dve", lambda e: e.tensor_copy(tmpf[:, 1, :], tmpi[:]), reads=[S], writes=[S])
    c.op("dve", lambda e: e.tensor_scalar(tmpf[:, 2, :], src[:], float(shift), None, ALU.add), reads=[S], writes=[S])
    c.op("dve", lambda e: e.scalar_tensor_tensor(tmpf[:, 2, :], tmpf[:, 1, :], -TWO_PI, tmpf[:, 2, :], ALU.mult, ALU.add), reads=[S], writes=[S])
    c.op("dve", lambda e: e.tensor_scalar(tmpf[:, 3, :], tmpf[:, 2, :], float(np.pi), -TWO_PI, ALU.is_gt, ALU.mult), reads=[S], writes=[S])
    c.op("dve", lambda e: e.tensor_tensor(tmpf[:, 2, :], tmpf[:, 2, :], tmpf[:, 3, :], ALU.add), reads=[S], writes=[S])
    c.op("dve", lambda e: e.tensor_scalar(tmpf[:, 3, :], tmpf[:, 2, :], float(-np.pi), TWO_PI, ALU.is_lt, ALU.mult), reads=[S], writes=[S])
    c.op("dve", lambda e: e.tensor_tensor(tmpf[:, 2, :], tmpf[:, 2, :], tmpf[:, 3, :], ALU.add), reads=[S], writes=[S])
    c.op("act", lambda e: e.activation(out=out[:], in_=tmpf[:, 2, :], func=AF.Sin), reads=[S], writes=[S])


def even_setup(c, g, W, mi):
    bank = g.bank
    S = "es"
    if g.cfg.get("dbg") == "none":
        return
    with c.phase():
        nat = c.sb("es_nat", [32, 3, 64], F32)
        natm = c.sb("es_natm", [32, 3, 128], F32)
        par = c.sb("es_par", [32, 2], F32)
        ldt = c.sb("es_ldt", [32, 1], F32)
        q3 = c.sb("es_q3", [128, 3, 16], F32)
        tf = c.sb("es_tf", [128, 4, 16], F32)
        ti = c.sb("es_ti", [128, 16], I32)
        pt = c.sb("es_pt", [128, 32], F32)
        w = c.sb("es_w", [128, 12, 16], F32)
        Bn = c.sb("es_Bn", [128, 2, 16, 16], F32)
        Bf = c.sb("es_Bf", [128, 2, 16, 128], F32)
        Cf = c.sb("es_Cf", [128, 2, 16, 128], F32)
        c.dma("sp", lambda e: e.dma_start(out=nat[:, 0, :], in_=W["ssm_a_re"][mi]), cowrites=["es_nat"])
        c.dma("sp", lambda e: e.dma_start(out=nat[:, 1, :], in_=W["ssm_a_im"][mi]), cowrites=["es_nat"])
        c.dma("sp", lambda e: e.dma_start(out=ldt[:], in_=W["ssm_log_dt"][mi].rearrange("(g o) -> g o", o=1)), cowrites=["es_nat"])
        c.dma("sp", lambda e: e.dma_start(out=par[:], in_=W["c_par"][:, :]), cowrites=["es_nat"])
        for ri, nm in enumerate(("ssm_b_re", "ssm_b_im")):
            for gg in range(2):
                c.dma("sp", (lambda ri, nm, gg: lambda e: e.dma_start(out=Bn[gg * 64:(gg + 1) * 64, ri, :, :], in_=W[nm][mi].rearrange("(j gg) p h -> gg p j h", gg=2)[gg]))(ri, nm, gg), cowrites=["es_Bn"])
        c.op("dve", lambda e: e.memset(Bf[:], 0.0), writes=["es_Bf"])
        c.op("pool", lambda e: e.memset(Cf[:], 0.0), writes=["es_Cf"])
        for ri, nm in enumerate(("ssm_c_re", "ssm_c_im")):
            for j in range(16):
                for gg in range(2):
                    q = j % 4
                    p0 = 32 * q + 16 * gg
                    c.dma("sp", (lambda ri, nm, j, gg, p0: lambda e: e.dma_start(out=Cf[p0:p0 + 16, ri, j, gg * 64:(gg + 1) * 64], in_=W[nm][mi, 2 * j + gg]))(ri, nm, j, gg, p0),
                          reads=["es_Cf"], cowrites=["es_Cf2"])
        c.op("act", lambda e: e.activation(out=ldt[:], in_=ldt[:], func=AF.Exp), reads=["es_nat"], writes=[S])
        c.op("dve", lambda e: e.tensor_scalar_min(nat[:, 0, :], nat[:, 0, :], -1e-4), reads=["es_nat", S], writes=[S])
        c.op("dve", lambda e: e.memset(nat[:, 2, :], 1.0), reads=[S], writes=[S])
        c.op("dve", lambda e: e.tensor_scalar_mul(nat[:, 2, :], nat[:, 2, :], ldt[:, 0:1]), reads=[S], writes=[S])
        for m in range(3):
            c.op("dve", (lambda m: lambda e: e.tensor_scalar_mul(natm[:, m, 0:64], nat[:, m, :], par[:, 0:1]))(m), reads=[S], writes=[S])
            c.op("dve", (lambda m: lambda e: e.tensor_scalar_mul(natm[:, m, 64:128], nat[:, m, :], par[:, 1:2]))(m), reads=[S], writes=[S])
            c.op("pe", (lambda m: lambda e: e.transpose(bank[0][:, 0:32], natm[:, m, :], g.ident[0:32, 0:32]))(m), reads=[S, "ident"], writes=["bank0"])
            c.op("dve", lambda e: e.tensor_copy(pt[:], bank[0][:, 0:32]), reads=["bank0", S], writes=[S])
            c.op("dve", (lambda m: lambda e: e.tensor_tensor(q3[:, m, :], pt[:].rearrange("p (j gg) -> p j gg", gg=2)[:, :, 0], pt[:].rearrange("p (j gg) -> p j gg", gg=2)[:, :, 1], ALU.add))(m), reads=[S], writes=[S])
        LR, LI, DT = q3[:, 0, :], q3[:, 1, :], q3[:, 2, :]
        c.op("dve", lambda e: e.tensor_tensor(w[:, 0, :], LR, DT, ALU.mult), reads=[S], writes=[S])
        c.op("act", lambda e: e.activation(out=w[:, 0, :], in_=w[:, 0, :], func=AF.Exp), reads=[S], writes=[S])
        c.op("dve", lambda e: e.tensor_tensor(w[:, 1, :], LI, DT, ALU.mult), reads=[S], writes=[S])
        sin_reduced(c, w[:, 3, :], w[:, 1, :], 0.0, tf, ti, S)
        sin_reduced(c, w[:, 2, :], w[:, 1, :], np.pi / 2, tf, ti, S)
        c.op("dve", lambda e: e.tensor_tensor(w[:, 4, :], w[:, 0, :], w[:, 2, :], ALU.mult), reads=[S], writes=[S])
        c.op("dve", lambda e: e.tensor_tensor(w[:, 5, :], w[:, 0, :], w[:, 3, :], ALU.mult), reads=[S], writes=[S])
        c.op("dve", lambda e: e.tensor_scalar_add(w[:, 6, :], w[:, 4, :], -1.0), reads=[S], writes=[S])
        c.op("dve", lambda e: e.tensor_tensor(w[:, 7, :], LR, LR, ALU.mult), reads=[S], writes=[S])
        c.op("dve", lambda e: e.tensor_tensor(w[:, 10, :], LI, LI, ALU.mult), reads=[S], writes=[S])
        c.op("dve", lambda e: e.tensor_tensor(w[:, 7, :], w[:, 7, :], w[:, 10, :], ALU.add), reads=[S], writes=[S])
        c.op("dve", lambda e: e.reciprocal(w[:, 7, :], w[:, 7, :]), reads=[S], writes=[S])
        c.op("dve", lambda e: e.tensor_tensor(w[:, 8, :], w[:, 6, :], LR, ALU.mult), reads=[S], writes=[S])
        c.op("dve", lambda e: e.tensor_tensor(w[:, 10, :], w[:, 5, :], LI, ALU.mult), reads=[S], writes=[S])
        c.op("dve", lambda e: e.tensor_tensor(w[:, 8, :], w[:, 8, :], w[:, 10, :], ALU.add), reads=[S], writes=[S])
        c.op("dve", lambda e: e.tensor_tensor(w[:, 8, :], w[:, 8, :], w[:, 7, :], ALU.mult), reads=[S], writes=[S])
        c.op("dve", lambda e: e.tensor_tensor(w[:, 9, :], w[:, 5, :], LR, ALU.mult), reads=[S], writes=[S])
        c.op("dve", lambda e: e.tensor_tensor(w[:, 10, :], w[:, 6, :], LI, ALU.mult), reads=[S], writes=[S])
        c.op("dve", lambda e: e.tensor_tensor(w[:, 9, :], w[:, 9, :], w[:, 10, :], ALU.subtract), reads=[S], writes=[S])
        c.op("dve", lambda e: e.tensor_tensor(w[:, 9, :], w[:, 9, :], w[:, 7, :], ALU.mult), reads=[S], writes=[S])
        c.op("dve", lambda e: e.tensor_scalar_mul(w[:, 11, :], w[:, 9, :], -1.0), reads=[S], writes=[S])
        Pr, Pi, nPi = g.Pr, g.Pi, g.nPi
        c.op("dve", lambda e: e.tensor_copy(Pr[:, :, 0], w[:, 4, :]), reads=[S], writes=["P"])
        c.op("dve", lambda e: e.tensor_copy(Pi[:, :, 0], w[:, 5, :]), reads=[S, "P"], writes=["P"])
        for k in range(NRND):
            c.op("dve", (lambda k: lambda e: e.tensor_scalar_mul(nPi[:, :, k], Pi[:, :, k], -1.0))(k), reads=["P"], writes=["P"])
            if k + 1 < NRND:
                c.op("dve", (lambda k: lambda e: e.tensor_tensor(w[:, 10, :], Pi[:, :, k], Pi[:, :, k], ALU.mult))(k), reads=["P", S], writes=[S])
                c.op("dve", (lambda k: lambda e: e.tensor_tensor(Pr[:, :, k + 1], Pr[:, :, k], Pr[:, :, k], ALU.mult))(k), reads=["P"], writes=["P"])
                c.op("dve", (lambda k: lambda e: e.tensor_tensor(Pr[:, :, k + 1], Pr[:, :, k + 1], w[:, 10, :], ALU.subtract))(k), reads=["P", S], writes=["P"])
                c.op("dve", (lambda k: lambda e: e.scalar_tensor_tensor(Pi[:, :, k + 1], Pr[:, :, k], 2.0, Pi[:, :, k], ALU.mult, ALU.mult))(k), reads=["P"], writes=["P"])
        for j in range(16):
            q = j % 4
            for gg in range(2):
                ps_ = slice(gg * 64, (gg + 1) * 64)
                cs = slice(32 * q + 16 * gg, 32 * q + 16 * gg + 16)
                cr, ci, nci = w[ps_, 8, j:j + 1], w[ps_, 9, j:j + 1], w[ps_, 11, j:j + 1]
                c.op("dve", (lambda j, ps_, cs, cr: lambda e: e.tensor_scalar_mul(Bf[ps_, 0, j, cs], Bn[ps_, 0, j, :], cr))(j, ps_, cs, cr), reads=[S, "es_Bn", "es_Bf"], writes=["es_Bf"])
                c.op("dve", (lambda j, ps_, cs, nci: lambda e: e.scalar_tensor_tensor(Bf[ps_, 0, j, cs], Bn[ps_, 1, j, :], nci, Bf[ps_, 0, j, cs], ALU.mult, ALU.add))(j, ps_, cs, nci), reads=[S, "es_Bn", "es_Bf"], writes=["es_Bf"])
                c.op("dve", (lambda j, ps_, cs, cr: lambda e: e.tensor_scalar_mul(Bf[ps_, 1, j, cs], Bn[ps_, 1, j, :], cr))(j, ps_, cs, cr), reads=[S, "es_Bn", "es_Bf"], writes=["es_Bf"])
                c.op("dve", (lambda j, ps_, cs, ci: lambda e: e.scalar_tensor_tensor(Bf[ps_, 1, j, cs], Bn[ps_, 0, j, :], ci, Bf[ps_, 1, j, cs], ALU.mult, ALU.add))(j, ps_, cs, ci), reads=[S, "es_Bn", "es_Bf"], writes=["es_Bf"])
        for j in range(16):
            for ri, (src, srck, dst, scale) in enumerate(((Bf, "es_Bf", g.lBre, 1.0), (Bf, "es_Bf", g.lBim, 1.0), (Cf, "es_Cf2", g.lCre, 1.0), (Cf, "es_Cf2", g.lCim, -1.0))):
                r = ri % 2
                bk = bank[1 + (ri % 2)]
                bkk = "bank%d" % (1 + (ri % 2))
                c.op("pe", (lambda src, r, j, bk: lambda e: e.transpose(bk[:, 0:128], src[:, r, j, :], g.ident[:]))(src, r, j, bk), reads=[srck, "es_Cf", "ident"], writes=[bkk])
                c.op("act", (lambda dst, j, bk, scale: lambda e: e.activation(out=dst[:, j, :], in_=bk[:, 0:128], func=AF.Copy, scale=scale))(dst, j, bk, scale), reads=[bkk], writes=["lBC"])


def even_phase(c, g, W, layer, mi, SEQ, NSEQ, hsrc, hdst):
    bank = g.bank
    dbg = g.cfg.get("dbg")
    if dbg != "nosetup":
        even_setup(c, g, W, mi)
    if dbg in ("setup", "none"):
        c.dma("sp", lambda e: e.dma_start(out=hdst[:, :], in_=hsrc[:, :]), writes=["hdst"])
        return
    NB = SEQ // TB
    with c.phase():
        win = c.sb("e_win", [128, 8, 2048], BF16)
        wout = c.sb("e_wout", [128, 8, 1024], BF16)
        wglu = c.sb("e_wglu", [128, 4, 512], BF16)
        bglu = c.sb("e_bglu", [128, 4], F32)
        dsk = c.sb("e_dsk", [128, 4], F32)
        cw = c.sb("e_cw", [128, 4, 3], F32)
        gam = c.sb("e_gam", [128, D], F32)
        bet = c.sb("e_bet", [128, D], F32)
        hrow = [c.sb("e_hrow%d" % i, [128, D], F32) for i in range(2)]
        hT = c.sb("e_hT", [128, 8, TB], BF16)
        gb = c.sb("e_gb", [128, 4, TB], F32)
        vb = c.sb("e_vb", [128, 4, TB + 2], F32)
        gct = c.sb("e_gct", [128, TB], F32)
        uT = c.sb("e_uT", [128, 4, TB], BF16)
        u32 = c.sb("e_u32", [128, 4, TB], F32)
        xs = [[c.sb("e_x%d%d" % (a, b), [128, TB], F32) for b in range(2)] for a in range(2)]
        xbf = [c.sb("e_xbf%d" % b, [128, TB], BF16) for b in range(2)]
        Xst = c.sb("e_Xst", [128, 16, 2], F32)
        ycT = c.sb("e_ycT", [128, 8, TB], BF16)
        yt = c.sb("e_yt", [128, TB], F32)
        tt_ = c.sb("e_tt", [128, TB], F32)
        z32 = c.sb("e_z32", [128, 4, TB], F32)
        zT = c.sb("e_zT", [128, 4, TB], BF16)
        outt = [c.sb("e_out%d" % i, [128, D], F32) for i in range(2)]
        for kc in range(8):
            c.dma("pool", (lambda kc: lambda e: e.dma_start(out=win[:, kc, :], in_=W["hy_w_in"][mi, kc * 128:(kc + 1) * 128, :], max_dma_last_dim=8192))(kc), cowrites=["win"])
            c.dma("pool", (lambda kc: lambda e: e.dma_start(out=wout[:, kc, :], in_=W["hy_w_out"][mi, kc * 128:(kc + 1) * 128, :], max_dma_last_dim=4096))(kc), cowrites=["wout"])
        for kc in range(4):
            c.dma("pool", (lambda kc: lambda e: e.dma_start(out=wglu[:, kc, :], in_=W["ssm_w_glu"][mi, kc * 128:(kc + 1) * 128, :], max_dma_last_dim=2048))(kc), cowrites=["wglu"])
            c.dma("sp", (lambda kc: lambda e: e.dma_start(out=bglu[:, kc:kc + 1], in_=W["ssm_b_glu"][mi, kc * 128:(kc + 1) * 128].rearrange("(p o) -> p o", o=1)))(kc), cowrites=["small"])
            c.dma("sp", (lambda kc: lambda e: e.dma_start(out=dsk[:, kc:kc + 1], in_=W["ssm_d"][mi].rearrange("g h -> (g h)")[kc * 128:(kc + 1) * 128].rearrange("(p o) -> p o", o=1)))(kc), cowrites=["small"])
            for k in range(3):
                c.dma("sp", (lambda kc, k: lambda e: e.dma_start(out=cw[:, kc, k:k + 1], in_=W["conv_w"][mi, k, kc * 128:(kc + 1) * 128].rearrange("(p o) -> p o", o=1)))(kc, k), cowrites=["small"])
        c.dma("sp", lambda e: e.dma_start(out=gam[:], in_=W["ln_mix_g"][layer:layer + 1, :].partition_broadcast(128)), cowrites=["e_gam"])
        c.dma("sp", lambda e: e.dma_start(out=bet[:], in_=W["ln_mix_b"][layer:layer + 1, :].partition_broadcast(128)), cowrites=["e_gam"])
        for s in range(NSEQ):
            for tb in range(NB):
                t0 = s * SEQ + tb * TB
                for tt in range(TB // 128):
                    hr = hrow[tt % 2]
                    hk = "e_hrow%d" % (tt % 2)
                    c.dma("sp", (lambda tt, hr, t0: lambda e: e.dma_start(out=hr[:], in_=hsrc[t0 + tt * 128:t0 + (tt + 1) * 128, :]))(tt, hr, t0), writes=[hk])
                    for half in range(2):
                        for jj in range(4):
                            kc = half * 4 + jj
                            c.op("pe", (lambda kc, jj, hr: lambda e: e.transpose(bank[0][:, jj * 128:(jj + 1) * 128], hr[:, kc * 128:(kc + 1) * 128], g.ident[:]))(kc, jj, hr),
                                 reads=[hk, "ident"], writes=["bank0"])
                        c.op("act", (lambda half, tt: lambda e: e.activation(out=hT[:, half * 4:(half + 1) * 4, tt * 128:(tt + 1) * 128], in_=bank[0][:, :].rearrange("p (a b) -> p a b", a=4), func=AF.Copy))(half, tt),
                             reads=["bank0"], writes=["e_hT"])
                stg = g.cfg.get("stages", "abcdef")
                def proj(oc, bk, bkk):
                    for kc in range(8):
                        c.op("pe", (lambda kc: lambda e: e.matmul(bk[:, :], win[:, kc, oc * 128:(oc + 1) * 128], hT[:, kc, :], start=(kc == 0), stop=(kc == 7)))(kc),
                             reads=["win", "e_hT"], writes=[bkk])
                for cc in range(4 if "b" in stg else 0):
                    if "1" in stg or "c" in stg:
                        proj(cc, bank[1], "bank1")
                        c.op("act", (lambda cc: lambda e: e.activation(out=gb[:, cc, :], in_=bank[1][:, :], func=AF.Copy))(cc), reads=["bank1"], writes=["e_gb"])
                    if "2" in stg or "c" in stg:
                        proj(4 + cc, bank[2], "bank2")
                        c.op("act", lambda e: e.activation(out=gct[:], in_=bank[2][:, :], func=AF.Copy), reads=["bank2"], writes=["e_gct"])
                    if "3" in stg or "c" in stg:
                        proj(8 + cc, bank[1], "bank1")
                        if tb == 0:
                            c.op("dve", (lambda cc: lambda e: e.memset(vb[:, cc, 0:2], 0.0))(cc), writes=["e_vh%d" % cc], reads=["e_vb%d" % cc])
                        c.op("dve", (lambda cc: lambda e: e.tensor_tensor(vb[:, cc, 2:TB + 2], gct[:], bank[1][:, :], ALU.mult))(cc), reads=["bank1", "e_gct"], writes=["e_vb%d" % cc])
                    if "4" in stg or "d" in stg:
                        proj(12 + cc, bank[2], "bank2")
                        c.op("dve", (lambda cc: lambda e: e.tensor_copy(u32[:, cc, :], bank[2][:, :]))(cc), reads=["bank2"], writes=["e_u32"])
                        c.op("act", (lambda cc: lambda e: e.activation(out=uT[:, cc, :], in_=u32[:, cc, :], func=AF.Copy))(cc), reads=["e_u32"], writes=["e_uT"])
                    if "c" not in stg:
                        continue
                    vk = ["e_vb%d" % cc, "e_vh%d" % cc]
                    c.op("dve", (lambda cc: lambda e: e.tensor_scalar_mul(tt_[:], vb[:, cc, 0:TB], cw[:, cc, 0:1]))(cc), reads=vk + ["small"], writes=["e_tt"])
                    c.op("dve", (lambda cc: lambda e: e.scalar_tensor_tensor(tt_[:], vb[:, cc, 1:TB + 1], cw[:, cc, 1:2], tt_[:], ALU.mult, ALU.add))(cc), reads=vk + ["small", "e_tt"], writes=["e_tt"])
                    c.op("dve", (lambda cc: lambda e: e.scalar_tensor_tensor(tt_[:], vb[:, cc, 2:TB + 2], cw[:, cc, 2:3], tt_[:], ALU.mult, ALU.add))(cc), reads=vk + ["small", "e_tt"], writes=["e_tt"])
                    c.op("dve", (lambda cc: lambda e: e.tensor_tensor(ycT[:, cc, :], tt_[:], gb[:, cc, :], ALU.mult))(cc), reads=["e_tt", "e_gb"], writes=["e_ycT"])
                    c.op("dve", (lambda cc: lambda e: e.tensor_copy(vb[:, cc, 0:2], vb[:, cc, TB:TB + 2]))(cc), reads=vk, writes=["e_vh%d" % cc])
                for j in range(16 if "d" in stg else 0):
                    cc = j // 4
                    c.op("pe", (lambda j, cc: lambda e: e.matmul(bank[3][:, :], g.lBre[:, j, :], uT[:, cc, :], start=True, stop=True))(j, cc), reads=["lBC", "e_uT"], writes=["bank3"])
                    c.op("pe", (lambda j, cc: lambda e: e.matmul(bank[4][:, :], g.lBim[:, j, :], uT[:, cc, :], start=True, stop=True))(j, cc), reads=["lBC", "e_uT"], writes=["bank4"])
                    c.op("act", lambda e: e.activation(out=xs[0][0][:], in_=bank[3][:, :], func=AF.Copy), reads=["bank3"], writes=["e_xs0"])
                    c.op("dve", lambda e: e.tensor_copy(xs[0][1][:], bank[4][:, :]), reads=["bank4"], writes=["e_xs0"])
                    if tb > 0:
                        ar, ai, nai = g.Pr[:, j, 0:1], g.Pi[:, j, 0:1], g.nPi[:, j, 0:1]
                        Xr, Xi = Xst[:, j, 0:1], Xst[:, j, 1:2]
                        c.op("dve", (lambda ar, Xr: lambda e: e.scalar_tensor_tensor(xs[0][0][:, 0:1], Xr, ar, xs[0][0][:, 0:1], ALU.mult, ALU.add))(ar, Xr), reads=["P", "e_Xst", "e_xs0"], writes=["e_xs0"])
                        c.op("dve", (lambda nai, Xi: lambda e: e.scalar_tensor_tensor(xs[0][0][:, 0:1], Xi, nai, xs[0][0][:, 0:1], ALU.mult, ALU.add))(nai, Xi), reads=["P", "e_Xst", "e_xs0"], writes=["e_xs0"])
                        c.op("dve", (lambda ar, Xi: lambda e: e.scalar_tensor_tensor(xs[0][1][:, 0:1], Xi, ar, xs[0][1][:, 0:1], ALU.mult, ALU.add))(ar, Xi), reads=["P", "e_Xst", "e_xs0"], writes=["e_xs0"])
                        c.op("dve", (lambda ai, Xr: lambda e: e.scalar_tensor_tensor(xs[0][1][:, 0:1], Xr, ai, xs[0][1][:, 0:1], ALU.mult, ALU.add))(ai, Xr), reads=["P", "e_Xst", "e_xs0"], writes=["e_xs0"])
                    cur = 0
                    for k in range(NRND):
                        sft = 1 << k
                        src, dst = xs[cur], xs[1 - cur]
                        sk_, dk_ = "e_xs%d" % cur, "e_xs%d" % (1 - cur)
                        pr, pi, npi = g.Pr[:, j, k:k + 1], g.Pi[:, j, k:k + 1], g.nPi[:, j, k:k + 1]
                        c.op("pool", (lambda src, dst, sft: lambda e: e.tensor_copy(dst[0][:, 0:sft], src[0][:, 0:sft]))(src, dst, sft), reads=[sk_], writes=[dk_])
                        c.op("pool", (lambda src, dst, sft: lambda e: e.tensor_copy(dst[1][:, 0:sft], src[1][:, 0:sft]))(src, dst, sft), reads=[sk_, dk_], writes=[dk_])
                        c.op("dve", (lambda src, dst, sft, pr: lambda e: e.scalar_tensor_tensor(dst[0][:, sft:TB], src[0][:, 0:TB - sft], pr, src[0][:, sft:TB], ALU.mult, ALU.add))(src, dst, sft, pr), reads=[sk_, dk_, "P"], writes=[dk_])
                        c.op("dve", (lambda src, dst, sft, npi: lambda e: e.scalar_tensor_tensor(dst[0][:, sft:TB], src[1][:, 0:TB - sft], npi, dst[0][:, sft:TB], ALU.mult, ALU.add))(src, dst, sft, npi), reads=[sk_, dk_, "P"], writes=[dk_])
                        c.op("dve", (lambda src, dst, sft, pr: lambda e: e.scalar_tensor_tensor(dst[1][:, sft:TB], src[1][:, 0:TB - sft], pr, src[1][:, sft:TB], ALU.mult, ALU.add))(src, dst, sft, pr), reads=[sk_, dk_, "P"], writes=[dk_])
                        c.op("dve", (lambda src, dst, sft, pi: lambda e: e.scalar_tensor_tensor(dst[1][:, sft:TB], src[0][:, 0:TB - sft], pi, dst[1][:, sft:TB], ALU.mult, ALU.add))(src, dst, sft, pi), reads=[sk_, dk_, "P"], writes=[dk_])
                        cur = 1 - cur
                    fin = xs[cur]
                    fk = "e_xs%d" % cur
                    c.op("pool", (lambda fin, j: lambda e: e.tensor_copy(Xst[:, j, 0:1], fin[0][:, TB - 1:TB]))(fin, j), reads=[fk], writes=["e_Xst"])
                    c.op("pool", (lambda fin, j: lambda e: e.tensor_copy(Xst[:, j, 1:2], fin[1][:, TB - 1:TB]))(fin, j), reads=[fk, "e_Xst"], writes=["e_Xst"])
                    c.op("act", (lambda fin: lambda e: e.activation(out=xbf[0][:], in_=fin[0][:], func=AF.Copy))(fin), reads=[fk], writes=["e_xbf0"])
                    c.op("act", (lambda fin: lambda e: e.activation(out=xbf[1][:], in_=fin[1][:], func=AF.Copy))(fin), reads=[fk], writes=["e_xbf1"])
                    c.op("pe", (lambda j: lambda e: e.matmul(bank[5][:, :], g.lCre[:, j, :], xbf[0][:], start=(j % 4 == 0), stop=False))(j), reads=["lBC", "e_xbf0"], writes=["bank5"])
                    c.op("pe", (lambda j: lambda e: e.matmul(bank[5][:, :], g.lCim[:, j, :], xbf[1][:], start=False, stop=(j % 4 == 3)))(j), reads=["lBC", "e_xbf1"], writes=["bank5"])
                    if j % 4 == 3:
                        c.op("dve", (lambda cc: lambda e: e.scalar_tensor_tensor(yt[:], u32[:, cc, :], dsk[:, cc:cc + 1], bank[5][:, :], ALU.mult, ALU.add))(cc), reads=["bank5", "e_u32", "small"], writes=["e_yt"])
                        c.op("act", lambda e: e.activation(out=tt_[:], in_=yt[:], func=AF.Square), reads=["e_yt"], writes=["e_tt"])
                        c.op("dve", lambda e: e.tensor_scalar(tt_[:], tt_[:], 0.044715, 1.0, ALU.mult, ALU.add), reads=["e_tt"], writes=["e_tt"])
                        c.op("dve", lambda e: e.tensor_tensor(tt_[:], tt_[:], yt[:], ALU.mult), reads=["e_tt", "e_yt"], writes=["e_tt"])
                        c.op("act", lambda e: e.activation(out=tt_[:], in_=tt_[:], func=AF.Sigmoid, scale=1.5957691216057308), reads=["e_tt"], writes=["e_tt"])
                        c.op("dve", (lambda cc: lambda e: e.tensor_tensor(z32[:, cc, :], tt_[:], yt[:], ALU.mult))(cc), reads=["e_tt", "e_yt"], writes=["e_z32"])
                        c.op("act", (lambda cc: lambda e: e.activation(out=zT[:, cc, :], in_=z32[:, cc, :], func=AF.Copy))(cc), reads=["e_z32"], writes=["e_zT"])
                for oc in range(4 if "e" in stg else 0):
                    for kc in range(4):
                        c.op("pe", (lambda oc, kc: lambda e: e.matmul(bank[6][:, :], wglu[:, kc, oc * 128:(oc + 1) * 128], zT[:, kc, :], start=(kc == 0), stop=(kc == 3)))(oc, kc), reads=["wglu", "e_zT"], writes=["bank6"])
                    c.op("act", (lambda oc: lambda e: e.activation(out=tt_[:], in_=bank[6][:, :], func=AF.Sigmoid, bias=bglu[:, oc:oc + 1], scale=1.0))(oc), reads=["bank6", "small"], writes=["e_tt"])
                    c.op("dve", (lambda oc: lambda e: e.tensor_tensor(ycT[:, 4 + oc, :], tt_[:], z32[:, oc, :], ALU.mult))(oc), reads=["e_tt", "e_z32"], writes=["e_ycT"])
                for tt in range(TB // 128 if "f" in stg else 0):
                    hr = hrow[tt % 2]
                    hk = "e_hrow%d" % (tt % 2)
                    ob = outt[tt % 2]
                    ok = "e_out%d" % (tt % 2)
                    c.dma("sp", (lambda tt, hr, t0: lambda e: e.dma_start(out=hr[:], in_=hsrc[t0 + tt * 128:t0 + (tt + 1) * 128, :]))(tt, hr, t0), writes=[hk])
                    for n in range(2):
                        for kc in range(8):
                            c.op("pe", (lambda tt, n, kc: lambda e: e.matmul(bank[6 + n][:, :], ycT[:, kc, tt * 128:(tt + 1) * 128], wout[:, kc, n * 512:(n + 1) * 512], start=(kc == 0), stop=(kc == 7)))(tt, n, kc),
                                 reads=["e_ycT", "wout"], writes=["bank%d" % (6 + n)])
                        c.op("dve", (lambda n, hr: lambda e: e.scalar_tensor_tensor(hr[:, n * 512:(n + 1) * 512], hr[:, n * 512:(n + 1) * 512], float(DN_ALPHA), bank[6 + n][:, :], ALU.mult, ALU.add))(n, hr),
                             reads=["bank%d" % (6 + n), hk], writes=[hk])
                    layer_norm_tile(c, g, hr, hk, gam, bet, "e_gam", ob, ok, "e")
                    c.dma("sp", (lambda tt, ob, t0: lambda e: e.dma_start(out=hdst[t0 + tt * 128:t0 + (tt + 1) * 128, :], in_=ob[:]))(tt, ob, t0), reads=[ok], cowrites=["hdst"])
        if "f" not in g.cfg.get("stages", "abcdef"):
            c.dma("sp", lambda e: e.dma_start(out=hdst[:, :], in_=hsrc[:, :]), writes=["hdst"])


VW = NH * 65
RW = 2 * D + VW


def odd_phase(c, g, W, layer, mi, SEQ, NSEQ, hsrc, hdst):
    bank = g.bank
    TT = SEQ * NSEQ
    NT = TT // 128
    qkv_d = g.qkv_d
    O_d = g.O_d
    stg = g.cfg.get("stages", "ABC")
    with c.phase():
        wqkv = c.sb("o_wqkv", [128, 8, 3 * D], BF16)
        hrow = [c.sb("o_hrow%d" % i, [128, D], F32) for i in range(2)]
        hT = c.sb("o_hT", [128, 8, 128], BF16)
        cs = [c.sb("o_cs%d" % i, [128, 2, 512], F32) for i in range(2)]
        qk32 = c.sb("o_qk32", [128, 2, D], F32)
        tq = [c.sb("o_tq%d" % i, [128, 512], F32) for i in range(2)]
        tk = [c.sb("o_tk%d" % i, [128, 512], F32) for i in range(2)]
        rows = [c.sb("o_rows%d" % i, [128, RW], BF16) for i in range(2)]
        for kc in range(8):
            for part in range(3):
                c.dma("pool", (lambda kc, part: lambda e: e.dma_start(out=wqkv[:, kc, part * D:(part + 1) * D], in_=W["att_w_qkv"][mi, kc * 128:(kc + 1) * 128, part * D:(part + 1) * D], max_dma_last_dim=4096))(kc, part), cowrites=["wqkv"])
        for i in range(2):
            c.op("pool", (lambda i: lambda e: e.memset(rows[i][:, 2 * D:RW], 1.0))(i), writes=["o_rows%d" % i])
        for i in range(NT if "A" in stg else 0):
            b = i % 2
            hr, hk = hrow[b], "o_hrow%d" % b
            pos0 = (i * 128) % SEQ
            c.dma("sp", (lambda i, hr: lambda e: e.dma_start(out=hr[:], in_=hsrc[i * 128:(i + 1) * 128, :]))(i, hr), writes=[hk])
            c.dma("sp", (lambda b, pos0: lambda e: e.dma_start(out=cs[b][:, 0, :], in_=W["c_cos16"][pos0:pos0 + 128, :]))(b, pos0), cowrites=["o_cs%d" % b], reads=["o_csr%d" % b])
            c.dma("sp", (lambda b, pos0: lambda e: e.dma_start(out=cs[b][:, 1, :], in_=W["c_sin16"][pos0:pos0 + 128, :]))(b, pos0), cowrites=["o_cs%d" % b], reads=["o_csr%d" % b])
            transpose_rows_to_T(c, g, hr, hk, hT, "o_hT", bank[0], "bank0", fp32_in=True, evac="act")
            for n in range(6):
                for kc in range(8):
                    c.op("pe", (lambda n, kc: lambda e: e.matmul(bank[2 + n][:, :], hT[:, kc, :], wqkv[:, kc, n * 512:(n + 1) * 512], start=(kc == 0), stop=(kc == 7)))(n, kc),
                         reads=["o_hT", "wqkv"], writes=["bank%d" % (2 + n)])
            R = rows[b]
            Rk = "o_rows%d" % b
            for n in range(4):
                c.op("act", (lambda n: lambda e: e.activation(out=qk32[:, n // 2, (n % 2) * 512:(n % 2 + 1) * 512], in_=bank[2 + n][:, :], func=AF.Copy))(n),
                     reads=["bank%d" % (2 + n)], writes=["o_qk32_%d" % (n // 2)])
            for n in range(2):
                c.op("act", (lambda n, R: lambda e: e.activation(out=R[:, 2 * D:RW].rearrange("p (h e) -> p h e", e=65)[:, n * 8:(n + 1) * 8, 0:64], in_=bank[6 + n][:, :].rearrange("p (h e) -> p h e", e=64), func=AF.Copy))(n, R),
                     reads=["bank%d" % (6 + n), Rk], writes=[Rk + "v"])
            for which, eng, tmp in ((0, "dve", tq), (1, "pool", tk)):
                x3 = qk32[:, which, :].rearrange("p (h e) -> p h e", e=64)
                o3 = R[:, which * D:(which + 1) * D].rearrange("p (h e) -> p h e", e=64)
                cosv = cs[b][:, 0, :].rearrange("p (h e) -> p h e", e=32)
                sinv = cs[b][:, 1, :].rearrange("p (h e) -> p h e", e=32)
                t1 = tmp[0][:].rearrange("p (h e) -> p h e", e=32)
                t2 = tmp[1][:].rearrange("p (h e) -> p h e", e=32)
                xk = "o_qk32_%d" % which
                tk_ = "o_tmp%d" % which
                rk = Rk + ("q" if which == 0 else "k")
                csk = "o_cs%d" % b
                c.op(eng, (lambda t1, x3, cosv: lambda e: e.tensor_tensor(t1, x3[:, :, 0:32], cosv, ALU.mult))(t1, x3, cosv), reads=[xk, csk], writes=[tk_ + "a"])
                c.op(eng, (lambda t2, x3, sinv: lambda e: e.tensor_tensor(t2, x3[:, :, 32:64], sinv, ALU.mult))(t2, x3, sinv), reads=[xk, csk], writes=[tk_ + "b"])
                c.op(eng, (lambda o3, t1, t2: lambda e: e.tensor_tensor(o3[:, :, 0:32], t1, t2, ALU.subtract))(o3, t1, t2), reads=[tk_ + "a", tk_ + "b"], writes=[rk])
                c.op(eng, (lambda t1, x3, cosv: lambda e: e.tensor_tensor(t1, x3[:, :, 32:64], cosv, ALU.mult))(t1, x3, cosv), reads=[xk, csk, rk], writes=[tk_ + "a"])
                c.op(eng, (lambda t2, x3, sinv: lambda e: e.tensor_tensor(t2, x3[:, :, 0:32], sinv, ALU.mult))(t2, x3, sinv), reads=[xk, csk, rk], writes=[tk_ + "b"])
                ev = c.op(eng, (lambda o3, t1, t2: lambda e: e.tensor_tensor(o3[:, :, 32:64], t1, t2, ALU.add))(o3, t1, t2), reads=[tk_ + "a", tk_ + "b"], writes=[rk])
                c.readers.setdefault("o_csr%d" % b, []).append(ev)
            c.dma("sp", (lambda i, R: lambda e: e.dma_start(out=qkv_d[i * 128:(i + 1) * 128, :], in_=R[:, :]))(i, R),
                  reads=[Rk + "q", Rk + "k", Rk + "v", Rk], cowrites=["qkv_d"])
            for sfx in ("q", "k", "v"):
                c.readers.setdefault(Rk + sfx, []).append(c.last_w["qkv_d"][-1])

    with c.phase():
        R3 = [c.sb("o_R%d" % i, [128, RW], BF16) for i in range(3)]
        KT = [c.sb("o_KT%d" % i, [128, 8, 128], BF16) for i in range(2)]
        QTz = [c.sb("o_QT%d" % i, [128, 8, 128], BF16) for i in range(2)]
        for i in range(2):
            c.op("pool", (lambda i: lambda e: e.memset(QTz[i][:], 0.0))(i), writes=["o_QT"])
        PT = [c.sb("o_PT%d" % i, [128, 512], BF16) for i in range(2)]
        mcur = c.sb("o_mcur", [128, 512], BF16)
        mprev = c.sb("o_mprev", [128, 512], BF16)
        osb = [c.sb("o_osb%d" % i, [128, VW], F32) for i in range(2)]
        c.dma("pool", lambda e: e.dma_start(out=mcur[:], in_=W["c_mcur"][:, :]), writes=["o_mcur"])
        c.dma("pool", lambda e: e.dma_start(out=mprev[:], in_=W["c_mprev"][:, :]), writes=["o_mprev"])
        hb_ = [(0, 7), (7, 14), (14, 16)]
        ucount = 0
        for s in range(NSEQ if "B" in stg else 0):
            for pi, (win_, dil) in enumerate(PATTERNS):
                nb = SEQ // (128 * dil)
                qv = qkv_d[s * SEQ:(s + 1) * SEQ, :].rearrange("(m d) c -> d m c", d=dil)
                ov = O_d[pi, s * SEQ:(s + 1) * SEQ, :].rearrange("(m d) c -> d m c", d=dil)
                for r in range(dil):
                    for n in range(nb):
                        u = ucount
                        ucount += 1
                        R = R3[u % 3]
                        Rk = "o_R%d" % (u % 3)
                        Rp = R3[(u - 1) % 3]
                        Rpk = "o_R%d" % ((u - 1) % 3)
                        kt, ktk = KT[u % 2], "o_KT%d" % (u % 2)
                        ktp, ktpk = KT[(u - 1) % 2], "o_KT%d" % ((u - 1) % 2)
                        ob, obk = osb[u % 2], "o_osb%d" % (u % 2)
                        c.dma("sp", (lambda R, qv, r, n: lambda e: e.dma_start(out=R[:, :], in_=qv[r, n * 128:(n + 1) * 128, :]))(R, qv, r, n), reads=["qkv_d"], writes=[Rk])
                        b0 = bank[0][:].bitcast(BF16)
                        b1 = bank[1][:].bitcast(BF16)
                        for pr in range(8):
                            c.op("pe", (lambda pr, R, b0: lambda e: e.transpose(b0[:, pr * 128:(pr + 1) * 128], R[:, D + pr * 128:D + (pr + 1) * 128], g.identb[:]))(pr, R, b0), reads=[Rk, "identb"], writes=["bank0"])
                        c.op("dve", (lambda kt, b0: lambda e: e.tensor_copy(kt[:].rearrange("p a b -> p (a b)"), b0[:, 0:1024]))(kt, b0), reads=["bank0"], writes=[ktk])
                        for pr in range(8):
                            c.op("pe", (lambda pr, R, b1: lambda e: e.transpose(b1[:, pr * 128:(pr + 1) * 128], R[:, pr * 128:(pr + 1) * 128], g.identb[:]))(pr, R, b1), reads=[Rk, "identb"], writes=["bank1"])
                        c.op("dve", (lambda b1: lambda e: e.tensor_copy(QTz[0][0:64].rearrange("p a b -> p (a b)"), b1[0:64, 0:1024]))(b1), reads=["bank1"], writes=["o_QT"])
                        c.op("dve", (lambda b1: lambda e: e.tensor_copy(QTz[1][64:128].rearrange("p a b -> p (a b)"), b1[64:128, 0:1024]))(b1), reads=["bank1", "o_QT"], writes=["o_QT"])
                        kbs = ([("prev", ktp, ktpk, Rp, Rpk, mprev, "o_mprev")] if n > 0 else []) + [("cur", kt, ktk, R, Rk, mcur, "o_mcur")]
                        bsub = g.cfg.get("bsub", "spo")
                        if "s" not in bsub:
                            kbs = []
                        started = [False, False, False]
                        gi = 0
                        for (nm, kT_, kTk, Rv, Rvk, msk, mskk) in kbs:
                            for grp in range(4):
                                sb_ = bank[2 + gi % 2]
                                sbk = "bank%d" % (2 + gi % 2)
                                pt, ptk = PT[gi % 2], "o_PT%d" % (gi % 2)
                                gi += 1
                                for hh in range(4):
                                    h = grp * 4 + hh
                                    pr, hf = h // 2, h % 2
                                    c.op("pe", (lambda sb_, hh, kT_, pr, hf: lambda e: e.matmul(sb_[:, hh * 128:(hh + 1) * 128], kT_[:, pr, :], QTz[hf][:, pr, :], start=True, stop=True))(sb_, hh, kT_, pr, hf),
                                         reads=[kTk, "o_QT"], writes=[sbk])
                                c.op("act", (lambda pt, sb_: lambda e: e.activation(out=pt[:], in_=sb_[:, :], func=AF.Exp, scale=0.125))(pt, sb_), reads=[sbk], writes=[ptk])
                                c.op("pool", (lambda pt, msk: lambda e: e.tensor_tensor(pt[:], pt[:], msk[:], ALU.mult))(pt, msk), reads=[ptk, mskk], writes=[ptk])
                                for hh in range(4 if "p" in bsub else 0):
                                    h = grp * 4 + hh
                                    bi = 0 if h < 7 else (1 if h < 14 else 2)
                                    col = (h - hb_[bi][0]) * 65
                                    st = not started[bi]
                                    started[bi] = True
                                    c.op("pe", (lambda bi, col, pt, hh, Rv, h, st: lambda e: e.matmul(bank[4 + bi][:, col:col + 65], pt[:, hh * 128:(hh + 1) * 128], Rv[:, 2 * D + h * 65:2 * D + (h + 1) * 65], start=st, stop=True, skip_group_check=True))(bi, col, pt, hh, Rv, h, st),
                                         reads=[ptk, Rvk], writes=["bank%d" % (4 + bi)])
                        if "o" not in bsub:
                            continue
                        for bi, (h0, h1) in enumerate(hb_):
                            c.op("dve" if bi != 1 else "act", (lambda bi, h0, h1, ob: lambda e: ecopy(e, "dve" if bi != 1 else "act", ob[:, h0 * 65:h1 * 65], bank[4 + bi][:, 0:(h1 - h0) * 65]))(bi, h0, h1, ob),
                                 reads=["bank%d" % (4 + bi)], writes=[obk + "_%d" % bi])
                        c.dma("sp", (lambda ov, r, n, ob: lambda e: e.dma_start(out=ov[r, n * 128:(n + 1) * 128, :], in_=ob[:, :]))(ov, r, n, ob),
                              reads=[obk + "_0", obk + "_1", obk + "_2"], cowrites=["O_d"])
                        for bi in range(3):
                            c.readers.setdefault(obk + "_%d" % bi, []).append(c.last_w["O_d"][-1])

    with c.phase():
        wo = c.sb("o_wo", [128, 8, D], BF16)
        gam = c.sb("o_gam", [128, D], F32)
        bet = c.sb("o_bet", [128, D], F32)
        hrow = [c.sb("o_hrow%d" % i, [128, D], F32) for i in range(2)]
        ot = [[c.sb("o_ot%d_%d" % (i, p), [128, VW], F32) for p in range(3)] for i in range(2)]
        rden = c.sb("o_rden", [128, NH], F32)
        osb2 = c.sb("o_o", [128, D], BF16)
        oT = c.sb("o_oT", [128, 8, 128], BF16)
        outt = [c.sb("o_out%d" % i, [128, D], F32) for i in range(2)]
        for kc in range(8):
            c.dma("pool", (lambda kc: lambda e: e.dma_start(out=wo[:, kc, :], in_=W["att_w_o"][mi, kc * 128:(kc + 1) * 128, :], max_dma_last_dim=4096))(kc), cowrites=["wo"])
        c.dma("sp", lambda e: e.dma_start(out=gam[:], in_=W["ln_mix_g"][layer:layer + 1, :].partition_broadcast(128)), cowrites=["o_gam"])
        c.dma("sp", lambda e: e.dma_start(out=bet[:], in_=W["ln_mix_b"][layer:layer + 1, :].partition_broadcast(128)), cowrites=["o_gam"])
        for i in range(NT if "C" in stg else 0):
            b = i % 2
            hr, hk = hrow[b], "o_hrow%d" % b
            c.dma("sp", (lambda i, hr: lambda e: e.dma_start(out=hr[:], in_=hsrc[i * 128:(i + 1) * 128, :]))(i, hr), writes=[hk])
            for p in range(3):
                c.dma("sp", (lambda i, b, p: lambda e: e.dma_start(out=ot[b][p][:, :], in_=O_d[p, i * 128:(i + 1) * 128, :]))(i, b, p), reads=["O_d"], writes=["o_ot%d_%d" % (b, p)])
            A = ot[b][0]
            Ak = "o_ot%d_0" % b
            c.op("pool", (lambda b, A: lambda e: e.tensor_tensor(A[:], A[:], ot[b][1][:], ALU.add))(b, A), reads=[Ak, "o_ot%d_1" % b], writes=[Ak])
            c.op("dve", (lambda b, A: lambda e: e.tensor_tensor(A[:], A[:], ot[b][2][:], ALU.add))(b, A), reads=[Ak, "o_ot%d_2" % b], writes=[Ak])
            A3 = A[:].rearrange("p (h e) -> p h e", e=65)
            c.op("dve", (lambda A3: lambda e: e.reciprocal(rden[:], A3[:, :, 64]))(A3), reads=[Ak], writes=["o_rden"])
            for h in range(NH):
                c.op("dve" if h % 2 == 0 else "pool", (lambda h, A3: lambda e: e.tensor_scalar(osb2[:, h * 64:(h + 1) * 64], A3[:, h, 0:64], rden[:, h:h + 1], None, ALU.mult))(h, A3),
                     reads=[Ak, "o_rden"], writes=["o_o%d" % (h % 2)])
            bb = bank[0][:].bitcast(BF16)
            for kc in range(8):
                c.op("pe", (lambda kc, bb: lambda e: e.transpose(bb[:, kc * 128:(kc + 1) * 128], osb2[:, kc * 128:(kc + 1) * 128], g.identb[:]))(kc, bb), reads=["o_o0", "o_o1", "identb"], writes=["bank0"])
            c.op("act", (lambda bb: lambda e: e.activation(out=oT[:].rearrange("p a b -> p (a b)"), in_=bb[:, 0:1024], func=AF.Copy))(bb), reads=["bank0"], writes=["o_oT"])
            for n in range(2):
                for kc in range(8):
                    c.op("pe", (lambda n, kc: lambda e: e.matmul(bank[6 + n][:, :], oT[:, kc, :], wo[:, kc, n * 512:(n + 1) * 512], start=(kc == 0), stop=(kc == 7)))(n, kc),
                         reads=["o_oT", "wo"], writes=["bank%d" % (6 + n)])
                c.op("dve", (lambda n, hr: lambda e: e.scalar_tensor_tensor(hr[:, n * 512:(n + 1) * 512], hr[:, n * 512:(n + 1) * 512], float(DN_ALPHA), bank[6 + n][:, :], ALU.mult, ALU.add))(n, hr),
                     reads=["bank%d" % (6 + n), hk], writes=[hk])
            layer_norm_tile(c, g, hr, hk, gam, bet, "o_gam", outt[b], "o_out%d" % b, "o")
            c.dma("sp", (lambda i, b: lambda e: e.dma_start(out=hdst[i * 128:(i + 1) * 128, :], in_=outt[b][:]))(i, b), reads=["o_out%d" % b], cowrites=["hdst"])
        if "C" not in stg:
            c.dma("sp", lambda e: e.dma_start(out=hdst[:, :], in_=hsrc[:, :]), writes=["hdst"])


NCORES = 4
FULL_SEQ = 8192
FULL_BATCH = 4
RENAME = {"expert_w_gu": "w_gu", "expert_b_gu": "b_gu", "expert_w_down": "w_dn", "expert_b_down": "b_dn"}
_CACHE = {}


def full_plan():
    plan = []
    for layer in range(DEPTH):
        plan.append(("even" if layer % 2 == 0 else "odd", layer, layer // 2))
        plan.append(("moe", layer, layer // 2))
    return plan


def kernel(**inputs):
    nseq = FULL_BATCH // NCORES
    tt = nseq * FULL_SEQ
    nl = {}
    wmap = {}
    for name, arr in inputs.items():
        if name == "x":
            continue
        kname = RENAME.get(name, name)
        wmap[kname] = np.ascontiguousarray(arr, dtype=np.float32)
        nl[kname] = arr.shape[0]
    cap = (tt * TOPK // NE) * 5 // 4
    cap = (cap + 127) // 128 * 128
    cfg = dict(SEQ=FULL_SEQ, NSEQ=nseq, C=cap, plan=full_plan(), nl=nl)
    key = (FULL_SEQ, nseq, cap)
    if key not in _CACHE:
        _CACHE[key] = build_program(cfg)
    nc, cst, _ = _CACHE[key]
    x = np.ascontiguousarray(inputs["x"], dtype=np.float32).reshape(NCORES, tt, D)
    in_maps = []
    for ci in range(NCORES):
        m = {"x": x[ci]}
        m.update(wmap)
        m.update(cst)
        in_maps.append(m)
    res = run_bass_kernel_spmd(nc, in_maps, core_ids=list(range(NCORES)))
    out = np.stack([res.results[ci]["out"] for ci in range(NCORES)], axis=0)
    return out.reshape(FULL_BATCH, FULL_SEQ, D).astype(np.float32)
```

```python
import contextlib
import numpy as np
import ml_dtypes
import concourse.bass as bass
import concourse.mybir as mybir
from concourse.bass_utils import run_bass_kernel_spmd

F32 = mybir.dt.float32
BF16 = mybir.dt.bfloat16
I32 = mybir.dt.int32
U32 = mybir.dt.uint32
AF = mybir.ActivationFunctionType
ALU = mybir.AluOpType
AX = mybir.AxisListType

D = 1024
NE = 32
TOPK = 4
DEPTH = 4
DN_ALPHA = (2 * DEPTH) ** 0.25
LN_EPS = 1e-5
NH = 16
HD = 64
PATTERNS = ((128, 1), (512, 4), (2048, 16))

SAME_ENG_WAIT = True
ENGS = ("pe", "dve", "act", "pool", "sp")
RING = {"sp": 12, "act": 6, "pool": 12}


class Ctx:
    def __init__(self, nc):
        self.nc = nc
        self.stack = contextlib.ExitStack()
        self.prog = {e: [] for e in ENGS}
        self.cnt = {e: 0 for e in ENGS}
        self.semobj = {}
        for e in ENGS:
            self.semobj[("e", e)] = self.stack.enter_context(nc.semaphore("s_" + e))
        for q, n in RING.items():
            for i in range(n):
                self.semobj[("r", q, i)] = self.stack.enter_context(nc.semaphore("r_%s%d" % (q, i)))
        self.dcnt = {q: 0 for q in RING}
        self.seen = {e: {} for e in ENGS}
        self.last_w = {}
        self.readers = {}
        self.n_instr = 0
        self.pstack = None

    def sb(self, name, shape, dt=F32):
        st = self.pstack if self.pstack is not None else self.stack
        self.uid = getattr(self, "uid", 0) + 1
        return st.enter_context(self.nc.sbuf_tensor("%s_u%d" % (name, self.uid), list(shape), dt))

    def reg(self, e, val):
        if not hasattr(self, "_regs"):
            self._regs = {}
        if val not in self._regs:
            self._regs[val] = e.to_reg(val)
        return self._regs[val]

    def ps(self, name, shape, dt=F32):
        return self.stack.enter_context(self.nc.psum_tensor(name, list(shape), dt))

    @contextlib.contextmanager
    def phase(self):
        self.barrier()
        self.pstack = contextlib.ExitStack()
        try:
            yield
        finally:
            self.barrier()
            self.pstack.close()
            self.pstack = None

    def cur_events(self):
        evs = []
        for e in ENGS:
            if self.cnt[e]:
                evs.append((("e", e), self.cnt[e]))
        for q, n in RING.items():
            i = self.dcnt[q]
            for slot in range(n):
                if i > slot:
                    last = ((i - 1 - slot) // n) * n + slot
                    evs.append((("r", q, slot), 16 * (last // n + 1)))
        return evs

    def barrier(self):
        evs = self.cur_events()
        for eng in ENGS:
            for k, v in evs:
                if self.seen[eng].get(k, 0) < v:
                    self.seen[eng][k] = v
                    self.prog[eng].append(("wait", k, v))
        self.last_w = {}
        self.readers = {}

    def _need(self, eng, reads, writes, cowrites=()):
        need = {}

        def add(ev):
            k, v = ev
            if need.get(k, 0) < v:
                need[k] = v
        for k in reads:
            for ev in self.last_w.get(k, ()):
                add(ev)
        for k in writes:
            for ev in self.last_w.get(k, ()):
                add(ev)
            for ev in self.readers.get(k, ()):
                add(ev)
        for k in cowrites:
            for ev in self.readers.get(k, ()):
                add(ev)
        for k, v in need.items():
            if k == ("e", eng) and (eng == "pe" or not SAME_ENG_WAIT):
                continue
            if self.seen[eng].get(k, 0) >= v:
                continue
            self.seen[eng][k] = v
            self.prog[eng].append(("wait", k, v))

    def _commit(self, ev, reads, writes, cowrites=()):
        for k in reads:
            self.readers.setdefault(k, []).append(ev)
        for k in writes:
            self.last_w[k] = [ev]
            self.readers[k] = []
        for k in cowrites:
            self.last_w.setdefault(k, []).append(ev)

    def op(self, eng, fn, reads=(), writes=()):
        self._need(eng, reads, writes)
        self.cnt[eng] += 1
        ev = (("e", eng), self.cnt[eng])
        self.prog[eng].append(("ins", fn, ("e", eng), 1))
        self._commit(ev, reads, writes)
        self.n_instr += 1
        return ev

    def dma(self, q, fn, reads=(), writes=(), cowrites=()):
        self._need(q, reads, writes, cowrites)
        i = self.dcnt[q]
        n = RING[q]
        slot = i % n
        if i >= n:
            k = ("r", q, slot)
            v = 16 * (i // n)
            if self.seen[q].get(k, 0) < v:
                self.seen[q][k] = v
                self.prog[q].append(("wait", k, v))
        self.dcnt[q] += 1
        ev = (("r", q, slot), 16 * (i // n + 1))
        self.prog[q].append(("ins", fn, ("r", q, slot), 16))
        self._commit(ev, reads, writes, cowrites)
        self.n_instr += 1
        return ev

    def emit(self):
        nc = self.nc
        self.barrier()
        with nc.Block() as block:
            def body(engname):
                def f(e):
                    for it in self.prog[engname]:
                        if it[0] == "wait":
                            e.wait_ge(self.semobj[it[1]], it[2])
                        else:
                            it[1](e).then_inc(self.semobj[it[2]], it[3])
                return f
            block.tensor(body("pe"))
            block.vector(body("dve"))
            block.scalar(body("act"))
            block.gpsimd(body("pool"))
            block.sync(body("sp"))
        self.stack.close()


class G:
    pass


def setup_globals(c, g, W):
    g.bank = [c.ps("bank%d" % i, [128, 512], F32) for i in range(8)]
    g.ident = c.sb("ident", [128, 128], F32)
    g.identb = c.sb("identb", [128, 128], BF16)
    g.onesrow = c.sb("onesrow", [1, 128], BF16)
    c.dma("sp", lambda e: e.dma_start(out=g.ident[:], in_=W["c_ident"][:, :]), writes=["ident"])
    c.dma("pool", lambda e: e.dma_start(out=g.identb[:], in_=W["c_ident"][:, :]), writes=["identb"])
    c.dma("pool", lambda e: e.dma_start(out=g.onesrow[:], in_=W["c_ones"][0:1, :]), writes=["onesrow"])


def ecopy(e, eng, out, in_):
    if eng == "act":
        return e.activation(out=out, in_=in_, func=AF.Copy)
    return e.tensor_copy(out, in_)


def load_bcast(c, tile, key, src_row_ap, q="sp"):
    c.dma(q, lambda e: e.dma_start(out=tile[:], in_=src_row_ap.partition_broadcast(128)), writes=[key])


def layer_norm_tile(c, g, acc, acck, gam, bet, gbk, out_tile, outk, tag):
    st = g.ln_stats
    mv = g.ln_mv
    sk = "ln_small"
    c.op("dve", lambda e: e.bn_stats(st[:, 0, :], acc[:, 0:512]), reads=[acck], writes=[sk])
    c.op("dve", lambda e: e.bn_stats(st[:, 1, :], acc[:, 512:1024]), reads=[acck, sk], writes=[sk])
    c.op("dve", lambda e: e.bn_aggr(mv[:, 0:2], st[:].rearrange("p a b -> p (a b)")), reads=[sk], writes=[sk])
    c.op("act", lambda e: e.activation(out=mv[:, 2:3], in_=mv[:, 1:2], func=AF.Sqrt, bias=g.eps_t[:, 0:1], scale=1.0),
         reads=[sk, "eps"], writes=[sk])
    c.op("dve", lambda e: e.reciprocal(mv[:, 3:4], mv[:, 2:3]), reads=[sk], writes=[sk])
    c.op("dve", lambda e: e.tensor_scalar(mv[:, 4:5], mv[:, 0:1], mv[:, 3:4], -1.0, ALU.mult, ALU.mult),
         reads=[sk], writes=[sk])
    c.op("act", lambda e: e.activation(out=acc[:], in_=acc[:], func=AF.Identity, bias=mv[:, 4:5], scale=mv[:, 3:4]),
         reads=[sk, acck], writes=[acck])
    c.op("dve", lambda e: e.tensor_tensor(acc[:], acc[:], gam[:], ALU.mult), reads=[acck, gbk], writes=[acck])
    c.op("dve", lambda e: e.tensor_tensor(out_tile[:], acc[:], bet[:], ALU.add), reads=[acck, gbk], writes=[outk])


def transpose_rows_to_T(c, g, rows, rowsk, xT, xTk, bank, bankk, nchunks=8, fp32_in=True, evac="dve"):
    if fp32_in:
        for half in range(nchunks // 4):
            for j in range(4):
                kc = half * 4 + j
                c.op("pe", (lambda kc, j: lambda e: e.transpose(bank[:, j * 128:(j + 1) * 128], rows[:, kc * 128:(kc + 1) * 128], g.ident[:]))(kc, j),
                     reads=[rowsk, "ident"], writes=[bankk])
            c.op(evac, (lambda half: lambda e: ecopy(e, evac, xT[:, half * 4:(half + 1) * 4, :].rearrange("p a b -> p (a b)"), bank[:, :]))(half),
                 reads=[bankk], writes=[xTk])
    else:
        bb = bank[:].bitcast(BF16)
        for kc in range(nchunks):
            c.op("pe", (lambda kc: lambda e: e.transpose(bb[:, kc * 128:(kc + 1) * 128], rows[:, kc * 128:(kc + 1) * 128], g.identb[:]))(kc),
                 reads=[rowsk, "identb"], writes=[bankk])
        c.op(evac, lambda e: ecopy(e, evac, xT[:].rearrange("p a b -> p (a b)"), bb[:, 0:nchunks * 128]),
             reads=[bankk], writes=[xTk])


def moe_phase(c, g, W, layer, TT, hsrc, hdst, C):
    NT = TT // 128
    NSL = C // 128
    bank = g.bank
    with c.phase():
        hrow = [c.sb("m_hrow%d" % i, [128, D], F32) for i in range(2)]
        hT = c.sb("m_hT", [128, 8, 128], F32)
        rw = c.sb("m_rw", [128, 8, NE], F32)
        rb = c.sb("m_rb", [128, NE], F32)
        ltri = c.sb("m_ltri", [128, 128], F32)
        ones = c.sb("m_ones", [128, 128], F32)
        iota = c.sb("m_iota", [128, NE], F32)
        base = c.sb("m_base", [128, NE], F32)
        logit = c.sb("m_logit", [128, NE], F32)
        top8 = c.sb("m_top8", [128, 8], F32)
        idx8 = c.sb("m_idx8", [128, 8], U32)
        idxf = c.sb("m_idxf", [128, 4], F32)
        sm = c.sb("m_sm", [128, 16], F32)
        oh = c.sb("m_oh", [128, 4, NE], F32)
        Mt = c.sb("m_M", [128, NE], F32)
        rank = c.sb("m_rank", [128, NE], F32)
        tmp = c.sb("m_tmp", [128, NE], F32)
        pos = c.sb("m_pos", [128, 8], F32)
        c.dma("sp", lambda e: e.dma_start(out=rw[:], in_=W["router_w"][layer].rearrange("(kc p) e -> p kc e", p=128)), writes=["rw"])
        load_bcast(c, rb, "rb", W["router_b"][layer:layer + 1, :])
        c.dma("sp", lambda e: e.dma_start(out=ltri[:], in_=W["c_ltri"][:, :]), writes=["ltri"])
        c.dma("sp", lambda e: e.dma_start(out=ones[:], in_=W["c_onesq"][:, :]), writes=["ones"])
        c.dma("sp", lambda e: e.dma_start(out=iota[:], in_=W["c_iota"][:, :]), writes=["iota"])
        c.op("dve", lambda e: e.memset(base[:], 0.0), writes=["base"])
        S = "m_small"
        for i in range(NT):
            hr = hrow[i % 2]
            hk = "m_hrow%d" % (i % 2)
            c.dma("sp", (lambda i, hr: lambda e: e.dma_start(out=hr[:], in_=hsrc[i * 128:(i + 1) * 128, :]))(i, hr), writes=[hk])
            transpose_rows_to_T(c, g, hr, hk, hT, "m_hT", bank[0], "bank0", fp32_in=True, evac="act")
            for kc in range(8):
                c.op("pe", (lambda kc: lambda e: e.matmul(bank[1][:, 0:NE], hT[:, kc, :], rw[:, kc, :], start=(kc == 0), stop=(kc == 7)))(kc),
                     reads=["m_hT", "rw"], writes=["bank1"])
            c.op("dve", lambda e: e.tensor_tensor(logit[:], bank[1][:, 0:NE], rb[:], ALU.add), reads=["bank1", "rb"], writes=[S])
            c.op("dve", lambda e: e.max(top8[:], logit[:]), reads=[S], writes=[S])
            c.op("dve", lambda e: e.max_index(idx8[:], top8[:], logit[:]), reads=[S], writes=[S])
            c.op("dve", lambda e: e.tensor_copy(idxf[:], idx8[:, 0:4]), reads=[S], writes=[S])
            c.op("dve", lambda e: e.tensor_scalar_mul(sm[:, 0:1], top8[:, 0:1], -1.0), reads=[S], writes=[S])
            c.op("act", lambda e: e.activation(out=sm[:, 4:8], in_=top8[:, 0:4], func=AF.Exp, bias=sm[:, 0:1], scale=1.0, accum_out=sm[:, 1:2]),
                 reads=[S], writes=[S])
            c.op("dve", lambda e: e.reciprocal(sm[:, 2:3], sm[:, 1:2]), reads=[S], writes=[S])
            c.op("dve", (lambda i: lambda e: e.tensor_scalar_mul(g.gates_all[:, i, :], sm[:, 4:8], sm[:, 2:3]))(i), reads=[S], writes=["gates_all"])
            for k in range(4):
                c.op("dve", (lambda k: lambda e: e.tensor_scalar(oh[:, k, :], iota[:], idxf[:, k:k + 1], None, ALU.is_equal))(k),
                     reads=[S, "iota"], writes=[S])
            c.op("dve", lambda e: e.tensor_tensor(Mt[:], oh[:, 0, :], oh[:, 1, :], ALU.add), reads=[S], writes=["m_M"])
            c.op("dve", lambda e: e.tensor_tensor(Mt[:], Mt[:], oh[:, 2, :], ALU.add), reads=[S, "m_M"], writes=["m_M"])
            c.op("dve", lambda e: e.tensor_tensor(Mt[:], Mt[:], oh[:, 3, :], ALU.add), reads=[S, "m_M"], writes=["m_M"])
            c.op("pe", lambda e: e.matmul(bank[2][:, 0:NE], ltri[:], Mt[:], start=True, stop=True), reads=["m_M", "ltri"], writes=["bank2"])
            c.op("pe", lambda e: e.matmul(bank[3][:, 0:NE], ones[:], Mt[:], start=True, stop=True), reads=["m_M", "ones"], writes=["bank3"])
            c.op("dve", lambda e: e.tensor_tensor(rank[:], bank[2][:, 0:NE], base[:], ALU.add), reads=["bank2", "base"], writes=[S])
            c.op("dve", lambda e: e.tensor_tensor(base[:], bank[3][:, 0:NE], base[:], ALU.add), reads=["bank3", S], writes=["base"])
            for k in range(4):
                c.op("dve", (lambda k: lambda e: e.scalar_tensor_tensor(tmp[:], oh[:, k, :], 1.0, rank[:], ALU.mult, ALU.mult, accum_out=pos[:, k:k + 1]))(k),
                     reads=[S], writes=[S])
            c.op("dve", lambda e: e.tensor_scalar(pos[:, 4:8], pos[:, 0:4], float(C), 1.0e6, ALU.is_ge, ALU.mult), reads=[S], writes=[S])
            c.op("dve", lambda e: e.tensor_tensor(pos[:, 0:4], pos[:, 0:4], pos[:, 4:8], ALU.add), reads=[S], writes=[S])
            c.op("dve", lambda e: e.scalar_tensor_tensor(pos[:, 4:8], idxf[:], float(C), pos[:, 0:4], ALU.mult, ALU.add), reads=[S], writes=[S])
            c.op("dve", (lambda i: lambda e: e.tensor_copy(g.drow_all[:, i, :], pos[:, 4:8]))(i), reads=[S], writes=["drow_all"])
            for k in range(4):
                c.dma("pool", (lambda i, k, hr: lambda e: e.indirect_dma_start(
                    out=g.Xd[:, :], out_offset=bass.IndirectOffsetOnAxis(ap=g.drow_all[:, i, k:k + 1], axis=0),
                    in_=hr[:, :], in_offset=None, bounds_check=c.reg(e, NE * C - 1), oob_is_err=False))(i, k, hr),
                    reads=[hk, "drow_all"], cowrites=["Xd"])

    with c.phase():
        wgu = [c.sb("m_wgu%d" % i, [128, 8, 2048], BF16) for i in range(2)]
        wdn = [c.sb("m_wdn%d" % i, [128, 8, 1024], BF16) for i in range(2)]
        bgu = [c.sb("m_bgu%d" % i, [1, 2048], BF16) for i in range(2)]
        bdn = [c.sb("m_bdn%d" % i, [1, 1024], BF16) for i in range(2)]
        xrow = [c.sb("m_xrow%d" % i, [128, D], BF16) for i in range(2)]
        xT = [c.sb("m_xT%d" % i, [128, 8, 128], BF16) for i in range(2)]
        gt = [c.sb("m_g%d" % i, [128, 1024], F32) for i in range(2)]
        sg = [c.sb("m_sg%d" % i, [128, 1024], F32) for i in range(2)]
        lin = [c.sb("m_lin%d" % i, [128, 1024], F32) for i in range(2)]
        act = [c.sb("m_act%d" % i, [128, 1024], BF16) for i in range(2)]
        actT = [c.sb("m_actT%d" % i, [128, 8, 128], BF16) for i in range(2)]
        yt = [c.sb("m_y%d" % i, [128, 1024], F32) for i in range(2)]

        def load_w(ex):
            p = ex % 2
            wg_src = W["w_gu"][layer, ex].rearrange("(kc p) n -> p kc n", p=128)
            wd_src = W["w_dn"][layer, ex].rearrange("(kc p) n -> p kc n", p=128)
            for kc in range(8):
                c.dma("pool", (lambda kc: lambda e: e.dma_start(out=wgu[p][:, kc, :], in_=wg_src[:, kc, :], max_dma_last_dim=8192))(kc),
                      cowrites=["wgu%d" % p], reads=[], writes=[])
            for kc in range(8):
                c.dma("pool", (lambda kc: lambda e: e.dma_start(out=wdn[p][:, kc, :], in_=wd_src[:, kc, :], max_dma_last_dim=4096))(kc),
                      cowrites=["wdn%d" % p])
            c.dma("pool", lambda e: e.dma_start(out=bgu[p][:], in_=W["b_gu"][layer, ex:ex + 1, :], max_dma_last_dim=8192), cowrites=["wgu%d" % p])
            c.dma("pool", lambda e: e.dma_start(out=bdn[p][:], in_=W["b_dn"][layer, ex:ex + 1, :], max_dma_last_dim=4096), cowrites=["wdn%d" % p])

        units = [(ex, i) for ex in range(NE) for i in range(NSL)]

        def st_load(u):
            ex, i = units[u]
            b = u % 2
            r0 = ex * C + i * 128
            c.dma("sp", lambda e: e.dma_start(out=xrow[b][:], in_=g.Xd[r0:r0 + 128, :]), reads=["Xd"], writes=["xrow%d" % b])

        def st_tx(u):
            b = u % 2
            transpose_rows_to_T(c, g, xrow[b], "xrow%d" % b, xT[b], "xT%d" % b, bank[0], "bank0", fp32_in=False, evac="dve")

        def st_gu(u):
            ex, i = units[u]
            b = u % 2
            p = ex % 2
            for n in range(4):
                for kc in range(8):
                    c.op("pe", (lambda n, kc: lambda e: e.matmul(bank[2 + n][:, :], xT[b][:, kc, :], wgu[p][:, kc, n * 512:(n + 1) * 512], start=(kc == 0), stop=False))(n, kc),
                         reads=["xT%d" % b, "wgu%d" % p], writes=["gu%d" % n])
                c.op("pe", (lambda n: lambda e: e.matmul(bank[2 + n][:, :], g.onesrow[0:1, :], bgu[p][0:1, n * 512:(n + 1) * 512], start=False, stop=True))(n),
                     reads=["onesrow", "wgu%d" % p], writes=["gu%d" % n])
            sls = [slice(n * 512, (n + 1) * 512) for n in range(2)]
            for n in range(2):
                sl = sls[n]
                c.op("dve", (lambda n, sl: lambda e: e.tensor_scalar_min(gt[b][:, sl], bank[2 + n][:, :], 7.0))(n, sl), reads=["gu%d" % n], writes=["g%d_%d" % (b, n)])
                c.op("act", (lambda sl: lambda e: e.activation(out=sg[b][:, sl], in_=gt[b][:, sl], func=AF.Sigmoid, scale=1.702))(sl), reads=["g%d_%d" % (b, n)], writes=["sg%d_%d" % (b, n)])
            for n in range(2):
                sl = sls[n]
                c.op("dve", (lambda n, sl: lambda e: e.tensor_scalar(lin[b][:, sl], bank[4 + n][:, :], 7.0, -7.0, ALU.min, ALU.max))(n, sl), reads=["gu%d" % (2 + n)], writes=["lin%d_%d" % (b, n)])
            for n in range(2):
                sl = sls[n]
                c.op("dve", (lambda sl: lambda e: e.scalar_tensor_tensor(lin[b][:, sl], lin[b][:, sl], 1.0, gt[b][:, sl], ALU.add, ALU.mult))(sl),
                     reads=["lin%d_%d" % (b, n), "g%d_%d" % (b, n)], writes=["lin%d_%d" % (b, n)])
            for n in range(2):
                sl = sls[n]
                c.op("pool" if n == 1 else "dve", (lambda sl: lambda e: e.tensor_tensor(act[b][:, sl], lin[b][:, sl], sg[b][:, sl], ALU.mult))(sl),
                     reads=["lin%d_%d" % (b, n), "sg%d_%d" % (b, n)], writes=["act%d_%d" % (b, n)])

        def st_tact(u):
            b = u % 2
            bb = bank[1][:].bitcast(BF16)
            for kc in range(8):
                c.op("pe", (lambda kc: lambda e: e.transpose(bb[:, kc * 128:(kc + 1) * 128], act[b][:, kc * 128:(kc + 1) * 128], g.identb[:]))(kc),
                     reads=["act%d_%d" % (b, kc // 4), "identb"], writes=["bank1"])
            c.op("act", lambda e: e.activation(out=actT[b][:].rearrange("p a b -> p (a b)"), in_=bb[:, 0:1024], func=AF.Copy), reads=["bank1"], writes=["actT%d" % b])

        def st_down(u):
            ex, i = units[u]
            b = u % 2
            p = ex % 2
            for n in range(2):
                for kc in range(8):
                    c.op("pe", (lambda n, kc: lambda e: e.matmul(bank[6 + n][:, :], actT[b][:, kc, :], wdn[p][:, kc, n * 512:(n + 1) * 512], start=(kc == 0), stop=False))(n, kc),
                         reads=["actT%d" % b, "wdn%d" % p], writes=["dn%d" % n])
                c.op("pe", (lambda n: lambda e: e.matmul(bank[6 + n][:, :], g.onesrow[0:1, :], bdn[p][0:1, n * 512:(n + 1) * 512], start=False, stop=True))(n),
                     reads=["onesrow", "wdn%d" % p], writes=["dn%d" % n])
                c.op("act", (lambda n: lambda e: e.activation(out=yt[b][:, n * 512:(n + 1) * 512], in_=bank[6 + n][:, :], func=AF.Copy))(n),
                     reads=["dn%d" % n], writes=["y%d" % b])
            r0 = ex * C + i * 128
            c.dma("sp", lambda e: e.dma_start(out=g.Yb[r0:r0 + 128, :], in_=yt[b][:]), reads=["y%d" % b], cowrites=["Yb"])

        NU = len(units)
        load_w(0)
        st_load(0)
        st_tx(0)
        for u in range(NU):
            ex, i = units[u]
            if u + 1 < NU:
                st_load(u + 1)
            if u >= 1:
                st_tact(u - 1)
            if u + 1 < NU:
                st_tx(u + 1)
            st_gu(u)
            if u >= 1:
                st_down(u - 1)
            if i == 0 and ex + 1 < NE:
                load_w(ex + 1)
        st_tact(NU - 1)
        st_down(NU - 1)

    with c.phase():
        hrow = [c.sb("c_hrow%d" % i, [128, D], F32) for i in range(2)]
        yk = [[c.sb("c_y%d_%d" % (i, k), [128, D], F32) for k in range(4)] for i in range(2)]
        outt = [c.sb("c_out%d" % i, [128, D], F32) for i in range(2)]
        gam = c.sb("c_gam", [128, D], F32)
        bet = c.sb("c_bet", [128, D], F32)
        load_bcast(c, gam, "c_gb", W["ln_ffn_g"][layer:layer + 1, :])
        c.dma("sp", lambda e: e.dma_start(out=bet[:], in_=W["ln_ffn_b"][layer:layer + 1, :].partition_broadcast(128)), cowrites=["c_gb"])
        for i in range(NT):
            b = i % 2
            hr = hrow[b]
            hk = "c_hrow%d" % b
            c.dma("sp", (lambda i, hr: lambda e: e.dma_start(out=hr[:], in_=hsrc[i * 128:(i + 1) * 128, :]))(i, hr), writes=[hk])
            for k in range(4):
                c.dma("pool", (lambda i, k, b: lambda e: e.indirect_dma_start(
                    out=yk[b][k][:, :], out_offset=None, in_=g.Yb[:, :],
                    in_offset=bass.IndirectOffsetOnAxis(ap=g.drow_all[:, i, k:k + 1], axis=0),
                    bounds_check=c.reg(e, NE * C - 1), oob_is_err=False))(i, k, b),
                    reads=["Yb", "drow_all"], writes=["c_y%d_%d" % (b, k)])
            c.op("act", (lambda hr: lambda e: e.mul(hr[:], hr[:], float(DN_ALPHA)))(hr), reads=[hk], writes=[hk])
            for k in range(4):
                c.op("dve", (lambda i, k, b, hr: lambda e: e.scalar_tensor_tensor(hr[:], yk[b][k][:], g.gates_all[:, i, k:k + 1], hr[:], ALU.mult, ALU.add))(i, k, b, hr),
                     reads=[hk, "c_y%d_%d" % (b, k), "gates_all"], writes=[hk])
            layer_norm_tile(c, g, hr, hk, gam, bet, "c_gb", outt[b], "c_out%d" % b, "c")
            c.dma("sp", (lambda i, b: lambda e: e.dma_start(out=hdst[i * 128:(i + 1) * 128, :], in_=outt[b][:]))(i, b),
                  reads=["c_out%d" % b], cowrites=["hdst"])


def host_constants(SEQ):
    cst = {}
    cst["c_ident"] = np.eye(128, dtype=np.float32)
    cst["c_ones"] = np.ones((1, 128), np.float32)
    cst["c_onesq"] = np.ones((128, 128), np.float32)
    k = np.arange(128)
    cst["c_ltri"] = (k[:, None] < k[None, :]).astype(np.float32)
    cst["c_iota"] = np.tile(np.arange(NE, dtype=np.float32)[None, :], (128, 1))
    mc = (k[:, None] <= k[None, :]).astype(np.float32)
    mp = (k[:, None] >= k[None, :]).astype(np.float32)
    cst["c_mcur"] = np.tile(mc, (1, 4)).astype(np.float32)
    cst["c_mprev"] = np.tile(mp, (1, 4)).astype(np.float32)
    par = np.zeros((32, 2), np.float32)
    par[0::2, 0] = 1.0
    par[1::2, 1] = 1.0
    cst["c_par"] = par
    half = HD // 2
    inv = (10000.0 ** (-np.arange(half, dtype=np.float32) / half)).astype(np.float32)
    ang = (np.arange(SEQ, dtype=np.float32)[:, None] * inv[None, :]).astype(np.float32)
    cst["c_cos16"] = np.tile(np.cos(ang).astype(np.float32), (1, NH))
    cst["c_sin16"] = np.tile(np.sin(ang).astype(np.float32), (1, NH))
    return cst


WEIGHT_SHAPES = {
    "hy_w_in": (D, 2048), "conv_w": (3, 512), "ssm_a_re": (32, 64), "ssm_a_im": (32, 64), "ssm_log_dt": (32,),
    "ssm_b_re": (32, 64, 16), "ssm_b_im": (32, 64, 16), "ssm_c_re": (32, 16, 64), "ssm_c_im": (32, 16, 64),
    "ssm_d": (32, 16), "ssm_w_glu": (512, 512), "ssm_b_glu": (512,), "hy_w_out": (D, D),
    "att_w_qkv": (D, 3 * D), "att_w_o": (D, D),
    "ln_mix_g": (D,), "ln_mix_b": (D,), "ln_ffn_g": (D,), "ln_ffn_b": (D,),
    "router_w": (D, NE), "router_b": (NE,), "w_gu": (NE, D, 2 * D), "b_gu": (NE, 2 * D),
    "w_dn": (NE, D, D), "b_dn": (NE, D),
}


def build_program(cfg):
    SEQ, NSEQ, C = cfg["SEQ"], cfg["NSEQ"], cfg["C"]
    TT = SEQ * NSEQ
    nc = bass.Bass("TRN2", target_bir_lowering=False)
    W = {}
    W["x"] = nc.dram_tensor("x", [TT, D], F32, kind="ExternalInput").ap()
    for name, shp in WEIGHT_SHAPES.items():
        n0 = cfg["nl"].get(name, 0)
        if n0 == 0:
            continue
        W[name] = nc.dram_tensor(name, [n0] + list(shp), F32, kind="ExternalInput").ap()
    cst = host_constants(SEQ)
    for name, arr in cst.items():
        W[name] = nc.dram_tensor(name, list(arr.shape), F32, kind="ExternalInput").ap()
    out = nc.dram_tensor("out", [TT, D], F32, kind="ExternalOutput").ap()
    c = Ctx(nc)
    g = G()
    setup_globals(c, g, W)
    NT = TT // 128
    g.gates_all = c.sb("gates_all", [128, NT, 4], F32)
    g.drow_all = c.sb("drow_all", [128, NT, 4], U32)
    g.ln_stats = c.sb("ln_stats", [128, 2, 6], F32)
    g.ln_mv = c.sb("ln_mv", [128, 8], F32)
    g.eps_t = c.sb("eps_t", [128, 1], F32)
    c.op("dve", lambda e: e.memset(g.eps_t[:], LN_EPS), writes=["eps"])
    g.Pr = c.sb("ssm_Pr", [128, 16, NRND], F32)
    g.Pi = c.sb("ssm_Pi", [128, 16, NRND], F32)
    g.nPi = c.sb("ssm_nPi", [128, 16, NRND], F32)
    g.lBre = c.sb("ssm_lBre", [128, 16, 128], BF16)
    g.lBim = c.sb("ssm_lBim", [128, 16, 128], BF16)
    g.lCre = c.sb("ssm_lCre", [128, 16, 128], BF16)
    g.lCim = c.sb("ssm_lCim", [128, 16, 128], BF16)
    g.Xd = nc.dram_tensor("Xd", [NE * C, D], BF16).ap()
    g.Yb = nc.dram_tensor("Yb", [NE * C, D], F32).ap()
    hb = [nc.dram_tensor("hbuf%d" % i, [TT, D], F32).ap() for i in range(2)]
    g.qkv_d = nc.dram_tensor("qkv_d", [TT, RW], BF16).ap()
    g.O_d = nc.dram_tensor("O_d", [3, TT, VW], F32).ap()
    g.cfg = cfg
    cur = W["x"]
    plan = cfg["plan"]
    for pi, (kind, layer, mi) in enumerate(plan):
        last = pi == len(plan) - 1
        dst = out if last else hb[pi % 2]
        if kind == "moe":
            moe_phase(c, g, W, layer, TT, cur, dst, C)
        elif kind == "even":
            even_phase(c, g, W, layer, mi, SEQ, NSEQ, cur, dst)
        elif kind == "odd":
            odd_phase(c, g, W, layer, mi, SEQ, NSEQ, cur, dst)
        cur = dst
    c.emit()
    return nc, cst, c


TB = 512
NRND = 9
TWO_PI = 6.283185307179586


def sin_reduced(c, out, src, shift, tmpf, tmpi, S):
    c.op("dve", lambda e: e.tensor_scalar(tmpf[:, 0, :], src[:], float(shift), 1.0 / TWO_PI, ALU.add, ALU.mult), reads=[S], writes=[S])
    c.op("dve", lambda e: e.tensor_copy(tmpi[:], tmpf[:, 0, :]), reads=[S], writes=[S])
    c.op("dve", lambda e: e.tensor_copy(tmpf[:, 1, :], tmpi[:]), reads=[S], writes=[S])
    c.op("dve", lambda e: e.tensor_scalar(tmpf[:, 2, :], src[:], float(shift), None, ALU.add), reads=[S], writes=[S])
    c.op("dve", lambda e: e.scalar_tensor_tensor(tmpf[:, 2, :], tmpf[:, 1, :], -TWO_PI, tmpf[:, 2, :], ALU.mult, ALU.add), reads=[S], writes=[S])
    c.op("dve", lambda e: e.tensor_scalar(tmpf[:, 3, :], tmpf[:, 2, :], float(np.pi), -TWO_PI, ALU.is_gt, ALU.mult), reads=[S], writes=[S])
    c.op("dve", lambda e: e.tensor_tensor(tmpf[:, 2, :], tmpf[:, 2, :], tmpf[:, 3, :], ALU.add), reads=[S], writes=[S])
    c.op("dve", lambda e: e.tensor_scalar(tmpf[:, 3, :], tmpf[:, 2, :], float(-np.pi), TWO_PI, ALU.is_lt, ALU.mult), reads=[S], writes=[S])
    c.op("dve", lambda e: e.tensor_tensor(tmpf[:, 2, :], tmpf[:, 2, :], tmpf[:, 3, :], ALU.add), reads=[S], writes=[S])
    c.op("act", lambda e: e.activation(out=out[:], in_=tmpf[:, 2, :], func=AF.Sin), reads=[S], writes=[S])


def even_setup(c, g, W, mi):
    bank = g.bank
    S = "es"
    if g.cfg.get("dbg") == "none":
        return
    with c.phase():
        nat = c.sb("es_nat", [32, 3, 64], F32)
        natm = c.sb("es_natm", [32, 3, 128], F32)
        par = c.sb("es_par", [32, 2], F32)
        ldt = c.sb("es_ldt", [32, 1], F32)
        q3 = c.sb("es_q3", [128, 3, 16], F32)
        tf = c.sb("es_tf", [128, 4, 16], F32)
        ti = c.sb("es_ti", [128, 16], I32)
        pt = c.sb("es_pt", [128, 32], F32)
        w = c.sb("es_w", [128, 12, 16], F32)
        Bn = c.sb("es_Bn", [128, 2, 16, 16], F32)
        Bf = c.sb("es_Bf", [128, 2, 16, 128], F32)
        Cf = c.sb("es_Cf", [128, 2, 16, 128], F32)
        c.dma("sp", lambda e: e.dma_start(out=nat[:, 0, :], in_=W["ssm_a_re"][mi]), cowrites=["es_nat"])
        c.dma("sp", lambda e: e.dma_start(out=nat[:, 1, :], in_=W["ssm_a_im"][mi]), cowrites=["es_nat"])
        c.dma("sp", lambda e: e.dma_start(out=ldt[:], in_=W["ssm_log_dt"][mi].rearrange("(g o) -> g o", o=1)), cowrites=["es_nat"])
        c.dma("sp", lambda e: e.dma_start(out=par[:], in_=W["c_par"][:, :]), cowrites=["es_nat"])
        for ri, nm in enumerate(("ssm_b_re", "ssm_b_im")):
            for gg in range(2):
                c.dma("sp", (lambda ri, nm, gg: lambda e: e.dma_start(out=Bn[gg * 64:(gg + 1) * 64, ri, :, :], in_=W[nm][mi].rearrange("(j gg) p h -> gg p j h", gg=2)[gg]))(ri, nm, gg), cowrites=["es_Bn"])
        c.op("dve", lambda e: e.memset(Bf[:], 0.0), writes=["es_Bf"])
        c.op("pool", lambda e: e.memset(Cf[:], 0.0), writes=["es_Cf"])
        for ri, nm in enumerate(("ssm_c_re", "ssm_c_im")):
            for j in range(16):
                for gg in range(2):
                    q = j % 4
                    p0 = 32 * q + 16 * gg
                    c.dma("sp", (lambda ri, nm, j, gg, p0: lambda e: e.dma_start(out=Cf[p0:p0 + 16, ri, j, gg * 64:(gg + 1) * 64], in_=W[nm][mi, 2 * j + gg]))(ri, nm, j, gg, p0),
                          reads=["es_Cf"], cowrites=["es_Cf2"])
        c.op("act", lambda e: e.activation(out=ldt[:], in_=ldt[:], func=AF.Exp), reads=["es_nat"], writes=[S])
        c.op("dve", lambda e: e.tensor_scalar_min(nat[:, 0, :], nat[:, 0, :], -1e-4), reads=["es_nat", S], writes=[S])
        c.op("dve", lambda e: e.memset(nat[:, 2, :], 1.0), reads=[S], writes=[S])
        c.op("dve", lambda e: e.tensor_scalar_mul(nat[:, 2, :], nat[:, 2, :], ldt[:, 0:1]), reads=[S], writes=[S])
        for m in range(3):
            c.op("dve", (lambda m: lambda e: e.tensor_scalar_mul(natm[:, m, 0:64], nat[:, m, :], par[:, 0:1]))(m), reads=[S], writes=[S])
            c.op("dve", (lambda m: lambda e: e.tensor_scalar_mul(natm[:, m, 64:128], nat[:, m, :], par[:, 1:2]))(m), reads=[S], writes=[S])
            c.op("pe", (lambda m: lambda e: e.transpose(bank[0][:, 0:32], natm[:, m, :], g.ident[0:32, 0:32]))(m), reads=[S, "ident"], writes=["bank0"])
            c.op("dve", lambda e: e.tensor_copy(pt[:], bank[0][:, 0:32]), reads=["bank0", S], writes=[S])
            c.op("dve", (lambda m: lambda e: e.tensor_tensor(q3[:, m, :], pt[:].rearrange("p (j gg) -> p j gg", gg=2)[:, :, 0], pt[:].rearrange("p (j gg) -> p j gg", gg=2)[:, :, 1], ALU.add))(m), reads=[S], writes=[S])
        LR, LI, DT = q3[:, 0, :], q3[:, 1, :], q3[:, 2, :]
        c.op("dve", lambda e: e.tensor_tensor(w[:, 0, :], LR, DT, ALU.mult), reads=[S], writes=[S])
        c.op("act", lambda e: e.activation(out=w[:, 0, :], in_=w[:, 0, :], func=AF.Exp), reads=[S], writes=[S])
        c.op("dve", lambda e: e.tensor_tensor(w[:, 1, :], LI, DT, ALU.mult), reads=[S], writes=[S])
        sin_reduced(c, w[:, 3, :], w[:, 1, :], 0.0, tf, ti, S)
        sin_reduced(c, w[:, 2, :], w[:, 1, :], np.pi / 2, tf, ti, S)
        c.op("dve", lambda e: e.tensor_tensor(w[:, 4, :], w[:, 0, :], w[:, 2, :], ALU.mult), reads=[S], writes=[S])
        c.op("dve", lambda e: e.tensor_tensor(w[:, 5, :], w[:, 0, :], w[:, 3, :], ALU.mult), reads=[S], writes=[S])
        c.op("dve", lambda e: e.tensor_scalar_add(w[:, 6, :], w[:, 4, :], -1.0), reads=[S], writes=[S])
        c.op("dve", lambda e: e.tensor_tensor(w[:, 7, :], LR, LR, ALU.mult), reads=[S], writes=[S])
        c.op("dve", lambda e: e.tensor_tensor(w[:, 10, :], LI, LI, ALU.mult), reads=[S], writes=[S])
        c.op("dve", lambda e: e.tensor_tensor(w[:, 7, :], w[:, 7, :], w[:, 10, :], ALU.add), reads=[S], writes=[S])
        c.op("dve", lambda e: e.reciprocal(w[:, 7, :], w[:, 7, :]), reads=[S], writes=[S])
        c.op("dve", lambda e: e.tensor_tensor(w[:, 8, :], w[:, 6, :], LR, ALU.mult), reads=[S], writes=[S])
        c.op("dve", lambda e: e.tensor_tensor(w[:, 10, :], w[:, 5, :], LI, ALU.mult), reads=[S], writes=[S])
        c.op("dve", lambda e: e.tensor_tensor(w[:, 8, :], w[:, 8, :], w[:, 10, :], ALU.add), reads=[S], writes=[S])
        c.op("dve", lambda e: e.tensor_tensor(w[:, 8, :], w[:, 8, :], w[:, 7, :], ALU.mult), reads=[S], writes=[S])
        c.op("dve", lambda e: e.tensor_tensor(w[:, 9, :], w[:, 5, :], LR, ALU.mult), reads=[S], writes=[S])
        c.op("dve", lambda e: e.tensor_tensor(w[:, 10, :], w[:, 6, :], LI, ALU.mult), reads=[S], writes=[S])
        c.op("dve", lambda e: e.tensor_tensor(w[:, 9, :], w[:, 9, :], w[:, 10, :], ALU.subtract), reads=[S], writes=[S])
        c.op("dve", lambda e: e.tensor_tensor(w[:, 9, :], w[:, 9, :], w[:, 7, :], ALU.mult), reads=[S], writes=[S])
        c.op("dve", lambda e: e.tensor_scalar_mul(w[:, 11, :], w[:, 9, :], -1.0), reads=[S], writes=[S])
        Pr, Pi, nPi = g.Pr, g.Pi, g.nPi
        c.op("dve", lambda e: e.tensor_copy(Pr[:, :, 0], w[:, 4, :]), reads=[S], writes=["P"])
        c.op("dve", lambda e: e.tensor_copy(Pi[:, :, 0], w[:, 5, :]), reads=[S, "P"], writes=["P"])
        for k in range(NRND):
            c.op("dve", (lambda k: lambda e: e.tensor_scalar_mul(nPi[:, :, k], Pi[:, :, k], -1.0))(k), reads=["P"], writes=["P"])
            if k + 1 < NRND:
                c.op("dve", (lambda k: lambda e: e.tensor_tensor(w[:, 10, :], Pi[:, :, k], Pi[:, :, k], ALU.mult))(k), reads=["P", S], writes=[S])
                c.op("dve", (lambda k: lambda e: e.tensor_tensor(Pr[:, :, k + 1], Pr[:, :, k], Pr[:, :, k], ALU.mult))(k), reads=["P"], writes=["P"])
                c.op("dve", (lambda k: lambda e: e.tensor_tensor(Pr[:, :, k + 1], Pr[:, :, k + 1], w[:, 10, :], ALU.subtract))(k), reads=["P", S], writes=["P"])
                c.op("dve", (lambda k: lambda e: e.scalar_tensor_tensor(Pi[:, :, k + 1], Pr[:, :, k], 2.0, Pi[:, :, k], ALU.mult, ALU.mult))(k), reads=["P"], writes=["P"])
        for j in range(16):
            q = j % 4
            for gg in range(2):
                ps_ = slice(gg * 64, (gg + 1) * 64)
                cs = slice(32 * q + 16 * gg, 32 * q + 16 * gg + 16)
                cr, ci, nci = w[ps_, 8, j:j + 1], w[ps_, 9, j:j + 1], w[ps_, 11, j:j + 1]
                c.op("dve", (lambda j, ps_, cs, cr: lambda e: e.tensor_scalar_mul(Bf[ps_, 0, j, cs], Bn[ps_, 0, j, :], cr))(j, ps_, cs, cr), reads=[S, "es_Bn", "es_Bf"], writes=["es_Bf"])
                c.op("dve", (lambda j, ps_, cs, nci: lambda e: e.scalar_tensor_tensor(Bf[ps_, 0, j, cs], Bn[ps_, 1, j, :], nci, Bf[ps_, 0, j, cs], ALU.mult, ALU.add))(j, ps_, cs, nci), reads=[S, "es_Bn", "es_Bf"], writes=["es_Bf"])
                c.op("dve", (lambda j, ps_, cs, cr: lambda e: e.tensor_scalar_mul(Bf[ps_, 1, j, cs], Bn[ps_, 1, j, :], cr))(j, ps_, cs, cr), reads=[S, "es_Bn", "es_Bf"], writes=["es_Bf"])
                c.op("dve", (lambda j, ps_, cs, ci: lambda e: e.scalar_tensor_tensor(Bf[ps_, 1, j, cs], Bn[ps_, 0, j, :], ci, Bf[ps_, 1, j, cs], ALU.mult, ALU.add))(j, ps_, cs, ci), reads=[S, "es_Bn", "es_Bf"], writes=["es_Bf"])
        for j in range(16):
            for ri, (src, srck, dst, scale) in enumerate(((Bf, "es_Bf", g.lBre, 1.0), (Bf, "es_Bf", g.lBim, 1.0), (Cf, "es_Cf2", g.lCre, 1.0), (Cf, "es_Cf2", g.lCim, -1.0))):
                r = ri % 2
                bk = bank[1 + (ri % 2)]
                bkk = "bank%d" % (1 + (ri % 2))
                c.op("pe", (lambda src, r, j, bk: lambda e: e.transpose(bk[:, 0:128], src[:, r, j, :], g.ident[:]))(src, r, j, bk), reads=[srck, "es_Cf", "ident"], writes=[bkk])
                c.op("act", (lambda dst, j, bk, scale: lambda e: e.activation(out=dst[:, j, :], in_=bk[:, 0:128], func=AF.Copy, scale=scale))(dst, j, bk, scale), reads=[bkk], writes=["lBC"])


def even_phase(c, g, W, layer, mi, SEQ, NSEQ, hsrc, hdst):
    bank = g.bank
    dbg = g.cfg.get("dbg")
    if dbg != "nosetup":
        even_setup(c, g, W, mi)
    if dbg in ("setup", "none"):
        c.dma("sp", lambda e: e.dma_start(out=hdst[:, :], in_=hsrc[:, :]), writes=["hdst"])
        return
    NB = SEQ // TB
    with c.phase():
        win = c.sb("e_win", [128, 8, 2048], BF16)
        wout = c.sb("e_wout", [128, 8, 1024], BF16)
        wglu = c.sb("e_wglu", [128, 4, 512], BF16)
        bglu = c.sb("e_bglu", [128, 4], F32)
        dsk = c.sb("e_dsk", [128, 4], F32)
        cw = c.sb("e_cw", [128, 4, 3], F32)
        gam = c.sb("e_gam", [128, D], F32)
        bet = c.sb("e_bet", [128, D], F32)
        hrow = [c.sb("e_hrow%d" % i, [128, D], F32) for i in range(2)]
        hT = c.sb("e_hT", [128, 8, TB], BF16)
        gb = c.sb("e_gb", [128, 4, TB], F32)
        vb = c.sb("e_vb", [128, 4, TB + 2], F32)
        gct = c.sb("e_gct", [128, TB], F32)
        uT = c.sb("e_uT", [128, 4, TB], BF16)
        u32 = c.sb("e_u32", [128, 4, TB], F32)
        xs = [[c.sb("e_x%d%d" % (a, b), [128, TB], F32) for b in range(2)] for a in range(2)]
        xbf = [c.sb("e_xbf%d" % b, [128, TB], BF16) for b in range(2)]
        Xst = c.sb("e_Xst", [128, 16, 2], F32)
        ycT = c.sb("e_ycT", [128, 8, TB], BF16)
        yt = c.sb("e_yt", [128, TB], F32)
        tt_ = c.sb("e_tt", [128, TB], F32)
        z32 = c.sb("e_z32", [128, 4, TB], F32)
        zT = c.sb("e_zT", [128, 4, TB], BF16)
        outt = [c.sb("e_out%d" % i, [128, D], F32) for i in range(2)]
        for kc in range(8):
            c.dma("pool", (lambda kc: lambda e: e.dma_start(out=win[:, kc, :], in_=W["hy_w_in"][mi, kc * 128:(kc + 1) * 128, :], max_dma_last_dim=8192))(kc), cowrites=["win"])
            c.dma("pool", (lambda kc: lambda e: e.dma_start(out=wout[:, kc, :], in_=W["hy_w_out"][mi, kc * 128:(kc + 1) * 128, :], max_dma_last_dim=4096))(kc), cowrites=["wout"])
        for kc in range(4):
            c.dma("pool", (lambda kc: lambda e: e.dma_start(out=wglu[:, kc, :], in_=W["ssm_w_glu"][mi, kc * 128:(kc + 1) * 128, :], max_dma_last_dim=2048))(kc), cowrites=["wglu"])
            c.dma("sp", (lambda kc: lambda e: e.dma_start(out=bglu[:, kc:kc + 1], in_=W["ssm_b_glu"][mi, kc * 128:(kc + 1) * 128].rearrange("(p o) -> p o", o=1)))(kc), cowrites=["small"])
            c.dma("sp", (lambda kc: lambda e: e.dma_start(out=dsk[:, kc:kc + 1], in_=W["ssm_d"][mi].rearrange("g h -> (g h)")[kc * 128:(kc + 1) * 128].rearrange("(p o) -> p o", o=1)))(kc), cowrites=["small"])
            for k in range(3):
                c.dma("sp", (lambda kc, k: lambda e: e.dma_start(out=cw[:, kc, k:k + 1], in_=W["conv_w"][mi, k, kc * 128:(kc + 1) * 128].rearrange("(p o) -> p o", o=1)))(kc, k), cowrites=["small"])
        c.dma("sp", lambda e: e.dma_start(out=gam[:], in_=W["ln_mix_g"][layer:layer + 1, :].partition_broadcast(128)), cowrites=["e_gam"])
        c.dma("sp", lambda e: e.dma_start(out=bet[:], in_=W["ln_mix_b"][layer:layer + 1, :].partition_broadcast(128)), cowrites=["e_gam"])
        for s in range(NSEQ):
            for tb in range(NB):
                t0 = s * SEQ + tb * TB
                for tt in range(TB // 128):
                    hr = hrow[tt % 2]
                    hk = "e_hrow%d" % (tt % 2)
                    c.dma("sp", (lambda tt, hr, t0: lambda e: e.dma_start(out=hr[:], in_=hsrc[t0 + tt * 128:t0 + (tt + 1) * 128, :]))(tt, hr, t0), writes=[hk])
                    for half in range(2):
                        for jj in range(4):
                            kc = half * 4 + jj
                            c.op("pe", (lambda kc, jj, hr: lambda e: e.transpose(bank[0][:, jj * 128:(jj + 1) * 128], hr[:, kc * 128:(kc + 1) * 128], g.ident[:]))(kc, jj, hr),
                                 reads=[hk, "ident"], writes=["bank0"])
                        c.op("act", (lambda half, tt: lambda e: e.activation(out=hT[:, half * 4:(half + 1) * 4, tt * 128:(tt + 1) * 128], in_=bank[0][:, :].rearrange("p (a b) -> p a b", a=4), func=AF.Copy))(half, tt),
                             reads=["bank0"], writes=["e_hT"])
                stg = g.cfg.get("stages", "abcdef")
                def proj(oc, bk, bkk):
                    for kc in range(8):
                        c.op("pe", (lambda kc: lambda e: e.matmul(bk[:, :], win[:, kc, oc * 128:(oc + 1) * 128], hT[:, kc, :], start=(kc == 0), stop=(kc == 7)))(kc),
                             reads=["win", "e_hT"], writes=[bkk])
                for cc in range(4 if "b" in stg else 0):
                    if "1" in stg or "c" in stg:
                        proj(cc, bank[1], "bank1")
                        c.op("act", (lambda cc: lambda e: e.activation(out=gb[:, cc, :], in_=bank[1][:, :], func=AF.Copy))(cc), reads=["bank1"], writes=["e_gb"])
                    if "2" in stg or "c" in stg:
                        proj(4 + cc, bank[2], "bank2")
                        c.op("act", lambda e: e.activation(out=gct[:], in_=bank[2][:, :], func=AF.Copy), reads=["bank2"], writes=["e_gct"])
                    if "3" in stg or "c" in stg:
                        proj(8 + cc, bank[1], "bank1")
                        if tb == 0:
                            c.op("dve", (lambda cc: lambda e: e.memset(vb[:, cc, 0:2], 0.0))(cc), writes=["e_vh%d" % cc], reads=["e_vb%d" % cc])
                        c.op("dve", (lambda cc: lambda e: e.tensor_tensor(vb[:, cc, 2:TB + 2], gct[:], bank[1][:, :], ALU.mult))(cc), reads=["bank1", "e_gct"], writes=["e_vb%d" % cc])
                    if "4" in stg or "d" in stg:
                        proj(12 + cc, bank[2], "bank2")
                        c.op("dve", (lambda cc: lambda e: e.tensor_copy(u32[:, cc, :], bank[2][:, :]))(cc), reads=["bank2"], writes=["e_u32"])
                        c.op("act", (lambda cc: lambda e: e.activation(out=uT[:, cc, :], in_=u32[:, cc, :], func=AF.Copy))(cc), reads=["e_u32"], writes=["e_uT"])
                    if "c" not in stg:
                        continue
                    vk = ["e_vb%d" % cc, "e_vh%d" % cc]
                    c.op("dve", (lambda cc: lambda e: e.tensor_scalar_mul(tt_[:], vb[:, cc, 0:TB], cw[:, cc, 0:1]))(cc), reads=vk + ["small"], writes=["e_tt"])
                    c.op("dve", (lambda cc: lambda e: e.scalar_tensor_tensor(tt_[:], vb[:, cc, 1:TB + 1], cw[:, cc, 1:2], tt_[:], ALU.mult, ALU.add))(cc), reads=vk + ["small", "e_tt"], writes=["e_tt"])
                    c.op("dve", (lambda cc: lambda e: e.scalar_tensor_tensor(tt_[:], vb[:, cc, 2:TB + 2], cw[:, cc, 2:3], tt_[:], ALU.mult, ALU.add))(cc), reads=vk + ["small", "e_tt"], writes=["e_tt"])
                    c.op("dve", (lambda cc: lambda e: e.tensor_tensor(ycT[:, cc, :], tt_[:], gb[:, cc, :], ALU.mult))(cc), reads=["e_tt", "e_gb"], writes=["e_ycT"])
                    c.op("dve", (lambda cc: lambda e: e.tensor_copy(vb[:, cc, 0:2], vb[:, cc, TB:TB + 2]))(cc), reads=vk, writes=["e_vh%d" % cc])
                for j in range(16 if "d" in stg else 0):
                    cc = j // 4
                    c.op("pe", (lambda j, cc: lambda e: e.matmul(bank[3][:, :], g.lBre[:, j, :], uT[:, cc, :], start=True, stop=True))(j, cc), reads=["lBC", "e_uT"], writes=["bank3"])
                    c.op("pe", (lambda j, cc: lambda e: e.matmul(bank[4][:, :], g.lBim[:, j, :], uT[:, cc, :], start=True, stop=True))(j, cc), reads=["lBC", "e_uT"], writes=["bank4"])
                    c.op("act", lambda e: e.activation(out=xs[0][0][:], in_=bank[3][:, :], func=AF.Copy), reads=["bank3"], writes=["e_xs0"])
                    c.op("dve", lambda e: e.tensor_copy(xs[0][1][:], bank[4][:, :]), reads=["bank4"], writes=["e_xs0"])
                    if tb > 0:
                        ar, ai, nai = g.Pr[:, j, 0:1], g.Pi[:, j, 0:1], g.nPi[:, j, 0:1]
                        Xr, Xi = Xst[:, j, 0:1], Xst[:, j, 1:2]
                        c.op("dve", (lambda ar, Xr: lambda e: e.scalar_tensor_tensor(xs[0][0][:, 0:1], Xr, ar, xs[0][0][:, 0:1], ALU.mult, ALU.add))(ar, Xr), reads=["P", "e_Xst", "e_xs0"], writes=["e_xs0"])
                        c.op("dve", (lambda nai, Xi: lambda e: e.scalar_tensor_tensor(xs[0][0][:, 0:1], Xi, nai, xs[0][0][:, 0:1], ALU.mult, ALU.add))(nai, Xi), reads=["P", "e_Xst", "e_xs0"], writes=["e_xs0"])
                        c.op("dve", (lambda ar, Xi: lambda e: e.scalar_tensor_tensor(xs[0][1][:, 0:1], Xi, ar, xs[0][1][:, 0:1], ALU.mult, ALU.add))(ar, Xi), reads=["P", "e_Xst", "e_xs0"], writes=["e_xs0"])
                        c.op("dve", (lambda ai, Xr: lambda e: e.scalar_tensor_tensor(xs[0][1][:, 0:1], Xr, ai, xs[0][1][:, 0:1], ALU.mult, ALU.add))(ai, Xr), reads=["P", "e_Xst", "e_xs0"], writes=["e_xs0"])
                    cur = 0
                    for k in range(NRND):
                        sft = 1 << k
                        src, dst = xs[cur], xs[1 - cur]
                        sk_, dk_ = "e_xs%d" % cur, "e_xs%d" % (1 - cur)
                        pr, pi, npi = g.Pr[:, j, k:k + 1], g.Pi[:, j, k:k + 1], g.nPi[:, j, k:k + 1]
                        c.op("pool", (lambda src, dst, sft: lambda e: e.tensor_copy(dst[0][:, 0:sft], src[0][:, 0:sft]))(src, dst, sft), reads=[sk_], writes=[dk_])
                        c.op("pool", (lambda src, dst, sft: lambda e: e.tensor_copy(dst[1][:, 0:sft], src[1][:, 0:sft]))(src, dst, sft), reads=[sk_, dk_], writes=[dk_])
                        c.op("dve", (lambda src, dst, sft, pr: lambda e: e.scalar_tensor_tensor(dst[0][:, sft:TB], src[0][:, 0:TB - sft], pr, src[0][:, sft:TB], ALU.mult, ALU.add))(src, dst, sft, pr), reads=[sk_, dk_, "P"], writes=[dk_])
                        c.op("dve", (lambda src, dst, sft, npi: lambda e: e.scalar_tensor_tensor(dst[0][:, sft:TB], src[1][:, 0:TB - sft], npi, dst[0][:, sft:TB], ALU.mult, ALU.add))(src, dst, sft, npi), reads=[sk_, dk_, "P"], writes=[dk_])
                        c.op("dve", (lambda src, dst, sft, pr: lambda e: e.scalar_tensor_tensor(dst[1][:, sft:TB], src[1][:, 0:TB - sft], pr, src[1][:, sft:TB], ALU.mult, ALU.add))(src, dst, sft, pr), reads=[sk_, dk_, "P"], writes=[dk_])
                        c.op("dve", (lambda src, dst, sft, pi: lambda e: e.scalar_tensor_tensor(dst[1][:, sft:TB], src[0][:, 0:TB - sft], pi, dst[1][:, sft:TB], ALU.mult, ALU.add))(src, dst, sft, pi), reads=[sk_, dk_, "P"], writes=[dk_])
                        cur = 1 - cur
                    fin = xs[cur]
                    fk = "e_xs%d" % cur
                    c.op("pool", (lambda fin, j: lambda e: e.tensor_copy(Xst[:, j, 0:1], fin[0][:, TB - 1:TB]))(fin, j), reads=[fk], writes=["e_Xst"])
                    c.op("pool", (lambda fin, j: lambda e: e.tensor_copy(Xst[:, j, 1:2], fin[1][:, TB - 1:TB]))(fin, j), reads=[fk, "e_Xst"], writes=["e_Xst"])
                    c.op("act", (lambda fin: lambda e: e.activation(out=xbf[0][:], in_=fin[0][:], func=AF.Copy))(fin), reads=[fk], writes=["e_xbf0"])
                    c.op("act", (lambda fin: lambda e: e.activation(out=xbf[1][:], in_=fin[1][:], func=AF.Copy))(fin), reads=[fk], writes=["e_xbf1"])
                    c.op("pe", (lambda j: lambda e: e.matmul(bank[5][:, :], g.lCre[:, j, :], xbf[0][:], start=(j % 4 == 0), stop=False))(j), reads=["lBC", "e_xbf0"], writes=["bank5"])
                    c.op("pe", (lambda j: lambda e: e.matmul(bank[5][:, :], g.lCim[:, j, :], xbf[1][:], start=False, stop=(j % 4 == 3)))(j), reads=["lBC", "e_xbf1"], writes=["bank5"])
                    if j % 4 == 3:
                        c.op("dve", (lambda cc: lambda e: e.scalar_tensor_tensor(yt[:], u32[:, cc, :], dsk[:, cc:cc + 1], bank[5][:, :], ALU.mult, ALU.add))(cc), reads=["bank5", "e_u32", "small"], writes=["e_yt"])
                        c.op("act", lambda e: e.activation(out=tt_[:], in_=yt[:], func=AF.Square), reads=["e_yt"], writes=["e_tt"])
                        c.op("dve", lambda e: e.tensor_scalar(tt_[:], tt_[:], 0.044715, 1.0, ALU.mult, ALU.add), reads=["e_tt"], writes=["e_tt"])
                        c.op("dve", lambda e: e.tensor_tensor(tt_[:], tt_[:], yt[:], ALU.mult), reads=["e_tt", "e_yt"], writes=["e_tt"])
                        c.op("act", lambda e: e.activation(out=tt_[:], in_=tt_[:], func=AF.Sigmoid, scale=1.5957691216057308), reads=["e_tt"], writes=["e_tt"])
                        c.op("dve", (lambda cc: lambda e: e.tensor_tensor(z32[:, cc, :], tt_[:], yt[:], ALU.mult))(cc), reads=["e_tt", "e_yt"], writes=["e_z32"])
                        c.op("act", (lambda cc: lambda e: e.activation(out=zT[:, cc, :], in_=z32[:, cc, :], func=AF.Copy))(cc), reads=["e_z32"], writes=["e_zT"])
                for oc in range(4 if "e" in stg else 0):
                    for kc in range(4):
                        c.op("pe", (lambda oc, kc: lambda e: e.matmul(bank[6][:, :], wglu[:, kc, oc * 128:(oc + 1) * 128], zT[:, kc, :], start=(kc == 0), stop=(kc == 3)))(oc, kc), reads=["wglu", "e_zT"], writes=["bank6"])
                    c.op("act", (lambda oc: lambda e: e.activation(out=tt_[:], in_=bank[6][:, :], func=AF.Sigmoid, bias=bglu[:, oc:oc + 1], scale=1.0))(oc), reads=["bank6", "small"], writes=["e_tt"])
                    c.op("dve", (lambda oc: lambda e: e.tensor_tensor(ycT[:, 4 + oc, :], tt_[:], z32[:, oc, :], ALU.mult))(oc), reads=["e_tt", "e_z32"], writes=["e_ycT"])
                for tt in range(TB // 128 if "f" in stg else 0):
                    hr = hrow[tt % 2]
                    hk = "e_hrow%d" % (tt % 2)
                    ob = outt[tt % 2]
                    ok = "e_out%d" % (tt % 2)
                    c.dma("sp", (lambda tt, hr, t0: lambda e: e.dma_start(out=hr[:], in_=hsrc[t0 + tt * 128:t0 + (tt + 1) * 128, :]))(tt, hr, t0), writes=[hk])
                    for n in range(2):
                        for kc in range(8):
                            c.op("pe", (lambda tt, n, kc: lambda e: e.matmul(bank[6 + n][:, :], ycT[:, kc, tt * 128:(tt + 1) * 128], wout[:, kc, n * 512:(n + 1) * 512], start=(kc == 0), stop=(kc == 7)))(tt, n, kc),
                                 reads=["e_ycT", "wout"], writes=["bank%d" % (6 + n)])
                        c.op("dve", (lambda n, hr: lambda e: e.scalar_tensor_tensor(hr[:, n * 512:(n + 1) * 512], hr[:, n * 512:(n + 1) * 512], float(DN_ALPHA), bank[6 + n][:, :], ALU.mult, ALU.add))(n, hr),
                             reads=["bank%d" % (6 + n), hk], writes=[hk])
                    layer_norm_tile(c, g, hr, hk, gam, bet, "e_gam", ob, ok, "e")
                    c.dma("sp", (lambda tt, ob, t0: lambda e: e.dma_start(out=hdst[t0 + tt * 128:t0 + (tt + 1) * 128, :], in_=ob[:]))(tt, ob, t0), reads=[ok], cowrites=["hdst"])
        if "f" not in g.cfg.get("stages", "abcdef"):
            c.dma("sp", lambda e: e.dma_start(out=hdst[:, :], in_=hsrc[:, :]), writes=["hdst"])


VW = NH * 65
RW = 2 * D + VW


def odd_phase(c, g, W, layer, mi, SEQ, NSEQ, hsrc, hdst):
    bank = g.bank
    TT = SEQ * NSEQ
    NT = TT // 128
    qkv_d = g.qkv_d
    O_d = g.O_d
    stg = g.cfg.get("stages", "ABC")
    with c.phase():
        wqkv = c.sb("o_wqkv", [128, 8, 3 * D], BF16)
        hrow = [c.sb("o_hrow%d" % i, [128, D], F32) for i in range(2)]
        hT = c.sb("o_hT", [128, 8, 128], BF16)
        cs = [c.sb("o_cs%d" % i, [128, 2, 512], F32) for i in range(2)]
        qk32 = c.sb("o_qk32", [128, 2, D], F32)
        tq = [c.sb("o_tq%d" % i, [128, 512], F32) for i in range(2)]
        tk = [c.sb("o_tk%d" % i, [128, 512], F32) for i in range(2)]
        rows = [c.sb("o_rows%d" % i, [128, RW], BF16) for i in range(2)]
        for kc in range(8):
            for part in range(3):
                c.dma("pool", (lambda kc, part: lambda e: e.dma_start(out=wqkv[:, kc, part * D:(part + 1) * D], in_=W["att_w_qkv"][mi, kc * 128:(kc + 1) * 128, part * D:(part + 1) * D], max_dma_last_dim=4096))(kc, part), cowrites=["wqkv"])
        for i in range(2):
            c.op("pool", (lambda i: lambda e: e.memset(rows[i][:, 2 * D:RW], 1.0))(i), writes=["o_rows%d" % i])
        for i in range(NT if "A" in stg else 0):
            b = i % 2
            hr, hk = hrow[b], "o_hrow%d" % b
            pos0 = (i * 128) % SEQ
            c.dma("sp", (lambda i, hr: lambda e: e.dma_start(out=hr[:], in_=hsrc[i * 128:(i + 1) * 128, :]))(i, hr), writes=[hk])
            c.dma("sp", (lambda b, pos0: lambda e: e.dma_start(out=cs[b][:, 0, :], in_=W["c_cos16"][pos0:pos0 + 128, :]))(b, pos0), cowrites=["o_cs%d" % b], reads=["o_csr%d" % b])
            c.dma("sp", (lambda b, pos0: lambda e: e.dma_start(out=cs[b][:, 1, :], in_=W["c_sin16"][pos0:pos0 + 128, :]))(b, pos0), cowrites=["o_cs%d" % b], reads=["o_csr%d" % b])
            transpose_rows_to_T(c, g, hr, hk, hT, "o_hT", bank[0], "bank0", fp32_in=True, evac="act")
            for n in range(6):
                for kc in range(8):
                    c.op("pe", (lambda n, kc: lambda e: e.matmul(bank[2 + n][:, :], hT[:, kc, :], wqkv[:, kc, n * 512:(n + 1) * 512], start=(kc == 0), stop=(kc == 7)))(n, kc),
                         reads=["o_hT", "wqkv"], writes=["bank%d" % (2 + n)])
            R = rows[b]
            Rk = "o_rows%d" % b
            for n in range(4):
                c.op("act", (lambda n: lambda e: e.activation(out=qk32[:, n // 2, (n % 2) * 512:(n % 2 + 1) * 512], in_=bank[2 + n][:, :], func=AF.Copy))(n),
                     reads=["bank%d" % (2 + n)], writes=["o_qk32_%d" % (n // 2)])
            for n in range(2):
                c.op("act", (lambda n, R: lambda e: e.activation(out=R[:, 2 * D:RW].rearrange("p (h e) -> p h e", e=65)[:, n * 8:(n + 1) * 8, 0:64], in_=bank[6 + n][:, :].rearrange("p (h e) -> p h e", e=64), func=AF.Copy))(n, R),
                     reads=["bank%d" % (6 + n), Rk], writes=[Rk + "v"])
            for which, eng, tmp in ((0, "dve", tq), (1, "pool", tk)):
                x3 = qk32[:, which, :].rearrange("p (h e) -> p h e", e=64)
                o3 = R[:, which * D:(which + 1) * D].rearrange("p (h e) -> p h e", e=64)
                cosv = cs[b][:, 0, :].rearrange("p (h e) -> p h e", e=32)
                sinv = cs[b][:, 1, :].rearrange("p (h e) -> p h e", e=32)
                t1 = tmp[0][:].rearrange("p (h e) -> p h e", e=32)
                t2 = tmp[1][:].rearrange("p (h e) -> p h e", e=32)
                xk = "o_qk32_%d" % which
                tk_ = "o_tmp%d" % which
                rk = Rk + ("q" if which == 0 else "k")
                csk = "o_cs%d" % b
                c.op(eng, (lambda t1, x3, cosv: lambda e: e.tensor_tensor(t1, x3[:, :, 0:32], cosv, ALU.mult))(t1, x3, cosv), reads=[xk, csk], writes=[tk_ + "a"])
                c.op(eng, (lambda t2, x3, sinv: lambda e: e.tensor_tensor(t2, x3[:, :, 32:64], sinv, ALU.mult))(t2, x3, sinv), reads=[xk, csk], writes=[tk_ + "b"])
                c.op(eng, (lambda o3, t1, t2: lambda e: e.tensor_tensor(o3[:, :, 0:32], t1, t2, ALU.subtract))(o3, t1, t2), reads=[tk_ + "a", tk_ + "b"], writes=[rk])
                c.op(eng, (lambda t1, x3, cosv: lambda e: e.tensor_tensor(t1, x3[:, :, 32:64], cosv, ALU.mult))(t1, x3, cosv), reads=[xk, csk, rk], writes=[tk_ + "a"])
                c.op(eng, (lambda t2, x3, sinv: lambda e: e.tensor_tensor(t2, x3[:, :, 0:32], sinv, ALU.mult))(t2, x3, sinv), reads=[xk, csk, rk], writes=[tk_ + "b"])
                ev = c.op(eng, (lambda o3, t1, t2: lambda e: e.tensor_tensor(o3[:, :, 32:64], t1, t2, ALU.add))(o3, t1, t2), reads=[tk_ + "a", tk_ + "b"], writes=[rk])
                c.readers.setdefault("o_csr%d" % b, []).append(ev)
            c.dma("sp", (lambda i, R: lambda e: e.dma_start(out=qkv_d[i * 128:(i + 1) * 128, :], in_=R[:, :]))(i, R),
                  reads=[Rk + "q", Rk + "k", Rk + "v", Rk], cowrites=["qkv_d"])
            for sfx in ("q", "k", "v"):
                c.readers.setdefault(Rk + sfx, []).append(c.last_w["qkv_d"][-1])

    with c.phase():
        R3 = [c.sb("o_R%d" % i, [128, RW], BF16) for i in range(3)]
        KT = [c.sb("o_KT%d" % i, [128, 8, 128], BF16) for i in range(2)]
        QTz = [[c.sb("o_QT%d_%d" % (j, i), [128, 8, 128], BF16) for i in range(2)] for j in range(2)]
        for j in range(2):
            for i in range(2):
                c.op("pool", (lambda i, j: lambda e: e.memset(QTz[j][i][:], 0.0))(i, j), writes=["o_QT%d" % j])
        PT = [c.sb("o_PT%d" % i, [128, 512], BF16) for i in range(3)]
        mcur = c.sb("o_mcur", [128, 512], BF16)
        mprev = c.sb("o_mprev", [128, 512], BF16)
        osb = [c.sb("o_osb%d" % i, [128, VW], F32) for i in range(2)]
        c.dma("pool", lambda e: e.dma_start(out=mcur[:], in_=W["c_mcur"][:, :]), writes=["o_mcur"])
        c.dma("pool", lambda e: e.dma_start(out=mprev[:], in_=W["c_mprev"][:, :]), writes=["o_mprev"])
        hb_ = [(0, 7), (7, 14), (14, 16)]
        ulist = []
        for s_ in range(NSEQ if "B" in stg else 0):
            for pi, (win_, dil) in enumerate(PATTERNS):
                nb = SEQ // (128 * dil)
                for r in range(dil):
                    for n in range(nb):
                        ulist.append((s_, pi, dil, r, n))
        b0 = bank[0][:].bitcast(BF16)
        b1 = bank[1][:].bitcast(BF16)

        def views(u):
            s_, pi, dil, r, n = ulist[u]
            qv = qkv_d[s_ * SEQ:(s_ + 1) * SEQ, :].rearrange("(m d) c -> d m c", d=dil)
            ov = O_d[pi, s_ * SEQ:(s_ + 1) * SEQ, :].rearrange("(m d) c -> d m c", d=dil)
            return qv, ov

        def prologue(u):
            s_, pi, dil, r, n = ulist[u]
            qv, ov = views(u)
            R, Rk = R3[u % 3], "o_R%d" % (u % 3)
            kt, ktk = KT[u % 2], "o_KT%d" % (u % 2)
            qt, qtk = QTz[u % 2], "o_QT%d" % (u % 2)
            c.dma("sp", lambda e: e.dma_start(out=R[:, :], in_=qv[r, n * 128:(n + 1) * 128, :]), reads=["qkv_d"], writes=[Rk])
            for pr in range(8):
                c.op("pe", (lambda pr: lambda e: e.transpose(b0[:, pr * 128:(pr + 1) * 128], R[:, D + pr * 128:D + (pr + 1) * 128], g.identb[:]))(pr), reads=[Rk, "identb"], writes=["bank0"])
            c.op("act", lambda e: e.activation(out=kt[:].rearrange("p a b -> p (a b)"), in_=b0[:, 0:1024], func=AF.Copy), reads=["bank0"], writes=[ktk])
            for pr in range(8):
                c.op("pe", (lambda pr: lambda e: e.transpose(b1[:, pr * 128:(pr + 1) * 128], R[:, pr * 128:(pr + 1) * 128], g.identb[:]))(pr), reads=[Rk, "identb"], writes=["bank1"])
            c.op("dve", lambda e: e.tensor_copy(qt[0][0:64].rearrange("p a b -> p (a b)"), b1[0:64, 0:1024]), reads=["bank1"], writes=[qtk])
            c.op("dve", lambda e: e.tensor_copy(qt[1][64:128].rearrange("p a b -> p (a b)"), b1[64:128, 0:1024]), reads=["bank1", qtk], writes=[qtk])

        gcount = [0]

        def groups_of(u):
            s_, pi, dil, r, n = ulist[u]
            R, Rk = R3[u % 3], "o_R%d" % (u % 3)
            Rp, Rpk = R3[(u - 1) % 3], "o_R%d" % ((u - 1) % 3)
            kt, ktk = KT[u % 2], "o_KT%d" % (u % 2)
            ktp, ktpk = KT[(u - 1) % 2], "o_KT%d" % ((u - 1) % 2)
            qt, qtk = QTz[u % 2], "o_QT%d" % (u % 2)
            kbs = ([(ktp, ktpk, Rp, Rpk, mprev, "o_mprev")] if n > 0 else []) + [(kt, ktk, R, Rk, mcur, "o_mcur")]
            started = [False, False, False]
            out = []
            for (kT_, kTk, Rv, Rvk, msk, mskk) in kbs:
                for grp in range(4):
                    gi = gcount[0]
                    gcount[0] += 1
                    sb_, sbk = bank[2 + gi % 2], "bank%d" % (2 + gi % 2)
                    pt, ptk = PT[gi % 3], "o_PT%d" % (gi % 3)

                    def S(sb_=sb_, sbk=sbk, pt=pt, ptk=ptk, kT_=kT_, kTk=kTk, msk=msk, mskk=mskk, grp=grp):
                        for hh in range(4):
                            h = grp * 4 + hh
                            pr, hf = h // 2, h % 2
                            c.op("pe", (lambda hh, pr, hf: lambda e: e.matmul(sb_[:, hh * 128:(hh + 1) * 128], kT_[:, pr, :], qt[hf][:, pr, :], start=True, stop=True))(hh, pr, hf),
                                 reads=[kTk, qtk], writes=[sbk])
                        c.op("act", lambda e: e.activation(out=pt[:], in_=sb_[:, :], func=AF.Exp, scale=0.125), reads=[sbk], writes=[ptk])
                        c.op("dve", lambda e: e.tensor_tensor(pt[:], pt[:], msk[:], ALU.mult), reads=[ptk, mskk], writes=[ptk])

                    sts = []
                    for hh in range(4):
                        h = grp * 4 + hh
                        bi = 0 if h < 7 else (1 if h < 14 else 2)
                        sts.append(not started[bi])
                        started[bi] = True

                    def P(pt=pt, ptk=ptk, Rv=Rv, Rvk=Rvk, grp=grp, sts=sts):
                        for hh in range(4):
                            h = grp * 4 + hh
                            bi = 0 if h < 7 else (1 if h < 14 else 2)
                            col = (h - hb_[bi][0]) * 65
                            c.op("pe", (lambda bi, col, hh, h, st: lambda e: e.matmul(bank[4 + bi][:, col:col + 65], pt[:, hh * 128:(hh + 1) * 128], Rv[:, 2 * D + h * 65:2 * D + (h + 1) * 65], start=st, stop=True, skip_group_check=True))(bi, col, hh, h, sts[hh]),
                                 reads=[ptk, Rvk], writes=["bank%d" % (4 + bi)])
                    out.append((S, P))
            return out

        def epilogue(u):
            s_, pi, dil, r, n = ulist[u]
            qv, ov = views(u)
            ob, obk = osb[u % 2], "o_osb%d" % (u % 2)
            for bi, (h0, h1) in enumerate(hb_):
                eng = "act" if bi == 1 else "dve"
                c.op(eng, (lambda bi, h0, h1, eng: lambda e: ecopy(e, eng, ob[:, h0 * 65:h1 * 65], bank[4 + bi][:, 0:(h1 - h0) * 65]))(bi, h0, h1, eng),
                     reads=["bank%d" % (4 + bi)], writes=[obk + "_%d" % bi])
            c.dma("sp", lambda e: e.dma_start(out=ov[r, n * 128:(n + 1) * 128, :], in_=ob[:, :]),
                  reads=[obk + "_0", obk + "_1", obk + "_2"], cowrites=["O_d"])

        NU = len(ulist)
        if NU:
            prologue(0)
        pendingP = None
        for u in range(NU):
            grps = groups_of(u)
            ng = len(grps)
            for gi_, (S, P) in enumerate(grps):
                S()
                if pendingP is not None:
                    pendingP[0]()
                    if pendingP[1] is not None:
                        epilogue(pendingP[1])
                pendingP = (P, u if gi_ == ng - 1 else None)
                if gi_ == ng // 2 and u + 1 < NU:
                    prologue(u + 1)
        if pendingP is not None:
            pendingP[0]()
            epilogue(pendingP[1])

    with c.phase():
        wo = c.sb("o_wo", [128, 8, D], BF16)
        gam = c.sb("o_gam", [128, D], F32)
        bet = c.sb("o_bet", [128, D], F32)
        hrow = [c.sb("o_hrow%d" % i, [128, D], F32) for i in range(2)]
        ot = [[c.sb("o_ot%d_%d" % (i, p), [128, VW], F32) for p in range(3)] for i in range(2)]
        rden = c.sb("o_rden", [128, NH], F32)
        osb2 = c.sb("o_o", [128, D], BF16)
        oT = c.sb("o_oT", [128, 8, 128], BF16)
        outt = [c.sb("o_out%d" % i, [128, D], F32) for i in range(2)]
        for kc in range(8):
            c.dma("pool", (lambda kc: lambda e: e.dma_start(out=wo[:, kc, :], in_=W["att_w_o"][mi, kc * 128:(kc + 1) * 128, :], max_dma_last_dim=4096))(kc), cowrites=["wo"])
        c.dma("sp", lambda e: e.dma_start(out=gam[:], in_=W["ln_mix_g"][layer:layer + 1, :].partition_broadcast(128)), cowrites=["o_gam"])
        c.dma("sp", lambda e: e.dma_start(out=bet[:], in_=W["ln_mix_b"][layer:layer + 1, :].partition_broadcast(128)), cowrites=["o_gam"])
        for i in range(NT if "C" in stg else 0):
            b = i % 2
            hr, hk = hrow[b], "o_hrow%d" % b
            c.dma("sp", (lambda i, hr: lambda e: e.dma_start(out=hr[:], in_=hsrc[i * 128:(i + 1) * 128, :]))(i, hr), writes=[hk])
            for p in range(3):
                c.dma("sp", (lambda i, b, p: lambda e: e.dma_start(out=ot[b][p][:, :], in_=O_d[p, i * 128:(i + 1) * 128, :]))(i, b, p), reads=["O_d"], writes=["o_ot%d_%d" % (b, p)])
            A = ot[b][0]
            Ak = "o_ot%d_0" % b
            c.op("pool", (lambda b, A: lambda e: e.tensor_tensor(A[:], A[:], ot[b][1][:], ALU.add))(b, A), reads=[Ak, "o_ot%d_1" % b], writes=[Ak])
            c.op("dve", (lambda b, A: lambda e: e.tensor_tensor(A[:], A[:], ot[b][2][:], ALU.add))(b, A), reads=[Ak, "o_ot%d_2" % b], writes=[Ak])
            A3 = A[:].rearrange("p (h e) -> p h e", e=65)
            c.op("dve", (lambda A3: lambda e: e.reciprocal(rden[:], A3[:, :, 64]))(A3), reads=[Ak], writes=["o_rden"])
            for h in range(NH):
                c.op("dve" if h % 2 == 0 else "pool", (lambda h, A3: lambda e: e.tensor_scalar(osb2[:, h * 64:(h + 1) * 64], A3[:, h, 0:64], rden[:, h:h + 1], None, ALU.mult))(h, A3),
                     reads=[Ak, "o_rden"], writes=["o_o%d" % (h % 2)])
            bb = bank[0][:].bitcast(BF16)
            for kc in range(8):
                c.op("pe", (lambda kc, bb: lambda e: e.transpose(bb[:, kc * 128:(kc + 1) * 128], osb2[:, kc * 128:(kc + 1) * 128], g.identb[:]))(kc, bb), reads=["o_o0", "o_o1", "identb"], writes=["bank0"])
            c.op("act", (lambda bb: lambda e: e.activation(out=oT[:].rearrange("p a b -> p (a b)"), in_=bb[:, 0:1024], func=AF.Copy))(bb), reads=["bank0"], writes=["o_oT"])
            for n in range(2):
                for kc in range(8):
                    c.op("pe", (lambda n, kc: lambda e: e.matmul(bank[6 + n][:, :], oT[:, kc, :], wo[:, kc, n * 512:(n + 1) * 512], start=(kc == 0), stop=(kc == 7)))(n, kc),
                         reads=["o_oT", "wo"], writes=["bank%d" % (6 + n)])
                c.op("dve", (lambda n, hr: lambda e: e.scalar_tensor_tensor(hr[:, n * 512:(n + 1) * 512], hr[:, n * 512:(n + 1) * 512], float(DN_ALPHA), bank[6 + n][:, :], ALU.mult, ALU.add))(n, hr),
                     reads=["bank%d" % (6 + n), hk], writes=[hk])
            layer_norm_tile(c, g, hr, hk, gam, bet, "o_gam", outt[b], "o_out%d" % b, "o")
            c.dma("sp", (lambda i, b: lambda e: e.dma_start(out=hdst[i * 128:(i + 1) * 128, :], in_=outt[b][:]))(i, b), reads=["o_out%d" % b], cowrites=["hdst"])
        if "C" not in stg:
            c.dma("sp", lambda e: e.dma_start(out=hdst[:, :], in_=hsrc[:, :]), writes=["hdst"])


NCORES = 4
FULL_SEQ = 8192
FULL_BATCH = 4
RENAME = {"expert_w_gu": "w_gu", "expert_b_gu": "b_gu", "expert_w_down": "w_dn", "expert_b_down": "b_dn"}
_CACHE = {}


def full_plan():
    plan = []
    for layer in range(DEPTH):
        plan.append(("even" if layer % 2 == 0 else "odd", layer, layer // 2))
        plan.append(("moe", layer, layer // 2))
    return plan


def kernel(**inputs):
    nseq = FULL_BATCH // NCORES
    tt = nseq * FULL_SEQ
    nl = {}
    wmap = {}
    for name, arr in inputs.items():
        if name == "x":
            continue
        kname = RENAME.get(name, name)
        wmap[kname] = np.ascontiguousarray(arr, dtype=np.float32)
        nl[kname] = arr.shape[0]
    cap = (tt * TOPK // NE) * 5 // 4
    cap = (cap + 127) // 128 * 128
    cfg = dict(SEQ=FULL_SEQ, NSEQ=nseq, C=cap, plan=full_plan(), nl=nl)
    key = (FULL_SEQ, nseq, cap)
    if key not in _CACHE:
        _CACHE[key] = build_program(cfg)
    nc, cst, _ = _CACHE[key]
    x = np.ascontiguousarray(inputs["x"], dtype=np.float32).reshape(NCORES, tt, D)
    in_maps = []
    for ci in range(NCORES):
        m = {"x": x[ci]}
        m.update(wmap)
        m.update(cst)
        in_maps.append(m)
    res = run_bass_kernel_spmd(nc, in_maps, core_ids=list(range(NCORES)))
    out = np.stack([res.results[ci]["out"] for ci in range(NCORES)], axis=0)
    return out.reshape(FULL_BATCH, FULL_SEQ, D).astype(np.float32)
```

```python
import contextlib
import numpy as np
import ml_dtypes
import concourse.bass as bass
import concourse.mybir as mybir
from concourse.bass_utils import run_bass_kernel_spmd

F32 = mybir.dt.float32
BF16 = mybir.dt.bfloat16
I32 = mybir.dt.int32
U32 = mybir.dt.uint32
AF = mybir.ActivationFunctionType
ALU = mybir.AluOpType
AX = mybir.AxisListType

D = 1024
NE = 32
TOPK = 4
DEPTH = 4
DN_ALPHA = (2 * DEPTH) ** 0.25
LN_EPS = 1e-5
NH = 16
HD = 64
PATTERNS = ((128, 1), (512, 4), (2048, 16))

SAME_ENG_WAIT = True
ENGS = ("pe", "dve", "act", "pool", "sp")
RING = {"sp": 12, "act": 6, "pool": 12}


class Ctx:
    def __init__(self, nc):
        self.nc = nc
        self.stack = contextlib.ExitStack()
        self.prog = {e: [] for e in ENGS}
        self.cnt = {e: 0 for e in ENGS}
        self.semobj = {}
        for e in ENGS:
            self.semobj[("e", e)] = self.stack.enter_context(nc.semaphore("s_" + e))
        for q, n in RING.items():
            for i in range(n):
                self.semobj[("r", q, i)] = self.stack.enter_context(nc.semaphore("r_%s%d" % (q, i)))
        self.dcnt = {q: 0 for q in RING}
        self.seen = {e: {} for e in ENGS}
        self.last_w = {}
        self.readers = {}
        self.n_instr = 0
        self.pstack = None

    def sb(self, name, shape, dt=F32):
        st = self.pstack if self.pstack is not None else self.stack
        self.uid = getattr(self, "uid", 0) + 1
        return st.enter_context(self.nc.sbuf_tensor("%s_u%d" % (name, self.uid), list(shape), dt))

    def reg(self, e, val):
        if not hasattr(self, "_regs"):
            self._regs = {}
        if val not in self._regs:
            self._regs[val] = e.to_reg(val)
        return self._regs[val]

    def ps(self, name, shape, dt=F32):
        return self.stack.enter_context(self.nc.psum_tensor(name, list(shape), dt))

    @contextlib.contextmanager
    def phase(self):
        self.barrier()
        self.pstack = contextlib.ExitStack()
        try:
            yield
        finally:
            self.barrier()
            self.pstack.close()
            self.pstack = None

    def cur_events(self):
        evs = []
        for e in ENGS:
            if self.cnt[e]:
                evs.append((("e", e), self.cnt[e]))
        for q, n in RING.items():
            i = self.dcnt[q]
            for slot in range(n):
                if i > slot:
                    last = ((i - 1 - slot) // n) * n + slot
                    evs.append((("r", q, slot), 16 * (last // n + 1)))
        return evs

    def barrier(self):
        evs = self.cur_events()
        for eng in ENGS:
            for k, v in evs:
                if self.seen[eng].get(k, 0) < v:
                    self.seen[eng][k] = v
                    self.prog[eng].append(("wait", k, v))
        self.last_w = {}
        self.readers = {}

    def _need(self, eng, reads, writes, cowrites=()):
        need = {}

        def add(ev):
            k, v = ev
            if need.get(k, 0) < v:
                need[k] = v
        for k in reads:
            for ev in self.last_w.get(k, ()):
                add(ev)
        for k in writes:
            for ev in self.last_w.get(k, ()):
                add(ev)
            for ev in self.readers.get(k, ()):
                add(ev)
        for k in cowrites:
            for ev in self.readers.get(k, ()):
                add(ev)
        for k, v in need.items():
            if k == ("e", eng) and (eng == "pe" or not SAME_ENG_WAIT):
                continue
            if self.seen[eng].get(k, 0) >= v:
                continue
            self.seen[eng][k] = v
            self.prog[eng].append(("wait", k, v))

    def _commit(self, ev, reads, writes, cowrites=()):
        for k in reads:
            self.readers.setdefault(k, []).append(ev)
        for k in writes:
            self.last_w[k] = [ev]
            self.readers[k] = []
        for k in cowrites:
            self.last_w.setdefault(k, []).append(ev)

    def op(self, eng, fn, reads=(), writes=()):
        self._need(eng, reads, writes)
        self.cnt[eng] += 1
        ev = (("e", eng), self.cnt[eng])
        self.prog[eng].append(("ins", fn, ("e", eng), 1))
        self._commit(ev, reads, writes)
        self.n_instr += 1
        return ev

    def dma(self, q, fn, reads=(), writes=(), cowrites=()):
        self._need(q, reads, writes, cowrites)
        i = self.dcnt[q]
        n = RING[q]
        slot = i % n
        if i >= n:
            k = ("r", q, slot)
            v = 16 * (i // n)
            if self.seen[q].get(k, 0) < v:
                self.seen[q][k] = v
                self.prog[q].append(("wait", k, v))
        self.dcnt[q] += 1
        ev = (("r", q, slot), 16 * (i // n + 1))
        self.prog[q].append(("ins", fn, ("r", q, slot), 16))
        self._commit(ev, reads, writes, cowrites)
        self.n_instr += 1
        return ev

    def emit(self):
        nc = self.nc
        self.barrier()
        with nc.Block() as block:
            def body(engname):
                def f(e):
                    for it in self.prog[engname]:
                        if it[0] == "wait":
                            e.wait_ge(self.semobj[it[1]], it[2])
                        else:
                            it[1](e).then_inc(self.semobj[it[2]], it[3])
                return f
            block.tensor(body("pe"))
            block.vector(body("dve"))
            block.scalar(body("act"))
            block.gpsimd(body("pool"))
            block.sync(body("sp"))
        self.stack.close()


class G:
    pass


def setup_globals(c, g, W):
    g.bank = [c.ps("bank%d" % i, [128, 512], F32) for i in range(8)]
    g.ident = c.sb("ident", [128, 128], F32)
    g.identb = c.sb("identb", [128, 128], BF16)
    g.onesrow = c.sb("onesrow", [1, 128], BF16)
    c.dma("sp", lambda e: e.dma_start(out=g.ident[:], in_=W["c_ident"][:, :]), writes=["ident"])
    c.dma("pool", lambda e: e.dma_start(out=g.identb[:], in_=W["c_ident"][:, :]), writes=["identb"])
    c.dma("pool", lambda e: e.dma_start(out=g.onesrow[:], in_=W["c_ones"][0:1, :]), writes=["onesrow"])


def ecopy(e, eng, out, in_):
    if eng == "act":
        return e.activation(out=out, in_=in_, func=AF.Copy)
    return e.tensor_copy(out, in_)


def load_bcast(c, tile, key, src_row_ap, q="sp"):
    c.dma(q, lambda e: e.dma_start(out=tile[:], in_=src_row_ap.partition_broadcast(128)), writes=[key])


def layer_norm_tile(c, g, acc, acck, gam, bet, gbk, out_tile, outk, tag):
    st = g.ln_stats
    mv = g.ln_mv
    sk = "ln_small"
    c.op("dve", lambda e: e.bn_stats(st[:, 0, :], acc[:, 0:512]), reads=[acck], writes=[sk])
    c.op("dve", lambda e: e.bn_stats(st[:, 1, :], acc[:, 512:1024]), reads=[acck, sk], writes=[sk])
    c.op("dve", lambda e: e.bn_aggr(mv[:, 0:2], st[:].rearrange("p a b -> p (a b)")), reads=[sk], writes=[sk])
    c.op("act", lambda e: e.activation(out=mv[:, 2:3], in_=mv[:, 1:2], func=AF.Sqrt, bias=g.eps_t[:, 0:1], scale=1.0),
         reads=[sk, "eps"], writes=[sk])
    c.op("dve", lambda e: e.reciprocal(mv[:, 3:4], mv[:, 2:3]), reads=[sk], writes=[sk])
    c.op("dve", lambda e: e.tensor_scalar(mv[:, 4:5], mv[:, 0:1], mv[:, 3:4], -1.0, ALU.mult, ALU.mult),
         reads=[sk], writes=[sk])
    c.op("act", lambda e: e.activation(out=acc[:], in_=acc[:], func=AF.Identity, bias=mv[:, 4:5], scale=mv[:, 3:4]),
         reads=[sk, acck], writes=[acck])
    c.op("dve", lambda e: e.tensor_tensor(acc[:], acc[:], gam[:], ALU.mult), reads=[acck, gbk], writes=[acck])
    c.op("dve", lambda e: e.tensor_tensor(out_tile[:], acc[:], bet[:], ALU.add), reads=[acck, gbk], writes=[outk])


def transpose_rows_to_T(c, g, rows, rowsk, xT, xTk, bank, bankk, nchunks=8, fp32_in=True, evac="dve"):
    if fp32_in:
        for half in range(nchunks // 4):
            for j in range(4):
                kc = half * 4 + j
                c.op("pe", (lambda kc, j: lambda e: e.transpose(bank[:, j * 128:(j + 1) * 128], rows[:, kc * 128:(kc + 1) * 128], g.ident[:]))(kc, j),
                     reads=[rowsk, "ident"], writes=[bankk])
            c.op(evac, (lambda half: lambda e: ecopy(e, evac, xT[:, half * 4:(half + 1) * 4, :].rearrange("p a b -> p (a b)"), bank[:, :]))(half),
                 reads=[bankk], writes=[xTk])
    else:
        bb = bank[:].bitcast(BF16)
        for kc in range(nchunks):
            c.op("pe", (lambda kc: lambda e: e.transpose(bb[:, kc * 128:(kc + 1) * 128], rows[:, kc * 128:(kc + 1) * 128], g.identb[:]))(kc),
                 reads=[rowsk, "identb"], writes=[bankk])
        c.op(evac, lambda e: ecopy(e, evac, xT[:].rearrange("p a b -> p (a b)"), bb[:, 0:nchunks * 128]),
             reads=[bankk], writes=[xTk])


def moe_phase(c, g, W, layer, TT, hsrc, hdst, C):
    NT = TT // 128
    NSL = C // 128
    bank = g.bank
    with c.phase():
        hrow = [c.sb("m_hrow%d" % i, [128, D], F32) for i in range(2)]
        hT = c.sb("m_hT", [128, 8, 128], F32)
        rw = c.sb("m_rw", [128, 8, NE], F32)
        rb = c.sb("m_rb", [128, NE], F32)
        ltri = c.sb("m_ltri", [128, 128], F32)
        ones = c.sb("m_ones", [128, 128], F32)
        iota = c.sb("m_iota", [128, NE], F32)
        base = c.sb("m_base", [128, NE], F32)
        logit = c.sb("m_logit", [128, NE], F32)
        top8 = c.sb("m_top8", [128, 8], F32)
        idx8 = c.sb("m_idx8", [128, 8], U32)
        idxf = c.sb("m_idxf", [128, 4], F32)
        sm = c.sb("m_sm", [128, 16], F32)
        oh = c.sb("m_oh", [128, 4, NE], F32)
        Mt = c.sb("m_M", [128, NE], F32)
        rank = c.sb("m_rank", [128, NE], F32)
        tmp = c.sb("m_tmp", [128, NE], F32)
        pos = c.sb("m_pos", [128, 8], F32)
        c.dma("sp", lambda e: e.dma_start(out=rw[:], in_=W["router_w"][layer].rearrange("(kc p) e -> p kc e", p=128)), writes=["rw"])
        load_bcast(c, rb, "rb", W["router_b"][layer:layer + 1, :])
        c.dma("sp", lambda e: e.dma_start(out=ltri[:], in_=W["c_ltri"][:, :]), writes=["ltri"])
        c.dma("sp", lambda e: e.dma_start(out=ones[:], in_=W["c_onesq"][:, :]), writes=["ones"])
        c.dma("sp", lambda e: e.dma_start(out=iota[:], in_=W["c_iota"][:, :]), writes=["iota"])
        c.op("dve", lambda e: e.memset(base[:], 0.0), writes=["base"])
        S = "m_small"
        for i in range(NT):
            hr = hrow[i % 2]
            hk = "m_hrow%d" % (i % 2)
            c.dma("sp", (lambda i, hr: lambda e: e.dma_start(out=hr[:], in_=hsrc[i * 128:(i + 1) * 128, :]))(i, hr), writes=[hk])
            transpose_rows_to_T(c, g, hr, hk, hT, "m_hT", bank[0], "bank0", fp32_in=True, evac="act")
            for kc in range(8):
                c.op("pe", (lambda kc: lambda e: e.matmul(bank[1][:, 0:NE], hT[:, kc, :], rw[:, kc, :], start=(kc == 0), stop=(kc == 7)))(kc),
                     reads=["m_hT", "rw"], writes=["bank1"])
            c.op("dve", lambda e: e.tensor_tensor(logit[:], bank[1][:, 0:NE], rb[:], ALU.add), reads=["bank1", "rb"], writes=[S])
            c.op("dve", lambda e: e.max(top8[:], logit[:]), reads=[S], writes=[S])
            c.op("dve", lambda e: e.max_index(idx8[:], top8[:], logit[:]), reads=[S], writes=[S])
            c.op("dve", lambda e: e.tensor_copy(idxf[:], idx8[:, 0:4]), reads=[S], writes=[S])
            c.op("dve", lambda e: e.tensor_scalar_mul(sm[:, 0:1], top8[:, 0:1], -1.0), reads=[S], writes=[S])
            c.op("act", lambda e: e.activation(out=sm[:, 4:8], in_=top8[:, 0:4], func=AF.Exp, bias=sm[:, 0:1], scale=1.0, accum_out=sm[:, 1:2]),
                 reads=[S], writes=[S])
            c.op("dve", lambda e: e.reciprocal(sm[:, 2:3], sm[:, 1:2]), reads=[S], writes=[S])
            c.op("dve", (lambda i: lambda e: e.tensor_scalar_mul(g.gates_all[:, i, :], sm[:, 4:8], sm[:, 2:3]))(i), reads=[S], writes=["gates_all"])
            for k in range(4):
                c.op("dve", (lambda k: lambda e: e.tensor_scalar(oh[:, k, :], iota[:], idxf[:, k:k + 1], None, ALU.is_equal))(k),
                     reads=[S, "iota"], writes=[S])
            c.op("dve", lambda e: e.tensor_tensor(Mt[:], oh[:, 0, :], oh[:, 1, :], ALU.add), reads=[S], writes=["m_M"])
            c.op("dve", lambda e: e.tensor_tensor(Mt[:], Mt[:], oh[:, 2, :], ALU.add), reads=[S, "m_M"], writes=["m_M"])
            c.op("dve", lambda e: e.tensor_tensor(Mt[:], Mt[:], oh[:, 3, :], ALU.add), reads=[S, "m_M"], writes=["m_M"])
            c.op("pe", lambda e: e.matmul(bank[2][:, 0:NE], ltri[:], Mt[:], start=True, stop=True), reads=["m_M", "ltri"], writes=["bank2"])
            c.op("pe", lambda e: e.matmul(bank[3][:, 0:NE], ones[:], Mt[:], start=True, stop=True), reads=["m_M", "ones"], writes=["bank3"])
            c.op("dve", lambda e: e.tensor_tensor(rank[:], bank[2][:, 0:NE], base[:], ALU.add), reads=["bank2", "base"], writes=[S])
            c.op("dve", lambda e: e.tensor_tensor(base[:], bank[3][:, 0:NE], base[:], ALU.add), reads=["bank3", S], writes=["base"])
            for k in range(4):
                c.op("dve", (lambda k: lambda e: e.scalar_tensor_tensor(tmp[:], oh[:, k, :], 1.0, rank[:], ALU.mult, ALU.mult, accum_out=pos[:, k:k + 1]))(k),
                     reads=[S], writes=[S])
            c.op("dve", lambda e: e.tensor_scalar(pos[:, 4:8], pos[:, 0:4], float(C), 1.0e6, ALU.is_ge, ALU.mult), reads=[S], writes=[S])
            c.op("dve", lambda e: e.tensor_tensor(pos[:, 0:4], pos[:, 0:4], pos[:, 4:8], ALU.add), reads=[S], writes=[S])
            c.op("dve", lambda e: e.scalar_tensor_tensor(pos[:, 4:8], idxf[:], float(C), pos[:, 0:4], ALU.mult, ALU.add), reads=[S], writes=[S])
            c.op("dve", (lambda i: lambda e: e.tensor_copy(g.drow_all[:, i, :], pos[:, 4:8]))(i), reads=[S], writes=["drow_all"])
            for k in range(4):
                c.dma("pool", (lambda i, k, hr: lambda e: e.indirect_dma_start(
                    out=g.Xd[:, :], out_offset=bass.IndirectOffsetOnAxis(ap=g.drow_all[:, i, k:k + 1], axis=0),
                    in_=hr[:, :], in_offset=None, bounds_check=c.reg(e, NE * C - 1), oob_is_err=False))(i, k, hr),
                    reads=[hk, "drow_all"], cowrites=["Xd"])

    with c.phase():
        wgu = [c.sb("m_wgu%d" % i, [128, 8, 2048], BF16) for i in range(2)]
        wdn = [c.sb("m_wdn%d" % i, [128, 8, 1024], BF16) for i in range(2)]
        bgu = [c.sb("m_bgu%d" % i, [1, 2048], BF16) for i in range(2)]
        bdn = [c.sb("m_bdn%d" % i, [1, 1024], BF16) for i in range(2)]
        xrow = [c.sb("m_xrow%d" % i, [128, D], BF16) for i in range(2)]
        xT = [c.sb("m_xT%d" % i, [128, 8, 128], BF16) for i in range(2)]
        gt = [c.sb("m_g%d" % i, [128, 1024], F32) for i in range(2)]
        sg = [c.sb("m_sg%d" % i, [128, 1024], F32) for i in range(2)]
        lin = [c.sb("m_lin%d" % i, [128, 1024], F32) for i in range(2)]
        act = [c.sb("m_act%d" % i, [128, 1024], BF16) for i in range(2)]
        actT = [c.sb("m_actT%d" % i, [128, 8, 128], BF16) for i in range(2)]
        yt = [c.sb("m_y%d" % i, [128, 1024], F32) for i in range(2)]

        def load_w(ex):
            p = ex % 2
            wg_src = W["w_gu"][layer, ex].rearrange("(kc p) n -> p kc n", p=128)
            wd_src = W["w_dn"][layer, ex].rearrange("(kc p) n -> p kc n", p=128)
            for kc in range(8):
                c.dma("pool", (lambda kc: lambda e: e.dma_start(out=wgu[p][:, kc, :], in_=wg_src[:, kc, :], max_dma_last_dim=8192))(kc),
                      cowrites=["wgu%d" % p], reads=[], writes=[])
            for kc in range(8):
                c.dma("pool", (lambda kc: lambda e: e.dma_start(out=wdn[p][:, kc, :], in_=wd_src[:, kc, :], max_dma_last_dim=4096))(kc),
                      cowrites=["wdn%d" % p])
            c.dma("pool", lambda e: e.dma_start(out=bgu[p][:], in_=W["b_gu"][layer, ex:ex + 1, :], max_dma_last_dim=8192), cowrites=["wgu%d" % p])
            c.dma("pool", lambda e: e.dma_start(out=bdn[p][:], in_=W["b_dn"][layer, ex:ex + 1, :], max_dma_last_dim=4096), cowrites=["wdn%d" % p])

        units = [(ex, i) for ex in range(NE) for i in range(NSL)]

        def st_load(u):
            ex, i = units[u]
            b = u % 2
            r0 = ex * C + i * 128
            c.dma("sp", lambda e: e.dma_start(out=xrow[b][:], in_=g.Xd[r0:r0 + 128, :]), reads=["Xd"], writes=["xrow%d" % b])

        def st_tx(u):
            b = u % 2
            transpose_rows_to_T(c, g, xrow[b], "xrow%d" % b, xT[b], "xT%d" % b, bank[0], "bank0", fp32_in=False, evac="dve")

        def st_gu(u):
            ex, i = units[u]
            b = u % 2
            p = ex % 2
            for n in range(4):
                for kc in range(8):
                    c.op("pe", (lambda n, kc: lambda e: e.matmul(bank[2 + n][:, :], xT[b][:, kc, :], wgu[p][:, kc, n * 512:(n + 1) * 512], start=(kc == 0), stop=False))(n, kc),
                         reads=["xT%d" % b, "wgu%d" % p], writes=["gu%d" % n])
                c.op("pe", (lambda n: lambda e: e.matmul(bank[2 + n][:, :], g.onesrow[0:1, :], bgu[p][0:1, n * 512:(n + 1) * 512], start=False, stop=True))(n),
                     reads=["onesrow", "wgu%d" % p], writes=["gu%d" % n])
            sls = [slice(n * 512, (n + 1) * 512) for n in range(2)]
            for n in range(2):
                sl = sls[n]
                c.op("dve", (lambda n, sl: lambda e: e.tensor_scalar_min(gt[b][:, sl], bank[2 + n][:, :], 7.0))(n, sl), reads=["gu%d" % n], writes=["g%d_%d" % (b, n)])
                c.op("act", (lambda sl: lambda e: e.activation(out=sg[b][:, sl], in_=gt[b][:, sl], func=AF.Sigmoid, scale=1.702))(sl), reads=["g%d_%d" % (b, n)], writes=["sg%d_%d" % (b, n)])
            for n in range(2):
                sl = sls[n]
                c.op("dve", (lambda n, sl: lambda e: e.tensor_scalar(lin[b][:, sl], bank[4 + n][:, :], 7.0, -7.0, ALU.min, ALU.max))(n, sl), reads=["gu%d" % (2 + n)], writes=["lin%d_%d" % (b, n)])
            for n in range(2):
                sl = sls[n]
                c.op("dve", (lambda sl: lambda e: e.scalar_tensor_tensor(lin[b][:, sl], lin[b][:, sl], 1.0, gt[b][:, sl], ALU.add, ALU.mult))(sl),
                     reads=["lin%d_%d" % (b, n), "g%d_%d" % (b, n)], writes=["lin%d_%d" % (b, n)])
            for n in range(2):
                sl = sls[n]
                c.op("pool" if n == 1 else "dve", (lambda sl: lambda e: e.tensor_tensor(act[b][:, sl], lin[b][:, sl], sg[b][:, sl], ALU.mult))(sl),
                     reads=["lin%d_%d" % (b, n), "sg%d_%d" % (b, n)], writes=["act%d_%d" % (b, n)])

        def st_tact(u):
            b = u % 2
            bb = bank[1][:].bitcast(BF16)
            for kc in range(8):
                c.op("pe", (lambda kc: lambda e: e.transpose(bb[:, kc * 128:(kc + 1) * 128], act[b][:, kc * 128:(kc + 1) * 128], g.identb[:]))(kc),
                     reads=["act%d_%d" % (b, kc // 4), "identb"], writes=["bank1"])
            c.op("act", lambda e: e.activation(out=actT[b][:].rearrange("p a b -> p (a b)"), in_=bb[:, 0:1024], func=AF.Copy), reads=["bank1"], writes=["actT%d" % b])

        def st_down(u):
            ex, i = units[u]
            b = u % 2
            p = ex % 2
            for n in range(2):
                for kc in range(8):
                    c.op("pe", (lambda n, kc: lambda e: e.matmul(bank[6 + n][:, :], actT[b][:, kc, :], wdn[p][:, kc, n * 512:(n + 1) * 512], start=(kc == 0), stop=False))(n, kc),
                         reads=["actT%d" % b, "wdn%d" % p], writes=["dn%d" % n])
                c.op("pe", (lambda n: lambda e: e.matmul(bank[6 + n][:, :], g.onesrow[0:1, :], bdn[p][0:1, n * 512:(n + 1) * 512], start=False, stop=True))(n),
                     reads=["onesrow", "wdn%d" % p], writes=["dn%d" % n])
                c.op("act", (lambda n: lambda e: e.activation(out=yt[b][:, n * 512:(n + 1) * 512], in_=bank[6 + n][:, :], func=AF.Copy))(n),
                     reads=["dn%d" % n], writes=["y%d" % b])
            r0 = ex * C + i * 128
            c.dma("sp", lambda e: e.dma_start(out=g.Yb[r0:r0 + 128, :], in_=yt[b][:]), reads=["y%d" % b], cowrites=["Yb"])

        NU = len(units)
        load_w(0)
        st_load(0)
        st_tx(0)
        for u in range(NU):
            ex, i = units[u]
            if u + 1 < NU:
                st_load(u + 1)
            if u >= 1:
                st_tact(u - 1)
            if u + 1 < NU:
                st_tx(u + 1)
            st_gu(u)
            if u >= 1:
                st_down(u - 1)
            if i == 0 and ex + 1 < NE:
                load_w(ex + 1)
        st_tact(NU - 1)
        st_down(NU - 1)

    with c.phase():
        hrow = [c.sb("c_hrow%d" % i, [128, D], F32) for i in range(2)]
        yk = [[c.sb("c_y%d_%d" % (i, k), [128, D], F32) for k in range(4)] for i in range(2)]
        outt = [c.sb("c_out%d" % i, [128, D], F32) for i in range(2)]
        gam = c.sb("c_gam", [128, D], F32)
        bet = c.sb("c_bet", [128, D], F32)
        load_bcast(c, gam, "c_gb", W["ln_ffn_g"][layer:layer + 1, :])
        c.dma("sp", lambda e: e.dma_start(out=bet[:], in_=W["ln_ffn_b"][layer:layer + 1, :].partition_broadcast(128)), cowrites=["c_gb"])
        for i in range(NT):
            b = i % 2
            hr = hrow[b]
            hk = "c_hrow%d" % b
            c.dma("sp", (lambda i, hr: lambda e: e.dma_start(out=hr[:], in_=hsrc[i * 128:(i + 1) * 128, :]))(i, hr), writes=[hk])
            for k in range(4):
                c.dma("pool", (lambda i, k, b: lambda e: e.indirect_dma_start(
                    out=yk[b][k][:, :], out_offset=None, in_=g.Yb[:, :],
                    in_offset=bass.IndirectOffsetOnAxis(ap=g.drow_all[:, i, k:k + 1], axis=0),
                    bounds_check=c.reg(e, NE * C - 1), oob_is_err=False))(i, k, b),
                    reads=["Yb", "drow_all"], writes=["c_y%d_%d" % (b, k)])
            c.op("act", (lambda hr: lambda e: e.mul(hr[:], hr[:], float(DN_ALPHA)))(hr), reads=[hk], writes=[hk])
            for k in range(4):
                c.op("dve", (lambda i, k, b, hr: lambda e: e.scalar_tensor_tensor(hr[:], yk[b][k][:], g.gates_all[:, i, k:k + 1], hr[:], ALU.mult, ALU.add))(i, k, b, hr),
                     reads=[hk, "c_y%d_%d" % (b, k), "gates_all"], writes=[hk])
            layer_norm_tile(c, g, hr, hk, gam, bet, "c_gb", outt[b], "c_out%d" % b, "c")
            c.dma("sp", (lambda i, b: lambda e: e.dma_start(out=hdst[i * 128:(i + 1) * 128, :], in_=outt[b][:]))(i, b),
                  reads=["c_out%d" % b], cowrites=["hdst"])


def host_constants(SEQ):
    cst = {}
    cst["c_ident"] = np.eye(128, dtype=np.float32)
    cst["c_ones"] = np.ones((1, 128), np.float32)
    cst["c_onesq"] = np.ones((128, 128), np.float32)
    k = np.arange(128)
    cst["c_ltri"] = (k[:, None] < k[None, :]).astype(np.float32)
    cst["c_iota"] = np.tile(np.arange(NE, dtype=np.float32)[None, :], (128, 1))
    mc = (k[:, None] <= k[None, :]).astype(np.float32)
    mp = (k[:, None] >= k[None, :]).astype(np.float32)
    cst["c_mcur"] = np.tile(mc, (1, 4)).astype(np.float32)
    cst["c_mprev"] = np.tile(mp, (1, 4)).astype(np.float32)
    par = np.zeros((32, 2), np.float32)
    par[0::2, 0] = 1.0
    par[1::2, 1] = 1.0
    cst["c_par"] = par
    cst["c_iotaT"] = np.tile(np.arange(TB + 1, dtype=np.float32)[None, :], (128, 1))
    half = HD // 2
    inv = (10000.0 ** (-np.arange(half, dtype=np.float32) / half)).astype(np.float32)
    ang = (np.arange(SEQ, dtype=np.float32)[:, None] * inv[None, :]).astype(np.float32)
    cst["c_cos16"] = np.tile(np.cos(ang).astype(np.float32), (1, NH))
    cst["c_sin16"] = np.tile(np.sin(ang).astype(np.float32), (1, NH))
    return cst


WEIGHT_SHAPES = {
    "hy_w_in": (D, 2048), "conv_w": (3, 512), "ssm_a_re": (32, 64), "ssm_a_im": (32, 64), "ssm_log_dt": (32,),
    "ssm_b_re": (32, 64, 16), "ssm_b_im": (32, 64, 16), "ssm_c_re": (32, 16, 64), "ssm_c_im": (32, 16, 64),
    "ssm_d": (32, 16), "ssm_w_glu": (512, 512), "ssm_b_glu": (512,), "hy_w_out": (D, D),
    "att_w_qkv": (D, 3 * D), "att_w_o": (D, D),
    "ln_mix_g": (D,), "ln_mix_b": (D,), "ln_ffn_g": (D,), "ln_ffn_b": (D,),
    "router_w": (D, NE), "router_b": (NE,), "w_gu": (NE, D, 2 * D), "b_gu": (NE, 2 * D),
    "w_dn": (NE, D, D), "b_dn": (NE, D),
}


def build_program(cfg):
    SEQ, NSEQ, C = cfg["SEQ"], cfg["NSEQ"], cfg["C"]
    TT = SEQ * NSEQ
    nc = bass.Bass("TRN2", target_bir_lowering=False)
    W = {}
    W["x"] = nc.dram_tensor("x", [TT, D], F32, kind="ExternalInput").ap()
    for name, shp in WEIGHT_SHAPES.items():
        n0 = cfg["nl"].get(name, 0)
        if n0 == 0:
            continue
        W[name] = nc.dram_tensor(name, [n0] + list(shp), F32, kind="ExternalInput").ap()
    cst = host_constants(SEQ)
    for name, arr in cst.items():
        W[name] = nc.dram_tensor(name, list(arr.shape), F32, kind="ExternalInput").ap()
    out = nc.dram_tensor("out", [TT, D], F32, kind="ExternalOutput").ap()
    c = Ctx(nc)
    g = G()
    setup_globals(c, g, W)
    NT = TT // 128
    g.gates_all = c.sb("gates_all", [128, NT, 4], F32)
    g.drow_all = c.sb("drow_all", [128, NT, 4], U32)
    g.ln_stats = c.sb("ln_stats", [128, 2, 6], F32)
    g.ln_mv = c.sb("ln_mv", [128, 8], F32)
    g.eps_t = c.sb("eps_t", [128, 1], F32)
    c.op("dve", lambda e: e.memset(g.eps_t[:], LN_EPS), writes=["eps"])
    g.rho = c.sb("ssm_rho", [128, 16], F32)
    g.ctab = c.sb("ssm_ctab", [128, 16, TB + 1], F32)
    g.stab = c.sb("ssm_stab", [128, 16, TB + 1], F32)
    g.Pr = c.sb("ssm_Pr", [128, 16, NRND], F32)
    g.Pi = c.sb("ssm_Pi", [128, 16, NRND], F32)
    g.nPi = c.sb("ssm_nPi", [128, 16, NRND], F32)
    g.lBre = c.sb("ssm_lBre", [128, 16, 128], BF16)
    g.lBim = c.sb("ssm_lBim", [128, 16, 128], BF16)
    g.lCre = c.sb("ssm_lCre", [128, 16, 128], BF16)
    g.lCim = c.sb("ssm_lCim", [128, 16, 128], BF16)
    g.Xd = nc.dram_tensor("Xd", [NE * C, D], BF16).ap()
    g.Yb = nc.dram_tensor("Yb", [NE * C, D], F32).ap()
    hb = [nc.dram_tensor("hbuf%d" % i, [TT, D], F32).ap() for i in range(2)]
    g.qkv_d = nc.dram_tensor("qkv_d", [TT, RW], BF16).ap()
    g.O_d = nc.dram_tensor("O_d", [3, TT, VW], F32).ap()
    g.cfg = cfg
    cur = W["x"]
    plan = cfg["plan"]
    for pi, (kind, layer, mi) in enumerate(plan):
        last = pi == len(plan) - 1
        dst = out if last else hb[pi % 2]
        if kind == "moe":
            moe_phase(c, g, W, layer, TT, cur, dst, C)
        elif kind == "even":
            even_phase(c, g, W, layer, mi, SEQ, NSEQ, cur, dst)
        elif kind == "odd":
            odd_phase(c, g, W, layer, mi, SEQ, NSEQ, cur, dst)
        cur = dst
    c.emit()
    return nc, cst, c


TB = 256
NRND = 1
TWO_PI = 6.283185307179586


def sin_reduced(c, out, src, shift, tmpf, tmpi, S):
    c.op("dve", lambda e: e.tensor_scalar(tmpf[:, 0, :], src, float(shift), 1.0 / TWO_PI, ALU.add, ALU.mult), reads=[S], writes=[S])
    c.op("dve", lambda e: e.tensor_copy(tmpi[:], tmpf[:, 0, :]), reads=[S], writes=[S])
    c.op("dve", lambda e: e.tensor_copy(tmpf[:, 1, :], tmpi[:]), reads=[S], writes=[S])
    c.op("dve", lambda e: e.tensor_scalar(tmpf[:, 2, :], src, float(shift), None, ALU.add), reads=[S], writes=[S])
    c.op("dve", lambda e: e.scalar_tensor_tensor(tmpf[:, 2, :], tmpf[:, 1, :], -TWO_PI, tmpf[:, 2, :], ALU.mult, ALU.add), reads=[S], writes=[S])
    c.op("dve", lambda e: e.tensor_scalar(tmpf[:, 3, :], tmpf[:, 2, :], float(np.pi), -TWO_PI, ALU.is_gt, ALU.mult), reads=[S], writes=[S])
    c.op("dve", lambda e: e.tensor_tensor(tmpf[:, 2, :], tmpf[:, 2, :], tmpf[:, 3, :], ALU.add), reads=[S], writes=[S])
    c.op("dve", lambda e: e.tensor_scalar(tmpf[:, 3, :], tmpf[:, 2, :], float(-np.pi), TWO_PI, ALU.is_lt, ALU.mult), reads=[S], writes=[S])
    c.op("dve", lambda e: e.tensor_tensor(tmpf[:, 2, :], tmpf[:, 2, :], tmpf[:, 3, :], ALU.add), reads=[S], writes=[S])
    c.op("act", lambda e: e.activation(out=out, in_=tmpf[:, 2, :], func=AF.Sin), reads=[S], writes=[S])


def even_setup(c, g, W, mi):
    bank = g.bank
    S = "es"
    if g.cfg.get("dbg") == "none":
        return
    with c.phase():
        nat = c.sb("es_nat", [32, 3, 64], F32)
        natm = c.sb("es_natm", [32, 3, 128], F32)
        par = c.sb("es_par", [32, 2], F32)
        ldt = c.sb("es_ldt", [32, 1], F32)
        q3 = c.sb("es_q3", [128, 3, 16], F32)
        tf = c.sb("es_tf", [128, 4, 16], F32)
        ti = c.sb("es_ti", [128, 16], I32)
        pt = c.sb("es_pt", [128, 32], F32)
        w = c.sb("es_w", [128, 12, 16], F32)
        Bn = c.sb("es_Bn", [128, 2, 16, 16], F32)
        Bf = c.sb("es_Bf", [128, 2, 16, 128], F32)
        Cf = c.sb("es_Cf", [128, 2, 16, 128], F32)
        c.dma("sp", lambda e: e.dma_start(out=nat[:, 0, :], in_=W["ssm_a_re"][mi]), cowrites=["es_nat"])
        c.dma("sp", lambda e: e.dma_start(out=nat[:, 1, :], in_=W["ssm_a_im"][mi]), cowrites=["es_nat"])
        c.dma("sp", lambda e: e.dma_start(out=ldt[:], in_=W["ssm_log_dt"][mi].rearrange("(g o) -> g o", o=1)), cowrites=["es_nat"])
        c.dma("sp", lambda e: e.dma_start(out=par[:], in_=W["c_par"][:, :]), cowrites=["es_nat"])
        for ri, nm in enumerate(("ssm_b_re", "ssm_b_im")):
            for gg in range(2):
                c.dma("sp", (lambda ri, nm, gg: lambda e: e.dma_start(out=Bn[gg * 64:(gg + 1) * 64, ri, :, :], in_=W[nm][mi].rearrange("(j gg) p h -> gg p j h", gg=2)[gg]))(ri, nm, gg), cowrites=["es_Bn"])
        c.op("dve", lambda e: e.memset(Bf[:], 0.0), writes=["es_Bf"])
        c.op("pool", lambda e: e.memset(Cf[:], 0.0), writes=["es_Cf"])
        for ri, nm in enumerate(("ssm_c_re", "ssm_c_im")):
            for j in range(16):
                for gg in range(2):
                    q = j % 4
                    p0 = 32 * q + 16 * gg
                    c.dma("sp", (lambda ri, nm, j, gg, p0: lambda e: e.dma_start(out=Cf[p0:p0 + 16, ri, j, gg * 64:(gg + 1) * 64], in_=W[nm][mi, 2 * j + gg]))(ri, nm, j, gg, p0),
                          reads=["es_Cf"], cowrites=["es_Cf2"])
        c.op("act", lambda e: e.activation(out=ldt[:], in_=ldt[:], func=AF.Exp), reads=["es_nat"], writes=[S])
        c.op("dve", lambda e: e.tensor_scalar_min(nat[:, 0, :], nat[:, 0, :], -1e-4), reads=["es_nat", S], writes=[S])
        c.op("dve", lambda e: e.memset(nat[:, 2, :], 1.0), reads=[S], writes=[S])
        c.op("dve", lambda e: e.tensor_scalar_mul(nat[:, 2, :], nat[:, 2, :], ldt[:, 0:1]), reads=[S], writes=[S])
        for m in range(3):
            c.op("dve", (lambda m: lambda e: e.tensor_scalar_mul(natm[:, m, 0:64], nat[:, m, :], par[:, 0:1]))(m), reads=[S], writes=[S])
            c.op("dve", (lambda m: lambda e: e.tensor_scalar_mul(natm[:, m, 64:128], nat[:, m, :], par[:, 1:2]))(m), reads=[S], writes=[S])
            c.op("pe", (lambda m: lambda e: e.transpose(bank[0][:, 0:32], natm[:, m, :], g.ident[0:32, 0:32]))(m), reads=[S, "ident"], writes=["bank0"])
            c.op("dve", lambda e: e.tensor_copy(pt[:], bank[0][:, 0:32]), reads=["bank0", S], writes=[S])
            c.op("dve", (lambda m: lambda e: e.tensor_tensor(q3[:, m, :], pt[:].rearrange("p (j gg) -> p j gg", gg=2)[:, :, 0], pt[:].rearrange("p (j gg) -> p j gg", gg=2)[:, :, 1], ALU.add))(m), reads=[S], writes=[S])
        LR, LI, DT = q3[:, 0, :], q3[:, 1, :], q3[:, 2, :]
        c.op("dve", lambda e: e.tensor_tensor(w[:, 0, :], LR, DT, ALU.mult), reads=[S], writes=[S])
        c.op("act", lambda e: e.activation(out=w[:, 0, :], in_=w[:, 0, :], func=AF.Exp), reads=[S], writes=[S])
        c.op("dve", lambda e: e.tensor_tensor(w[:, 1, :], LI, DT, ALU.mult), reads=[S], writes=[S])
        sin_reduced(c, w[:, 3, :], w[:, 1, :], 0.0, tf, ti, S)
        sin_reduced(c, w[:, 2, :], w[:, 1, :], np.pi / 2, tf, ti, S)
        c.op("dve", lambda e: e.tensor_tensor(w[:, 4, :], w[:, 0, :], w[:, 2, :], ALU.mult), reads=[S], writes=[S])
        c.op("dve", lambda e: e.tensor_tensor(w[:, 5, :], w[:, 0, :], w[:, 3, :], ALU.mult), reads=[S], writes=[S])
        c.op("dve", lambda e: e.tensor_scalar_add(w[:, 6, :], w[:, 4, :], -1.0), reads=[S], writes=[S])
        c.op("dve", lambda e: e.tensor_tensor(w[:, 7, :], LR, LR, ALU.mult), reads=[S], writes=[S])
        c.op("dve", lambda e: e.tensor_tensor(w[:, 10, :], LI, LI, ALU.mult), reads=[S], writes=[S])
        c.op("dve", lambda e: e.tensor_tensor(w[:, 7, :], w[:, 7, :], w[:, 10, :], ALU.add), reads=[S], writes=[S])
        c.op("dve", lambda e: e.reciprocal(w[:, 7, :], w[:, 7, :]), reads=[S], writes=[S])
        c.op("dve", lambda e: e.tensor_tensor(w[:, 8, :], w[:, 6, :], LR, ALU.mult), reads=[S], writes=[S])
        c.op("dve", lambda e: e.tensor_tensor(w[:, 10, :], w[:, 5, :], LI, ALU.mult), reads=[S], writes=[S])
        c.op("dve", lambda e: e.tensor_tensor(w[:, 8, :], w[:, 8, :], w[:, 10, :], ALU.add), reads=[S], writes=[S])
        c.op("dve", lambda e: e.tensor_tensor(w[:, 8, :], w[:, 8, :], w[:, 7, :], ALU.mult), reads=[S], writes=[S])
        c.op("dve", lambda e: e.tensor_tensor(w[:, 9, :], w[:, 5, :], LR, ALU.mult), reads=[S], writes=[S])
        c.op("dve", lambda e: e.tensor_tensor(w[:, 10, :], w[:, 6, :], LI, ALU.mult), reads=[S], writes=[S])
        c.op("dve", lambda e: e.tensor_tensor(w[:, 9, :], w[:, 9, :], w[:, 10, :], ALU.subtract), reads=[S], writes=[S])
        c.op("dve", lambda e: e.tensor_tensor(w[:, 9, :], w[:, 9, :], w[:, 7, :], ALU.mult), reads=[S], writes=[S])
        c.op("dve", lambda e: e.tensor_scalar_mul(w[:, 11, :], w[:, 9, :], -1.0), reads=[S], writes=[S])
        c.op("dve", lambda e: e.tensor_copy(g.rho[:], w[:, 0, :]), reads=[S], writes=["P"])
        io = c.sb("es_io", [128, TB + 1], F32)
        ang = c.sb("es_ang", [128, TB + 1], F32)
        tf2 = c.sb("es_tf2", [128, 4, TB + 1], F32)
        ti2 = c.sb("es_ti2", [128, TB + 1], I32)
        c.dma("sp", lambda e: e.dma_start(out=io[:], in_=W["c_iotaT"][:, :]), writes=["es_io"])
        for j in range(16):
            c.op("dve", (lambda j: lambda e: e.tensor_scalar_mul(ang[:], io[:], w[:, 1, j:j + 1]))(j), reads=[S, "es_io"], writes=[S])
            sin_reduced(c, g.stab[:, j, :], ang[:], 0.0, tf2, ti2, S)
            sin_reduced(c, g.ctab[:, j, :], ang[:], np.pi / 2, tf2, ti2, S)
        for j in range(16):
            q = j % 4
            for gg in range(2):
                ps_ = slice(gg * 64, (gg + 1) * 64)
                cs = slice(32 * q + 16 * gg, 32 * q + 16 * gg + 16)
                cr, ci, nci = w[ps_, 8, j:j + 1], w[ps_, 9, j:j + 1], w[ps_, 11, j:j + 1]
                c.op("dve", (lambda j, ps_, cs, cr: lambda e: e.tensor_scalar_mul(Bf[ps_, 0, j, cs], Bn[ps_, 0, j, :], cr))(j, ps_, cs, cr), reads=[S, "es_Bn", "es_Bf"], writes=["es_Bf"])
                c.op("dve", (lambda j, ps_, cs, nci: lambda e: e.scalar_tensor_tensor(Bf[ps_, 0, j, cs], Bn[ps_, 1, j, :], nci, Bf[ps_, 0, j, cs], ALU.mult, ALU.add))(j, ps_, cs, nci), reads=[S, "es_Bn", "es_Bf"], writes=["es_Bf"])
                c.op("dve", (lambda j, ps_, cs, cr: lambda e: e.tensor_scalar_mul(Bf[ps_, 1, j, cs], Bn[ps_, 1, j, :], cr))(j, ps_, cs, cr), reads=[S, "es_Bn", "es_Bf"], writes=["es_Bf"])
                c.op("dve", (lambda j, ps_, cs, ci: lambda e: e.scalar_tensor_tensor(Bf[ps_, 1, j, cs], Bn[ps_, 0, j, :], ci, Bf[ps_, 1, j, cs], ALU.mult, ALU.add))(j, ps_, cs, ci), reads=[S, "es_Bn", "es_Bf"], writes=["es_Bf"])
        for j in range(16):
            for ri, (src, srck, dst, scale) in enumerate(((Bf, "es_Bf", g.lBre, 1.0), (Bf, "es_Bf", g.lBim, 1.0), (Cf, "es_Cf2", g.lCre, 1.0), (Cf, "es_Cf2", g.lCim, -1.0))):
                r = ri % 2
                bk = bank[1 + (ri % 2)]
                bkk = "bank%d" % (1 + (ri % 2))
                c.op("pe", (lambda src, r, j, bk: lambda e: e.transpose(bk[:, 0:128], src[:, r, j, :], g.ident[:]))(src, r, j, bk), reads=[srck, "es_Cf", "ident"], writes=[bkk])
                c.op("act", (lambda dst, j, bk, scale: lambda e: e.activation(out=dst[:, j, :], in_=bk[:, 0:128], func=AF.Copy, scale=scale))(dst, j, bk, scale), reads=[bkk], writes=["lBC"])


def even_phase(c, g, W, layer, mi, SEQ, NSEQ, hsrc, hdst):
    bank = g.bank
    dbg = g.cfg.get("dbg")
    if dbg != "nosetup":
        even_setup(c, g, W, mi)
    if dbg in ("setup", "none"):
        c.dma("sp", lambda e: e.dma_start(out=hdst[:, :], in_=hsrc[:, :]), writes=["hdst"])
        return
    NB = SEQ // TB
    with c.phase():
        win = c.sb("e_win", [128, 8, 2048], BF16)
        wout = c.sb("e_wout", [128, 8, 1024], BF16)
        wglu = c.sb("e_wglu", [128, 4, 512], BF16)
        bglu = c.sb("e_bglu", [128, 4], F32)
        dsk = c.sb("e_dsk", [128, 4], F32)
        cw = c.sb("e_cw", [128, 4, 3], F32)
        gam = c.sb("e_gam", [128, D], F32)
        bet = c.sb("e_bet", [128, D], F32)
        hrow = [c.sb("e_hrow%d" % i, [128, D], F32) for i in range(2)]
        hT = c.sb("e_hT", [128, 8, TB], BF16)
        gb = c.sb("e_gb", [128, 4, TB], F32)
        vb = c.sb("e_vb", [128, 4, TB + 2], F32)
        gct = c.sb("e_gct", [128, TB], F32)
        uT = c.sb("e_uT", [128, 4, TB], BF16)
        u32 = c.sb("e_u32", [128, 4, TB], F32)
        sbr = [c.sb("e_sbr%d" % b, [128, TB], F32) for b in range(2)]
        sbi = [c.sb("e_sbi%d" % b, [128, TB], F32) for b in range(2)]
        st1 = [c.sb("e_st1%d" % b, [128, TB], F32) for b in range(2)]
        st2 = [c.sb("e_st2%d" % b, [128, TB], F32) for b in range(2)]
        st3 = [c.sb("e_st3%d" % b, [128, TB], F32) for b in range(2)]
        st4 = [c.sb("e_st4%d" % b, [128, TB], F32) for b in range(2)]
        sp3 = [c.sb("e_sp3%d" % b, [128, TB], F32) for b in range(2)]
        sp4 = [c.sb("e_sp4%d" % b, [128, TB], F32) for b in range(2)]
        svr = [c.sb("e_svr%d" % b, [128, TB], F32) for b in range(2)]
        svi = [c.sb("e_svi%d" % b, [128, TB], F32) for b in range(2)]
        xbf = [[c.sb("e_xbf%d%d" % (a, b), [128, TB], BF16) for b in range(2)] for a in range(2)]
        Xst = c.sb("e_Xst", [128, 16, 2], F32)
        Xtmp = c.sb("e_Xtmp", [128, 16, 2], F32)
        ycT = c.sb("e_ycT", [128, 8, TB], BF16)
        yt = c.sb("e_yt", [128, TB], F32)
        tt_ = c.sb("e_tt", [128, TB], F32)
        z32 = c.sb("e_z32", [128, 4, TB], F32)
        zT = c.sb("e_zT", [128, 4, TB], BF16)
        outt = [c.sb("e_out%d" % i, [128, D], F32) for i in range(2)]
        for kc in range(8):
            c.dma("pool", (lambda kc: lambda e: e.dma_start(out=win[:, kc, :], in_=W["hy_w_in"][mi, kc * 128:(kc + 1) * 128, :], max_dma_last_dim=8192))(kc), cowrites=["win"])
            c.dma("pool", (lambda kc: lambda e: e.dma_start(out=wout[:, kc, :], in_=W["hy_w_out"][mi, kc * 128:(kc + 1) * 128, :], max_dma_last_dim=4096))(kc), cowrites=["wout"])
        for kc in range(4):
            c.dma("pool", (lambda kc: lambda e: e.dma_start(out=wglu[:, kc, :], in_=W["ssm_w_glu"][mi, kc * 128:(kc + 1) * 128, :], max_dma_last_dim=2048))(kc), cowrites=["wglu"])
            c.dma("sp", (lambda kc: lambda e: e.dma_start(out=bglu[:, kc:kc + 1], in_=W["ssm_b_glu"][mi, kc * 128:(kc + 1) * 128].rearrange("(p o) -> p o", o=1)))(kc), cowrites=["small"])
            c.dma("sp", (lambda kc: lambda e: e.dma_start(out=dsk[:, kc:kc + 1], in_=W["ssm_d"][mi].rearrange("g h -> (g h)")[kc * 128:(kc + 1) * 128].rearrange("(p o) -> p o", o=1)))(kc), cowrites=["small"])
            for k in range(3):
                c.dma("sp", (lambda kc, k: lambda e: e.dma_start(out=cw[:, kc, k:k + 1], in_=W["conv_w"][mi, k, kc * 128:(kc + 1) * 128].rearrange("(p o) -> p o", o=1)))(kc, k), cowrites=["small"])
        c.dma("sp", lambda e: e.dma_start(out=gam[:], in_=W["ln_mix_g"][layer:layer + 1, :].partition_broadcast(128)), cowrites=["e_gam"])
        c.dma("sp", lambda e: e.dma_start(out=bet[:], in_=W["ln_mix_b"][layer:layer + 1, :].partition_broadcast(128)), cowrites=["e_gam"])
        for s in range(NSEQ):
            for tb in range(NB):
                t0 = s * SEQ + tb * TB
                for tt in range(TB // 128):
                    hr = hrow[tt % 2]
                    hk = "e_hrow%d" % (tt % 2)
                    c.dma("sp", (lambda tt, hr, t0: lambda e: e.dma_start(out=hr[:], in_=hsrc[t0 + tt * 128:t0 + (tt + 1) * 128, :]))(tt, hr, t0), writes=[hk])
                    for half in range(2):
                        for jj in range(4):
                            kc = half * 4 + jj
                            c.op("pe", (lambda kc, jj, hr: lambda e: e.transpose(bank[0][:, jj * 128:(jj + 1) * 128], hr[:, kc * 128:(kc + 1) * 128], g.ident[:]))(kc, jj, hr),
                                 reads=[hk, "ident"], writes=["bank0"])
                        c.op("act", (lambda half, tt: lambda e: e.activation(out=hT[:, half * 4:(half + 1) * 4, tt * 128:(tt + 1) * 128], in_=bank[0][:, :].rearrange("p (a b) -> p a b", a=4), func=AF.Copy))(half, tt),
                             reads=["bank0"], writes=["e_hT"])
                stg = g.cfg.get("stages", "abcdef")
                def proj(oc, bk, bkk):
                    for kc in range(8):
                        c.op("pe", (lambda kc: lambda e: e.matmul(bk[:, 0:TB], win[:, kc, oc * 128:(oc + 1) * 128], hT[:, kc, :], start=(kc == 0), stop=(kc == 7)))(kc),
                             reads=["win", "e_hT"], writes=[bkk])
                for cc in range(4 if "b" in stg else 0):
                    if "1" in stg or "c" in stg:
                        proj(cc, bank[1], "bank1")
                        c.op("act", (lambda cc: lambda e: e.activation(out=gb[:, cc, :], in_=bank[1][:, 0:TB], func=AF.Copy))(cc), reads=["bank1"], writes=["e_gb"])
                    if "2" in stg or "c" in stg:
                        proj(4 + cc, bank[2], "bank2")
                        c.op("act", lambda e: e.activation(out=gct[:], in_=bank[2][:, 0:TB], func=AF.Copy), reads=["bank2"], writes=["e_gct"])
                    if "3" in stg or "c" in stg:
                        proj(8 + cc, bank[1], "bank1")
                        if tb == 0:
                            c.op("dve", (lambda cc: lambda e: e.memset(vb[:, cc, 0:2], 0.0))(cc), writes=["e_vh%d" % cc], reads=["e_vb%d" % cc])
                        c.op("dve", (lambda cc: lambda e: e.tensor_tensor(vb[:, cc, 2:TB + 2], gct[:], bank[1][:, 0:TB], ALU.mult))(cc), reads=["bank1", "e_gct"], writes=["e_vb%d" % cc])
                    if "4" in stg or "d" in stg:
                        proj(12 + cc, bank[2], "bank2")
                        c.op("dve", (lambda cc: lambda e: e.tensor_copy(u32[:, cc, :], bank[2][:, 0:TB]))(cc), reads=["bank2"], writes=["e_u32"])
                        c.op("act", (lambda cc: lambda e: e.activation(out=uT[:, cc, :], in_=u32[:, cc, :], func=AF.Copy))(cc), reads=["e_u32"], writes=["e_uT"])
                    if "c" not in stg:
                        continue
                    vk = ["e_vb%d" % cc, "e_vh%d" % cc]
                    c.op("dve", (lambda cc: lambda e: e.tensor_scalar_mul(tt_[:], vb[:, cc, 0:TB], cw[:, cc, 0:1]))(cc), reads=vk + ["small"], writes=["e_tt"])
                    c.op("dve", (lambda cc: lambda e: e.scalar_tensor_tensor(tt_[:], vb[:, cc, 1:TB + 1], cw[:, cc, 1:2], tt_[:], ALU.mult, ALU.add))(cc), reads=vk + ["small", "e_tt"], writes=["e_tt"])
                    c.op("dve", (lambda cc: lambda e: e.scalar_tensor_tensor(tt_[:], vb[:, cc, 2:TB + 2], cw[:, cc, 2:3], tt_[:], ALU.mult, ALU.add))(cc), reads=vk + ["small", "e_tt"], writes=["e_tt"])
                    c.op("dve", (lambda cc: lambda e: e.tensor_tensor(ycT[:, cc, :], tt_[:], gb[:, cc, :], ALU.mult))(cc), reads=["e_tt", "e_gb"], writes=["e_ycT"])
                    c.op("dve", (lambda cc: lambda e: e.tensor_copy(vb[:, cc, 0:2], vb[:, cc, TB:TB + 2]))(cc), reads=vk, writes=["e_vh%d" % cc])
                for j in range(16 if "d" in stg else 0):
                    cc = j // 4
                    jb = j % 2
                    cj, sj = g.ctab[:, j, 0:TB], g.stab[:, j, 0:TB]
                    Er, Ei = g.ctab[:, j, TB:TB + 1], g.stab[:, j, TB:TB + 1]
                    br, bi_ = sbr[jb], sbi[jb]
                    K = "e_s%d_" % jb
                    c.op("pe", (lambda j, cc: lambda e: e.matmul(bank[3][:, 0:TB], g.lBre[:, j, :], uT[:, cc, :], start=True, stop=True))(j, cc), reads=["lBC", "e_uT"], writes=["bank3"])
                    c.op("pe", (lambda j, cc: lambda e: e.matmul(bank[4][:, 0:TB], g.lBim[:, j, :], uT[:, cc, :], start=True, stop=True))(j, cc), reads=["lBC", "e_uT"], writes=["bank4"])
                    c.op("act", (lambda br: lambda e: e.activation(out=br[:], in_=bank[3][:, 0:TB], func=AF.Copy))(br), reads=["bank3"], writes=[K + "br"])
                    c.op("act", (lambda bi_: lambda e: e.activation(out=bi_[:], in_=bank[4][:, 0:TB], func=AF.Copy))(bi_), reads=["bank4"], writes=[K + "bi"])
                    t1, t2, t3, t4 = st1[jb], st2[jb], st3[jb], st4[jb]
                    c.op("dve", (lambda t1, br, cj: lambda e: e.tensor_tensor(t1[:], br[:], cj, ALU.mult))(t1, br, cj), reads=[K + "br", "P"], writes=[K + "t1"])
                    c.op("pool", (lambda t2, bi_, sj: lambda e: e.tensor_tensor(t2[:], bi_[:], sj, ALU.mult))(t2, bi_, sj), reads=[K + "bi", "P"], writes=[K + "t2"])
                    c.op("dve", (lambda t3, bi_, cj: lambda e: e.tensor_tensor(t3[:], bi_[:], cj, ALU.mult))(t3, bi_, cj), reads=[K + "bi", "P"], writes=[K + "t3"])
                    c.op("pool", (lambda t4, br, sj: lambda e: e.tensor_tensor(t4[:], br[:], sj, ALU.mult))(t4, br, sj), reads=[K + "br", "P"], writes=[K + "t4"])
                    c.op("dve", (lambda t1, t2: lambda e: e.tensor_tensor(t1[:], t1[:], t2[:], ALU.add))(t1, t2), reads=[K + "t1", K + "t2"], writes=[K + "t1"])
                    c.op("dve", (lambda t3, t4: lambda e: e.tensor_tensor(t3[:], t3[:], t4[:], ALU.subtract))(t3, t4), reads=[K + "t3", K + "t4"], writes=[K + "t3"])
                    vr, vi = svr[jb], svi[jb]
                    rb = g.rho[:, j:j + 1].to_broadcast([128, TB])
                    ir = 0.0 if tb == 0 else Xst[:, j, 0:1]
                    ii = 0.0 if tb == 0 else Xst[:, j, 1:2]
                    c.op("dve", (lambda vr, rb, t1, ir: lambda e: e.tensor_tensor_scan(vr[:], rb, t1[:], ir, ALU.mult, ALU.add))(vr, rb, t1, ir), reads=[K + "t1", "P", "e_Xst"], writes=[K + "vr"])
                    c.op("dve", (lambda vi, rb, t3, ii: lambda e: e.tensor_tensor_scan(vi[:], rb, t3[:], ii, ALU.mult, ALU.add))(vi, rb, t3, ii), reads=[K + "t3", "P", "e_Xst"], writes=[K + "vi"])
                    c.op("dve", (lambda vi, Ei, j: lambda e: e.tensor_scalar_mul(Xtmp[:, j, 0:1], vi[:, TB - 1:TB], Ei))(vi, Ei, j), reads=[K + "vi", "P"], writes=["e_Xtmp"])
                    c.op("dve", (lambda vr, Ei, j: lambda e: e.tensor_scalar_mul(Xtmp[:, j, 1:2], vr[:, TB - 1:TB], Ei))(vr, Ei, j), reads=[K + "vr", "P", "e_Xtmp"], writes=["e_Xtmp"])
                    c.op("dve", (lambda vr, Er, j: lambda e: e.scalar_tensor_tensor(Xst[:, j, 0:1], vr[:, TB - 1:TB], Er, Xtmp[:, j, 0:1], ALU.mult, ALU.subtract))(vr, Er, j), reads=[K + "vr", "P", "e_Xtmp"], writes=["e_Xst"])
                    c.op("dve", (lambda vi, Er, j: lambda e: e.scalar_tensor_tensor(Xst[:, j, 1:2], vi[:, TB - 1:TB], Er, Xtmp[:, j, 1:2], ALU.mult, ALU.add))(vi, Er, j), reads=[K + "vi", "P", "e_Xtmp", "e_Xst"], writes=["e_Xst"])
                    p1, p2, p3, p4 = t2, t4, sp3[jb], sp4[jb]
                    c.op("pool", (lambda p1, vr, cj: lambda e: e.tensor_tensor(p1[:], vr[:], cj, ALU.mult))(p1, vr, cj), reads=[K + "vr", "P", K + "t2", K + "t1"], writes=[K + "t2"])
                    c.op("pool", (lambda p2, vi, sj: lambda e: e.tensor_tensor(p2[:], vi[:], sj, ALU.mult))(p2, vi, sj), reads=[K + "vi", "P", K + "t4", K + "t3"], writes=[K + "t4"])
                    c.op("pool", (lambda p3, vi, cj: lambda e: e.tensor_tensor(p3[:], vi[:], cj, ALU.mult))(p3, vi, cj), reads=[K + "vi", "P"], writes=[K + "p3"])
                    c.op("pool", (lambda p4, vr, sj: lambda e: e.tensor_tensor(p4[:], vr[:], sj, ALU.mult))(p4, vr, sj), reads=[K + "vr", "P"], writes=[K + "p4"])
                    xr_b, xi_b = xbf[jb][0], xbf[jb][1]
                    c.op("dve", (lambda xr_b, p1, p2: lambda e: e.tensor_tensor(xr_b[:], p1[:], p2[:], ALU.subtract))(xr_b, p1, p2), reads=[K + "t2", K + "t4"], writes=[K + "xr"])
                    c.op("dve", (lambda xi_b, p3, p4: lambda e: e.tensor_tensor(xi_b[:], p3[:], p4[:], ALU.add))(xi_b, p3, p4), reads=[K + "p3", K + "p4"], writes=[K + "xi"])
                    c.op("pe", (lambda j, xr_b: lambda e: e.matmul(bank[5][:, 0:TB], g.lCre[:, j, :], xr_b[:], start=(j % 4 == 0), stop=False))(j, xr_b), reads=["lBC", K + "xr"], writes=["bank5"])
                    c.op("pe", (lambda j, xi_b: lambda e: e.matmul(bank[5][:, 0:TB], g.lCim[:, j, :], xi_b[:], start=False, stop=(j % 4 == 3)))(j, xi_b), reads=["lBC", K + "xi"], writes=["bank5"])
                    if j % 4 == 3:
                        c.op("dve", (lambda cc: lambda e: e.scalar_tensor_tensor(yt[:], u32[:, cc, :], dsk[:, cc:cc + 1], bank[5][:, 0:TB], ALU.mult, ALU.add))(cc), reads=["bank5", "e_u32", "small"], writes=["e_yt"])
                        c.op("act", lambda e: e.activation(out=tt_[:], in_=yt[:], func=AF.Square), reads=["e_yt"], writes=["e_tt"])
                        c.op("dve", lambda e: e.tensor_scalar(tt_[:], tt_[:], 0.044715, 1.0, ALU.mult, ALU.add), reads=["e_tt"], writes=["e_tt"])
                        c.op("dve", lambda e: e.tensor_tensor(tt_[:], tt_[:], yt[:], ALU.mult), reads=["e_tt", "e_yt"], writes=["e_tt"])
                        c.op("act", lambda e: e.activation(out=tt_[:], in_=tt_[:], func=AF.Sigmoid, scale=1.5957691216057308), reads=["e_tt"], writes=["e_tt"])
                        c.op("dve", (lambda cc: lambda e: e.tensor_tensor(z32[:, cc, :], tt_[:], yt[:], ALU.mult))(cc), reads=["e_tt", "e_yt"], writes=["e_z32"])
                        c.op("act", (lambda cc: lambda e: e.activation(out=zT[:, cc, :], in_=z32[:, cc, :], func=AF.Copy))(cc), reads=["e_z32"], writes=["e_zT"])
                for oc in range(4 if "e" in stg else 0):
                    for kc in range(4):
                        c.op("pe", (lambda oc, kc: lambda e: e.matmul(bank[6][:, 0:TB], wglu[:, kc, oc * 128:(oc + 1) * 128], zT[:, kc, :], start=(kc == 0), stop=(kc == 3)))(oc, kc), reads=["wglu", "e_zT"], writes=["bank6"])
                    c.op("act", (lambda oc: lambda e: e.activation(out=tt_[:], in_=bank[6][:, 0:TB], func=AF.Sigmoid, bias=bglu[:, oc:oc + 1], scale=1.0))(oc), reads=["bank6", "small"], writes=["e_tt"])
                    c.op("dve", (lambda oc: lambda e: e.tensor_tensor(ycT[:, 4 + oc, :], tt_[:], z32[:, oc, :], ALU.mult))(oc), reads=["e_tt", "e_z32"], writes=["e_ycT"])
                for tt in range(TB // 128 if "f" in stg else 0):
                    hr = hrow[tt % 2]
                    hk = "e_hrow%d" % (tt % 2)
                    ob = outt[tt % 2]
                    ok = "e_out%d" % (tt % 2)
                    c.dma("sp", (lambda tt, hr, t0: lambda e: e.dma_start(out=hr[:], in_=hsrc[t0 + tt * 128:t0 + (tt + 1) * 128, :]))(tt, hr, t0), writes=[hk])
                    for n in range(2):
                        for kc in range(8):
                            c.op("pe", (lambda tt, n, kc: lambda e: e.matmul(bank[6 + n][:, :], ycT[:, kc, tt * 128:(tt + 1) * 128], wout[:, kc, n * 512:(n + 1) * 512], start=(kc == 0), stop=(kc == 7)))(tt, n, kc),
                                 reads=["e_ycT", "wout"], writes=["bank%d" % (6 + n)])
                        c.op("dve", (lambda n, hr: lambda e: e.scalar_tensor_tensor(hr[:, n * 512:(n + 1) * 512], hr[:, n * 512:(n + 1) * 512], float(DN_ALPHA), bank[6 + n][:, :], ALU.mult, ALU.add))(n, hr),
                             reads=["bank%d" % (6 + n), hk], writes=[hk])
                    layer_norm_tile(c, g, hr, hk, gam, bet, "e_gam", ob, ok, "e")
                    c.dma("sp", (lambda tt, ob, t0: lambda e: e.dma_start(out=hdst[t0 + tt * 128:t0 + (tt + 1) * 128, :], in_=ob[:]))(tt, ob, t0), reads=[ok], cowrites=["hdst"])
        if "f" not in g.cfg.get("stages", "abcdef"):
            c.dma("sp", lambda e: e.dma_start(out=hdst[:, :], in_=hsrc[:, :]), writes=["hdst"])


VW = NH * 65
RW = 2 * D + VW


def odd_phase(c, g, W, layer, mi, SEQ, NSEQ, hsrc, hdst):
    bank = g.bank
    TT = SEQ * NSEQ
    NT = TT // 128
    qkv_d = g.qkv_d
    O_d = g.O_d
    stg = g.cfg.get("stages", "ABC")
    with c.phase():
        wqkv = c.sb("o_wqkv", [128, 8, 3 * D], BF16)
        hrow = [c.sb("o_hrow%d" % i, [128, D], F32) for i in range(2)]
        hT = c.sb("o_hT", [128, 8, 128], BF16)
        cs = [c.sb("o_cs%d" % i, [128, 2, 512], F32) for i in range(2)]
        qk32 = c.sb("o_qk32", [128, 2, D], F32)
        tq = [c.sb("o_tq%d" % i, [128, 512], F32) for i in range(2)]
        tk = [c.sb("o_tk%d" % i, [128, 512], F32) for i in range(2)]
        rows = [c.sb("o_rows%d" % i, [128, RW], BF16) for i in range(2)]
        for kc in range(8):
            for part in range(3):
                c.dma("pool", (lambda kc, part: lambda e: e.dma_start(out=wqkv[:, kc, part * D:(part + 1) * D], in_=W["att_w_qkv"][mi, kc * 128:(kc + 1) * 128, part * D:(part + 1) * D], max_dma_last_dim=4096))(kc, part), cowrites=["wqkv"])
        for i in range(2):
            c.op("pool", (lambda i: lambda e: e.memset(rows[i][:, 2 * D:RW], 1.0))(i), writes=["o_rows%d" % i])
        for i in range(NT if "A" in stg else 0):
            b = i % 2
            hr, hk = hrow[b], "o_hrow%d" % b
            pos0 = (i * 128) % SEQ
            c.dma("sp", (lambda i, hr: lambda e: e.dma_start(out=hr[:], in_=hsrc[i * 128:(i + 1) * 128, :]))(i, hr), writes=[hk])
            c.dma("sp", (lambda b, pos0: lambda e: e.dma_start(out=cs[b][:, 0, :], in_=W["c_cos16"][pos0:pos0 + 128, :]))(b, pos0), cowrites=["o_cs%d" % b], reads=["o_csr%d" % b])
            c.dma("sp", (lambda b, pos0: lambda e: e.dma_start(out=cs[b][:, 1, :], in_=W["c_sin16"][pos0:pos0 + 128, :]))(b, pos0), cowrites=["o_cs%d" % b], reads=["o_csr%d" % b])
            transpose_rows_to_T(c, g, hr, hk, hT, "o_hT", bank[0], "bank0", fp32_in=True, evac="act")
            for n in range(6):
                for kc in range(8):
                    c.op("pe", (lambda n, kc: lambda e: e.matmul(bank[2 + n][:, :], hT[:, kc, :], wqkv[:, kc, n * 512:(n + 1) * 512], start=(kc == 0), stop=(kc == 7)))(n, kc),
                         reads=["o_hT", "wqkv"], writes=["bank%d" % (2 + n)])
            R = rows[b]
            Rk = "o_rows%d" % b
            for n in range(4):
                c.op("act", (lambda n: lambda e: e.activation(out=qk32[:, n // 2, (n % 2) * 512:(n % 2 + 1) * 512], in_=bank[2 + n][:, :], func=AF.Copy))(n),
                     reads=["bank%d" % (2 + n)], writes=["o_qk32_%d" % (n // 2)])
            for n in range(2):
                c.op("act", (lambda n, R: lambda e: e.activation(out=R[:, 2 * D:RW].rearrange("p (h e) -> p h e", e=65)[:, n * 8:(n + 1) * 8, 0:64], in_=bank[6 + n][:, :].rearrange("p (h e) -> p h e", e=64), func=AF.Copy))(n, R),
                     reads=["bank%d" % (6 + n), Rk], writes=[Rk + "v"])
            for which, eng, tmp in ((0, "dve", tq), (1, "pool", tk)):
                x3 = qk32[:, which, :].rearrange("p (h e) -> p h e", e=64)
                o3 = R[:, which * D:(which + 1) * D].rearrange("p (h e) -> p h e", e=64)
                cosv = cs[b][:, 0, :].rearrange("p (h e) -> p h e", e=32)
                sinv = cs[b][:, 1, :].rearrange("p (h e) -> p h e", e=32)
                t1 = tmp[0][:].rearrange("p (h e) -> p h e", e=32)
                t2 = tmp[1][:].rearrange("p (h e) -> p h e", e=32)
                xk = "o_qk32_%d" % which
                tk_ = "o_tmp%d" % which
                rk = Rk + ("q" if which == 0 else "k")
                csk = "o_cs%d" % b
                c.op(eng, (lambda t1, x3, cosv: lambda e: e.tensor_tensor(t1, x3[:, :, 0:32], cosv, ALU.mult))(t1, x3, cosv), reads=[xk, csk], writes=[tk_ + "a"])
                c.op(eng, (lambda t2, x3, sinv: lambda e: e.tensor_tensor(t2, x3[:, :, 32:64], sinv, ALU.mult))(t2, x3, sinv), reads=[xk, csk], writes=[tk_ + "b"])
                c.op(eng, (lambda o3, t1, t2: lambda e: e.tensor_tensor(o3[:, :, 0:32], t1, t2, ALU.subtract))(o3, t1, t2), reads=[tk_ + "a", tk_ + "b"], writes=[rk])
                c.op(eng, (lambda t1, x3, cosv: lambda e: e.tensor_tensor(t1, x3[:, :, 32:64], cosv, ALU.mult))(t1, x3, cosv), reads=[xk, csk, rk], writes=[tk_ + "a"])
                c.op(eng, (lambda t2, x3, sinv: lambda e: e.tensor_tensor(t2, x3[:, :, 0:32], sinv, ALU.mult))(t2, x3, sinv), reads=[xk, csk, rk], writes=[tk_ + "b"])
                ev = c.op(eng, (lambda o3, t1, t2: lambda e: e.tensor_tensor(o3[:, :, 32:64], t1, t2, ALU.add))(o3, t1, t2), reads=[tk_ + "a", tk_ + "b"], writes=[rk])
                c.readers.setdefault("o_csr%d" % b, []).append(ev)
            c.dma("sp", (lambda i, R: lambda e: e.dma_start(out=qkv_d[i * 128:(i + 1) * 128, :], in_=R[:, :]))(i, R),
                  reads=[Rk + "q", Rk + "k", Rk + "v", Rk], cowrites=["qkv_d"])
            for sfx in ("q", "k", "v"):
                c.readers.setdefault(Rk + sfx, []).append(c.last_w["qkv_d"][-1])

    with c.phase():
        R3 = [c.sb("o_R%d" % i, [128, RW], BF16) for i in range(3)]
        KT = [c.sb("o_KT%d" % i, [128, 8, 128], BF16) for i in range(2)]
        QTz = [[c.sb("o_QT%d_%d" % (j, i), [128, 8, 128], BF16) for i in range(2)] for j in range(2)]
        for j in range(2):
            for i in range(2):
                c.op("pool", (lambda i, j: lambda e: e.memset(QTz[j][i][:], 0.0))(i, j), writes=["o_QT%d" % j])
        PT = [c.sb("o_PT%d" % i, [128, 512], BF16) for i in range(3)]
        mcur = c.sb("o_mcur", [128, 512], BF16)
        mprev = c.sb("o_mprev", [128, 512], BF16)
        osb = [c.sb("o_osb%d" % i, [128, VW], F32) for i in range(2)]
        c.dma("pool", lambda e: e.dma_start(out=mcur[:], in_=W["c_mcur"][:, :]), writes=["o_mcur"])
        c.dma("pool", lambda e: e.dma_start(out=mprev[:], in_=W["c_mprev"][:, :]), writes=["o_mprev"])
        hb_ = [(0, 7), (7, 14), (14, 16)]
        ulist = []
        for s_ in range(NSEQ if "B" in stg else 0):
            for pi, (win_, dil) in enumerate(PATTERNS):
                nb = SEQ // (128 * dil)
                for r in range(dil):
                    for n in range(nb):
                        ulist.append((s_, pi, dil, r, n))
        b0 = bank[0][:].bitcast(BF16)
        b1 = bank[1][:].bitcast(BF16)

        def views(u):
            s_, pi, dil, r, n = ulist[u]
            qv = qkv_d[s_ * SEQ:(s_ + 1) * SEQ, :].rearrange("(m d) c -> d m c", d=dil)
            ov = O_d[pi, s_ * SEQ:(s_ + 1) * SEQ, :].rearrange("(m d) c -> d m c", d=dil)
            return qv, ov

        def prologue(u):
            s_, pi, dil, r, n = ulist[u]
            qv, ov = views(u)
            R, Rk = R3[u % 3], "o_R%d" % (u % 3)
            kt, ktk = KT[u % 2], "o_KT%d" % (u % 2)
            qt, qtk = QTz[u % 2], "o_QT%d" % (u % 2)
            c.dma("sp", lambda e: e.dma_start(out=R[:, :], in_=qv[r, n * 128:(n + 1) * 128, :]), reads=["qkv_d"], writes=[Rk])
            for pr in range(8):
                c.op("pe", (lambda pr: lambda e: e.transpose(b0[:, pr * 128:(pr + 1) * 128], R[:, D + pr * 128:D + (pr + 1) * 128], g.identb[:]))(pr), reads=[Rk, "identb"], writes=["bank0"])
            c.op("act", lambda e: e.activation(out=kt[:].rearrange("p a b -> p (a b)"), in_=b0[:, 0:1024], func=AF.Copy), reads=["bank0"], writes=[ktk])
            for pr in range(8):
                c.op("pe", (lambda pr: lambda e: e.transpose(b1[:, pr * 128:(pr + 1) * 128], R[:, pr * 128:(pr + 1) * 128], g.identb[:]))(pr), reads=[Rk, "identb"], writes=["bank1"])
            c.op("dve", lambda e: e.tensor_copy(qt[0][0:64].rearrange("p a b -> p (a b)"), b1[0:64, 0:1024]), reads=["bank1"], writes=[qtk])
            c.op("dve", lambda e: e.tensor_copy(qt[1][64:128].rearrange("p a b -> p (a b)"), b1[64:128, 0:1024]), reads=["bank1", qtk], writes=[qtk])

        gcount = [0]

        def groups_of(u):
            s_, pi, dil, r, n = ulist[u]
            R, Rk = R3[u % 3], "o_R%d" % (u % 3)
            Rp, Rpk = R3[(u - 1) % 3], "o_R%d" % ((u - 1) % 3)
            kt, ktk = KT[u % 2], "o_KT%d" % (u % 2)
            ktp, ktpk = KT[(u - 1) % 2], "o_KT%d" % ((u - 1) % 2)
            qt, qtk = QTz[u % 2], "o_QT%d" % (u % 2)
            kbs = ([(ktp, ktpk, Rp, Rpk, mprev, "o_mprev")] if n > 0 else []) + [(kt, ktk, R, Rk, mcur, "o_mcur")]
            started = [False, False, False]
            out = []
            for (kT_, kTk, Rv, Rvk, msk, mskk) in kbs:
                for grp in range(4):
                    gi = gcount[0]
                    gcount[0] += 1
                    sb_, sbk = bank[2 + gi % 2], "bank%d" % (2 + gi % 2)
                    pt, ptk = PT[gi % 3], "o_PT%d" % (gi % 3)

                    def S(sb_=sb_, sbk=sbk, pt=pt, ptk=ptk, kT_=kT_, kTk=kTk, msk=msk, mskk=mskk, grp=grp):
                        for hh in range(4):
                            h = grp * 4 + hh
                            pr, hf = h // 2, h % 2
                            c.op("pe", (lambda hh, pr, hf: lambda e: e.matmul(sb_[:, hh * 128:(hh + 1) * 128], kT_[:, pr, :], qt[hf][:, pr, :], start=True, stop=True))(hh, pr, hf),
                                 reads=[kTk, qtk], writes=[sbk])
                        c.op("act", lambda e: e.activation(out=pt[:], in_=sb_[:, :], func=AF.Exp, scale=0.125), reads=[sbk], writes=[ptk])
                        c.op("dve", lambda e: e.tensor_tensor(pt[:], pt[:], msk[:], ALU.mult), reads=[ptk, mskk], writes=[ptk])

                    sts = []
                    for hh in range(4):
                        h = grp * 4 + hh
                        bi = 0 if h < 7 else (1 if h < 14 else 2)
                        sts.append(not started[bi])
                        started[bi] = True

                    def P(pt=pt, ptk=ptk, Rv=Rv, Rvk=Rvk, grp=grp, sts=sts):
                        for hh in range(4):
                            h = grp * 4 + hh
                            bi = 0 if h < 7 else (1 if h < 14 else 2)
                            col = (h - hb_[bi][0]) * 65
                            c.op("pe", (lambda bi, col, hh, h, st: lambda e: e.matmul(bank[4 + bi][:, col:col + 65], pt[:, hh * 128:(hh + 1) * 128], Rv[:, 2 * D + h * 65:2 * D + (h + 1) * 65], start=st, stop=True, skip_group_check=True))(bi, col, hh, h, sts[hh]),
                                 reads=[ptk, Rvk], writes=["bank%d" % (4 + bi)])
                    out.append((S, P))
            return out

        def epilogue(u):
            s_, pi, dil, r, n = ulist[u]
            qv, ov = views(u)
            ob, obk = osb[u % 2], "o_osb%d" % (u % 2)
            for bi, (h0, h1) in enumerate(hb_):
                eng = "act" if bi == 1 else "dve"
                c.op(eng, (lambda bi, h0, h1, eng: lambda e: ecopy(e, eng, ob[:, h0 * 65:h1 * 65], bank[4 + bi][:, 0:(h1 - h0) * 65]))(bi, h0, h1, eng),
                     reads=["bank%d" % (4 + bi)], writes=[obk + "_%d" % bi])
            c.dma("sp", lambda e: e.dma_start(out=ov[r, n * 128:(n + 1) * 128, :], in_=ob[:, :]),
                  reads=[obk + "_0", obk + "_1", obk + "_2"], cowrites=["O_d"])

        NU = len(ulist)
        if NU:
            prologue(0)
        pendingP = None
        for u in range(NU):
            grps = groups_of(u)
            ng = len(grps)
            for gi_, (S, P) in enumerate(grps):
                S()
                if pendingP is not None:
                    pendingP[0]()
                    if pendingP[1] is not None:
                        epilogue(pendingP[1])
                pendingP = (P, u if gi_ == ng - 1 else None)
                if gi_ == ng // 2 and u + 1 < NU:
                    prologue(u + 1)
        if pendingP is not None:
            pendingP[0]()
            epilogue(pendingP[1])

    with c.phase():
        wo = c.sb("o_wo", [128, 8, D], BF16)
        gam = c.sb("o_gam", [128, D], F32)
        bet = c.sb("o_bet", [128, D], F32)
        hrow = [c.sb("o_hrow%d" % i, [128, D], F32) for i in range(2)]
        ot = [[c.sb("o_ot%d_%d" % (i, p), [128, VW], F32) for p in range(3)] for i in range(2)]
        rden = c.sb("o_rden", [128, NH], F32)
        osb2 = c.sb("o_o", [128, D], BF16)
        oT = c.sb("o_oT", [128, 8, 128], BF16)
        outt = [c.sb("o_out%d" % i, [128, D], F32) for i in range(2)]
        for kc in range(8):
            c.dma("pool", (lambda kc: lambda e: e.dma_start(out=wo[:, kc, :], in_=W["att_w_o"][mi, kc * 128:(kc + 1) * 128, :], max_dma_last_dim=4096))(kc), cowrites=["wo"])
        c.dma("sp", lambda e: e.dma_start(out=gam[:], in_=W["ln_mix_g"][layer:layer + 1, :].partition_broadcast(128)), cowrites=["o_gam"])
        c.dma("sp", lambda e: e.dma_start(out=bet[:], in_=W["ln_mix_b"][layer:layer + 1, :].partition_broadcast(128)), cowrites=["o_gam"])
        for i in range(NT if "C" in stg else 0):
            b = i % 2
            hr, hk = hrow[b], "o_hrow%d" % b
            c.dma("sp", (lambda i, hr: lambda e: e.dma_start(out=hr[:], in_=hsrc[i * 128:(i + 1) * 128, :]))(i, hr), writes=[hk])
            for p in range(3):
                c.dma("sp", (lambda i, b, p: lambda e: e.dma_start(out=ot[b][p][:, :], in_=O_d[p, i * 128:(i + 1) * 128, :]))(i, b, p), reads=["O_d"], writes=["o_ot%d_%d" % (b, p)])
            A = ot[b][0]
            Ak = "o_ot%d_0" % b
            c.op("pool", (lambda b, A: lambda e: e.tensor_tensor(A[:], A[:], ot[b][1][:], ALU.add))(b, A), reads=[Ak, "o_ot%d_1" % b], writes=[Ak])
            c.op("dve", (lambda b, A: lambda e: e.tensor_tensor(A[:], A[:], ot[b][2][:], ALU.add))(b, A), reads=[Ak, "o_ot%d_2" % b], writes=[Ak])
            A3 = A[:].rearrange("p (h e) -> p h e", e=65)
            c.op("dve", (lambda A3: lambda e: e.reciprocal(rden[:], A3[:, :, 64]))(A3), reads=[Ak], writes=["o_rden"])
            for h in range(NH):
                c.op("dve" if h % 2 == 0 else "pool", (lambda h, A3: lambda e: e.tensor_scalar(osb2[:, h * 64:(h + 1) * 64], A3[:, h, 0:64], rden[:, h:h + 1], None, ALU.mult))(h, A3),
                     reads=[Ak, "o_rden"], writes=["o_o%d" % (h % 2)])
            bb = bank[0][:].bitcast(BF16)
            for kc in range(8):
                c.op("pe", (lambda kc, bb: lambda e: e.transpose(bb[:, kc * 128:(kc + 1) * 128], osb2[:, kc * 128:(kc + 1) * 128], g.identb[:]))(kc, bb), reads=["o_o0", "o_o1", "identb"], writes=["bank0"])
            c.op("act", (lambda bb: lambda e: e.activation(out=oT[:].rearrange("p a b -> p (a b)"), in_=bb[:, 0:1024], func=AF.Copy))(bb), reads=["bank0"], writes=["o_oT"])
            for n in range(2):
                for kc in range(8):
                    c.op("pe", (lambda n, kc: lambda e: e.matmul(bank[6 + n][:, :], oT[:, kc, :], wo[:, kc, n * 512:(n + 1) * 512], start=(kc == 0), stop=(kc == 7)))(n, kc),
                         reads=["o_oT", "wo"], writes=["bank%d" % (6 + n)])
                c.op("dve", (lambda n, hr: lambda e: e.scalar_tensor_tensor(hr[:, n * 512:(n + 1) * 512], hr[:, n * 512:(n + 1) * 512], float(DN_ALPHA), bank[6 + n][:, :], ALU.mult, ALU.add))(n, hr),
                     reads=["bank%d" % (6 + n), hk], writes=[hk])
            layer_norm_tile(c, g, hr, hk, gam, bet, "o_gam", outt[b], "o_out%d" % b, "o")
            c.dma("sp", (lambda i, b: lambda e: e.dma_start(out=hdst[i * 128:(i + 1) * 128, :], in_=outt[b][:]))(i, b), reads=["o_out%d" % b], cowrites=["hdst"])
        if "C" not in stg:
            c.dma("sp", lambda e: e.dma_start(out=hdst[:, :], in_=hsrc[:, :]), writes=["hdst"])


NCORES = 4
FULL_SEQ = 8192
FULL_BATCH = 4
RENAME = {"expert_w_gu": "w_gu", "expert_b_gu": "b_gu", "expert_w_down": "w_dn", "expert_b_down": "b_dn"}
_CACHE = {}


def full_plan():
    plan = []
    for layer in range(DEPTH):
        plan.append(("even" if layer % 2 == 0 else "odd", layer, layer // 2))
        plan.append(("moe", layer, layer // 2))
    return plan


def kernel(**inputs):
    nseq = FULL_BATCH // NCORES
    tt = nseq * FULL_SEQ
    nl = {}
    wmap = {}
    for name, arr in inputs.items():
        if name == "x":
            continue
        kname = RENAME.get(name, name)
        wmap[kname] = np.ascontiguousarray(arr, dtype=np.float32)
        nl[kname] = arr.shape[0]
    cap = (tt * TOPK // NE) * 5 // 4
    cap = (cap + 127) // 128 * 128
    cfg = dict(SEQ=FULL_SEQ, NSEQ=nseq, C=cap, plan=full_plan(), nl=nl)
    key = (FULL_SEQ, nseq, cap)
    if key not in _CACHE:
        _CACHE[key] = build_program(cfg)
    nc, cst, _ = _CACHE[key]
    x = np.ascontiguousarray(inputs["x"], dtype=np.float32).reshape(NCORES, tt, D)
    in_maps = []
    for ci in range(NCORES):
        m = {"x": x[ci]}
        m.update(wmap)
        m.update(cst)
        in_maps.append(m)
    res = run_bass_kernel_spmd(nc, in_maps, core_ids=list(range(NCORES)))
    out = np.stack([res.results[ci]["out"] for ci in range(NCORES)], axis=0)
    return out.reshape(FULL_BATCH, FULL_SEQ, D).astype(np.float32)
```

```python
import contextlib
import numpy as np
import ml_dtypes
import concourse.bass as bass
import concourse.mybir as mybir
from concourse.bass_utils import run_bass_kernel_spmd

F32 = mybir.dt.float32
BF16 = mybir.dt.bfloat16
I32 = mybir.dt.int32
U32 = mybir.dt.uint32
AF = mybir.ActivationFunctionType
ALU = mybir.AluOpType
AX = mybir.AxisListType

D = 1024
NE = 32
TOPK = 4
DEPTH = 4
DN_ALPHA = (2 * DEPTH) ** 0.25
LN_EPS = 1e-5
NH = 16
HD = 64
PATTERNS = ((128, 1), (512, 4), (2048, 16))

SAME_ENG_WAIT = True
ENGS = ("pe", "dve", "act", "pool", "sp")
RING = {"sp": 12, "act": 6, "pool": 12}


class Ctx:
    def __init__(self, nc):
        self.nc = nc
        self.stack = contextlib.ExitStack()
        self.prog = {e: [] for e in ENGS}
        self.cnt = {e: 0 for e in ENGS}
        self.semobj = {}
        for e in ENGS:
            self.semobj[("e", e)] = self.stack.enter_context(nc.semaphore("s_" + e))
        for q, n in RING.items():
            for i in range(n):
                self.semobj[("r", q, i)] = self.stack.enter_context(nc.semaphore("r_%s%d" % (q, i)))
        self.dcnt = {q: 0 for q in RING}
        self.seen = {e: {} for e in ENGS}
        self.last_w = {}
        self.readers = {}
        self.n_instr = 0
        self.pstack = None

    def sb(self, name, shape, dt=F32):
        st = self.pstack if self.pstack is not None else self.stack
        self.uid = getattr(self, "uid", 0) + 1
        return st.enter_context(self.nc.sbuf_tensor("%s_u%d" % (name, self.uid), list(shape), dt))

    def reg(self, e, val):
        if not hasattr(self, "_regs"):
            self._regs = {}
        if val not in self._regs:
            self._regs[val] = e.to_reg(val)
        return self._regs[val]

    def ps(self, name, shape, dt=F32):
        return self.stack.enter_context(self.nc.psum_tensor(name, list(shape), dt))

    @contextlib.contextmanager
    def phase(self):
        self.barrier()
        self.pstack = contextlib.ExitStack()
        try:
            yield
        finally:
            self.barrier()
            self.pstack.close()
            self.pstack = None

    def cur_events(self):
        evs = []
        for e in ENGS:
            if self.cnt[e]:
                evs.append((("e", e), self.cnt[e]))
        for q, n in RING.items():
            i = self.dcnt[q]
            for slot in range(n):
                if i > slot:
                    last = ((i - 1 - slot) // n) * n + slot
                    evs.append((("r", q, slot), 16 * (last // n + 1)))
        return evs

    def barrier(self):
        evs = self.cur_events()
        for eng in ENGS:
            for k, v in evs:
                if self.seen[eng].get(k, 0) < v:
                    self.seen[eng][k] = v
                    self.prog[eng].append(("wait", k, v))
        self.last_w = {}
        self.readers = {}

    def _need(self, eng, reads, writes, cowrites=()):
        need = {}

        def add(ev):
            k, v = ev
            if need.get(k, 0) < v:
                need[k] = v
        for k in reads:
            for ev in self.last_w.get(k, ()):
                add(ev)
        for k in writes:
            for ev in self.last_w.get(k, ()):
                add(ev)
            for ev in self.readers.get(k, ()):
                add(ev)
        for k in cowrites:
            for ev in self.readers.get(k, ()):
                add(ev)
        for k, v in need.items():
            if k == ("e", eng) and (eng == "pe" or not SAME_ENG_WAIT):
                continue
            if self.seen[eng].get(k, 0) >= v:
                continue
            self.seen[eng][k] = v
            self.prog[eng].append(("wait", k, v))

    def _commit(self, ev, reads, writes, cowrites=()):
        for k in reads:
            self.readers.setdefault(k, []).append(ev)
        for k in writes:
            self.last_w[k] = [ev]
            self.readers[k] = []
        for k in cowrites:
            self.last_w.setdefault(k, []).append(ev)

    def op(self, eng, fn, reads=(), writes=()):
        self._need(eng, reads, writes)
        self.cnt[eng] += 1
        ev = (("e", eng), self.cnt[eng])
        self.prog[eng].append(("ins", fn, ("e", eng), 1))
        self._commit(ev, reads, writes)
        self.n_instr += 1
        return ev

    def dma(self, q, fn, reads=(), writes=(), cowrites=()):
        self._need(q, reads, writes, cowrites)
        i = self.dcnt[q]
        n = RING[q]
        slot = i % n
        if i >= n:
            k = ("r", q, slot)
            v = 16 * (i // n)
            if self.seen[q].get(k, 0) < v:
                self.seen[q][k] = v
                self.prog[q].append(("wait", k, v))
        self.dcnt[q] += 1
        ev = (("r", q, slot), 16 * (i // n + 1))
        self.prog[q].append(("ins", fn, ("r", q, slot), 16))
        self._commit(ev, reads, writes, cowrites)
        self.n_instr += 1
        return ev

    def emit(self):
        nc = self.nc
        self.barrier()
        with nc.Block() as block:
            def body(engname):
                def f(e):
                    for it in self.prog[engname]:
                        if it[0] == "wait":
                            e.wait_ge(self.semobj[it[1]], it[2])
                        else:
                            it[1](e).then_inc(self.semobj[it[2]], it[3])
                return f
            block.tensor(body("pe"))
            block.vector(body("dve"))
            block.scalar(body("act"))
            block.gpsimd(body("pool"))
            block.sync(body("sp"))
        self.stack.close()


class G:
    pass


def setup_globals(c, g, W):
    g.bank = [c.ps("bank%d" % i, [128, 512], F32) for i in range(8)]
    g.ident = c.sb("ident", [128, 128], F32)
    g.identb = c.sb("identb", [128, 128], BF16)
    g.onesrow = c.sb("onesrow", [1, 128], BF16)
    c.dma("sp", lambda e: e.dma_start(out=g.ident[:], in_=W["c_ident"][:, :]), writes=["ident"])
    c.dma("pool", lambda e: e.dma_start(out=g.identb[:], in_=W["c_ident"][:, :]), writes=["identb"])
    c.dma("pool", lambda e: e.dma_start(out=g.onesrow[:], in_=W["c_ones"][0:1, :]), writes=["onesrow"])


def ecopy(e, eng, out, in_):
    if eng == "act":
        return e.activation(out=out, in_=in_, func=AF.Copy)
    return e.tensor_copy(out, in_)


def load_bcast(c, tile, key, src_row_ap, q="sp"):
    c.dma(q, lambda e: e.dma_start(out=tile[:], in_=src_row_ap.partition_broadcast(128)), writes=[key])


def layer_norm_tile(c, g, acc, acck, gam, bet, gbk, out_tile, outk, tag):
    st = g.ln_stats
    mv = g.ln_mv
    sk = "ln_small"
    c.op("dve", lambda e: e.bn_stats(st[:, 0, :], acc[:, 0:512]), reads=[acck], writes=[sk])
    c.op("dve", lambda e: e.bn_stats(st[:, 1, :], acc[:, 512:1024]), reads=[acck, sk], writes=[sk])
    c.op("dve", lambda e: e.bn_aggr(mv[:, 0:2], st[:].rearrange("p a b -> p (a b)")), reads=[sk], writes=[sk])
    c.op("act", lambda e: e.activation(out=mv[:, 2:3], in_=mv[:, 1:2], func=AF.Sqrt, bias=g.eps_t[:, 0:1], scale=1.0),
         reads=[sk, "eps"], writes=[sk])
    c.op("dve", lambda e: e.reciprocal(mv[:, 3:4], mv[:, 2:3]), reads=[sk], writes=[sk])
    c.op("dve", lambda e: e.tensor_scalar(mv[:, 4:5], mv[:, 0:1], mv[:, 3:4], -1.0, ALU.mult, ALU.mult),
         reads=[sk], writes=[sk])
    c.op("act", lambda e: e.activation(out=acc[:], in_=acc[:], func=AF.Identity, bias=mv[:, 4:5], scale=mv[:, 3:4]),
         reads=[sk, acck], writes=[acck])
    c.op("dve", lambda e: e.tensor_tensor(acc[:], acc[:], gam[:], ALU.mult), reads=[acck, gbk], writes=[acck])
    c.op("dve", lambda e: e.tensor_tensor(out_tile[:], acc[:], bet[:], ALU.add), reads=[acck, gbk], writes=[outk])


def transpose_rows_to_T(c, g, rows, rowsk, xT, xTk, bank, bankk, nchunks=8, fp32_in=True, evac="dve"):
    if fp32_in:
        for half in range(nchunks // 4):
            for j in range(4):
                kc = half * 4 + j
                c.op("pe", (lambda kc, j: lambda e: e.transpose(bank[:, j * 128:(j + 1) * 128], rows[:, kc * 128:(kc + 1) * 128], g.ident[:]))(kc, j),
                     reads=[rowsk, "ident"], writes=[bankk])
            c.op(evac, (lambda half: lambda e: ecopy(e, evac, xT[:, half * 4:(half + 1) * 4, :].rearrange("p a b -> p (a b)"), bank[:, :]))(half),
                 reads=[bankk], writes=[xTk])
    else:
        bb = bank[:].bitcast(BF16)
        for kc in range(nchunks):
            c.op("pe", (lambda kc: lambda e: e.transpose(bb[:, kc * 128:(kc + 1) * 128], rows[:, kc * 128:(kc + 1) * 128], g.identb[:]))(kc),
                 reads=[rowsk, "identb"], writes=[bankk])
        c.op(evac, lambda e: ecopy(e, evac, xT[:].rearrange("p a b -> p (a b)"), bb[:, 0:nchunks * 128]),
             reads=[bankk], writes=[xTk])


def moe_phase(c, g, W, layer, TT, hsrc, hdst, C):
    NT = TT // 128
    NSL = C // 128
    bank = g.bank
    with c.phase():
        hrow = [c.sb("m_hrow%d" % i, [128, D], F32) for i in range(2)]
        hT = c.sb("m_hT", [128, 8, 128], F32)
        rw = c.sb("m_rw", [128, 8, NE], F32)
        rb = c.sb("m_rb", [128, NE], F32)
        ltri = c.sb("m_ltri", [128, 128], F32)
        ones = c.sb("m_ones", [128, 128], F32)
        iota = c.sb("m_iota", [128, NE], F32)
        base = c.sb("m_base", [128, NE], F32)
        logit = c.sb("m_logit", [128, NE], F32)
        top8 = c.sb("m_top8", [128, 8], F32)
        idx8 = c.sb("m_idx8", [128, 8], U32)
        idxf = c.sb("m_idxf", [128, 4], F32)
        sm = c.sb("m_sm", [128, 16], F32)
        oh = c.sb("m_oh", [128, 4, NE], F32)
        Mt = c.sb("m_M", [128, NE], F32)
        rank = c.sb("m_rank", [128, NE], F32)
        tmp = c.sb("m_tmp", [128, NE], F32)
        pos = c.sb("m_pos", [128, 8], F32)
        c.dma("sp", lambda e: e.dma_start(out=rw[:], in_=W["router_w"][layer].rearrange("(kc p) e -> p kc e", p=128)), writes=["rw"])
        load_bcast(c, rb, "rb", W["router_b"][layer:layer + 1, :])
        c.dma("sp", lambda e: e.dma_start(out=ltri[:], in_=W["c_ltri"][:, :]), writes=["ltri"])
        c.dma("sp", lambda e: e.dma_start(out=ones[:], in_=W["c_onesq"][:, :]), writes=["ones"])
        c.dma("sp", lambda e: e.dma_start(out=iota[:], in_=W["c_iota"][:, :]), writes=["iota"])
        c.op("dve", lambda e: e.memset(base[:], 0.0), writes=["base"])
        S = "m_small"
        for i in range(NT):
            hr = hrow[i % 2]
            hk = "m_hrow%d" % (i % 2)
            c.dma("sp", (lambda i, hr: lambda e: e.dma_start(out=hr[:], in_=hsrc[i * 128:(i + 1) * 128, :]))(i, hr), writes=[hk])
            transpose_rows_to_T(c, g, hr, hk, hT, "m_hT", bank[0], "bank0", fp32_in=True, evac="act")
            for kc in range(8):
                c.op("pe", (lambda kc: lambda e: e.matmul(bank[1][:, 0:NE], hT[:, kc, :], rw[:, kc, :], start=(kc == 0), stop=(kc == 7)))(kc),
                     reads=["m_hT", "rw"], writes=["bank1"])
            c.op("dve", lambda e: e.tensor_tensor(logit[:], bank[1][:, 0:NE], rb[:], ALU.add), reads=["bank1", "rb"], writes=[S])
            c.op("dve", lambda e: e.max(top8[:], logit[:]), reads=[S], writes=[S])
            c.op("dve", lambda e: e.max_index(idx8[:], top8[:], logit[:]), reads=[S], writes=[S])
            c.op("dve", lambda e: e.tensor_copy(idxf[:], idx8[:, 0:4]), reads=[S], writes=[S])
            c.op("dve", lambda e: e.tensor_scalar_mul(sm[:, 0:1], top8[:, 0:1], -1.0), reads=[S], writes=[S])
            c.op("act", lambda e: e.activation(out=sm[:, 4:8], in_=top8[:, 0:4], func=AF.Exp, bias=sm[:, 0:1], scale=1.0, accum_out=sm[:, 1:2]),
                 reads=[S], writes=[S])
            c.op("dve", lambda e: e.reciprocal(sm[:, 2:3], sm[:, 1:2]), reads=[S], writes=[S])
            c.op("dve", (lambda i: lambda e: e.tensor_scalar_mul(g.gates_all[:, i, :], sm[:, 4:8], sm[:, 2:3]))(i), reads=[S], writes=["gates_all"])
            for k in range(4):
                c.op("dve", (lambda k: lambda e: e.tensor_scalar(oh[:, k, :], iota[:], idxf[:, k:k + 1], None, ALU.is_equal))(k),
                     reads=[S, "iota"], writes=[S])
            c.op("dve", lambda e: e.tensor_tensor(Mt[:], oh[:, 0, :], oh[:, 1, :], ALU.add), reads=[S], writes=["m_M"])
            c.op("dve", lambda e: e.tensor_tensor(Mt[:], Mt[:], oh[:, 2, :], ALU.add), reads=[S, "m_M"], writes=["m_M"])
            c.op("dve", lambda e: e.tensor_tensor(Mt[:], Mt[:], oh[:, 3, :], ALU.add), reads=[S, "m_M"], writes=["m_M"])
            c.op("pe", lambda e: e.matmul(bank[2][:, 0:NE], ltri[:], Mt[:], start=True, stop=True), reads=["m_M", "ltri"], writes=["bank2"])
            c.op("pe", lambda e: e.matmul(bank[3][:, 0:NE], ones[:], Mt[:], start=True, stop=True), reads=["m_M", "ones"], writes=["bank3"])
            c.op("dve", lambda e: e.tensor_tensor(rank[:], bank[2][:, 0:NE], base[:], ALU.add), reads=["bank2", "base"], writes=[S])
            c.op("dve", lambda e: e.tensor_tensor(base[:], bank[3][:, 0:NE], base[:], ALU.add), reads=["bank3", S], writes=["base"])
            for k in range(4):
                c.op("dve", (lambda k: lambda e: e.scalar_tensor_tensor(tmp[:], oh[:, k, :], 1.0, rank[:], ALU.mult, ALU.mult, accum_out=pos[:, k:k + 1]))(k),
                     reads=[S], writes=[S])
            c.op("dve", lambda e: e.tensor_scalar(pos[:, 4:8], pos[:, 0:4], float(C), 1.0e6, ALU.is_ge, ALU.mult), reads=[S], writes=[S])
            c.op("dve", lambda e: e.tensor_tensor(pos[:, 0:4], pos[:, 0:4], pos[:, 4:8], ALU.add), reads=[S], writes=[S])
            c.op("dve", lambda e: e.scalar_tensor_tensor(pos[:, 4:8], idxf[:], float(C), pos[:, 0:4], ALU.mult, ALU.add), reads=[S], writes=[S])
            c.op("dve", (lambda i: lambda e: e.tensor_copy(g.drow_all[:, i, :], pos[:, 4:8]))(i), reads=[S], writes=["drow_all"])
            for k in range(4):
                c.dma("pool", (lambda i, k, hr: lambda e: e.indirect_dma_start(
                    out=g.Xd[:, :], out_offset=bass.IndirectOffsetOnAxis(ap=g.drow_all[:, i, k:k + 1], axis=0),
                    in_=hr[:, :], in_offset=None, bounds_check=c.reg(e, NE * C - 1), oob_is_err=False))(i, k, hr),
                    reads=[hk, "drow_all"], cowrites=["Xd"])

    with c.phase():
        wgu = [c.sb("m_wgu%d" % i, [128, 8, 2048], BF16) for i in range(2)]
        wdn = [c.sb("m_wdn%d" % i, [128, 8, 1024], BF16) for i in range(2)]
        bgu = [c.sb("m_bgu%d" % i, [1, 2048], BF16) for i in range(2)]
        bdn = [c.sb("m_bdn%d" % i, [1, 1024], BF16) for i in range(2)]
        xrow = [c.sb("m_xrow%d" % i, [128, D], BF16) for i in range(3)]
        xT = [c.sb("m_xT%d" % i, [128, 8, 128], BF16) for i in range(2)]
        gt = [c.sb("m_g", [128, 1024], F32)] * 2
        sg = [c.sb("m_sg", [128, 1024], F32)] * 2
        lin = [c.sb("m_lin", [128, 1024], F32)] * 2
        act = [c.sb("m_act%d" % i, [128, 1024], BF16) for i in range(2)]
        actT = [c.sb("m_actT%d" % i, [128, 8, 128], BF16) for i in range(2)]
        yt = [c.sb("m_y%d" % i, [128, 1024], F32) for i in range(2)]

        def load_w(ex):
            p = ex % 2
            wg_src = W["w_gu"][layer, ex].rearrange("(kc p) n -> p kc n", p=128)
            wd_src = W["w_dn"][layer, ex].rearrange("(kc p) n -> p kc n", p=128)
            for kc in range(8):
                c.dma("pool", (lambda kc: lambda e: e.dma_start(out=wgu[p][:, kc, :], in_=wg_src[:, kc, :], max_dma_last_dim=8192))(kc),
                      cowrites=["wgu%d" % p], reads=[], writes=[])
            for kc in range(8):
                c.dma("pool", (lambda kc: lambda e: e.dma_start(out=wdn[p][:, kc, :], in_=wd_src[:, kc, :], max_dma_last_dim=4096))(kc),
                      cowrites=["wdn%d" % p])
            c.dma("pool", lambda e: e.dma_start(out=bgu[p][:], in_=W["b_gu"][layer, ex:ex + 1, :], max_dma_last_dim=8192), cowrites=["wgu%d" % p])
            c.dma("pool", lambda e: e.dma_start(out=bdn[p][:], in_=W["b_dn"][layer, ex:ex + 1, :], max_dma_last_dim=4096), cowrites=["wdn%d" % p])

        units = [(ex, i) for ex in range(NE) for i in range(NSL)]

        def st_load(u):
            ex, i = units[u]
            b3 = u % 3
            r0 = ex * C + i * 128
            c.dma("sp", lambda e: e.dma_start(out=xrow[b3][:], in_=g.Xd[r0:r0 + 128, :]), reads=["Xd"], writes=["xrow%d" % b3])

        def st_tx(u):
            b = u % 2
            b3 = u % 3
            transpose_rows_to_T(c, g, xrow[b3], "xrow%d" % b3, xT[b], "xT%d" % b, bank[0], "bank0", fp32_in=False, evac="dve")

        def st_gu(u):
            ex, i = units[u]
            b = u % 2
            p = ex % 2
            for n in range(4):
                for kc in range(8):
                    c.op("pe", (lambda n, kc: lambda e: e.matmul(bank[2 + n][:, :], xT[b][:, kc, :], wgu[p][:, kc, n * 512:(n + 1) * 512], start=(kc == 0), stop=False))(n, kc),
                         reads=["xT%d" % b, "wgu%d" % p], writes=["gu%d" % n])
                c.op("pe", (lambda n: lambda e: e.matmul(bank[2 + n][:, :], g.onesrow[0:1, :], bgu[p][0:1, n * 512:(n + 1) * 512], start=False, stop=True))(n),
                     reads=["onesrow", "wgu%d" % p], writes=["gu%d" % n])
            sls = [slice(n * 512, (n + 1) * 512) for n in range(2)]
            for n in range(2):
                sl = sls[n]
                c.op("dve", (lambda n, sl: lambda e: e.tensor_scalar_min(gt[b][:, sl], bank[2 + n][:, :], 7.0))(n, sl), reads=["gu%d" % n], writes=["g_%d" % n])
                c.op("act", (lambda sl: lambda e: e.activation(out=sg[b][:, sl], in_=gt[b][:, sl], func=AF.Sigmoid, scale=1.702))(sl), reads=["g_%d" % n], writes=["sg_%d" % n])
            for n in range(2):
                sl = sls[n]
                c.op("dve", (lambda n, sl: lambda e: e.tensor_scalar(lin[b][:, sl], bank[4 + n][:, :], 7.0, -7.0, ALU.min, ALU.max))(n, sl), reads=["gu%d" % (2 + n)], writes=["lin_%d" % n])
            for n in range(2):
                sl = sls[n]
                c.op("dve", (lambda sl: lambda e: e.scalar_tensor_tensor(lin[b][:, sl], lin[b][:, sl], 1.0, gt[b][:, sl], ALU.add, ALU.mult))(sl),
                     reads=["lin_%d" % n, "g_%d" % n], writes=["lin_%d" % n])
            for n in range(2):
                sl = sls[n]
                c.op("dve", (lambda sl: lambda e: e.tensor_tensor(act[b][:, sl], lin[b][:, sl], sg[b][:, sl], ALU.mult))(sl),
                     reads=["lin_%d" % n, "sg_%d" % n], writes=["act%d_%d" % (b, n)])

        def st_tact(u):
            b = u % 2
            bb = bank[1][:].bitcast(BF16)
            for kc in range(8):
                c.op("pe", (lambda kc: lambda e: e.transpose(bb[:, kc * 128:(kc + 1) * 128], act[b][:, kc * 128:(kc + 1) * 128], g.identb[:]))(kc),
                     reads=["act%d_%d" % (b, kc // 4), "identb"], writes=["bank1"])
            c.op("act", lambda e: e.activation(out=actT[b][:].rearrange("p a b -> p (a b)"), in_=bb[:, 0:1024], func=AF.Copy), reads=["bank1"], writes=["actT%d" % b])

        def st_down(u):
            ex, i = units[u]
            b = u % 2
            p = ex % 2
            for n in range(2):
                for kc in range(8):
                    c.op("pe", (lambda n, kc: lambda e: e.matmul(bank[6 + n][:, :], actT[b][:, kc, :], wdn[p][:, kc, n * 512:(n + 1) * 512], start=(kc == 0), stop=False))(n, kc),
                         reads=["actT%d" % b, "wdn%d" % p], writes=["dn%d" % n])
                c.op("pe", (lambda n: lambda e: e.matmul(bank[6 + n][:, :], g.onesrow[0:1, :], bdn[p][0:1, n * 512:(n + 1) * 512], start=False, stop=True))(n),
                     reads=["onesrow", "wdn%d" % p], writes=["dn%d" % n])
                c.op("act", (lambda n: lambda e: e.activation(out=yt[b][:, n * 512:(n + 1) * 512], in_=bank[6 + n][:, :], func=AF.Copy))(n),
                     reads=["dn%d" % n], writes=["y%d" % b])
            r0 = ex * C + i * 128
            c.dma("sp", lambda e: e.dma_start(out=g.Yb[r0:r0 + 128, :], in_=yt[b][:]), reads=["y%d" % b], cowrites=["Yb"])

        NU = len(units)
        load_w(0)
        st_load(0)
        if NU > 1:
            st_load(1)
        st_tx(0)
        for u in range(NU):
            ex, i = units[u]
            if u + 2 < NU:
                st_load(u + 2)
            if u >= 1:
                st_tact(u - 1)
            if u + 1 < NU:
                st_tx(u + 1)
            st_gu(u)
            if u >= 1:
                st_down(u - 1)
            if i == 0 and ex + 1 < NE:
                load_w(ex + 1)
        st_tact(NU - 1)
        st_down(NU - 1)

    with c.phase():
        hrow = [c.sb("c_hrow%d" % i, [128, D], F32) for i in range(2)]
        yk = [[c.sb("c_y%d_%d" % (i, k), [128, D], F32) for k in range(4)] for i in range(2)]
        outt = [c.sb("c_out%d" % i, [128, D], F32) for i in range(2)]
        gam = c.sb("c_gam", [128, D], F32)
        bet = c.sb("c_bet", [128, D], F32)
        load_bcast(c, gam, "c_gb", W["ln_ffn_g"][layer:layer + 1, :])
        c.dma("sp", lambda e: e.dma_start(out=bet[:], in_=W["ln_ffn_b"][layer:layer + 1, :].partition_broadcast(128)), cowrites=["c_gb"])
        def c_loads(i):
            b = i % 2
            hr = hrow[b]
            hk = "c_hrow%d" % b
            c.dma("sp", (lambda i, hr: lambda e: e.dma_start(out=hr[:], in_=hsrc[i * 128:(i + 1) * 128, :]))(i, hr), writes=[hk])
            for k in range(4):
                c.dma("pool", (lambda i, k, b: lambda e: e.indirect_dma_start(
                    out=yk[b][k][:, :], out_offset=None, in_=g.Yb[:, :],
                    in_offset=bass.IndirectOffsetOnAxis(ap=g.drow_all[:, i, k:k + 1], axis=0),
                    bounds_check=c.reg(e, NE * C - 1), oob_is_err=False))(i, k, b),
                    reads=["Yb", "drow_all"], writes=["c_y%d_%d" % (b, k)])

        c_loads(0)
        for i in range(NT):
            b = i % 2
            hr = hrow[b]
            hk = "c_hrow%d" % b
            if i + 1 < NT:
                c_loads(i + 1)
            c.op("act", (lambda hr: lambda e: e.mul(hr[:], hr[:], float(DN_ALPHA)))(hr), reads=[hk], writes=[hk])
            for k in range(4):
                c.op("dve", (lambda i, k, b, hr: lambda e: e.scalar_tensor_tensor(hr[:], yk[b][k][:], g.gates_all[:, i, k:k + 1], hr[:], ALU.mult, ALU.add))(i, k, b, hr),
                     reads=[hk, "c_y%d_%d" % (b, k), "gates_all"], writes=[hk])
            layer_norm_tile(c, g, hr, hk, gam, bet, "c_gb", outt[b], "c_out%d" % b, "c")
            c.dma("sp", (lambda i, b: lambda e: e.dma_start(out=hdst[i * 128:(i + 1) * 128, :], in_=outt[b][:]))(i, b),
                  reads=["c_out%d" % b], cowrites=["hdst"])


def host_constants(SEQ):
    cst = {}
    cst["c_ident"] = np.eye(128, dtype=np.float32)
    cst["c_ones"] = np.ones((1, 128), np.float32)
    cst["c_onesq"] = np.ones((128, 128), np.float32)
    k = np.arange(128)
    cst["c_ltri"] = (k[:, None] < k[None, :]).astype(np.float32)
    cst["c_iota"] = np.tile(np.arange(NE, dtype=np.float32)[None, :], (128, 1))
    mc = (k[:, None] <= k[None, :]).astype(np.float32)
    mp = (k[:, None] >= k[None, :]).astype(np.float32)
    cst["c_mcur"] = np.tile(mc, (1, 4)).astype(np.float32)
    cst["c_mprev"] = np.tile(mp, (1, 4)).astype(np.float32)
    par = np.zeros((32, 2), np.float32)
    par[0::2, 0] = 1.0
    par[1::2, 1] = 1.0
    cst["c_par"] = par
    cst["c_iotaT"] = np.tile(np.arange(TB + 1, dtype=np.float32)[None, :], (128, 1))
    half = HD // 2
    inv = (10000.0 ** (-np.arange(half, dtype=np.float32) / half)).astype(np.float32)
    ang = (np.arange(SEQ, dtype=np.float32)[:, None] * inv[None, :]).astype(np.float32)
    cst["c_cos16"] = np.tile(np.cos(ang).astype(np.float32), (1, NH))
    cst["c_sin16"] = np.tile(np.sin(ang).astype(np.float32), (1, NH))
    return cst


WEIGHT_SHAPES = {
    "hy_w_in": (D, 2048), "conv_w": (3, 512), "ssm_a_re": (32, 64), "ssm_a_im": (32, 64), "ssm_log_dt": (32,),
    "ssm_b_re": (32, 64, 16), "ssm_b_im": (32, 64, 16), "ssm_c_re": (32, 16, 64), "ssm_c_im": (32, 16, 64),
    "ssm_d": (32, 16), "ssm_w_glu": (512, 512), "ssm_b_glu": (512,), "hy_w_out": (D, D),
    "att_w_qkv": (D, 3 * D), "att_w_o": (D, D),
    "ln_mix_g": (D,), "ln_mix_b": (D,), "ln_ffn_g": (D,), "ln_ffn_b": (D,),
    "router_w": (D, NE), "router_b": (NE,), "w_gu": (NE, D, 2 * D), "b_gu": (NE, 2 * D),
    "w_dn": (NE, D, D), "b_dn": (NE, D),
}


def build_program(cfg):
    SEQ, NSEQ, C = cfg["SEQ"], cfg["NSEQ"], cfg["C"]
    TT = SEQ * NSEQ
    nc = bass.Bass("TRN2", target_bir_lowering=False)
    W = {}
    W["x"] = nc.dram_tensor("x", [TT, D], F32, kind="ExternalInput").ap()
    for name, shp in WEIGHT_SHAPES.items():
        n0 = cfg["nl"].get(name, 0)
        if n0 == 0:
            continue
        W[name] = nc.dram_tensor(name, [n0] + list(shp), F32, kind="ExternalInput").ap()
    cst = host_constants(SEQ)
    for name, arr in cst.items():
        W[name] = nc.dram_tensor(name, list(arr.shape), F32, kind="ExternalInput").ap()
    out = nc.dram_tensor("out", [TT, D], F32, kind="ExternalOutput").ap()
    c = Ctx(nc)
    g = G()
    setup_globals(c, g, W)
    NT = TT // 128
    g.gates_all = c.sb("gates_all", [128, NT, 4], F32)
    g.drow_all = c.sb("drow_all", [128, NT, 4], U32)
    g.ln_stats = c.sb("ln_stats", [128, 2, 6], F32)
    g.ln_mv = c.sb("ln_mv", [128, 8], F32)
    g.eps_t = c.sb("eps_t", [128, 1], F32)
    c.op("dve", lambda e: e.memset(g.eps_t[:], LN_EPS), writes=["eps"])
    g.rho = c.sb("ssm_rho", [128, 16], F32)
    g.ctab = c.sb("ssm_ctab", [128, 16, TB + 1], F32)
    g.stab = c.sb("ssm_stab", [128, 16, TB + 1], F32)
    g.Pr = c.sb("ssm_Pr", [128, 16, NRND], F32)
    g.Pi = c.sb("ssm_Pi", [128, 16, NRND], F32)
    g.nPi = c.sb("ssm_nPi", [128, 16, NRND], F32)
    g.lBre = c.sb("ssm_lBre", [128, 16, 128], BF16)
    g.lBim = c.sb("ssm_lBim", [128, 16, 128], BF16)
    g.lCre = c.sb("ssm_lCre", [128, 16, 128], BF16)
    g.lCim = c.sb("ssm_lCim", [128, 16, 128], BF16)
    g.Xd = nc.dram_tensor("Xd", [NE * C, D], BF16).ap()
    g.Yb = nc.dram_tensor("Yb", [NE * C, D], F32).ap()
    hb = [nc.dram_tensor("hbuf%d" % i, [TT, D], F32).ap() for i in range(2)]
    g.qkv_d = nc.dram_tensor("qkv_d", [TT, RW], BF16).ap()
    g.O_d = nc.dram_tensor("O_d", [3, TT, VW], F32).ap()
    g.cfg = cfg
    cur = W["x"]
    plan = cfg["plan"]
    for pi, (kind, layer, mi) in enumerate(plan):
        last = pi == len(plan) - 1
        dst = out if last else hb[pi % 2]
        if kind == "moe":
            moe_phase(c, g, W, layer, TT, cur, dst, C)
        elif kind == "even":
            even_phase(c, g, W, layer, mi, SEQ, NSEQ, cur, dst)
        elif kind == "odd":
            odd_phase(c, g, W, layer, mi, SEQ, NSEQ, cur, dst)
        cur = dst
    c.emit()
    return nc, cst, c


TB = 256
NRND = 1
TWO_PI = 6.283185307179586


def sin_reduced(c, out, src, shift, tmpf, tmpi, S):
    c.op("dve", lambda e: e.tensor_scalar(tmpf[:, 0, :], src, float(shift), 1.0 / TWO_PI, ALU.add, ALU.mult), reads=[S], writes=[S])
    c.op("dve", lambda e: e.tensor_copy(tmpi[:], tmpf[:, 0, :]), reads=[S], writes=[S])
    c.op("dve", lambda e: e.tensor_copy(tmpf[:, 1, :], tmpi[:]), reads=[S], writes=[S])
    c.op("dve", lambda e: e.tensor_scalar(tmpf[:, 2, :], src, float(shift), None, ALU.add), reads=[S], writes=[S])
    c.op("dve", lambda e: e.scalar_tensor_tensor(tmpf[:, 2, :], tmpf[:, 1, :], -TWO_PI, tmpf[:, 2, :], ALU.mult, ALU.add), reads=[S], writes=[S])
    c.op("dve", lambda e: e.tensor_scalar(tmpf[:, 3, :], tmpf[:, 2, :], float(np.pi), -TWO_PI, ALU.is_gt, ALU.mult), reads=[S], writes=[S])
    c.op("dve", lambda e: e.tensor_tensor(tmpf[:, 2, :], tmpf[:, 2, :], tmpf[:, 3, :], ALU.add), reads=[S], writes=[S])
    c.op("dve", lambda e: e.tensor_scalar(tmpf[:, 3, :], tmpf[:, 2, :], float(-np.pi), TWO_PI, ALU.is_lt, ALU.mult), reads=[S], writes=[S])
    c.op("dve", lambda e: e.tensor_tensor(tmpf[:, 2, :], tmpf[:, 2, :], tmpf[:, 3, :], ALU.add), reads=[S], writes=[S])
    c.op("act", lambda e: e.activation(out=out, in_=tmpf[:, 2, :], func=AF.Sin), reads=[S], writes=[S])


def even_setup(c, g, W, mi):
    bank = g.bank
    S = "es"
    if g.cfg.get("dbg") == "none":
        return
    with c.phase():
        nat = c.sb("es_nat", [32, 3, 64], F32)
        natm = c.sb("es_natm", [32, 3, 128], F32)
        par = c.sb("es_par", [32, 2], F32)
        ldt = c.sb("es_ldt", [32, 1], F32)
        q3 = c.sb("es_q3", [128, 3, 16], F32)
        tf = c.sb("es_tf", [128, 4, 16], F32)
        ti = c.sb("es_ti", [128, 16], I32)
        pt = c.sb("es_pt", [128, 32], F32)
        w = c.sb("es_w", [128, 12, 16], F32)
        Bn = c.sb("es_Bn", [128, 2, 16, 16], F32)
        Bf = c.sb("es_Bf", [128, 2, 16, 128], F32)
        Cf = c.sb("es_Cf", [128, 2, 16, 128], F32)
        c.dma("sp", lambda e: e.dma_start(out=nat[:, 0, :], in_=W["ssm_a_re"][mi]), cowrites=["es_nat"])
        c.dma("sp", lambda e: e.dma_start(out=nat[:, 1, :], in_=W["ssm_a_im"][mi]), cowrites=["es_nat"])
        c.dma("sp", lambda e: e.dma_start(out=ldt[:], in_=W["ssm_log_dt"][mi].rearrange("(g o) -> g o", o=1)), cowrites=["es_nat"])
        c.dma("sp", lambda e: e.dma_start(out=par[:], in_=W["c_par"][:, :]), cowrites=["es_nat"])
        for ri, nm in enumerate(("ssm_b_re", "ssm_b_im")):
            for gg in range(2):
                c.dma("sp", (lambda ri, nm, gg: lambda e: e.dma_start(out=Bn[gg * 64:(gg + 1) * 64, ri, :, :], in_=W[nm][mi].rearrange("(j gg) p h -> gg p j h", gg=2)[gg]))(ri, nm, gg), cowrites=["es_Bn"])
        c.op("dve", lambda e: e.memset(Bf[:], 0.0), writes=["es_Bf"])
        c.op("pool", lambda e: e.memset(Cf[:], 0.0), writes=["es_Cf"])
        for ri, nm in enumerate(("ssm_c_re", "ssm_c_im")):
            for j in range(16):
                for gg in range(2):
                    q = j % 4
                    p0 = 32 * q + 16 * gg
                    c.dma("sp", (lambda ri, nm, j, gg, p0: lambda e: e.dma_start(out=Cf[p0:p0 + 16, ri, j, gg * 64:(gg + 1) * 64], in_=W[nm][mi, 2 * j + gg]))(ri, nm, j, gg, p0),
                          reads=["es_Cf"], cowrites=["es_Cf2"])
        c.op("act", lambda e: e.activation(out=ldt[:], in_=ldt[:], func=AF.Exp), reads=["es_nat"], writes=[S])
        c.op("dve", lambda e: e.tensor_scalar_min(nat[:, 0, :], nat[:, 0, :], -1e-4), reads=["es_nat", S], writes=[S])
        c.op("dve", lambda e: e.memset(nat[:, 2, :], 1.0), reads=[S], writes=[S])
        c.op("dve", lambda e: e.tensor_scalar_mul(nat[:, 2, :], nat[:, 2, :], ldt[:, 0:1]), reads=[S], writes=[S])
        for m in range(3):
            c.op("dve", (lambda m: lambda e: e.tensor_scalar_mul(natm[:, m, 0:64], nat[:, m, :], par[:, 0:1]))(m), reads=[S], writes=[S])
            c.op("dve", (lambda m: lambda e: e.tensor_scalar_mul(natm[:, m, 64:128], nat[:, m, :], par[:, 1:2]))(m), reads=[S], writes=[S])
            c.op("pe", (lambda m: lambda e: e.transpose(bank[0][:, 0:32], natm[:, m, :], g.ident[0:32, 0:32]))(m), reads=[S, "ident"], writes=["bank0"])
            c.op("dve", lambda e: e.tensor_copy(pt[:], bank[0][:, 0:32]), reads=["bank0", S], writes=[S])
            c.op("dve", (lambda m: lambda e: e.tensor_tensor(q3[:, m, :], pt[:].rearrange("p (j gg) -> p j gg", gg=2)[:, :, 0], pt[:].rearrange("p (j gg) -> p j gg", gg=2)[:, :, 1], ALU.add))(m), reads=[S], writes=[S])
        LR, LI, DT = q3[:, 0, :], q3[:, 1, :], q3[:, 2, :]
        c.op("dve", lambda e: e.tensor_tensor(w[:, 0, :], LR, DT, ALU.mult), reads=[S], writes=[S])
        c.op("act", lambda e: e.activation(out=w[:, 0, :], in_=w[:, 0, :], func=AF.Exp), reads=[S], writes=[S])
        c.op("dve", lambda e: e.tensor_tensor(w[:, 1, :], LI, DT, ALU.mult), reads=[S], writes=[S])
        sin_reduced(c, w[:, 3, :], w[:, 1, :], 0.0, tf, ti, S)
        sin_reduced(c, w[:, 2, :], w[:, 1, :], np.pi / 2, tf, ti, S)
        c.op("dve", lambda e: e.tensor_tensor(w[:, 4, :], w[:, 0, :], w[:, 2, :], ALU.mult), reads=[S], writes=[S])
        c.op("dve", lambda e: e.tensor_tensor(w[:, 5, :], w[:, 0, :], w[:, 3, :], ALU.mult), reads=[S], writes=[S])
        c.op("dve", lambda e: e.tensor_scalar_add(w[:, 6, :], w[:, 4, :], -1.0), reads=[S], writes=[S])
        c.op("dve", lambda e: e.tensor_tensor(w[:, 7, :], LR, LR, ALU.mult), reads=[S], writes=[S])
        c.op("dve", lambda e: e.tensor_tensor(w[:, 10, :], LI, LI, ALU.mult), reads=[S], writes=[S])
        c.op("dve", lambda e: e.tensor_tensor(w[:, 7, :], w[:, 7, :], w[:, 10, :], ALU.add), reads=[S], writes=[S])
        c.op("dve", lambda e: e.reciprocal(w[:, 7, :], w[:, 7, :]), reads=[S], writes=[S])
        c.op("dve", lambda e: e.tensor_tensor(w[:, 8, :], w[:, 6, :], LR, ALU.mult), reads=[S], writes=[S])
        c.op("dve", lambda e: e.tensor_tensor(w[:, 10, :], w[:, 5, :], LI, ALU.mult), reads=[S], writes=[S])
        c.op("dve", lambda e: e.tensor_tensor(w[:, 8, :], w[:, 8, :], w[:, 10, :], ALU.add), reads=[S], writes=[S])
        c.op("dve", lambda e: e.tensor_tensor(w[:, 8, :], w[:, 8, :], w[:, 7, :], ALU.mult), reads=[S], writes=[S])
        c.op("dve", lambda e: e.tensor_tensor(w[:, 9, :], w[:, 5, :], LR, ALU.mult), reads=[S], writes=[S])
        c.op("dve", lambda e: e.tensor_tensor(w[:, 10, :], w[:, 6, :], LI, ALU.mult), reads=[S], writes=[S])
        c.op("dve", lambda e: e.tensor_tensor(w[:, 9, :], w[:, 9, :], w[:, 10, :], ALU.subtract), reads=[S], writes=[S])
        c.op("dve", lambda e: e.tensor_tensor(w[:, 9, :], w[:, 9, :], w[:, 7, :], ALU.mult), reads=[S], writes=[S])
        c.op("dve", lambda e: e.tensor_scalar_mul(w[:, 11, :], w[:, 9, :], -1.0), reads=[S], writes=[S])
        c.op("dve", lambda e: e.tensor_copy(g.rho[:], w[:, 0, :]), reads=[S], writes=["P"])
        io = c.sb("es_io", [128, TB + 1], F32)
        ang = c.sb("es_ang", [128, TB + 1], F32)
        tf2 = c.sb("es_tf2", [128, 4, TB + 1], F32)
        ti2 = c.sb("es_ti2", [128, TB + 1], I32)
        c.dma("sp", lambda e: e.dma_start(out=io[:], in_=W["c_iotaT"][:, :]), writes=["es_io"])
        for j in range(16):
            c.op("dve", (lambda j: lambda e: e.tensor_scalar_mul(ang[:], io[:], w[:, 1, j:j + 1]))(j), reads=[S, "es_io"], writes=[S])
            sin_reduced(c, g.stab[:, j, :], ang[:], 0.0, tf2, ti2, S)
            sin_reduced(c, g.ctab[:, j, :], ang[:], np.pi / 2, tf2, ti2, S)
        for j in range(16):
            q = j % 4
            for gg in range(2):
                ps_ = slice(gg * 64, (gg + 1) * 64)
                cs = slice(32 * q + 16 * gg, 32 * q + 16 * gg + 16)
                cr, ci, nci = w[ps_, 8, j:j + 1], w[ps_, 9, j:j + 1], w[ps_, 11, j:j + 1]
                c.op("dve", (lambda j, ps_, cs, cr: lambda e: e.tensor_scalar_mul(Bf[ps_, 0, j, cs], Bn[ps_, 0, j, :], cr))(j, ps_, cs, cr), reads=[S, "es_Bn", "es_Bf"], writes=["es_Bf"])
                c.op("dve", (lambda j, ps_, cs, nci: lambda e: e.scalar_tensor_tensor(Bf[ps_, 0, j, cs], Bn[ps_, 1, j, :], nci, Bf[ps_, 0, j, cs], ALU.mult, ALU.add))(j, ps_, cs, nci), reads=[S, "es_Bn", "es_Bf"], writes=["es_Bf"])
                c.op("dve", (lambda j, ps_, cs, cr: lambda e: e.tensor_scalar_mul(Bf[ps_, 1, j, cs], Bn[ps_, 1, j, :], cr))(j, ps_, cs, cr), reads=[S, "es_Bn", "es_Bf"], writes=["es_Bf"])
                c.op("dve", (lambda j, ps_, cs, ci: lambda e: e.scalar_tensor_tensor(Bf[ps_, 1, j, cs], Bn[ps_, 0, j, :], ci, Bf[ps_, 1, j, cs], ALU.mult, ALU.add))(j, ps_, cs, ci), reads=[S, "es_Bn", "es_Bf"], writes=["es_Bf"])
        for j in range(16):
            for ri, (src, srck, dst, scale) in enumerate(((Bf, "es_Bf", g.lBre, 1.0), (Bf, "es_Bf", g.lBim, 1.0), (Cf, "es_Cf2", g.lCre, 1.0), (Cf, "es_Cf2", g.lCim, -1.0))):
                r = ri % 2
                bk = bank[1 + (ri % 2)]
                bkk = "bank%d" % (1 + (ri % 2))
                c.op("pe", (lambda src, r, j, bk: lambda e: e.transpose(bk[:, 0:128], src[:, r, j, :], g.ident[:]))(src, r, j, bk), reads=[srck, "es_Cf", "ident"], writes=[bkk])
                c.op("act", (lambda dst, j, bk, scale: lambda e: e.activation(out=dst[:, j, :], in_=bk[:, 0:128], func=AF.Copy, scale=scale))(dst, j, bk, scale), reads=[bkk], writes=["lBC"])


def even_phase(c, g, W, layer, mi, SEQ, NSEQ, hsrc, hdst):
    bank = g.bank
    dbg = g.cfg.get("dbg")
    if dbg != "nosetup":
        even_setup(c, g, W, mi)
    if dbg in ("setup", "none"):
        c.dma("sp", lambda e: e.dma_start(out=hdst[:, :], in_=hsrc[:, :]), writes=["hdst"])
        return
    NB = SEQ // TB
    with c.phase():
        win = c.sb("e_win", [128, 8, 2048], BF16)
        wout = c.sb("e_wout", [128, 8, 1024], BF16)
        wglu = c.sb("e_wglu", [128, 4, 512], BF16)
        bglu = c.sb("e_bglu", [128, 4], F32)
        dsk = c.sb("e_dsk", [128, 4], F32)
        cw = c.sb("e_cw", [128, 4, 3], F32)
        gam = c.sb("e_gam", [128, D], F32)
        bet = c.sb("e_bet", [128, D], F32)
        NTB = TB // 128
        hrowA = [c.sb("e_hrowA%d" % i, [128, D], F32) for i in range(NTB)]
        hrowF = [c.sb("e_hrowF%d" % i, [128, D], F32) for i in range(NTB)]
        hT = c.sb("e_hT", [128, 8, TB], BF16)
        gb = c.sb("e_gb", [128, 4, TB], F32)
        vb = c.sb("e_vb", [128, 4, TB + 2], F32)
        gct = c.sb("e_gct", [128, TB], F32)
        uT = c.sb("e_uT", [128, 4, TB], BF16)
        u32 = c.sb("e_u32", [128, 4, TB], F32)
        sbr = [c.sb("e_sbr%d" % b, [128, TB], F32) for b in range(2)]
        sbi = [c.sb("e_sbi%d" % b, [128, TB], F32) for b in range(2)]
        st1 = [c.sb("e_st1%d" % b, [128, TB], F32) for b in range(2)]
        st2 = [c.sb("e_st2%d" % b, [128, TB], F32) for b in range(2)]
        st3 = [c.sb("e_st3%d" % b, [128, TB], F32) for b in range(2)]
        st4 = [c.sb("e_st4%d" % b, [128, TB], F32) for b in range(2)]
        sp3 = [c.sb("e_sp3%d" % b, [128, TB], F32) for b in range(2)]
        sp4 = [c.sb("e_sp4%d" % b, [128, TB], F32) for b in range(2)]
        svr = [c.sb("e_svr%d" % b, [128, TB], F32) for b in range(2)]
        svi = [c.sb("e_svi%d" % b, [128, TB], F32) for b in range(2)]
        xbf = [[c.sb("e_xbf%d%d" % (a, b), [128, TB], BF16) for b in range(2)] for a in range(2)]
        Xst = c.sb("e_Xst", [128, 16, 2], F32)
        Xtmp = c.sb("e_Xtmp", [128, 16, 2], F32)
        ycT = c.sb("e_ycT", [128, 8, TB], BF16)
        yt = c.sb("e_yt", [128, TB], F32)
        tt_ = c.sb("e_tt", [128, TB], F32)
        z32 = c.sb("e_z32", [128, 4, TB], F32)
        zT = c.sb("e_zT", [128, 4, TB], BF16)
        outt = [c.sb("e_out%d" % i, [128, D], F32) for i in range(2)]
        for kc in range(8):
            c.dma("pool", (lambda kc: lambda e: e.dma_start(out=win[:, kc, :], in_=W["hy_w_in"][mi, kc * 128:(kc + 1) * 128, :], max_dma_last_dim=8192))(kc), cowrites=["win"])
            c.dma("pool", (lambda kc: lambda e: e.dma_start(out=wout[:, kc, :], in_=W["hy_w_out"][mi, kc * 128:(kc + 1) * 128, :], max_dma_last_dim=4096))(kc), cowrites=["wout"])
        for kc in range(4):
            c.dma("pool", (lambda kc: lambda e: e.dma_start(out=wglu[:, kc, :], in_=W["ssm_w_glu"][mi, kc * 128:(kc + 1) * 128, :], max_dma_last_dim=2048))(kc), cowrites=["wglu"])
            c.dma("sp", (lambda kc: lambda e: e.dma_start(out=bglu[:, kc:kc + 1], in_=W["ssm_b_glu"][mi, kc * 128:(kc + 1) * 128].rearrange("(p o) -> p o", o=1)))(kc), cowrites=["small"])
            c.dma("sp", (lambda kc: lambda e: e.dma_start(out=dsk[:, kc:kc + 1], in_=W["ssm_d"][mi].rearrange("g h -> (g h)")[kc * 128:(kc + 1) * 128].rearrange("(p o) -> p o", o=1)))(kc), cowrites=["small"])
            for k in range(3):
                c.dma("sp", (lambda kc, k: lambda e: e.dma_start(out=cw[:, kc, k:k + 1], in_=W["conv_w"][mi, k, kc * 128:(kc + 1) * 128].rearrange("(p o) -> p o", o=1)))(kc, k), cowrites=["small"])
        c.dma("sp", lambda e: e.dma_start(out=gam[:], in_=W["ln_mix_g"][layer:layer + 1, :].partition_broadcast(128)), cowrites=["e_gam"])
        c.dma("sp", lambda e: e.dma_start(out=bet[:], in_=W["ln_mix_b"][layer:layer + 1, :].partition_broadcast(128)), cowrites=["e_gam"])
        blocks = [(s, tb) for s in range(NSEQ) for tb in range(NB)]

        def e_loads(bi_, bufs, pfx):
            s_, tb_ = blocks[bi_]
            t0_ = s_ * SEQ + tb_ * TB
            for tt in range(NTB):
                c.dma("sp", (lambda tt: lambda e: e.dma_start(out=bufs[tt][:], in_=hsrc[t0_ + tt * 128:t0_ + (tt + 1) * 128, :]))(tt), writes=["%s%d" % (pfx, tt)])

        e_loads(0, hrowA, "e_hrowA")
        for bi_, (s, tb) in enumerate(blocks):
            if True:
                t0 = s * SEQ + tb * TB
                e_loads(bi_, hrowF, "e_hrowF")
                for tt in range(TB // 128):
                    hr = hrowA[tt]
                    hk = "e_hrowA%d" % tt
                    for half in range(2):
                        for jj in range(4):
                            kc = half * 4 + jj
                            c.op("pe", (lambda kc, jj, hr: lambda e: e.transpose(bank[0][:, jj * 128:(jj + 1) * 128], hr[:, kc * 128:(kc + 1) * 128], g.ident[:]))(kc, jj, hr),
                                 reads=[hk, "ident"], writes=["bank0"])
                        c.op("act", (lambda half, tt: lambda e: e.activation(out=hT[:, half * 4:(half + 1) * 4, tt * 128:(tt + 1) * 128], in_=bank[0][:, :].rearrange("p (a b) -> p a b", a=4), func=AF.Copy))(half, tt),
                             reads=["bank0"], writes=["e_hT"])
                if bi_ + 1 < len(blocks):
                    e_loads(bi_ + 1, hrowA, "e_hrowA")
                stg = g.cfg.get("stages", "abcdef")
                def proj(oc, bk, bkk):
                    for kc in range(8):
                        c.op("pe", (lambda kc: lambda e: e.matmul(bk[:, 0:TB], win[:, kc, oc * 128:(oc + 1) * 128], hT[:, kc, :], start=(kc == 0), stop=(kc == 7)))(kc),
                             reads=["win", "e_hT"], writes=[bkk])
                for cc in range(4 if "b" in stg else 0):
                    if "1" in stg or "c" in stg:
                        proj(cc, bank[1], "bank1")
                        c.op("act", (lambda cc: lambda e: e.activation(out=gb[:, cc, :], in_=bank[1][:, 0:TB], func=AF.Copy))(cc), reads=["bank1"], writes=["e_gb"])
                    if "2" in stg or "c" in stg:
                        proj(4 + cc, bank[2], "bank2")
                        c.op("act", lambda e: e.activation(out=gct[:], in_=bank[2][:, 0:TB], func=AF.Copy), reads=["bank2"], writes=["e_gct"])
                    if "3" in stg or "c" in stg:
                        proj(8 + cc, bank[1], "bank1")
                        if tb == 0:
                            c.op("dve", (lambda cc: lambda e: e.memset(vb[:, cc, 0:2], 0.0))(cc), writes=["e_vh%d" % cc], reads=["e_vb%d" % cc])
                        c.op("dve", (lambda cc: lambda e: e.tensor_tensor(vb[:, cc, 2:TB + 2], gct[:], bank[1][:, 0:TB], ALU.mult))(cc), reads=["bank1", "e_gct"], writes=["e_vb%d" % cc])
                    if "4" in stg or "d" in stg:
                        proj(12 + cc, bank[2], "bank2")
                        c.op("dve", (lambda cc: lambda e: e.tensor_copy(u32[:, cc, :], bank[2][:, 0:TB]))(cc), reads=["bank2"], writes=["e_u32"])
                        c.op("act", (lambda cc: lambda e: e.activation(out=uT[:, cc, :], in_=u32[:, cc, :], func=AF.Copy))(cc), reads=["e_u32"], writes=["e_uT"])
                    if "c" not in stg:
                        continue
                    vk = ["e_vb%d" % cc, "e_vh%d" % cc]
                    c.op("dve", (lambda cc: lambda e: e.tensor_scalar_mul(tt_[:], vb[:, cc, 0:TB], cw[:, cc, 0:1]))(cc), reads=vk + ["small"], writes=["e_tt"])
                    c.op("dve", (lambda cc: lambda e: e.scalar_tensor_tensor(tt_[:], vb[:, cc, 1:TB + 1], cw[:, cc, 1:2], tt_[:], ALU.mult, ALU.add))(cc), reads=vk + ["small", "e_tt"], writes=["e_tt"])
                    c.op("dve", (lambda cc: lambda e: e.scalar_tensor_tensor(tt_[:], vb[:, cc, 2:TB + 2], cw[:, cc, 2:3], tt_[:], ALU.mult, ALU.add))(cc), reads=vk + ["small", "e_tt"], writes=["e_tt"])
                    c.op("dve", (lambda cc: lambda e: e.tensor_tensor(ycT[:, cc, :], tt_[:], gb[:, cc, :], ALU.mult))(cc), reads=["e_tt", "e_gb"], writes=["e_ycT"])
                    c.op("dve", (lambda cc: lambda e: e.tensor_copy(vb[:, cc, 0:2], vb[:, cc, TB:TB + 2]))(cc), reads=vk, writes=["e_vh%d" % cc])
                for j in range(16 if "d" in stg else 0):
                    cc = j // 4
                    jb = j % 2
                    cj, sj = g.ctab[:, j, 0:TB], g.stab[:, j, 0:TB]
                    Er, Ei = g.ctab[:, j, TB:TB + 1], g.stab[:, j, TB:TB + 1]
                    br, bi_ = sbr[jb], sbi[jb]
                    K = "e_s%d_" % jb
                    c.op("pe", (lambda j, cc: lambda e: e.matmul(bank[3][:, 0:TB], g.lBre[:, j, :], uT[:, cc, :], start=True, stop=True))(j, cc), reads=["lBC", "e_uT"], writes=["bank3"])
                    c.op("pe", (lambda j, cc: lambda e: e.matmul(bank[4][:, 0:TB], g.lBim[:, j, :], uT[:, cc, :], start=True, stop=True))(j, cc), reads=["lBC", "e_uT"], writes=["bank4"])
                    c.op("act", (lambda br: lambda e: e.activation(out=br[:], in_=bank[3][:, 0:TB], func=AF.Copy))(br), reads=["bank3"], writes=[K + "br"])
                    c.op("act", (lambda bi_: lambda e: e.activation(out=bi_[:], in_=bank[4][:, 0:TB], func=AF.Copy))(bi_), reads=["bank4"], writes=[K + "bi"])
                    t1, t2, t3, t4 = st1[jb], st2[jb], st3[jb], st4[jb]
                    c.op("dve", (lambda t1, br, cj: lambda e: e.tensor_tensor(t1[:], br[:], cj, ALU.mult))(t1, br, cj), reads=[K + "br", "P"], writes=[K + "t1"])
                    c.op("pool", (lambda t2, bi_, sj: lambda e: e.tensor_tensor(t2[:], bi_[:], sj, ALU.mult))(t2, bi_, sj), reads=[K + "bi", "P"], writes=[K + "t2"])
                    c.op("dve", (lambda t3, bi_, cj: lambda e: e.tensor_tensor(t3[:], bi_[:], cj, ALU.mult))(t3, bi_, cj), reads=[K + "bi", "P"], writes=[K + "t3"])
                    c.op("pool", (lambda t4, br, sj: lambda e: e.tensor_tensor(t4[:], br[:], sj, ALU.mult))(t4, br, sj), reads=[K + "br", "P"], writes=[K + "t4"])
                    c.op("dve", (lambda t1, t2: lambda e: e.tensor_tensor(t1[:], t1[:], t2[:], ALU.add))(t1, t2), reads=[K + "t1", K + "t2"], writes=[K + "t1"])
                    c.op("dve", (lambda t3, t4: lambda e: e.tensor_tensor(t3[:], t3[:], t4[:], ALU.subtract))(t3, t4), reads=[K + "t3", K + "t4"], writes=[K + "t3"])
                    vr, vi = svr[jb], svi[jb]
                    rb = g.rho[:, j:j + 1].to_broadcast([128, TB])
                    ir = 0.0 if tb == 0 else Xst[:, j, 0:1]
                    ii = 0.0 if tb == 0 else Xst[:, j, 1:2]
                    c.op("dve", (lambda vr, rb, t1, ir: lambda e: e.tensor_tensor_scan(vr[:], rb, t1[:], ir, ALU.mult, ALU.add))(vr, rb, t1, ir), reads=[K + "t1", "P", "e_Xst"], writes=[K + "vr"])
                    c.op("dve", (lambda vi, rb, t3, ii: lambda e: e.tensor_tensor_scan(vi[:], rb, t3[:], ii, ALU.mult, ALU.add))(vi, rb, t3, ii), reads=[K + "t3", "P", "e_Xst"], writes=[K + "vi"])
                    c.op("dve", (lambda vi, Ei, j: lambda e: e.tensor_scalar_mul(Xtmp[:, j, 0:1], vi[:, TB - 1:TB], Ei))(vi, Ei, j), reads=[K + "vi", "P"], writes=["e_Xtmp"])
                    c.op("dve", (lambda vr, Ei, j: lambda e: e.tensor_scalar_mul(Xtmp[:, j, 1:2], vr[:, TB - 1:TB], Ei))(vr, Ei, j), reads=[K + "vr", "P", "e_Xtmp"], writes=["e_Xtmp"])
                    c.op("dve", (lambda vr, Er, j: lambda e: e.scalar_tensor_tensor(Xst[:, j, 0:1], vr[:, TB - 1:TB], Er, Xtmp[:, j, 0:1], ALU.mult, ALU.subtract))(vr, Er, j), reads=[K + "vr", "P", "e_Xtmp"], writes=["e_Xst"])
                    c.op("dve", (lambda vi, Er, j: lambda e: e.scalar_tensor_tensor(Xst[:, j, 1:2], vi[:, TB - 1:TB], Er, Xtmp[:, j, 1:2], ALU.mult, ALU.add))(vi, Er, j), reads=[K + "vi", "P", "e_Xtmp", "e_Xst"], writes=["e_Xst"])
                    p1, p2, p3, p4 = t2, t4, sp3[jb], sp4[jb]
                    c.op("pool", (lambda p1, vr, cj: lambda e: e.tensor_tensor(p1[:], vr[:], cj, ALU.mult))(p1, vr, cj), reads=[K + "vr", "P", K + "t2", K + "t1"], writes=[K + "t2"])
                    c.op("pool", (lambda p2, vi, sj: lambda e: e.tensor_tensor(p2[:], vi[:], sj, ALU.mult))(p2, vi, sj), reads=[K + "vi", "P", K + "t4", K + "t3"], writes=[K + "t4"])
                    c.op("pool", (lambda p3, vi, cj: lambda e: e.tensor_tensor(p3[:], vi[:], cj, ALU.mult))(p3, vi, cj), reads=[K + "vi", "P"], writes=[K + "p3"])
                    c.op("pool", (lambda p4, vr, sj: lambda e: e.tensor_tensor(p4[:], vr[:], sj, ALU.mult))(p4, vr, sj), reads=[K + "vr", "P"], writes=[K + "p4"])
                    xr_b, xi_b = xbf[jb][0], xbf[jb][1]
                    c.op("dve", (lambda xr_b, p1, p2: lambda e: e.tensor_tensor(xr_b[:], p1[:], p2[:], ALU.subtract))(xr_b, p1, p2), reads=[K + "t2", K + "t4"], writes=[K + "xr"])
                    c.op("dve", (lambda xi_b, p3, p4: lambda e: e.tensor_tensor(xi_b[:], p3[:], p4[:], ALU.add))(xi_b, p3, p4), reads=[K + "p3", K + "p4"], writes=[K + "xi"])
                    c.op("pe", (lambda j, xr_b: lambda e: e.matmul(bank[5][:, 0:TB], g.lCre[:, j, :], xr_b[:], start=(j % 4 == 0), stop=False))(j, xr_b), reads=["lBC", K + "xr"], writes=["bank5"])
                    c.op("pe", (lambda j, xi_b: lambda e: e.matmul(bank[5][:, 0:TB], g.lCim[:, j, :], xi_b[:], start=False, stop=(j % 4 == 3)))(j, xi_b), reads=["lBC", K + "xi"], writes=["bank5"])
                    if j % 4 == 3:
                        c.op("dve", (lambda cc: lambda e: e.scalar_tensor_tensor(yt[:], u32[:, cc, :], dsk[:, cc:cc + 1], bank[5][:, 0:TB], ALU.mult, ALU.add))(cc), reads=["bank5", "e_u32", "small"], writes=["e_yt"])
                        c.op("act", lambda e: e.activation(out=tt_[:], in_=yt[:], func=AF.Square), reads=["e_yt"], writes=["e_tt"])
                        c.op("dve", lambda e: e.tensor_scalar(tt_[:], tt_[:], 0.044715, 1.0, ALU.mult, ALU.add), reads=["e_tt"], writes=["e_tt"])
                        c.op("dve", lambda e: e.tensor_tensor(tt_[:], tt_[:], yt[:], ALU.mult), reads=["e_tt", "e_yt"], writes=["e_tt"])
                        c.op("act", lambda e: e.activation(out=tt_[:], in_=tt_[:], func=AF.Sigmoid, scale=1.5957691216057308), reads=["e_tt"], writes=["e_tt"])
                        c.op("dve", (lambda cc: lambda e: e.tensor_tensor(z32[:, cc, :], tt_[:], yt[:], ALU.mult))(cc), reads=["e_tt", "e_yt"], writes=["e_z32"])
                        c.op("act", (lambda cc: lambda e: e.activation(out=zT[:, cc, :], in_=z32[:, cc, :], func=AF.Copy))(cc), reads=["e_z32"], writes=["e_zT"])
                for oc in range(4 if "e" in stg else 0):
                    for kc in range(4):
                        c.op("pe", (lambda oc, kc: lambda e: e.matmul(bank[6][:, 0:TB], wglu[:, kc, oc * 128:(oc + 1) * 128], zT[:, kc, :], start=(kc == 0), stop=(kc == 3)))(oc, kc), reads=["wglu", "e_zT"], writes=["bank6"])
                    c.op("act", (lambda oc: lambda e: e.activation(out=tt_[:], in_=bank[6][:, 0:TB], func=AF.Sigmoid, bias=bglu[:, oc:oc + 1], scale=1.0))(oc), reads=["bank6", "small"], writes=["e_tt"])
                    c.op("dve", (lambda oc: lambda e: e.tensor_tensor(ycT[:, 4 + oc, :], tt_[:], z32[:, oc, :], ALU.mult))(oc), reads=["e_tt", "e_z32"], writes=["e_ycT"])
                for tt in range(TB // 128 if "f" in stg else 0):
                    hr = hrowF[tt]
                    hk = "e_hrowF%d" % tt
                    ob = outt[tt % 2]
                    ok = "e_out%d" % (tt % 2)
                    for n in range(2):
                        for kc in range(8):
                            c.op("pe", (lambda tt, n, kc: lambda e: e.matmul(bank[6 + n][:, :], ycT[:, kc, tt * 128:(tt + 1) * 128], wout[:, kc, n * 512:(n + 1) * 512], start=(kc == 0), stop=(kc == 7)))(tt, n, kc),
                                 reads=["e_ycT", "wout"], writes=["bank%d" % (6 + n)])
                        c.op("dve", (lambda n, hr: lambda e: e.scalar_tensor_tensor(hr[:, n * 512:(n + 1) * 512], hr[:, n * 512:(n + 1) * 512], float(DN_ALPHA), bank[6 + n][:, :], ALU.mult, ALU.add))(n, hr),
                             reads=["bank%d" % (6 + n), hk], writes=[hk])
                    layer_norm_tile(c, g, hr, hk, gam, bet, "e_gam", ob, ok, "e")
                    c.dma("sp", (lambda tt, ob, t0: lambda e: e.dma_start(out=hdst[t0 + tt * 128:t0 + (tt + 1) * 128, :], in_=ob[:]))(tt, ob, t0), reads=[ok], cowrites=["hdst"])
        if "f" not in g.cfg.get("stages", "abcdef"):
            c.dma("sp", lambda e: e.dma_start(out=hdst[:, :], in_=hsrc[:, :]), writes=["hdst"])


VW = NH * 65
RW = 2 * D + VW


def odd_phase(c, g, W, layer, mi, SEQ, NSEQ, hsrc, hdst):
    bank = g.bank
    TT = SEQ * NSEQ
    NT = TT // 128
    qkv_d = g.qkv_d
    O_d = g.O_d
    stg = g.cfg.get("stages", "ABC")
    with c.phase():
        wqkv = c.sb("o_wqkv", [128, 8, 3 * D], BF16)
        hrow = [c.sb("o_hrow%d" % i, [128, D], F32) for i in range(2)]
        hT = c.sb("o_hT", [128, 8, 128], BF16)
        cs = [c.sb("o_cs%d" % i, [128, 2, 512], F32) for i in range(2)]
        qk32 = c.sb("o_qk32", [128, 2, D], F32)
        tq = [c.sb("o_tq%d" % i, [128, 512], F32) for i in range(2)]
        tk = [c.sb("o_tk%d" % i, [128, 512], F32) for i in range(2)]
        rows = [c.sb("o_rows%d" % i, [128, RW], BF16) for i in range(2)]
        for kc in range(8):
            for part in range(3):
                c.dma("pool", (lambda kc, part: lambda e: e.dma_start(out=wqkv[:, kc, part * D:(part + 1) * D], in_=W["att_w_qkv"][mi, kc * 128:(kc + 1) * 128, part * D:(part + 1) * D], max_dma_last_dim=4096))(kc, part), cowrites=["wqkv"])
        for i in range(2):
            c.op("pool", (lambda i: lambda e: e.memset(rows[i][:, 2 * D:RW], 1.0))(i), writes=["o_rows%d" % i])
        def a_loads(i):
            b = i % 2
            hr, hk = hrow[b], "o_hrow%d" % b
            pos0 = (i * 128) % SEQ
            c.dma("sp", lambda e: e.dma_start(out=hr[:], in_=hsrc[i * 128:(i + 1) * 128, :]), writes=[hk])
            c.dma("sp", lambda e: e.dma_start(out=cs[b][:, 0, :], in_=W["c_cos16"][pos0:pos0 + 128, :]), cowrites=["o_cs%d" % b])
            c.dma("sp", lambda e: e.dma_start(out=cs[b][:, 1, :], in_=W["c_sin16"][pos0:pos0 + 128, :]), cowrites=["o_cs%d" % b])

        if "A" in stg:
            a_loads(0)
        for i in range(NT if "A" in stg else 0):
            b = i % 2
            hr, hk = hrow[b], "o_hrow%d" % b
            if i + 1 < NT:
                a_loads(i + 1)
            transpose_rows_to_T(c, g, hr, hk, hT, "o_hT", bank[0], "bank0", fp32_in=True, evac="act")
            for n in range(6):
                for kc in range(8):
                    c.op("pe", (lambda n, kc: lambda e: e.matmul(bank[2 + n][:, :], hT[:, kc, :], wqkv[:, kc, n * 512:(n + 1) * 512], start=(kc == 0), stop=(kc == 7)))(n, kc),
                         reads=["o_hT", "wqkv"], writes=["bank%d" % (2 + n)])
            R = rows[b]
            Rk = "o_rows%d" % b
            for n in range(4):
                c.op("act", (lambda n: lambda e: e.activation(out=qk32[:, n // 2, (n % 2) * 512:(n % 2 + 1) * 512], in_=bank[2 + n][:, :], func=AF.Copy))(n),
                     reads=["bank%d" % (2 + n)], writes=["o_qk32_%d" % (n // 2)])
            for n in range(2):
                c.op("act", (lambda n, R: lambda e: e.activation(out=R[:, 2 * D:RW].rearrange("p (h e) -> p h e", e=65)[:, n * 8:(n + 1) * 8, 0:64], in_=bank[6 + n][:, :].rearrange("p (h e) -> p h e", e=64), func=AF.Copy))(n, R),
                     reads=["bank%d" % (6 + n), Rk], writes=[Rk + "v"])
            for which, eng, tmp in ((0, "dve", tq), (1, "pool", tk)):
                x3 = qk32[:, which, :].rearrange("p (h e) -> p h e", e=64)
                o3 = R[:, which * D:(which + 1) * D].rearrange("p (h e) -> p h e", e=64)
                cosv = cs[b][:, 0, :].rearrange("p (h e) -> p h e", e=32)
                sinv = cs[b][:, 1, :].rearrange("p (h e) -> p h e", e=32)
                t1 = tmp[0][:].rearrange("p (h e) -> p h e", e=32)
                t2 = tmp[1][:].rearrange("p (h e) -> p h e", e=32)
                xk = "o_qk32_%d" % which
                tk_ = "o_tmp%d" % which
                rk = Rk + ("q" if which == 0 else "k")
                csk = "o_cs%d" % b
                c.op(eng, (lambda t1, x3, cosv: lambda e: e.tensor_tensor(t1, x3[:, :, 0:32], cosv, ALU.mult))(t1, x3, cosv), reads=[xk, csk], writes=[tk_ + "a"])
                c.op(eng, (lambda t2, x3, sinv: lambda e: e.tensor_tensor(t2, x3[:, :, 32:64], sinv, ALU.mult))(t2, x3, sinv), reads=[xk, csk], writes=[tk_ + "b"])
                c.op(eng, (lambda o3, t1, t2: lambda e: e.tensor_tensor(o3[:, :, 0:32], t1, t2, ALU.subtract))(o3, t1, t2), reads=[tk_ + "a", tk_ + "b"], writes=[rk])
                c.op(eng, (lambda t1, x3, cosv: lambda e: e.tensor_tensor(t1, x3[:, :, 32:64], cosv, ALU.mult))(t1, x3, cosv), reads=[xk, csk, rk], writes=[tk_ + "a"])
                c.op(eng, (lambda t2, x3, sinv: lambda e: e.tensor_tensor(t2, x3[:, :, 0:32], sinv, ALU.mult))(t2, x3, sinv), reads=[xk, csk, rk], writes=[tk_ + "b"])
                ev = c.op(eng, (lambda o3, t1, t2: lambda e: e.tensor_tensor(o3[:, :, 32:64], t1, t2, ALU.add))(o3, t1, t2), reads=[tk_ + "a", tk_ + "b"], writes=[rk])
                c.readers.setdefault("o_csr%d" % b, []).append(ev)
            c.dma("sp", (lambda i, R: lambda e: e.dma_start(out=qkv_d[i * 128:(i + 1) * 128, :], in_=R[:, :]))(i, R),
                  reads=[Rk + "q", Rk + "k", Rk + "v", Rk], cowrites=["qkv_d"])
            for sfx in ("q", "k", "v"):
                c.readers.setdefault(Rk + sfx, []).append(c.last_w["qkv_d"][-1])

    with c.phase():
        R3 = [c.sb("o_R%d" % i, [128, RW], BF16) for i in range(3)]
        KT = [c.sb("o_KT%d" % i, [128, 8, 128], BF16) for i in range(2)]
        QTz = [[c.sb("o_QT%d_%d" % (j, i), [128, 8, 128], BF16) for i in range(2)] for j in range(2)]
        for j in range(2):
            for i in range(2):
                c.op("pool", (lambda i, j: lambda e: e.memset(QTz[j][i][:], 0.0))(i, j), writes=["o_QT%d" % j])
        PT = [c.sb("o_PT%d" % i, [128, 512], BF16) for i in range(3)]
        mcur = c.sb("o_mcur", [128, 512], BF16)
        mprev = c.sb("o_mprev", [128, 512], BF16)
        osb = [c.sb("o_osb%d" % i, [128, VW], F32) for i in range(2)]
        c.dma("pool", lambda e: e.dma_start(out=mcur[:], in_=W["c_mcur"][:, :]), writes=["o_mcur"])
        c.dma("pool", lambda e: e.dma_start(out=mprev[:], in_=W["c_mprev"][:, :]), writes=["o_mprev"])
        hb_ = [(0, 7), (7, 14), (14, 16)]
        ulist = []
        for s_ in range(NSEQ if "B" in stg else 0):
            for pi, (win_, dil) in enumerate(PATTERNS):
                nb = SEQ // (128 * dil)
                for r in range(dil):
                    for n in range(nb):
                        ulist.append((s_, pi, dil, r, n))
        b0 = bank[0][:].bitcast(BF16)
        b1 = bank[1][:].bitcast(BF16)

        def views(u):
            s_, pi, dil, r, n = ulist[u]
            qv = qkv_d[s_ * SEQ:(s_ + 1) * SEQ, :].rearrange("(m d) c -> d m c", d=dil)
            ov = O_d[pi, s_ * SEQ:(s_ + 1) * SEQ, :].rearrange("(m d) c -> d m c", d=dil)
            return qv, ov

        def prologue(u):
            s_, pi, dil, r, n = ulist[u]
            qv, ov = views(u)
            R, Rk = R3[u % 3], "o_R%d" % (u % 3)
            kt, ktk = KT[u % 2], "o_KT%d" % (u % 2)
            qt, qtk = QTz[u % 2], "o_QT%d" % (u % 2)
            c.dma("sp", lambda e: e.dma_start(out=R[:, :], in_=qv[r, n * 128:(n + 1) * 128, :]), reads=["qkv_d"], writes=[Rk])
            for pr in range(8):
                c.op("pe", (lambda pr: lambda e: e.transpose(b0[:, pr * 128:(pr + 1) * 128], R[:, D + pr * 128:D + (pr + 1) * 128], g.identb[:]))(pr), reads=[Rk, "identb"], writes=["bank0"])
            c.op("act", lambda e: e.activation(out=kt[:].rearrange("p a b -> p (a b)"), in_=b0[:, 0:1024], func=AF.Copy), reads=["bank0"], writes=[ktk])
            for pr in range(8):
                c.op("pe", (lambda pr: lambda e: e.transpose(b1[:, pr * 128:(pr + 1) * 128], R[:, pr * 128:(pr + 1) * 128], g.identb[:]))(pr), reads=[Rk, "identb"], writes=["bank1"])
            c.op("dve", lambda e: e.tensor_copy(qt[0][0:64].rearrange("p a b -> p (a b)"), b1[0:64, 0:1024]), reads=["bank1"], writes=[qtk])
            c.op("dve", lambda e: e.tensor_copy(qt[1][64:128].rearrange("p a b -> p (a b)"), b1[64:128, 0:1024]), reads=["bank1", qtk], writes=[qtk])

        gcount = [0]

        def groups_of(u):
            s_, pi, dil, r, n = ulist[u]
            R, Rk = R3[u % 3], "o_R%d" % (u % 3)
            Rp, Rpk = R3[(u - 1) % 3], "o_R%d" % ((u - 1) % 3)
            kt, ktk = KT[u % 2], "o_KT%d" % (u % 2)
            ktp, ktpk = KT[(u - 1) % 2], "o_KT%d" % ((u - 1) % 2)
            qt, qtk = QTz[u % 2], "o_QT%d" % (u % 2)
            kbs = ([(ktp, ktpk, Rp, Rpk, mprev, "o_mprev")] if n > 0 else []) + [(kt, ktk, R, Rk, mcur, "o_mcur")]
            started = [False, False, False]
            out = []
            for (kT_, kTk, Rv, Rvk, msk, mskk) in kbs:
                for grp in range(4):
                    gi = gcount[0]
                    gcount[0] += 1
                    sb_, sbk = bank[2 + gi % 2], "bank%d" % (2 + gi % 2)
                    pt, ptk = PT[gi % 3], "o_PT%d" % (gi % 3)

                    def S(sb_=sb_, sbk=sbk, pt=pt, ptk=ptk, kT_=kT_, kTk=kTk, msk=msk, mskk=mskk, grp=grp):
                        for hh in range(4):
                            h = grp * 4 + hh
                            pr, hf = h // 2, h % 2
                            c.op("pe", (lambda hh, pr, hf: lambda e: e.matmul(sb_[:, hh * 128:(hh + 1) * 128], kT_[:, pr, :], qt[hf][:, pr, :], start=True, stop=True))(hh, pr, hf),
                                 reads=[kTk, qtk], writes=[sbk])
                        c.op("act", lambda e: e.activation(out=pt[:], in_=sb_[:, :], func=AF.Exp, scale=0.125), reads=[sbk], writes=[ptk])
                        c.op("dve", lambda e: e.tensor_tensor(pt[:], pt[:], msk[:], ALU.mult), reads=[ptk, mskk], writes=[ptk])

                    sts = []
                    for hh in range(4):
                        h = grp * 4 + hh
                        bi = 0 if h < 7 else (1 if h < 14 else 2)
                        sts.append(not started[bi])
                        started[bi] = True

                    def P(pt=pt, ptk=ptk, Rv=Rv, Rvk=Rvk, grp=grp, sts=sts):
                        for hh in range(4):
                            h = grp * 4 + hh
                            bi = 0 if h < 7 else (1 if h < 14 else 2)
                            col = (h - hb_[bi][0]) * 65
                            c.op("pe", (lambda bi, col, hh, h, st: lambda e: e.matmul(bank[4 + bi][:, col:col + 65], pt[:, hh * 128:(hh + 1) * 128], Rv[:, 2 * D + h * 65:2 * D + (h + 1) * 65], start=st, stop=True, skip_group_check=True))(bi, col, hh, h, sts[hh]),
                                 reads=[ptk, Rvk], writes=["bank%d" % (4 + bi)])
                    out.append((S, P))
            return out

        def epilogue(u):
            s_, pi, dil, r, n = ulist[u]
            qv, ov = views(u)
            ob, obk = osb[u % 2], "o_osb%d" % (u % 2)
            for bi, (h0, h1) in enumerate(hb_):
                eng = "act" if bi == 1 else "dve"
                c.op(eng, (lambda bi, h0, h1, eng: lambda e: ecopy(e, eng, ob[:, h0 * 65:h1 * 65], bank[4 + bi][:, 0:(h1 - h0) * 65]))(bi, h0, h1, eng),
                     reads=["bank%d" % (4 + bi)], writes=[obk + "_%d" % bi])
            c.dma("sp", lambda e: e.dma_start(out=ov[r, n * 128:(n + 1) * 128, :], in_=ob[:, :]),
                  reads=[obk + "_0", obk + "_1", obk + "_2"], cowrites=["O_d"])

        NU = len(ulist)
        if NU:
            prologue(0)
        pendingP = None
        for u in range(NU):
            grps = groups_of(u)
            ng = len(grps)
            for gi_, (S, P) in enumerate(grps):
                S()
                if pendingP is not None:
                    pendingP[0]()
                    if pendingP[1] is not None:
                        epilogue(pendingP[1])
                pendingP = (P, u if gi_ == ng - 1 else None)
                if gi_ == ng // 2 and u + 1 < NU:
                    prologue(u + 1)
        if pendingP is not None:
            pendingP[0]()
            epilogue(pendingP[1])

    with c.phase():
        wo = c.sb("o_wo", [128, 8, D], BF16)
        gam = c.sb("o_gam", [128, D], F32)
        bet = c.sb("o_bet", [128, D], F32)
        hrow = [c.sb("o_hrow%d" % i, [128, D], F32) for i in range(2)]
        ot = [[c.sb("o_ot%d_%d" % (i, p), [128, VW], F32) for p in range(3)] for i in range(2)]
        rden = c.sb("o_rden", [128, NH], F32)
        osb2 = c.sb("o_o", [128, D], BF16)
        oT = c.sb("o_oT", [128, 8, 128], BF16)
        outt = [c.sb("o_out%d" % i, [128, D], F32) for i in range(2)]
        for kc in range(8):
            c.dma("pool", (lambda kc: lambda e: e.dma_start(out=wo[:, kc, :], in_=W["att_w_o"][mi, kc * 128:(kc + 1) * 128, :], max_dma_last_dim=4096))(kc), cowrites=["wo"])
        c.dma("sp", lambda e: e.dma_start(out=gam[:], in_=W["ln_mix_g"][layer:layer + 1, :].partition_broadcast(128)), cowrites=["o_gam"])
        c.dma("sp", lambda e: e.dma_start(out=bet[:], in_=W["ln_mix_b"][layer:layer + 1, :].partition_broadcast(128)), cowrites=["o_gam"])
        def c_loads(i):
            b = i % 2
            hr, hk = hrow[b], "o_hrow%d" % b
            c.dma("sp", lambda e: e.dma_start(out=hr[:], in_=hsrc[i * 128:(i + 1) * 128, :]), writes=[hk])
            for p in range(3):
                c.dma("sp", (lambda p: lambda e: e.dma_start(out=ot[b][p][:, :], in_=O_d[p, i * 128:(i + 1) * 128, :]))(p), reads=["O_d"], writes=["o_ot%d_%d" % (b, p)])

        if "C" in stg:
            c_loads(0)
        for i in range(NT if "C" in stg else 0):
            b = i % 2
            hr, hk = hrow[b], "o_hrow%d" % b
            if i + 1 < NT:
                c_loads(i + 1)
            A = ot[b][0]
            Ak = "o_ot%d_0" % b
            c.op("pool", (lambda b, A: lambda e: e.tensor_tensor(A[:], A[:], ot[b][1][:], ALU.add))(b, A), reads=[Ak, "o_ot%d_1" % b], writes=[Ak])
            c.op("dve", (lambda b, A: lambda e: e.tensor_tensor(A[:], A[:], ot[b][2][:], ALU.add))(b, A), reads=[Ak, "o_ot%d_2" % b], writes=[Ak])
            A3 = A[:].rearrange("p (h e) -> p h e", e=65)
            c.op("dve", (lambda A3: lambda e: e.reciprocal(rden[:], A3[:, :, 64]))(A3), reads=[Ak], writes=["o_rden"])
            for h in range(NH):
                c.op("dve" if h % 2 == 0 else "pool", (lambda h, A3: lambda e: e.tensor_scalar(osb2[:, h * 64:(h + 1) * 64], A3[:, h, 0:64], rden[:, h:h + 1], None, ALU.mult))(h, A3),
                     reads=[Ak, "o_rden"], writes=["o_o%d" % (h % 2)])
            bb = bank[0][:].bitcast(BF16)
            for kc in range(8):
                c.op("pe", (lambda kc, bb: lambda e: e.transpose(bb[:, kc * 128:(kc + 1) * 128], osb2[:, kc * 128:(kc + 1) * 128], g.identb[:]))(kc, bb), reads=["o_o0", "o_o1", "identb"], writes=["bank0"])
            c.op("act", (lambda bb: lambda e: e.activation(out=oT[:].rearrange("p a b -> p (a b)"), in_=bb[:, 0:1024], func=AF.Copy))(bb), reads=["bank0"], writes=["o_oT"])
            for n in range(2):
                for kc in range(8):
                    c.op("pe", (lambda n, kc: lambda e: e.matmul(bank[6 + n][:, :], oT[:, kc, :], wo[:, kc, n * 512:(n + 1) * 512], start=(kc == 0), stop=(kc == 7)))(n, kc),
                         reads=["o_oT", "wo"], writes=["bank%d" % (6 + n)])
                c.op("dve", (lambda n, hr: lambda e: e.scalar_tensor_tensor(hr[:, n * 512:(n + 1) * 512], hr[:, n * 512:(n + 1) * 512], float(DN_ALPHA), bank[6 + n][:, :], ALU.mult, ALU.add))(n, hr),
                     reads=["bank%d" % (6 + n), hk], writes=[hk])
            layer_norm_tile(c, g, hr, hk, gam, bet, "o_gam", outt[b], "o_out%d" % b, "o")
            c.dma("sp", (lambda i, b: lambda e: e.dma_start(out=hdst[i * 128:(i + 1) * 128, :], in_=outt[b][:]))(i, b), reads=["o_out%d" % b], cowrites=["hdst"])
        if "C" not in stg:
            c.dma("sp", lambda e: e.dma_start(out=hdst[:, :], in_=hsrc[:, :]), writes=["hdst"])


NCORES = 4
FULL_SEQ = 8192
FULL_BATCH = 4
RENAME = {"expert_w_gu": "w_gu", "expert_b_gu": "b_gu", "expert_w_down": "w_dn", "expert_b_down": "b_dn"}
_CACHE = {}


def full_plan():
    plan = []
    for layer in range(DEPTH):
        plan.append(("even" if layer % 2 == 0 else "odd", layer, layer // 2))
        plan.append(("moe", layer, layer // 2))
    return plan


def kernel(**inputs):
    nseq = FULL_BATCH // NCORES
    tt = nseq * FULL_SEQ
    nl = {}
    wmap = {}
    for name, arr in inputs.items():
        if name == "x":
            continue
        kname = RENAME.get(name, name)
        wmap[kname] = np.ascontiguousarray(arr, dtype=np.float32)
        nl[kname] = arr.shape[0]
    cap = (tt * TOPK // NE) * 5 // 4
    cap = (cap + 127) // 128 * 128
    cfg = dict(SEQ=FULL_SEQ, NSEQ=nseq, C=cap, plan=full_plan(), nl=nl)
    key = (FULL_SEQ, nseq, cap)
    if key not in _CACHE:
        _CACHE[key] = build_program(cfg)
    nc, cst, _ = _CACHE[key]
    x = np.ascontiguousarray(inputs["x"], dtype=np.float32).reshape(NCORES, tt, D)
    in_maps = []
    for ci in range(NCORES):
        m = {"x": x[ci]}
        m.update(wmap)
        m.update(cst)
        in_maps.append(m)
    res = run_bass_kernel_spmd(nc, in_maps, core_ids=list(range(NCORES)))
    out = np.stack([res.results[ci]["out"] for ci in range(NCORES)], axis=0)
    return out.reshape(FULL_BATCH, FULL_SEQ, D).astype(np.float32)
```

```python
import contextlib
import numpy as np
import ml_dtypes
import concourse.bass as bass
import concourse.mybir as mybir
from concourse.bass_utils import run_bass_kernel_spmd

F32 = mybir.dt.float32
BF16 = mybir.dt.bfloat16
I32 = mybir.dt.int32
U32 = mybir.dt.uint32
AF = mybir.ActivationFunctionType
ALU = mybir.AluOpType
AX = mybir.AxisListType

D = 1024
NE = 32
TOPK = 4
DEPTH = 4
DN_ALPHA = (2 * DEPTH) ** 0.25
LN_EPS = 1e-5
NH = 16
HD = 64
PATTERNS = ((128, 1), (512, 4), (2048, 16))

SAME_ENG_WAIT = True
ENGS = ("pe", "dve", "act", "pool", "sp")
RING = {"sp": 12, "act": 6, "pool": 12}


class Ctx:
    def __init__(self, nc):
        self.nc = nc
        self.stack = contextlib.ExitStack()
        self.prog = {e: [] for e in ENGS}
        self.cnt = {e: 0 for e in ENGS}
        self.semobj = {}
        for e in ENGS:
            self.semobj[("e", e)] = self.stack.enter_context(nc.semaphore("s_" + e))
        for q, n in RING.items():
            for i in range(n):
                self.semobj[("r", q, i)] = self.stack.enter_context(nc.semaphore("r_%s%d" % (q, i)))
        self.dcnt = {q: 0 for q in RING}
        self.seen = {e: {} for e in ENGS}
        self.last_w = {}
        self.readers = {}
        self.n_instr = 0
        self.pstack = None

    def sb(self, name, shape, dt=F32):
        st = self.pstack if self.pstack is not None else self.stack
        self.uid = getattr(self, "uid", 0) + 1
        return st.enter_context(self.nc.sbuf_tensor("%s_u%d" % (name, self.uid), list(shape), dt))

    def reg(self, e, val):
        if not hasattr(self, "_regs"):
            self._regs = {}
        if val not in self._regs:
            self._regs[val] = e.to_reg(val)
        return self._regs[val]

    def ps(self, name, shape, dt=F32):
        return self.stack.enter_context(self.nc.psum_tensor(name, list(shape), dt))

    @contextlib.contextmanager
    def phase(self):
        self.barrier()
        self.pstack = contextlib.ExitStack()
        try:
            yield
        finally:
            self.barrier()
            self.pstack.close()
            self.pstack = None

    def cur_events(self):
        evs = []
        for e in ENGS:
            if self.cnt[e]:
                evs.append((("e", e), self.cnt[e]))
        for q, n in RING.items():
            i = self.dcnt[q]
            for slot in range(n):
                if i > slot:
                    last = ((i - 1 - slot) // n) * n + slot
                    evs.append((("r", q, slot), 16 * (last // n + 1)))
        return evs

    def barrier(self):
        evs = self.cur_events()
        for eng in ENGS:
            for k, v in evs:
                if self.seen[eng].get(k, 0) < v:
                    self.seen[eng][k] = v
                    self.prog[eng].append(("wait", k, v))
        self.last_w = {}
        self.readers = {}

    def _need(self, eng, reads, writes, cowrites=()):
        need = {}

        def add(ev):
            k, v = ev
            if need.get(k, 0) < v:
                need[k] = v
        for k in reads:
            for ev in self.last_w.get(k, ()):
                add(ev)
        for k in writes:
            for ev in self.last_w.get(k, ()):
                add(ev)
            for ev in self.readers.get(k, ()):
                add(ev)
        for k in cowrites:
            for ev in self.readers.get(k, ()):
                add(ev)
        for k, v in need.items():
            if k == ("e", eng) and (eng == "pe" or not SAME_ENG_WAIT):
                continue
            if self.seen[eng].get(k, 0) >= v:
                continue
            self.seen[eng][k] = v
            self.prog[eng].append(("wait", k, v))

    def _commit(self, ev, reads, writes, cowrites=()):
        for k in reads:
            self.readers.setdefault(k, []).append(ev)
        for k in writes:
            self.last_w[k] = [ev]
            self.readers[k] = []
        for k in cowrites:
            self.last_w.setdefault(k, []).append(ev)

    def op(self, eng, fn, reads=(), writes=()):
        self._need(eng, reads, writes)
        self.cnt[eng] += 1
        ev = (("e", eng), self.cnt[eng])
        self.prog[eng].append(("ins", fn, ("e", eng), 1))
        self._commit(ev, reads, writes)
        self.n_instr += 1
        return ev

    def dma(self, q, fn, reads=(), writes=(), cowrites=()):
        self._need(q, reads, writes, cowrites)
        i = self.dcnt[q]
        n = RING[q]
        slot = i % n
        if i >= n:
            k = ("r", q, slot)
            v = 16 * (i // n)
            if self.seen[q].get(k, 0) < v:
                self.seen[q][k] = v
                self.prog[q].append(("wait", k, v))
        self.dcnt[q] += 1
        ev = (("r", q, slot), 16 * (i // n + 1))
        self.prog[q].append(("ins", fn, ("r", q, slot), 16))
        self._commit(ev, reads, writes, cowrites)
        self.n_instr += 1
        return ev

    def emit(self):
        nc = self.nc
        self.barrier()
        with nc.Block() as block:
            def body(engname):
                def f(e):
                    for it in self.prog[engname]:
                        if it[0] == "wait":
                            e.wait_ge(self.semobj[it[1]], it[2])
                        else:
                            it[1](e).then_inc(self.semobj[it[2]], it[3])
                return f
            block.tensor(body("pe"))
            block.vector(body("dve"))
            block.scalar(body("act"))
            block.gpsimd(body("pool"))
            block.sync(body("sp"))
        self.stack.close()


class G:
    pass


def setup_globals(c, g, W):
    g.bank = [c.ps("bank%d" % i, [128, 512], F32) for i in range(8)]
    g.ident = c.sb("ident", [128, 128], F32)
    g.identb = c.sb("identb", [128, 128], BF16)
    g.onesrow = c.sb("onesrow", [1, 128], BF16)
    c.dma("sp", lambda e: e.dma_start(out=g.ident[:], in_=W["c_ident"][:, :]), writes=["ident"])
    c.dma("pool", lambda e: e.dma_start(out=g.identb[:], in_=W["c_ident"][:, :]), writes=["identb"])
    c.dma("pool", lambda e: e.dma_start(out=g.onesrow[:], in_=W["c_ones"][0:1, :]), writes=["onesrow"])


def ecopy(e, eng, out, in_):
    if eng == "act":
        return e.activation(out=out, in_=in_, func=AF.Copy)
    return e.tensor_copy(out, in_)


def load_bcast(c, tile, key, src_row_ap, q="sp"):
    c.dma(q, lambda e: e.dma_start(out=tile[:], in_=src_row_ap.partition_broadcast(128)), writes=[key])


def layer_norm_tile(c, g, acc, acck, gam, bet, gbk, out_tile, outk, tag):
    st = g.ln_stats
    mv = g.ln_mv
    sk = "ln_small"
    c.op("dve", lambda e: e.bn_stats(st[:, 0, :], acc[:, 0:512]), reads=[acck], writes=[sk])
    c.op("dve", lambda e: e.bn_stats(st[:, 1, :], acc[:, 512:1024]), reads=[acck, sk], writes=[sk])
    c.op("dve", lambda e: e.bn_aggr(mv[:, 0:2], st[:].rearrange("p a b -> p (a b)")), reads=[sk], writes=[sk])
    c.op("act", lambda e: e.activation(out=mv[:, 2:3], in_=mv[:, 1:2], func=AF.Sqrt, bias=g.eps_t[:, 0:1], scale=1.0),
         reads=[sk, "eps"], writes=[sk])
    c.op("dve", lambda e: e.reciprocal(mv[:, 3:4], mv[:, 2:3]), reads=[sk], writes=[sk])
    c.op("dve", lambda e: e.tensor_scalar(mv[:, 4:5], mv[:, 0:1], mv[:, 3:4], -1.0, ALU.mult, ALU.mult),
         reads=[sk], writes=[sk])
    c.op("act", lambda e: e.activation(out=acc[:], in_=acc[:], func=AF.Identity, bias=mv[:, 4:5], scale=mv[:, 3:4]),
         reads=[sk, acck], writes=[acck])
    c.op("dve", lambda e: e.tensor_tensor(acc[:], acc[:], gam[:], ALU.mult), reads=[acck, gbk], writes=[acck])
    c.op("dve", lambda e: e.tensor_tensor(out_tile[:], acc[:], bet[:], ALU.add), reads=[acck, gbk], writes=[outk])


def transpose_rows_to_T(c, g, rows, rowsk, xT, xTk, bank, bankk, nchunks=8, fp32_in=True, evac="dve"):
    if fp32_in:
        for half in range(nchunks // 4):
            for j in range(4):
                kc = half * 4 + j
                c.op("pe", (lambda kc, j: lambda e: e.transpose(bank[:, j * 128:(j + 1) * 128], rows[:, kc * 128:(kc + 1) * 128], g.ident[:]))(kc, j),
                     reads=[rowsk, "ident"], writes=[bankk])
            c.op(evac, (lambda half: lambda e: ecopy(e, evac, xT[:, half * 4:(half + 1) * 4, :].rearrange("p a b -> p (a b)"), bank[:, :]))(half),
                 reads=[bankk], writes=[xTk])
    else:
        bb = bank[:].bitcast(BF16)
        for kc in range(nchunks):
            c.op("pe", (lambda kc: lambda e: e.transpose(bb[:, kc * 128:(kc + 1) * 128], rows[:, kc * 128:(kc + 1) * 128], g.identb[:]))(kc),
                 reads=[rowsk, "identb"], writes=[bankk])
        c.op(evac, lambda e: ecopy(e, evac, xT[:].rearrange("p a b -> p (a b)"), bb[:, 0:nchunks * 128]),
             reads=[bankk], writes=[xTk])


def moe_phase(c, g, W, layer, TT, hsrc, hdst, C):
    NT = TT // 128
    NSL = C // 128
    bank = g.bank
    with c.phase():
        hrow = [c.sb("m_hrow%d" % i, [128, D], F32) for i in range(2)]
        hT = c.sb("m_hT", [128, 8, 128], F32)
        rw = c.sb("m_rw", [128, 8, NE], F32)
        rb = c.sb("m_rb", [128, NE], F32)
        ltri = c.sb("m_ltri", [128, 128], F32)
        ones = c.sb("m_ones", [128, 128], F32)
        iota = c.sb("m_iota", [128, NE], F32)
        base = c.sb("m_base", [128, NE], F32)
        logit = c.sb("m_logit", [128, NE], F32)
        top8 = c.sb("m_top8", [128, 8], F32)
        idx8 = c.sb("m_idx8", [128, 8], U32)
        idxf = c.sb("m_idxf", [128, 4], F32)
        sm = c.sb("m_sm", [128, 16], F32)
        oh = c.sb("m_oh", [128, 4, NE], F32)
        Mt = c.sb("m_M", [128, NE], F32)
        rank = c.sb("m_rank", [128, NE], F32)
        tmp = c.sb("m_tmp", [128, NE], F32)
        pos = c.sb("m_pos", [128, 8], F32)
        c.dma("sp", lambda e: e.dma_start(out=rw[:], in_=W["router_w"][layer].rearrange("(kc p) e -> p kc e", p=128)), writes=["rw"])
        load_bcast(c, rb, "rb", W["router_b"][layer:layer + 1, :])
        c.dma("sp", lambda e: e.dma_start(out=ltri[:], in_=W["c_ltri"][:, :]), writes=["ltri"])
        c.dma("sp", lambda e: e.dma_start(out=ones[:], in_=W["c_onesq"][:, :]), writes=["ones"])
        c.dma("sp", lambda e: e.dma_start(out=iota[:], in_=W["c_iota"][:, :]), writes=["iota"])
        c.op("dve", lambda e: e.memset(base[:], 0.0), writes=["base"])
        S = "m_small"
        for i in range(NT):
            hr = hrow[i % 2]
            hk = "m_hrow%d" % (i % 2)
            c.dma("sp", (lambda i, hr: lambda e: e.dma_start(out=hr[:], in_=hsrc[i * 128:(i + 1) * 128, :]))(i, hr), writes=[hk])
            transpose_rows_to_T(c, g, hr, hk, hT, "m_hT", bank[0], "bank0", fp32_in=True, evac="act")
            for kc in range(8):
                c.op("pe", (lambda kc: lambda e: e.matmul(bank[1][:, 0:NE], hT[:, kc, :], rw[:, kc, :], start=(kc == 0), stop=(kc == 7)))(kc),
                     reads=["m_hT", "rw"], writes=["bank1"])
            c.op("dve", lambda e: e.tensor_tensor(logit[:], bank[1][:, 0:NE], rb[:], ALU.add), reads=["bank1", "rb"], writes=[S])
            c.op("dve", lambda e: e.max(top8[:], logit[:]), reads=[S], writes=[S])
            c.op("dve", lambda e: e.max_index(idx8[:], top8[:], logit[:]), reads=[S], writes=[S])
            c.op("dve", lambda e: e.tensor_copy(idxf[:], idx8[:, 0:4]), reads=[S], writes=[S])
            c.op("dve", lambda e: e.tensor_scalar_mul(sm[:, 0:1], top8[:, 0:1], -1.0), reads=[S], writes=[S])
            c.op("act", lambda e: e.activation(out=sm[:, 4:8], in_=top8[:, 0:4], func=AF.Exp, bias=sm[:, 0:1], scale=1.0, accum_out=sm[:, 1:2]),
                 reads=[S], writes=[S])
            c.op("dve", lambda e: e.reciprocal(sm[:, 2:3], sm[:, 1:2]), reads=[S], writes=[S])
            c.op("dve", (lambda i: lambda e: e.tensor_scalar_mul(g.gates_all[:, i, :], sm[:, 4:8], sm[:, 2:3]))(i), reads=[S], writes=["gates_all"])
            for k in range(4):
                c.op("dve", (lambda k: lambda e: e.tensor_scalar(oh[:, k, :], iota[:], idxf[:, k:k + 1], None, ALU.is_equal))(k),
                     reads=[S, "iota"], writes=[S])
            c.op("dve", lambda e: e.tensor_tensor(Mt[:], oh[:, 0, :], oh[:, 1, :], ALU.add), reads=[S], writes=["m_M"])
            c.op("dve", lambda e: e.tensor_tensor(Mt[:], Mt[:], oh[:, 2, :], ALU.add), reads=[S, "m_M"], writes=["m_M"])
            c.op("dve", lambda e: e.tensor_tensor(Mt[:], Mt[:], oh[:, 3, :], ALU.add), reads=[S, "m_M"], writes=["m_M"])
            c.op("pe", lambda e: e.matmul(bank[2][:, 0:NE], ltri[:], Mt[:], start=True, stop=True), reads=["m_M", "ltri"], writes=["bank2"])
            c.op("pe", lambda e: e.matmul(bank[3][:, 0:NE], ones[:], Mt[:], start=True, stop=True), reads=["m_M", "ones"], writes=["bank3"])
            c.op("dve", lambda e: e.tensor_tensor(rank[:], bank[2][:, 0:NE], base[:], ALU.add), reads=["bank2", "base"], writes=[S])
            c.op("dve", lambda e: e.tensor_tensor(base[:], bank[3][:, 0:NE], base[:], ALU.add), reads=["bank3", S], writes=["base"])
            for k in range(4):
                c.op("dve", (lambda k: lambda e: e.scalar_tensor_tensor(tmp[:], oh[:, k, :], 1.0, rank[:], ALU.mult, ALU.mult, accum_out=pos[:, k:k + 1]))(k),
                     reads=[S], writes=[S])
            c.op("dve", lambda e: e.tensor_scalar(pos[:, 4:8], pos[:, 0:4], float(C), 1.0e6, ALU.is_ge, ALU.mult), reads=[S], writes=[S])
            c.op("dve", lambda e: e.tensor_tensor(pos[:, 0:4], pos[:, 0:4], pos[:, 4:8], ALU.add), reads=[S], writes=[S])
            c.op("dve", lambda e: e.scalar_tensor_tensor(pos[:, 4:8], idxf[:], float(C), pos[:, 0:4], ALU.mult, ALU.add), reads=[S], writes=[S])
            c.op("dve", (lambda i: lambda e: e.tensor_copy(g.drow_all[:, i, :], pos[:, 4:8]))(i), reads=[S], writes=["drow_all"])
            for k in range(4):
                c.dma("pool", (lambda i, k, hr: lambda e: e.indirect_dma_start(
                    out=g.Xd[:, :], out_offset=bass.IndirectOffsetOnAxis(ap=g.drow_all[:, i, k:k + 1], axis=0),
                    in_=hr[:, :], in_offset=None, bounds_check=c.reg(e, NE * C - 1), oob_is_err=False))(i, k, hr),
                    reads=[hk, "drow_all"], cowrites=["Xd"])

    with c.phase():
        wgu = [c.sb("m_wgu%d" % i, [128, 8, 2048], BF16) for i in range(2)]
        wdn = [c.sb("m_wdn%d" % i, [128, 8, 1024], BF16) for i in range(2)]
        bgu = [c.sb("m_bgu%d" % i, [1, 2048], BF16) for i in range(2)]
        bdn = [c.sb("m_bdn%d" % i, [1, 1024], BF16) for i in range(2)]
        xrow = [c.sb("m_xrow%d" % i, [128, D], BF16) for i in range(3)]
        xT = [c.sb("m_xT%d" % i, [128, 8, 128], BF16) for i in range(2)]
        gt = [c.sb("m_g", [128, 1024], F32)] * 2
        sg = [c.sb("m_sg", [128, 1024], F32)] * 2
        lin = [c.sb("m_lin", [128, 1024], F32)] * 2
        act = [c.sb("m_act%d" % i, [128, 1024], BF16) for i in range(2)]
        actT = [c.sb("m_actT%d" % i, [128, 8, 128], BF16) for i in range(2)]
        yt = [c.sb("m_y%d" % i, [128, 1024], F32) for i in range(2)]

        def load_w(ex):
            p = ex % 2
            wg_src = W["w_gu"][layer, ex].rearrange("(kc p) n -> p kc n", p=128)
            wd_src = W["w_dn"][layer, ex].rearrange("(kc p) n -> p kc n", p=128)
            for kc in range(8):
                c.dma("pool", (lambda kc: lambda e: e.dma_start(out=wgu[p][:, kc, :], in_=wg_src[:, kc, :], max_dma_last_dim=8192))(kc),
                      cowrites=["wgu%d" % p], reads=[], writes=[])
            for kc in range(8):
                c.dma("pool", (lambda kc: lambda e: e.dma_start(out=wdn[p][:, kc, :], in_=wd_src[:, kc, :], max_dma_last_dim=4096))(kc),
                      cowrites=["wdn%d" % p])
            c.dma("pool", lambda e: e.dma_start(out=bgu[p][:], in_=W["b_gu"][layer, ex:ex + 1, :], max_dma_last_dim=8192), cowrites=["wgu%d" % p])
            c.dma("pool", lambda e: e.dma_start(out=bdn[p][:], in_=W["b_dn"][layer, ex:ex + 1, :], max_dma_last_dim=4096), cowrites=["wdn%d" % p])

        units = [(ex, i) for ex in range(NE) for i in range(NSL)]

        def st_load(u):
            ex, i = units[u]
            b3 = u % 3
            r0 = ex * C + i * 128
            c.dma("sp", lambda e: e.dma_start(out=xrow[b3][:], in_=g.Xd[r0:r0 + 128, :]), reads=["Xd"], writes=["xrow%d" % b3])

        def st_tx(u):
            b = u % 2
            b3 = u % 3
            transpose_rows_to_T(c, g, xrow[b3], "xrow%d" % b3, xT[b], "xT%d" % b, bank[0], "bank0", fp32_in=False, evac="dve")

        def st_gu(u):
            ex, i = units[u]
            b = u % 2
            p = ex % 2
            for n in range(4):
                for kc in range(8):
                    c.op("pe", (lambda n, kc: lambda e: e.matmul(bank[2 + n][:, :], xT[b][:, kc, :], wgu[p][:, kc, n * 512:(n + 1) * 512], start=(kc == 0), stop=False))(n, kc),
                         reads=["xT%d" % b, "wgu%d" % p], writes=["gu%d" % n])
                c.op("pe", (lambda n: lambda e: e.matmul(bank[2 + n][:, :], g.onesrow[0:1, :], bgu[p][0:1, n * 512:(n + 1) * 512], start=False, stop=True))(n),
                     reads=["onesrow", "wgu%d" % p], writes=["gu%d" % n])
            sls = [slice(n * 512, (n + 1) * 512) for n in range(2)]
            for n in range(2):
                sl = sls[n]
                c.op("dve", (lambda n, sl: lambda e: e.tensor_scalar_min(gt[b][:, sl], bank[2 + n][:, :], 7.0))(n, sl), reads=["gu%d" % n], writes=["g_%d" % n])
                c.op("act", (lambda sl: lambda e: e.activation(out=sg[b][:, sl], in_=gt[b][:, sl], func=AF.Sigmoid, scale=1.702))(sl), reads=["g_%d" % n], writes=["sg_%d" % n])
            for n in range(2):
                sl = sls[n]
                c.op("dve", (lambda n, sl: lambda e: e.tensor_scalar(lin[b][:, sl], bank[4 + n][:, :], 7.0, -7.0, ALU.min, ALU.max))(n, sl), reads=["gu%d" % (2 + n)], writes=["lin_%d" % n])
            for n in range(2):
                sl = sls[n]
                c.op("dve", (lambda sl: lambda e: e.scalar_tensor_tensor(lin[b][:, sl], lin[b][:, sl], 1.0, gt[b][:, sl], ALU.add, ALU.mult))(sl),
                     reads=["lin_%d" % n, "g_%d" % n], writes=["lin_%d" % n])
            for n in range(2):
                sl = sls[n]
                c.op("dve", (lambda sl: lambda e: e.tensor_tensor(act[b][:, sl], lin[b][:, sl], sg[b][:, sl], ALU.mult))(sl),
                     reads=["lin_%d" % n, "sg_%d" % n], writes=["act%d_%d" % (b, n)])

        def st_tact(u):
            b = u % 2
            bb = bank[1][:].bitcast(BF16)
            for kc in range(8):
                c.op("pe", (lambda kc: lambda e: e.transpose(bb[:, kc * 128:(kc + 1) * 128], act[b][:, kc * 128:(kc + 1) * 128], g.identb[:]))(kc),
                     reads=["act%d_%d" % (b, kc // 4), "identb"], writes=["bank1"])
            c.op("act", lambda e: e.activation(out=actT[b][:].rearrange("p a b -> p (a b)"), in_=bb[:, 0:1024], func=AF.Copy), reads=["bank1"], writes=["actT%d" % b])

        def st_down(u):
            ex, i = units[u]
            b = u % 2
            p = ex % 2
            for n in range(2):
                for kc in range(8):
                    c.op("pe", (lambda n, kc: lambda e: e.matmul(bank[6 + n][:, :], actT[b][:, kc, :], wdn[p][:, kc, n * 512:(n + 1) * 512], start=(kc == 0), stop=False))(n, kc),
                         reads=["actT%d" % b, "wdn%d" % p], writes=["dn%d" % n])
                c.op("pe", (lambda n: lambda e: e.matmul(bank[6 + n][:, :], g.onesrow[0:1, :], bdn[p][0:1, n * 512:(n + 1) * 512], start=False, stop=True))(n),
                     reads=["onesrow", "wdn%d" % p], writes=["dn%d" % n])
                c.op("act", (lambda n: lambda e: e.activation(out=yt[b][:, n * 512:(n + 1) * 512], in_=bank[6 + n][:, :], func=AF.Copy))(n),
                     reads=["dn%d" % n], writes=["y%d" % b])
            r0 = ex * C + i * 128
            c.dma("sp", lambda e: e.dma_start(out=g.Yb[r0:r0 + 128, :], in_=yt[b][:]), reads=["y%d" % b], cowrites=["Yb"])

        NU = len(units)
        load_w(0)
        st_load(0)
        if NU > 1:
            st_load(1)
        st_tx(0)
        for u in range(NU):
            ex, i = units[u]
            if u + 2 < NU:
                st_load(u + 2)
            if u >= 1:
                st_tact(u - 1)
            if u + 1 < NU:
                st_tx(u + 1)
            st_gu(u)
            if u >= 1:
                st_down(u - 1)
            if i == 0 and ex + 1 < NE:
                load_w(ex + 1)
        st_tact(NU - 1)
        st_down(NU - 1)

    with c.phase():
        hrow = [c.sb("c_hrow%d" % i, [128, D], F32) for i in range(2)]
        yk = [[c.sb("c_y%d_%d" % (i, k), [128, D], F32) for k in range(4)] for i in range(2)]
        outt = [c.sb("c_out%d" % i, [128, D], F32) for i in range(2)]
        gam = c.sb("c_gam", [128, D], F32)
        bet = c.sb("c_bet", [128, D], F32)
        load_bcast(c, gam, "c_gb", W["ln_ffn_g"][layer:layer + 1, :])
        c.dma("sp", lambda e: e.dma_start(out=bet[:], in_=W["ln_ffn_b"][layer:layer + 1, :].partition_broadcast(128)), cowrites=["c_gb"])
        def c_loads(i):
            b = i % 2
            hr = hrow[b]
            hk = "c_hrow%d" % b
            c.dma("sp", (lambda i, hr: lambda e: e.dma_start(out=hr[:], in_=hsrc[i * 128:(i + 1) * 128, :]))(i, hr), writes=[hk])
            for k in range(4):
                c.dma("pool", (lambda i, k, b: lambda e: e.indirect_dma_start(
                    out=yk[b][k][:, :], out_offset=None, in_=g.Yb[:, :],
                    in_offset=bass.IndirectOffsetOnAxis(ap=g.drow_all[:, i, k:k + 1], axis=0),
                    bounds_check=c.reg(e, NE * C - 1), oob_is_err=False))(i, k, b),
                    reads=["Yb", "drow_all"], writes=["c_y%d_%d" % (b, k)])

        c_loads(0)
        for i in range(NT):
            b = i % 2
            hr = hrow[b]
            hk = "c_hrow%d" % b
            if i + 1 < NT:
                c_loads(i + 1)
            c.op("act", (lambda hr: lambda e: e.mul(hr[:], hr[:], float(DN_ALPHA)))(hr), reads=[hk], writes=[hk])
            for k in range(4):
                c.op("dve", (lambda i, k, b, hr: lambda e: e.scalar_tensor_tensor(hr[:], yk[b][k][:], g.gates_all[:, i, k:k + 1], hr[:], ALU.mult, ALU.add))(i, k, b, hr),
                     reads=[hk, "c_y%d_%d" % (b, k), "gates_all"], writes=[hk])
            layer_norm_tile(c, g, hr, hk, gam, bet, "c_gb", outt[b], "c_out%d" % b, "c")
            c.dma("sp", (lambda i, b: lambda e: e.dma_start(out=hdst[i * 128:(i + 1) * 128, :], in_=outt[b][:]))(i, b),
                  reads=["c_out%d" % b], cowrites=["hdst"])


def host_constants(SEQ):
    cst = {}
    cst["c_ident"] = np.eye(128, dtype=np.float32)
    cst["c_ones"] = np.ones((1, 128), np.float32)
    cst["c_onesq"] = np.ones((128, 128), np.float32)
    k = np.arange(128)
    cst["c_ltri"] = (k[:, None] < k[None, :]).astype(np.float32)
    cst["c_iota"] = np.tile(np.arange(NE, dtype=np.float32)[None, :], (128, 1))
    mc = (k[:, None] <= k[None, :]).astype(np.float32)
    mp = (k[:, None] >= k[None, :]).astype(np.float32)
    cst["c_mcur"] = np.tile(mc, (1, 4)).astype(np.float32)
    cst["c_mprev"] = np.tile(mp, (1, 4)).astype(np.float32)
    par = np.zeros((32, 2), np.float32)
    par[0::2, 0] = 1.0
    par[1::2, 1] = 1.0
    cst["c_par"] = par
    cst["c_iotaT"] = np.tile(np.arange(TB + 1, dtype=np.float32)[None, :], (128, 1))
    half = HD // 2
    inv = (10000.0 ** (-np.arange(half, dtype=np.float32) / half)).astype(np.float32)
    ang = (np.arange(SEQ, dtype=np.float32)[:, None] * inv[None, :]).astype(np.float32)
    cst["c_cos16"] = np.tile(np.cos(ang).astype(np.float32), (1, NH))
    cst["c_sin16"] = np.tile(np.sin(ang).astype(np.float32), (1, NH))
    return cst


WEIGHT_SHAPES = {
    "hy_w_in": (D, 2048), "conv_w": (3, 512), "ssm_a_re": (32, 64), "ssm_a_im": (32, 64), "ssm_log_dt": (32,),
    "ssm_b_re": (32, 64, 16), "ssm_b_im": (32, 64, 16), "ssm_c_re": (32, 16, 64), "ssm_c_im": (32, 16, 64),
    "ssm_d": (32, 16), "ssm_w_glu": (512, 512), "ssm_b_glu": (512,), "hy_w_out": (D, D),
    "att_w_qkv": (D, 3 * D), "att_w_o": (D, D),
    "ln_mix_g": (D,), "ln_mix_b": (D,), "ln_ffn_g": (D,), "ln_ffn_b": (D,),
    "router_w": (D, NE), "router_b": (NE,), "w_gu": (NE, D, 2 * D), "b_gu": (NE, 2 * D),
    "w_dn": (NE, D, D), "b_dn": (NE, D),
}


def build_program(cfg):
    SEQ, NSEQ, C = cfg["SEQ"], cfg["NSEQ"], cfg["C"]
    TT = SEQ * NSEQ
    nc = bass.Bass("TRN2", target_bir_lowering=False)
    W = {}
    W["x"] = nc.dram_tensor("x", [TT, D], F32, kind="ExternalInput").ap()
    for name, shp in WEIGHT_SHAPES.items():
        n0 = cfg["nl"].get(name, 0)
        if n0 == 0:
            continue
        W[name] = nc.dram_tensor(name, [n0] + list(shp), F32, kind="ExternalInput").ap()
    cst = host_constants(SEQ)
    for name, arr in cst.items():
        W[name] = nc.dram_tensor(name, list(arr.shape), F32, kind="ExternalInput").ap()
    out = nc.dram_tensor("out", [TT, D], F32, kind="ExternalOutput").ap()
    c = Ctx(nc)
    g = G()
    setup_globals(c, g, W)
    NT = TT // 128
    g.gates_all = c.sb("gates_all", [128, NT, 4], F32)
    g.drow_all = c.sb("drow_all", [128, NT, 4], U32)
    g.ln_stats = c.sb("ln_stats", [128, 2, 6], F32)
    g.ln_mv = c.sb("ln_mv", [128, 8], F32)
    g.eps_t = c.sb("eps_t", [128, 1], F32)
    c.op("dve", lambda e: e.memset(g.eps_t[:], LN_EPS), writes=["eps"])
    g.rho = c.sb("ssm_rho", [128, 16], F32)
    g.ctab = c.sb("ssm_ctab", [128, 16, TB + 1], F32)
    g.stab = c.sb("ssm_stab", [128, 16, TB + 1], F32)
    g.Pr = c.sb("ssm_Pr", [128, 16, NRND], F32)
    g.Pi = c.sb("ssm_Pi", [128, 16, NRND], F32)
    g.nPi = c.sb("ssm_nPi", [128, 16, NRND], F32)
    g.lBre = c.sb("ssm_lBre", [128, 16, 128], BF16)
    g.lBim = c.sb("ssm_lBim", [128, 16, 128], BF16)
    g.lCre = c.sb("ssm_lCre", [128, 16, 128], BF16)
    g.lCim = c.sb("ssm_lCim", [128, 16, 128], BF16)
    g.Xd = nc.dram_tensor("Xd", [NE * C, D], BF16).ap()
    g.Yb = nc.dram_tensor("Yb", [NE * C, D], F32).ap()
    hb = [nc.dram_tensor("hbuf%d" % i, [TT, D], F32).ap() for i in range(2)]
    g.qkv_d = nc.dram_tensor("qkv_d", [TT, RW], BF16).ap()
    g.O_d = nc.dram_tensor("O_d", [3, TT, VW], F32).ap()
    g.cfg = cfg
    cur = W["x"]
    plan = cfg["plan"]
    for pi, (kind, layer, mi) in enumerate(plan):
        last = pi == len(plan) - 1
        dst = out if last else hb[pi % 2]
        if kind == "moe":
            moe_phase(c, g, W, layer, TT, cur, dst, C)
        elif kind == "even":
            even_phase(c, g, W, layer, mi, SEQ, NSEQ, cur, dst)
        elif kind == "odd":
            odd_phase(c, g, W, layer, mi, SEQ, NSEQ, cur, dst)
        cur = dst
    c.emit()
    return nc, cst, c


TB = 256
NRND = 1
TWO_PI = 6.283185307179586


def sin_reduced(c, out, src, shift, tmpf, tmpi, S):
    c.op("dve", lambda e: e.tensor_scalar(tmpf[:, 0, :], src, float(shift), 1.0 / TWO_PI, ALU.add, ALU.mult), reads=[S], writes=[S])
    c.op("dve", lambda e: e.tensor_copy(tmpi[:], tmpf[:, 0, :]), reads=[S], writes=[S])
    c.op("dve", lambda e: e.tensor_copy(tmpf[:, 1, :], tmpi[:]), reads=[S], writes=[S])
    c.op("dve", lambda e: e.tensor_scalar(tmpf[:, 2, :], src, float(shift), None, ALU.add), reads=[S], writes=[S])
    c.op("dve", lambda e: e.scalar_tensor_tensor(tmpf[:, 2, :], tmpf[:, 1, :], -TWO_PI, tmpf[:, 2, :], ALU.mult, ALU.add), reads=[S], writes=[S])
    c.op("dve", lambda e: e.tensor_scalar(tmpf[:, 3, :], tmpf[:, 2, :], float(np.pi), -TWO_PI, ALU.is_gt, ALU.mult), reads=[S], writes=[S])
    c.op("dve", lambda e: e.tensor_tensor(tmpf[:, 2, :], tmpf[:, 2, :], tmpf[:, 3, :], ALU.add), reads=[S], writes=[S])
    c.op("dve", lambda e: e.tensor_scalar(tmpf[:, 3, :], tmpf[:, 2, :], float(-np.pi), TWO_PI, ALU.is_lt, ALU.mult), reads=[S], writes=[S])
    c.op("dve", lambda e: e.tensor_tensor(tmpf[:, 2, :], tmpf[:, 2, :], tmpf[:, 3, :], ALU.add), reads=[S], writes=[S])
    c.op("act", lambda e: e.activation(out=out, in_=tmpf[:, 2, :], func=AF.Sin), reads=[S], writes=[S])


def even_setup(c, g, W, mi):
    bank = g.bank
    S = "es"
    if g.cfg.get("dbg") == "none":
        return
    with c.phase():
        nat = c.sb("es_nat", [32, 3, 64], F32)
        natm = c.sb("es_natm", [32, 3, 128], F32)
        par = c.sb("es_par", [32, 2], F32)
        ldt = c.sb("es_ldt", [32, 1], F32)
        q3 = c.sb("es_q3", [128, 3, 16], F32)
        tf = c.sb("es_tf", [128, 4, 16], F32)
        ti = c.sb("es_ti", [128, 16], I32)
        pt = c.sb("es_pt", [128, 32], F32)
        w = c.sb("es_w", [128, 12, 16], F32)
        Bn = c.sb("es_Bn", [128, 2, 16, 16], F32)
        Bf = c.sb("es_Bf", [128, 2, 16, 128], F32)
        Cf = c.sb("es_Cf", [128, 2, 16, 128], F32)
        c.dma("sp", lambda e: e.dma_start(out=nat[:, 0, :], in_=W["ssm_a_re"][mi]), cowrites=["es_nat"])
        c.dma("sp", lambda e: e.dma_start(out=nat[:, 1, :], in_=W["ssm_a_im"][mi]), cowrites=["es_nat"])
        c.dma("sp", lambda e: e.dma_start(out=ldt[:], in_=W["ssm_log_dt"][mi].rearrange("(g o) -> g o", o=1)), cowrites=["es_nat"])
        c.dma("sp", lambda e: e.dma_start(out=par[:], in_=W["c_par"][:, :]), cowrites=["es_nat"])
        for ri, nm in enumerate(("ssm_b_re", "ssm_b_im")):
            for gg in range(2):
                c.dma("sp", (lambda ri, nm, gg: lambda e: e.dma_start(out=Bn[gg * 64:(gg + 1) * 64, ri, :, :], in_=W[nm][mi].rearrange("(j gg) p h -> gg p j h", gg=2)[gg]))(ri, nm, gg), cowrites=["es_Bn"])
        c.op("dve", lambda e: e.memset(Bf[:], 0.0), writes=["es_Bf"])
        c.op("pool", lambda e: e.memset(Cf[:], 0.0), writes=["es_Cf"])
        for ri, nm in enumerate(("ssm_c_re", "ssm_c_im")):
            for j in range(16):
                for gg in range(2):
                    q = j % 4
                    p0 = 32 * q + 16 * gg
                    c.dma("sp", (lambda ri, nm, j, gg, p0: lambda e: e.dma_start(out=Cf[p0:p0 + 16, ri, j, gg * 64:(gg + 1) * 64], in_=W[nm][mi, 2 * j + gg]))(ri, nm, j, gg, p0),
                          reads=["es_Cf"], cowrites=["es_Cf2"])
        c.op("act", lambda e: e.activation(out=ldt[:], in_=ldt[:], func=AF.Exp), reads=["es_nat"], writes=[S])
        c.op("dve", lambda e: e.tensor_scalar_min(nat[:, 0, :], nat[:, 0, :], -1e-4), reads=["es_nat", S], writes=[S])
        c.op("dve", lambda e: e.memset(nat[:, 2, :], 1.0), reads=[S], writes=[S])
        c.op("dve", lambda e: e.tensor_scalar_mul(nat[:, 2, :], nat[:, 2, :], ldt[:, 0:1]), reads=[S], writes=[S])
        for m in range(3):
            c.op("dve", (lambda m: lambda e: e.tensor_scalar_mul(natm[:, m, 0:64], nat[:, m, :], par[:, 0:1]))(m), reads=[S], writes=[S])
            c.op("dve", (lambda m: lambda e: e.tensor_scalar_mul(natm[:, m, 64:128], nat[:, m, :], par[:, 1:2]))(m), reads=[S], writes=[S])
            c.op("pe", (lambda m: lambda e: e.transpose(bank[0][:, 0:32], natm[:, m, :], g.ident[0:32, 0:32]))(m), reads=[S, "ident"], writes=["bank0"])
            c.op("dve", lambda e: e.tensor_copy(pt[:], bank[0][:, 0:32]), reads=["bank0", S], writes=[S])
            c.op("dve", (lambda m: lambda e: e.tensor_tensor(q3[:, m, :], pt[:].rearrange("p (j gg) -> p j gg", gg=2)[:, :, 0], pt[:].rearrange("p (j gg) -> p j gg", gg=2)[:, :, 1], ALU.add))(m), reads=[S], writes=[S])
        LR, LI, DT = q3[:, 0, :], q3[:, 1, :], q3[:, 2, :]
        c.op("dve", lambda e: e.tensor_tensor(w[:, 0, :], LR, DT, ALU.mult), reads=[S], writes=[S])
        c.op("act", lambda e: e.activation(out=w[:, 0, :], in_=w[:, 0, :], func=AF.Exp), reads=[S], writes=[S])
        c.op("dve", lambda e: e.tensor_tensor(w[:, 1, :], LI, DT, ALU.mult), reads=[S], writes=[S])
        sin_reduced(c, w[:, 3, :], w[:, 1, :], 0.0, tf, ti, S)
        sin_reduced(c, w[:, 2, :], w[:, 1, :], np.pi / 2, tf, ti, S)
        c.op("dve", lambda e: e.tensor_tensor(w[:, 4, :], w[:, 0, :], w[:, 2, :], ALU.mult), reads=[S], writes=[S])
        c.op("dve", lambda e: e.tensor_tensor(w[:, 5, :], w[:, 0, :], w[:, 3, :], ALU.mult), reads=[S], writes=[S])
        c.op("dve", lambda e: e.tensor_scalar_add(w[:, 6, :], w[:, 4, :], -1.0), reads=[S], writes=[S])
        c.op("dve", lambda e: e.tensor_tensor(w[:, 7, :], LR, LR, ALU.mult), reads=[S], writes=[S])
        c.op("dve", lambda e: e.tensor_tensor(w[:, 10, :], LI, LI, ALU.mult), reads=[S], writes=[S])
        c.op("dve", lambda e: e.tensor_tensor(w[:, 7, :], w[:, 7, :], w[:, 10, :], ALU.add), reads=[S], writes=[S])
        c.op("dve", lambda e: e.reciprocal(w[:, 7, :], w[:, 7, :]), reads=[S], writes=[S])
        c.op("dve", lambda e: e.tensor_tensor(w[:, 8, :], w[:, 6, :], LR, ALU.mult), reads=[S], writes=[S])
        c.op("dve", lambda e: e.tensor_tensor(w[:, 10, :], w[:, 5, :], LI, ALU.mult), reads=[S], writes=[S])
        c.op("dve", lambda e: e.tensor_tensor(w[:, 8, :], w[:, 8, :], w[:, 10, :], ALU.add), reads=[S], writes=[S])
        c.op("dve", lambda e: e.tensor_tensor(w[:, 8, :], w[:, 8, :], w[:, 7, :], ALU.mult), reads=[S], writes=[S])
        c.op("dve", lambda e: e.tensor_tensor(w[:, 9, :], w[:, 5, :], LR, ALU.mult), reads=[S], writes=[S])
        c.op("dve", lambda e: e.tensor_tensor(w[:, 10, :], w[:, 6, :], LI, ALU.mult), reads=[S], writes=[S])
        c.op("dve", lambda e: e.tensor_tensor(w[:, 9, :], w[:, 9, :], w[:, 10, :], ALU.subtract), reads=[S], writes=[S])
        c.op("dve", lambda e: e.tensor_tensor(w[:, 9, :], w[:, 9, :], w[:, 7, :], ALU.mult), reads=[S], writes=[S])
        c.op("dve", lambda e: e.tensor_scalar_mul(w[:, 11, :], w[:, 9, :], -1.0), reads=[S], writes=[S])
        c.op("dve", lambda e: e.tensor_copy(g.rho[:], w[:, 0, :]), reads=[S], writes=["P"])
        io = c.sb("es_io", [128, TB + 1], F32)
        ang = c.sb("es_ang", [128, TB + 1], F32)
        tf2 = c.sb("es_tf2", [128, 4, TB + 1], F32)
        ti2 = c.sb("es_ti2", [128, TB + 1], I32)
        c.dma("sp", lambda e: e.dma_start(out=io[:], in_=W["c_iotaT"][:, :]), writes=["es_io"])
        for j in range(16):
            c.op("dve", (lambda j: lambda e: e.tensor_scalar_mul(ang[:], io[:], w[:, 1, j:j + 1]))(j), reads=[S, "es_io"], writes=[S])
            sin_reduced(c, g.stab[:, j, :], ang[:], 0.0, tf2, ti2, S)
            sin_reduced(c, g.ctab[:, j, :], ang[:], np.pi / 2, tf2, ti2, S)
        for j in range(16):
            q = j % 4
            for gg in range(2):
                ps_ = slice(gg * 64, (gg + 1) * 64)
                cs = slice(32 * q + 16 * gg, 32 * q + 16 * gg + 16)
                cr, ci, nci = w[ps_, 8, j:j + 1], w[ps_, 9, j:j + 1], w[ps_, 11, j:j + 1]
                c.op("dve", (lambda j, ps_, cs, cr: lambda e: e.tensor_scalar_mul(Bf[ps_, 0, j, cs], Bn[ps_, 0, j, :], cr))(j, ps_, cs, cr), reads=[S, "es_Bn", "es_Bf"], writes=["es_Bf"])
                c.op("dve", (lambda j, ps_, cs, nci: lambda e: e.scalar_tensor_tensor(Bf[ps_, 0, j, cs], Bn[ps_, 1, j, :], nci, Bf[ps_, 0, j, cs], ALU.mult, ALU.add))(j, ps_, cs, nci), reads=[S, "es_Bn", "es_Bf"], writes=["es_Bf"])
                c.op("dve", (lambda j, ps_, cs, cr: lambda e: e.tensor_scalar_mul(Bf[ps_, 1, j, cs], Bn[ps_, 1, j, :], cr))(j, ps_, cs, cr), reads=[S, "es_Bn", "es_Bf"], writes=["es_Bf"])
                c.op("dve", (lambda j, ps_, cs, ci: lambda e: e.scalar_tensor_tensor(Bf[ps_, 1, j, cs], Bn[ps_, 0, j, :], ci, Bf[ps_, 1, j, cs], ALU.mult, ALU.add))(j, ps_, cs, ci), reads=[S, "es_Bn", "es_Bf"], writes=["es_Bf"])
        for j in range(16):
            for ri, (src, srck, dst, scale) in enumerate(((Bf, "es_Bf", g.lBre, 1.0), (Bf, "es_Bf", g.lBim, 1.0), (Cf, "es_Cf2", g.lCre, 1.0), (Cf, "es_Cf2", g.lCim, -1.0))):
                r = ri % 2
                bk = bank[1 + (ri % 2)]
                bkk = "bank%d" % (1 + (ri % 2))
                c.op("pe", (lambda src, r, j, bk: lambda e: e.transpose(bk[:, 0:128], src[:, r, j, :], g.ident[:]))(src, r, j, bk), reads=[srck, "es_Cf", "ident"], writes=[bkk])
                c.op("act", (lambda dst, j, bk, scale: lambda e: e.activation(out=dst[:, j, :], in_=bk[:, 0:128], func=AF.Copy, scale=scale))(dst, j, bk, scale), reads=[bkk], writes=["lBC"])


def even_phase(c, g, W, layer, mi, SEQ, NSEQ, hsrc, hdst):
    bank = g.bank
    dbg = g.cfg.get("dbg")
    if dbg != "nosetup":
        even_setup(c, g, W, mi)
    if dbg in ("setup", "none"):
        c.dma("sp", lambda e: e.dma_start(out=hdst[:, :], in_=hsrc[:, :]), writes=["hdst"])
        return
    NB = SEQ // TB
    with c.phase():
        win = c.sb("e_win", [128, 8, 2048], BF16)
        wout = c.sb("e_wout", [128, 8, 1024], BF16)
        wglu = c.sb("e_wglu", [128, 4, 512], BF16)
        bglu = c.sb("e_bglu", [128, 4], F32)
        dsk = c.sb("e_dsk", [128, 4], F32)
        cw = c.sb("e_cw", [128, 4, 3], F32)
        gam = c.sb("e_gam", [128, D], F32)
        bet = c.sb("e_bet", [128, D], F32)
        NTB = TB // 128
        hrowA = [c.sb("e_hrowA%d" % i, [128, D], F32) for i in range(NTB)]
        hrowF = [c.sb("e_hrowF%d" % i, [128, D], F32) for i in range(NTB)]
        hT = c.sb("e_hT", [128, 8, TB], BF16)
        gb = c.sb("e_gb", [128, 4, TB], F32)
        vb = c.sb("e_vb", [128, 4, TB + 2], F32)
        gct = c.sb("e_gct", [128, TB], F32)
        uT = c.sb("e_uT", [128, 4, TB], BF16)
        u32 = c.sb("e_u32", [128, 4, TB], F32)
        sbr = [c.sb("e_sbr%d" % b, [128, TB], F32) for b in range(2)]
        sbi = [c.sb("e_sbi%d" % b, [128, TB], F32) for b in range(2)]
        st1 = [c.sb("e_st1%d" % b, [128, TB], F32) for b in range(2)]
        st2 = [c.sb("e_st2%d" % b, [128, TB], F32) for b in range(2)]
        st3 = [c.sb("e_st3%d" % b, [128, TB], F32) for b in range(2)]
        st4 = [c.sb("e_st4%d" % b, [128, TB], F32) for b in range(2)]
        sp3 = [c.sb("e_sp3%d" % b, [128, TB], F32) for b in range(2)]
        sp4 = [c.sb("e_sp4%d" % b, [128, TB], F32) for b in range(2)]
        svr = [c.sb("e_svr%d" % b, [128, TB], F32) for b in range(2)]
        svi = [c.sb("e_svi%d" % b, [128, TB], F32) for b in range(2)]
        xbf = [[c.sb("e_xbf%d%d" % (a, b), [128, TB], BF16) for b in range(2)] for a in range(2)]
        Xst = c.sb("e_Xst", [128, 16, 2], F32)
        Xtmp = c.sb("e_Xtmp", [128, 16, 2], F32)
        ycT = c.sb("e_ycT", [128, 8, TB], BF16)
        yt = c.sb("e_yt", [128, TB], F32)
        tt_ = c.sb("e_tt", [128, TB], F32)
        z32 = c.sb("e_z32", [128, 4, TB], F32)
        zT = c.sb("e_zT", [128, 4, TB], BF16)
        outt = [c.sb("e_out%d" % i, [128, D], F32) for i in range(2)]
        for kc in range(8):
            c.dma("pool", (lambda kc: lambda e: e.dma_start(out=win[:, kc, :], in_=W["hy_w_in"][mi, kc * 128:(kc + 1) * 128, :], max_dma_last_dim=8192))(kc), cowrites=["win"])
            c.dma("pool", (lambda kc: lambda e: e.dma_start(out=wout[:, kc, :], in_=W["hy_w_out"][mi, kc * 128:(kc + 1) * 128, :], max_dma_last_dim=4096))(kc), cowrites=["wout"])
        for kc in range(4):
            c.dma("pool", (lambda kc: lambda e: e.dma_start(out=wglu[:, kc, :], in_=W["ssm_w_glu"][mi, kc * 128:(kc + 1) * 128, :], max_dma_last_dim=2048))(kc), cowrites=["wglu"])
            c.dma("sp", (lambda kc: lambda e: e.dma_start(out=bglu[:, kc:kc + 1], in_=W["ssm_b_glu"][mi, kc * 128:(kc + 1) * 128].rearrange("(p o) -> p o", o=1)))(kc), cowrites=["small"])
            c.dma("sp", (lambda kc: lambda e: e.dma_start(out=dsk[:, kc:kc + 1], in_=W["ssm_d"][mi].rearrange("g h -> (g h)")[kc * 128:(kc + 1) * 128].rearrange("(p o) -> p o", o=1)))(kc), cowrites=["small"])
            for k in range(3):
                c.dma("sp", (lambda kc, k: lambda e: e.dma_start(out=cw[:, kc, k:k + 1], in_=W["conv_w"][mi, k, kc * 128:(kc + 1) * 128].rearrange("(p o) -> p o", o=1)))(kc, k), cowrites=["small"])
        c.dma("sp", lambda e: e.dma_start(out=gam[:], in_=W["ln_mix_g"][layer:layer + 1, :].partition_broadcast(128)), cowrites=["e_gam"])
        c.dma("sp", lambda e: e.dma_start(out=bet[:], in_=W["ln_mix_b"][layer:layer + 1, :].partition_broadcast(128)), cowrites=["e_gam"])
        blocks = [(s, tb) for s in range(NSEQ) for tb in range(NB)]

        def e_loads(bi_, bufs, pfx):
            s_, tb_ = blocks[bi_]
            t0_ = s_ * SEQ + tb_ * TB
            for tt in range(NTB):
                c.dma("sp", (lambda tt: lambda e: e.dma_start(out=bufs[tt][:], in_=hsrc[t0_ + tt * 128:t0_ + (tt + 1) * 128, :]))(tt), writes=["%s%d" % (pfx, tt)])

        e_loads(0, hrowA, "e_hrowA")
        for bi_, (s, tb) in enumerate(blocks):
            if True:
                t0 = s * SEQ + tb * TB
                e_loads(bi_, hrowF, "e_hrowF")
                for tt in range(TB // 128):
                    hr = hrowA[tt]
                    hk = "e_hrowA%d" % tt
                    for half in range(2):
                        for jj in range(4):
                            kc = half * 4 + jj
                            c.op("pe", (lambda kc, jj, hr: lambda e: e.transpose(bank[0][:, jj * 128:(jj + 1) * 128], hr[:, kc * 128:(kc + 1) * 128], g.ident[:]))(kc, jj, hr),
                                 reads=[hk, "ident"], writes=["bank0"])
                        c.op("act", (lambda half, tt: lambda e: e.activation(out=hT[:, half * 4:(half + 1) * 4, tt * 128:(tt + 1) * 128], in_=bank[0][:, :].rearrange("p (a b) -> p a b", a=4), func=AF.Copy))(half, tt),
                             reads=["bank0"], writes=["e_hT"])
                if bi_ + 1 < len(blocks):
                    e_loads(bi_ + 1, hrowA, "e_hrowA")
                stg = g.cfg.get("stages", "abcdef")
                def proj(oc, bk, bkk):
                    for kc in range(8):
                        c.op("pe", (lambda kc: lambda e: e.matmul(bk[:, 0:TB], win[:, kc, oc * 128:(oc + 1) * 128], hT[:, kc, :], start=(kc == 0), stop=(kc == 7)))(kc),
                             reads=["win", "e_hT"], writes=[bkk])
                for cc in range(4 if "b" in stg else 0):
                    if "1" in stg or "c" in stg:
                        proj(cc, bank[1], "bank1")
                        c.op("act", (lambda cc: lambda e: e.activation(out=gb[:, cc, :], in_=bank[1][:, 0:TB], func=AF.Copy))(cc), reads=["bank1"], writes=["e_gb"])
                    if "2" in stg or "c" in stg:
                        proj(4 + cc, bank[2], "bank2")
                        c.op("act", lambda e: e.activation(out=gct[:], in_=bank[2][:, 0:TB], func=AF.Copy), reads=["bank2"], writes=["e_gct"])
                    if "3" in stg or "c" in stg:
                        proj(8 + cc, bank[1], "bank1")
                        if tb == 0:
                            c.op("dve", (lambda cc: lambda e: e.memset(vb[:, cc, 0:2], 0.0))(cc), writes=["e_vh%d" % cc], reads=["e_vb%d" % cc])
                        c.op("dve", (lambda cc: lambda e: e.tensor_tensor(vb[:, cc, 2:TB + 2], gct[:], bank[1][:, 0:TB], ALU.mult))(cc), reads=["bank1", "e_gct"], writes=["e_vb%d" % cc])
                    if "4" in stg or "d" in stg:
                        proj(12 + cc, bank[2], "bank2")
                        c.op("dve", (lambda cc: lambda e: e.tensor_copy(u32[:, cc, :], bank[2][:, 0:TB]))(cc), reads=["bank2"], writes=["e_u32"])
                        c.op("act", (lambda cc: lambda e: e.activation(out=uT[:, cc, :], in_=u32[:, cc, :], func=AF.Copy))(cc), reads=["e_u32"], writes=["e_uT"])
                    if "c" not in stg:
                        continue
                    vk = ["e_vb%d" % cc, "e_vh%d" % cc]
                    c.op("dve", (lambda cc: lambda e: e.tensor_scalar_mul(tt_[:], vb[:, cc, 0:TB], cw[:, cc, 0:1]))(cc), reads=vk + ["small"], writes=["e_tt"])
                    c.op("dve", (lambda cc: lambda e: e.scalar_tensor_tensor(tt_[:], vb[:, cc, 1:TB + 1], cw[:, cc, 1:2], tt_[:], ALU.mult, ALU.add))(cc), reads=vk + ["small", "e_tt"], writes=["e_tt"])
                    c.op("dve", (lambda cc: lambda e: e.scalar_tensor_tensor(tt_[:], vb[:, cc, 2:TB + 2], cw[:, cc, 2:3], tt_[:], ALU.mult, ALU.add))(cc), reads=vk + ["small", "e_tt"], writes=["e_tt"])
                    c.op("dve", (lambda cc: lambda e: e.tensor_tensor(ycT[:, cc, :], tt_[:], gb[:, cc, :], ALU.mult))(cc), reads=["e_tt", "e_gb"], writes=["e_ycT"])
                    c.op("dve", (lambda cc: lambda e: e.tensor_copy(vb[:, cc, 0:2], vb[:, cc, TB:TB + 2]))(cc), reads=vk, writes=["e_vh%d" % cc])
                for j in range(16 if "d" in stg else 0):
                    cc = j // 4
                    jb = j % 2
                    cj, sj = g.ctab[:, j, 0:TB], g.stab[:, j, 0:TB]
                    Er, Ei = g.ctab[:, j, TB:TB + 1], g.stab[:, j, TB:TB + 1]
                    br, bi_ = sbr[jb], sbi[jb]
                    K = "e_s%d_" % jb
                    c.op("pe", (lambda j, cc: lambda e: e.matmul(bank[3][:, 0:TB], g.lBre[:, j, :], uT[:, cc, :], start=True, stop=True))(j, cc), reads=["lBC", "e_uT"], writes=["bank3"])
                    c.op("pe", (lambda j, cc: lambda e: e.matmul(bank[4][:, 0:TB], g.lBim[:, j, :], uT[:, cc, :], start=True, stop=True))(j, cc), reads=["lBC", "e_uT"], writes=["bank4"])
                    c.op("act", (lambda br: lambda e: e.activation(out=br[:], in_=bank[3][:, 0:TB], func=AF.Copy))(br), reads=["bank3"], writes=[K + "br"])
                    c.op("act", (lambda bi_: lambda e: e.activation(out=bi_[:], in_=bank[4][:, 0:TB], func=AF.Copy))(bi_), reads=["bank4"], writes=[K + "bi"])
                    t1, t2, t3, t4 = st1[jb], st2[jb], st3[jb], st4[jb]
                    c.op("dve", (lambda t1, br, cj: lambda e: e.tensor_tensor(t1[:], br[:], cj, ALU.mult))(t1, br, cj), reads=[K + "br", "P"], writes=[K + "t1"])
                    c.op("pool", (lambda t2, bi_, sj: lambda e: e.tensor_tensor(t2[:], bi_[:], sj, ALU.mult))(t2, bi_, sj), reads=[K + "bi", "P"], writes=[K + "t2"])
                    c.op("dve", (lambda t3, bi_, cj: lambda e: e.tensor_tensor(t3[:], bi_[:], cj, ALU.mult))(t3, bi_, cj), reads=[K + "bi", "P"], writes=[K + "t3"])
                    c.op("pool", (lambda t4, br, sj: lambda e: e.tensor_tensor(t4[:], br[:], sj, ALU.mult))(t4, br, sj), reads=[K + "br", "P"], writes=[K + "t4"])
                    c.op("dve", (lambda t1, t2: lambda e: e.tensor_tensor(t1[:], t1[:], t2[:], ALU.add))(t1, t2), reads=[K + "t1", K + "t2"], writes=[K + "t1"])
                    c.op("dve", (lambda t3, t4: lambda e: e.tensor_tensor(t3[:], t3[:], t4[:], ALU.subtract))(t3, t4), reads=[K + "t3", K + "t4"], writes=[K + "t3"])
                    vr, vi = svr[jb], svi[jb]
                    rb = g.rho[:, j:j + 1].to_broadcast([128, TB])
                    ir = 0.0 if tb == 0 else Xst[:, j, 0:1]
                    ii = 0.0 if tb == 0 else Xst[:, j, 1:2]
                    c.op("dve", (lambda vr, rb, t1, ir: lambda e: e.tensor_tensor_scan(vr[:], rb, t1[:], ir, ALU.mult, ALU.add))(vr, rb, t1, ir), reads=[K + "t1", "P", "e_Xst"], writes=[K + "vr"])
                    c.op("dve", (lambda vi, rb, t3, ii: lambda e: e.tensor_tensor_scan(vi[:], rb, t3[:], ii, ALU.mult, ALU.add))(vi, rb, t3, ii), reads=[K + "t3", "P", "e_Xst"], writes=[K + "vi"])
                    c.op("dve", (lambda vi, Ei, j: lambda e: e.tensor_scalar_mul(Xtmp[:, j, 0:1], vi[:, TB - 1:TB], Ei))(vi, Ei, j), reads=[K + "vi", "P"], writes=["e_Xtmp"])
                    c.op("dve", (lambda vr, Ei, j: lambda e: e.tensor_scalar_mul(Xtmp[:, j, 1:2], vr[:, TB - 1:TB], Ei))(vr, Ei, j), reads=[K + "vr", "P", "e_Xtmp"], writes=["e_Xtmp"])
                    c.op("dve", (lambda vr, Er, j: lambda e: e.scalar_tensor_tensor(Xst[:, j, 0:1], vr[:, TB - 1:TB], Er, Xtmp[:, j, 0:1], ALU.mult, ALU.subtract))(vr, Er, j), reads=[K + "vr", "P", "e_Xtmp"], writes=["e_Xst"])
                    c.op("dve", (lambda vi, Er, j: lambda e: e.scalar_tensor_tensor(Xst[:, j, 1:2], vi[:, TB - 1:TB], Er, Xtmp[:, j, 1:2], ALU.mult, ALU.add))(vi, Er, j), reads=[K + "vi", "P", "e_Xtmp", "e_Xst"], writes=["e_Xst"])
                    p1, p2, p3, p4 = t2, t4, sp3[jb], sp4[jb]
                    c.op("pool", (lambda p1, vr, cj: lambda e: e.tensor_tensor(p1[:], vr[:], cj, ALU.mult))(p1, vr, cj), reads=[K + "vr", "P", K + "t2", K + "t1"], writes=[K + "t2"])
                    c.op("pool", (lambda p2, vi, sj: lambda e: e.tensor_tensor(p2[:], vi[:], sj, ALU.mult))(p2, vi, sj), reads=[K + "vi", "P", K + "t4", K + "t3"], writes=[K + "t4"])
                    c.op("pool", (lambda p3, vi, cj: lambda e: e.tensor_tensor(p3[:], vi[:], cj, ALU.mult))(p3, vi, cj), reads=[K + "vi", "P"], writes=[K + "p3"])
                    c.op("pool", (lambda p4, vr, sj: lambda e: e.tensor_tensor(p4[:], vr[:], sj, ALU.mult))(p4, vr, sj), reads=[K + "vr", "P"], writes=[K + "p4"])
                    xr_b, xi_b = xbf[jb][0], xbf[jb][1]
                    c.op("dve", (lambda xr_b, p1, p2: lambda e: e.tensor_tensor(xr_b[:], p1[:], p2[:], ALU.subtract))(xr_b, p1, p2), reads=[K + "t2", K + "t4"], writes=[K + "xr"])
                    c.op("dve", (lambda xi_b, p3, p4: lambda e: e.tensor_tensor(xi_b[:], p3[:], p4[:], ALU.add))(xi_b, p3, p4), reads=[K + "p3", K + "p4"], writes=[K + "xi"])
                    c.op("pe", (lambda j, xr_b: lambda e: e.matmul(bank[5][:, 0:TB], g.lCre[:, j, :], xr_b[:], start=(j % 4 == 0), stop=False))(j, xr_b), reads=["lBC", K + "xr"], writes=["bank5"])
                    c.op("pe", (lambda j, xi_b: lambda e: e.matmul(bank[5][:, 0:TB], g.lCim[:, j, :], xi_b[:], start=False, stop=(j % 4 == 3)))(j, xi_b), reads=["lBC", K + "xi"], writes=["bank5"])
                    if j % 4 == 3:
                        c.op("dve", (lambda cc: lambda e: e.scalar_tensor_tensor(yt[:], u32[:, cc, :], dsk[:, cc:cc + 1], bank[5][:, 0:TB], ALU.mult, ALU.add))(cc), reads=["bank5", "e_u32", "small"], writes=["e_yt"])
                        c.op("act", lambda e: e.activation(out=tt_[:], in_=yt[:], func=AF.Square), reads=["e_yt"], writes=["e_tt"])
                        c.op("dve", lambda e: e.tensor_scalar(tt_[:], tt_[:], 0.044715, 1.0, ALU.mult, ALU.add), reads=["e_tt"], writes=["e_tt"])
                        c.op("dve", lambda e: e.tensor_tensor(tt_[:], tt_[:], yt[:], ALU.mult), reads=["e_tt", "e_yt"], writes=["e_tt"])
                        c.op("act", lambda e: e.activation(out=tt_[:], in_=tt_[:], func=AF.Sigmoid, scale=1.5957691216057308), reads=["e_tt"], writes=["e_tt"])
                        c.op("dve", (lambda cc: lambda e: e.tensor_tensor(z32[:, cc, :], tt_[:], yt[:], ALU.mult))(cc), reads=["e_tt", "e_yt"], writes=["e_z32"])
                        c.op("act", (lambda cc: lambda e: e.activation(out=zT[:, cc, :], in_=z32[:, cc, :], func=AF.Copy))(cc), reads=["e_z32"], writes=["e_zT"])
                for oc in range(4 if "e" in stg else 0):
                    for kc in range(4):
                        c.op("pe", (lambda oc, kc: lambda e: e.matmul(bank[6][:, 0:TB], wglu[:, kc, oc * 128:(oc + 1) * 128], zT[:, kc, :], start=(kc == 0), stop=(kc == 3)))(oc, kc), reads=["wglu", "e_zT"], writes=["bank6"])
                    c.op("act", (lambda oc: lambda e: e.activation(out=tt_[:], in_=bank[6][:, 0:TB], func=AF.Sigmoid, bias=bglu[:, oc:oc + 1], scale=1.0))(oc), reads=["bank6", "small"], writes=["e_tt"])
                    c.op("dve", (lambda oc: lambda e: e.tensor_tensor(ycT[:, 4 + oc, :], tt_[:], z32[:, oc, :], ALU.mult))(oc), reads=["e_tt", "e_z32"], writes=["e_ycT"])
                for tt in range(TB // 128 if "f" in stg else 0):
                    hr = hrowF[tt]
                    hk = "e_hrowF%d" % tt
                    ob = outt[tt % 2]
                    ok = "e_out%d" % (tt % 2)
                    for n in range(2):
                        for kc in range(8):
                            c.op("pe", (lambda tt, n, kc: lambda e: e.matmul(bank[6 + n][:, :], ycT[:, kc, tt * 128:(tt + 1) * 128], wout[:, kc, n * 512:(n + 1) * 512], start=(kc == 0), stop=(kc == 7)))(tt, n, kc),
                                 reads=["e_ycT", "wout"], writes=["bank%d" % (6 + n)])
                        c.op("dve", (lambda n, hr: lambda e: e.scalar_tensor_tensor(hr[:, n * 512:(n + 1) * 512], hr[:, n * 512:(n + 1) * 512], float(DN_ALPHA), bank[6 + n][:, :], ALU.mult, ALU.add))(n, hr),
                             reads=["bank%d" % (6 + n), hk], writes=[hk])
                    layer_norm_tile(c, g, hr, hk, gam, bet, "e_gam", ob, ok, "e")
                    c.dma("sp", (lambda tt, ob, t0: lambda e: e.dma_start(out=hdst[t0 + tt * 128:t0 + (tt + 1) * 128, :], in_=ob[:]))(tt, ob, t0), reads=[ok], cowrites=["hdst"])
        if "f" not in g.cfg.get("stages", "abcdef"):
            c.dma("sp", lambda e: e.dma_start(out=hdst[:, :], in_=hsrc[:, :]), writes=["hdst"])


VW = NH * 65
RW = 2 * D + VW


def odd_phase(c, g, W, layer, mi, SEQ, NSEQ, hsrc, hdst):
    bank = g.bank
    TT = SEQ * NSEQ
    NT = TT // 128
    qkv_d = g.qkv_d
    O_d = g.O_d
    stg = g.cfg.get("stages", "ABC")
    with c.phase():
        wqkv = c.sb("o_wqkv", [128, 8, 3 * D], BF16)
        hrow = [c.sb("o_hrow%d" % i, [128, D], F32) for i in range(2)]
        hT = c.sb("o_hT", [128, 8, 128], BF16)
        cs = [c.sb("o_cs%d" % i, [128, 2, 512], F32) for i in range(2)]
        qk32s = [c.sb("o_qk32_%d" % i, [128, 2, D], F32) for i in range(2)]
        tq = [c.sb("o_tq%d" % i, [128, 512], F32) for i in range(2)]
        tk = [c.sb("o_tk%d" % i, [128, 512], F32) for i in range(4)]
        rows = [c.sb("o_rows%d" % i, [128, RW], BF16) for i in range(2)]
        for kc in range(8):
            for part in range(3):
                c.dma("pool", (lambda kc, part: lambda e: e.dma_start(out=wqkv[:, kc, part * D:(part + 1) * D], in_=W["att_w_qkv"][mi, kc * 128:(kc + 1) * 128, part * D:(part + 1) * D], max_dma_last_dim=4096))(kc, part), cowrites=["wqkv"])
        for i in range(2):
            c.op("pool", (lambda i: lambda e: e.memset(rows[i][:, 2 * D:RW], 1.0))(i), writes=["o_rows%d" % i])
        def a_loads(i):
            b = i % 2
            hr, hk = hrow[b], "o_hrow%d" % b
            pos0 = (i * 128) % SEQ
            c.dma("sp", lambda e: e.dma_start(out=hr[:], in_=hsrc[i * 128:(i + 1) * 128, :]), writes=[hk])
            c.dma("sp", lambda e: e.dma_start(out=cs[b][:, 0, :], in_=W["c_cos16"][pos0:pos0 + 128, :]), cowrites=["o_cs%d" % b])
            c.dma("sp", lambda e: e.dma_start(out=cs[b][:, 1, :], in_=W["c_sin16"][pos0:pos0 + 128, :]), cowrites=["o_cs%d" % b])

        if "A" in stg:
            a_loads(0)
        for i in range(NT if "A" in stg else 0):
            b = i % 2
            hr, hk = hrow[b], "o_hrow%d" % b
            if i + 1 < NT:
                a_loads(i + 1)
            transpose_rows_to_T(c, g, hr, hk, hT, "o_hT", bank[0], "bank0", fp32_in=True, evac="act")
            for n in range(6):
                for kc in range(8):
                    c.op("pe", (lambda n, kc: lambda e: e.matmul(bank[2 + n][:, :], hT[:, kc, :], wqkv[:, kc, n * 512:(n + 1) * 512], start=(kc == 0), stop=(kc == 7)))(n, kc),
                         reads=["o_hT", "wqkv"], writes=["bank%d" % (2 + n)])
            R = rows[b]
            Rk = "o_rows%d" % b
            qk32 = qk32s[b]
            for n in range(4):
                c.op("act", (lambda n, qk32: lambda e: e.activation(out=qk32[:, n // 2, (n % 2) * 512:(n % 2 + 1) * 512], in_=bank[2 + n][:, :], func=AF.Copy))(n, qk32),
                     reads=["bank%d" % (2 + n)], writes=["o_qk32_%d_%d" % (b, n // 2)])
            for n in range(2):
                c.op("act", (lambda n, R: lambda e: e.activation(out=R[:, 2 * D:RW].rearrange("p (h e) -> p h e", e=65)[:, n * 8:(n + 1) * 8, 0:64], in_=bank[6 + n][:, :].rearrange("p (h e) -> p h e", e=64), func=AF.Copy))(n, R),
                     reads=["bank%d" % (6 + n), Rk], writes=[Rk + "v"])
            cosv = cs[b][:, 0, :].rearrange("p (h e) -> p h e", e=32)
            sinv = cs[b][:, 1, :].rearrange("p (h e) -> p h e", e=32)
            csk = "o_cs%d" % b
            x3 = qk32[:, 0, :].rearrange("p (h e) -> p h e", e=64)
            o3 = R[:, 0:D].rearrange("p (h e) -> p h e", e=64)
            t1 = tq[0][:].rearrange("p (h e) -> p h e", e=32)
            t2 = tq[1][:].rearrange("p (h e) -> p h e", e=32)
            xk = "o_qk32_%d_0" % b
            c.op("dve", (lambda t1, x3, cosv: lambda e: e.tensor_tensor(t1, x3[:, :, 0:32], cosv, ALU.mult))(t1, x3, cosv), reads=[xk, csk], writes=["o_tqa"])
            c.op("dve", (lambda t2, x3, sinv: lambda e: e.tensor_tensor(t2, x3[:, :, 32:64], sinv, ALU.mult))(t2, x3, sinv), reads=[xk, csk], writes=["o_tqb"])
            c.op("dve", (lambda o3, t1, t2: lambda e: e.tensor_tensor(o3[:, :, 0:32], t1, t2, ALU.subtract))(o3, t1, t2), reads=["o_tqa", "o_tqb"], writes=[Rk + "q"])
            c.op("dve", (lambda t1, x3, cosv: lambda e: e.tensor_tensor(t1, x3[:, :, 32:64], cosv, ALU.mult))(t1, x3, cosv), reads=[xk, csk, Rk + "q"], writes=["o_tqa"])
            c.op("dve", (lambda t2, x3, sinv: lambda e: e.tensor_tensor(t2, x3[:, :, 0:32], sinv, ALU.mult))(t2, x3, sinv), reads=[xk, csk, Rk + "q"], writes=["o_tqb"])
            c.op("dve", (lambda o3, t1, t2: lambda e: e.tensor_tensor(o3[:, :, 32:64], t1, t2, ALU.add))(o3, t1, t2), reads=["o_tqa", "o_tqb"], writes=[Rk + "q"])
            x3 = qk32[:, 1, :].rearrange("p (h e) -> p h e", e=64)
            o3 = R[:, D:2 * D].rearrange("p (h e) -> p h e", e=64)
            k1, k2, k3, k4 = [tk[q_][:].rearrange("p (h e) -> p h e", e=32) for q_ in range(4)]
            xk = "o_qk32_%d_1" % b
            c.op("pool", (lambda k1, x3, cosv: lambda e: e.tensor_tensor(k1, x3[:, :, 0:32], cosv, ALU.mult))(k1, x3, cosv), reads=[xk, csk], writes=["o_tk1"])
            c.op("pool", (lambda k2, x3, sinv: lambda e: e.tensor_tensor(k2, x3[:, :, 32:64], sinv, ALU.mult))(k2, x3, sinv), reads=[xk, csk], writes=["o_tk2"])
            c.op("pool", (lambda k3, x3, cosv: lambda e: e.tensor_tensor(k3, x3[:, :, 32:64], cosv, ALU.mult))(k3, x3, cosv), reads=[xk, csk], writes=["o_tk3"])
            c.op("pool", (lambda k4, x3, sinv: lambda e: e.tensor_tensor(k4, x3[:, :, 0:32], sinv, ALU.mult))(k4, x3, sinv), reads=[xk, csk], writes=["o_tk4"])
            c.op("dve", (lambda o3, k1, k2: lambda e: e.tensor_tensor(o3[:, :, 0:32], k1, k2, ALU.subtract))(o3, k1, k2), reads=["o_tk1", "o_tk2"], writes=[Rk + "k"])
            c.op("dve", (lambda o3, k3, k4: lambda e: e.tensor_tensor(o3[:, :, 32:64], k3, k4, ALU.add))(o3, k3, k4), reads=["o_tk3", "o_tk4", Rk + "k"], writes=[Rk + "k"])
            c.dma("sp", (lambda i, R: lambda e: e.dma_start(out=qkv_d[i * 128:(i + 1) * 128, :], in_=R[:, :]))(i, R),
                  reads=[Rk + "q", Rk + "k", Rk + "v", Rk], cowrites=["qkv_d"])
            for sfx in ("q", "k", "v"):
                c.readers.setdefault(Rk + sfx, []).append(c.last_w["qkv_d"][-1])

    with c.phase():
        R3 = [c.sb("o_R%d" % i, [128, RW], BF16) for i in range(3)]
        KT = [c.sb("o_KT%d" % i, [128, 8, 128], BF16) for i in range(2)]
        QTz = [[c.sb("o_QT%d_%d" % (j, i), [128, 8, 128], BF16) for i in range(2)] for j in range(2)]
        for j in range(2):
            for i in range(2):
                c.op("pool", (lambda i, j: lambda e: e.memset(QTz[j][i][:], 0.0))(i, j), writes=["o_QT%d" % j])
        PT = [c.sb("o_PT%d" % i, [128, 512], BF16) for i in range(3)]
        mcur = c.sb("o_mcur", [128, 512], BF16)
        mprev = c.sb("o_mprev", [128, 512], BF16)
        osb = [c.sb("o_osb%d" % i, [128, VW], F32) for i in range(2)]
        c.dma("pool", lambda e: e.dma_start(out=mcur[:], in_=W["c_mcur"][:, :]), writes=["o_mcur"])
        c.dma("pool", lambda e: e.dma_start(out=mprev[:], in_=W["c_mprev"][:, :]), writes=["o_mprev"])
        hb_ = [(0, 7), (7, 14), (14, 16)]
        ulist = []
        for s_ in range(NSEQ if "B" in stg else 0):
            for pi, (win_, dil) in enumerate(PATTERNS):
                nb = SEQ // (128 * dil)
                for r in range(dil):
                    for n in range(nb):
                        ulist.append((s_, pi, dil, r, n))
        b0 = bank[0][:].bitcast(BF16)
        b1 = bank[1][:].bitcast(BF16)

        def views(u):
            s_, pi, dil, r, n = ulist[u]
            qv = qkv_d[s_ * SEQ:(s_ + 1) * SEQ, :].rearrange("(m d) c -> d m c", d=dil)
            ov = O_d[pi, s_ * SEQ:(s_ + 1) * SEQ, :].rearrange("(m d) c -> d m c", d=dil)
            return qv, ov

        def prologue(u):
            s_, pi, dil, r, n = ulist[u]
            qv, ov = views(u)
            R, Rk = R3[u % 3], "o_R%d" % (u % 3)
            kt, ktk = KT[u % 2], "o_KT%d" % (u % 2)
            qt, qtk = QTz[u % 2], "o_QT%d" % (u % 2)
            c.dma("sp", lambda e: e.dma_start(out=R[:, :], in_=qv[r, n * 128:(n + 1) * 128, :]), reads=["qkv_d"], writes=[Rk])
            for pr in range(8):
                c.op("pe", (lambda pr: lambda e: e.transpose(b0[:, pr * 128:(pr + 1) * 128], R[:, D + pr * 128:D + (pr + 1) * 128], g.identb[:]))(pr), reads=[Rk, "identb"], writes=["bank0"])
            c.op("act", lambda e: e.activation(out=kt[:].rearrange("p a b -> p (a b)"), in_=b0[:, 0:1024], func=AF.Copy), reads=["bank0"], writes=[ktk])
            for pr in range(8):
                c.op("pe", (lambda pr: lambda e: e.transpose(b1[:, pr * 128:(pr + 1) * 128], R[:, pr * 128:(pr + 1) * 128], g.identb[:]))(pr), reads=[Rk, "identb"], writes=["bank1"])
            c.op("dve", lambda e: e.tensor_copy(qt[0][0:64].rearrange("p a b -> p (a b)"), b1[0:64, 0:1024]), reads=["bank1"], writes=[qtk])
            c.op("dve", lambda e: e.tensor_copy(qt[1][64:128].rearrange("p a b -> p (a b)"), b1[64:128, 0:1024]), reads=["bank1", qtk], writes=[qtk])

        gcount = [0]

        def groups_of(u):
            s_, pi, dil, r, n = ulist[u]
            R, Rk = R3[u % 3], "o_R%d" % (u % 3)
            Rp, Rpk = R3[(u - 1) % 3], "o_R%d" % ((u - 1) % 3)
            kt, ktk = KT[u % 2], "o_KT%d" % (u % 2)
            ktp, ktpk = KT[(u - 1) % 2], "o_KT%d" % ((u - 1) % 2)
            qt, qtk = QTz[u % 2], "o_QT%d" % (u % 2)
            kbs = ([(ktp, ktpk, Rp, Rpk, mprev, "o_mprev")] if n > 0 else []) + [(kt, ktk, R, Rk, mcur, "o_mcur")]
            started = [False, False, False]
            out = []
            for (kT_, kTk, Rv, Rvk, msk, mskk) in kbs:
                for grp in range(4):
                    gi = gcount[0]
                    gcount[0] += 1
                    sb_, sbk = bank[2 + gi % 2], "bank%d" % (2 + gi % 2)
                    pt, ptk = PT[gi % 3], "o_PT%d" % (gi % 3)

                    def S(sb_=sb_, sbk=sbk, pt=pt, ptk=ptk, kT_=kT_, kTk=kTk, msk=msk, mskk=mskk, grp=grp):
                        for hh in range(4):
                            h = grp * 4 + hh
                            pr, hf = h // 2, h % 2
                            c.op("pe", (lambda hh, pr, hf: lambda e: e.matmul(sb_[:, hh * 128:(hh + 1) * 128], kT_[:, pr, :], qt[hf][:, pr, :], start=True, stop=True))(hh, pr, hf),
                                 reads=[kTk, qtk], writes=[sbk])
                        c.op("act", lambda e: e.activation(out=pt[:], in_=sb_[:, :], func=AF.Exp, scale=0.125), reads=[sbk], writes=[ptk])
                        c.op("dve", lambda e: e.tensor_tensor(pt[:], pt[:], msk[:], ALU.mult), reads=[ptk, mskk], writes=[ptk])

                    sts = []
                    for hh in range(4):
                        h = grp * 4 + hh
                        bi = 0 if h < 7 else (1 if h < 14 else 2)
                        sts.append(not started[bi])
                        started[bi] = True

                    def P(pt=pt, ptk=ptk, Rv=Rv, Rvk=Rvk, grp=grp, sts=sts):
                        for hh in range(4):
                            h = grp * 4 + hh
                            bi = 0 if h < 7 else (1 if h < 14 else 2)
                            col = (h - hb_[bi][0]) * 65
                            c.op("pe", (lambda bi, col, hh, h, st: lambda e: e.matmul(bank[4 + bi][:, col:col + 65], pt[:, hh * 128:(hh + 1) * 128], Rv[:, 2 * D + h * 65:2 * D + (h + 1) * 65], start=st, stop=True, skip_group_check=True))(bi, col, hh, h, sts[hh]),
                                 reads=[ptk, Rvk], writes=["bank%d" % (4 + bi)])
                    out.append((S, P))
            return out

        def epilogue(u):
            s_, pi, dil, r, n = ulist[u]
            qv, ov = views(u)
            ob, obk = osb[u % 2], "o_osb%d" % (u % 2)
            for bi, (h0, h1) in enumerate(hb_):
                eng = "act" if bi == 1 else "dve"
                c.op(eng, (lambda bi, h0, h1, eng: lambda e: ecopy(e, eng, ob[:, h0 * 65:h1 * 65], bank[4 + bi][:, 0:(h1 - h0) * 65]))(bi, h0, h1, eng),
                     reads=["bank%d" % (4 + bi)], writes=[obk + "_%d" % bi])
            c.dma("sp", lambda e: e.dma_start(out=ov[r, n * 128:(n + 1) * 128, :], in_=ob[:, :]),
                  reads=[obk + "_0", obk + "_1", obk + "_2"], cowrites=["O_d"])

        NU = len(ulist)
        if NU:
            prologue(0)
        pendingP = None
        for u in range(NU):
            grps = groups_of(u)
            ng = len(grps)
            for gi_, (S, P) in enumerate(grps):
                S()
                if pendingP is not None:
                    pendingP[0]()
                    if pendingP[1] is not None:
                        epilogue(pendingP[1])
                pendingP = (P, u if gi_ == ng - 1 else None)
                if gi_ == ng // 2 and u + 1 < NU:
                    prologue(u + 1)
        if pendingP is not None:
            pendingP[0]()
            epilogue(pendingP[1])

    with c.phase():
        wo = c.sb("o_wo", [128, 8, D], BF16)
        gam = c.sb("o_gam", [128, D], F32)
        bet = c.sb("o_bet", [128, D], F32)
        hrow = [c.sb("o_hrow%d" % i, [128, D], F32) for i in range(2)]
        ot = [[c.sb("o_ot%d_%d" % (i, p), [128, VW], F32) for p in range(3)] for i in range(2)]
        rden = c.sb("o_rden", [128, NH], F32)
        osb2 = c.sb("o_o", [128, D], BF16)
        oT = c.sb("o_oT", [128, 8, 128], BF16)
        outt = [c.sb("o_out%d" % i, [128, D], F32) for i in range(2)]
        for kc in range(8):
            c.dma("pool", (lambda kc: lambda e: e.dma_start(out=wo[:, kc, :], in_=W["att_w_o"][mi, kc * 128:(kc + 1) * 128, :], max_dma_last_dim=4096))(kc), cowrites=["wo"])
        c.dma("sp", lambda e: e.dma_start(out=gam[:], in_=W["ln_mix_g"][layer:layer + 1, :].partition_broadcast(128)), cowrites=["o_gam"])
        c.dma("sp", lambda e: e.dma_start(out=bet[:], in_=W["ln_mix_b"][layer:layer + 1, :].partition_broadcast(128)), cowrites=["o_gam"])
        def c_loads(i):
            b = i % 2
            hr, hk = hrow[b], "o_hrow%d" % b
            c.dma("sp", lambda e: e.dma_start(out=hr[:], in_=hsrc[i * 128:(i + 1) * 128, :]), writes=[hk])
            for p in range(3):
                c.dma("sp", (lambda p: lambda e: e.dma_start(out=ot[b][p][:, :], in_=O_d[p, i * 128:(i + 1) * 128, :]))(p), reads=["O_d"], writes=["o_ot%d_%d" % (b, p)])

        if "C" in stg:
            c_loads(0)
        for i in range(NT if "C" in stg else 0):
            b = i % 2
            hr, hk = hrow[b], "o_hrow%d" % b
            if i + 1 < NT:
                c_loads(i + 1)
            A = ot[b][0]
            Ak = "o_ot%d_0" % b
            c.op("pool", (lambda b, A: lambda e: e.tensor_tensor(A[:], A[:], ot[b][1][:], ALU.add))(b, A), reads=[Ak, "o_ot%d_1" % b], writes=[Ak])
            c.op("dve", (lambda b, A: lambda e: e.tensor_tensor(A[:], A[:], ot[b][2][:], ALU.add))(b, A), reads=[Ak, "o_ot%d_2" % b], writes=[Ak])
            A3 = A[:].rearrange("p (h e) -> p h e", e=65)
            c.op("dve", (lambda A3: lambda e: e.reciprocal(rden[:], A3[:, :, 64]))(A3), reads=[Ak], writes=["o_rden"])
            rb_ = rden[:].unsqueeze(2).to_broadcast([128, NH, 64])
            c.op("dve", (lambda A3, rb_: lambda e: e.tensor_tensor(osb2[:].rearrange("p (h e) -> p h e", e=64), A3[:, :, 0:64], rb_, ALU.mult))(A3, rb_),
                 reads=[Ak, "o_rden"], writes=["o_o0"])
            c.op("pool", lambda e: e.engine_nop(), reads=[], writes=["o_o1"]) if False else None
            bb = bank[0][:].bitcast(BF16)
            for kc in range(8):
                c.op("pe", (lambda kc, bb: lambda e: e.transpose(bb[:, kc * 128:(kc + 1) * 128], osb2[:, kc * 128:(kc + 1) * 128], g.identb[:]))(kc, bb), reads=["o_o0", "identb"], writes=["bank0"])
            c.op("act", (lambda bb: lambda e: e.activation(out=oT[:].rearrange("p a b -> p (a b)"), in_=bb[:, 0:1024], func=AF.Copy))(bb), reads=["bank0"], writes=["o_oT"])
            for n in range(2):
                for kc in range(8):
                    c.op("pe", (lambda n, kc: lambda e: e.matmul(bank[6 + n][:, :], oT[:, kc, :], wo[:, kc, n * 512:(n + 1) * 512], start=(kc == 0), stop=(kc == 7)))(n, kc),
                         reads=["o_oT", "wo"], writes=["bank%d" % (6 + n)])
                c.op("dve", (lambda n, hr: lambda e: e.scalar_tensor_tensor(hr[:, n * 512:(n + 1) * 512], hr[:, n * 512:(n + 1) * 512], float(DN_ALPHA), bank[6 + n][:, :], ALU.mult, ALU.add))(n, hr),
                     reads=["bank%d" % (6 + n), hk], writes=[hk])
            layer_norm_tile(c, g, hr, hk, gam, bet, "o_gam", outt[b], "o_out%d" % b, "o")
            c.dma("sp", (lambda i, b: lambda e: e.dma_start(out=hdst[i * 128:(i + 1) * 128, :], in_=outt[b][:]))(i, b), reads=["o_out%d" % b], cowrites=["hdst"])
        if "C" not in stg:
            c.dma("sp", lambda e: e.dma_start(out=hdst[:, :], in_=hsrc[:, :]), writes=["hdst"])


NCORES = 4
FULL_SEQ = 8192
FULL_BATCH = 4
RENAME = {"expert_w_gu": "w_gu", "expert_b_gu": "b_gu", "expert_w_down": "w_dn", "expert_b_down": "b_dn"}
_CACHE = {}


def full_plan():
    plan = []
    for layer in range(DEPTH):
        plan.append(("even" if layer % 2 == 0 else "odd", layer, layer // 2))
        plan.append(("moe", layer, layer // 2))
    return plan


def kernel(**inputs):
    nseq = FULL_BATCH // NCORES
    tt = nseq * FULL_SEQ
    nl = {}
    wmap = {}
    for name, arr in inputs.items():
        if name == "x":
            continue
        kname = RENAME.get(name, name)
        wmap[kname] = np.ascontiguousarray(arr, dtype=np.float32)
        nl[kname] = arr.shape[0]
    cap = (tt * TOPK // NE) * 5 // 4
    cap = (cap + 127) // 128 * 128
    cfg = dict(SEQ=FULL_SEQ, NSEQ=nseq, C=cap, plan=full_plan(), nl=nl)
    key = (FULL_SEQ, nseq, cap)
    if key not in _CACHE:
        _CACHE[key] = build_program(cfg)
    nc, cst, _ = _CACHE[key]
    x = np.ascontiguousarray(inputs["x"], dtype=np.float32).reshape(NCORES, tt, D)
    in_maps = []
    for ci in range(NCORES):
        m = {"x": x[ci]}
        m.update(wmap)
        m.update(cst)
        in_maps.append(m)
    res = run_bass_kernel_spmd(nc, in_maps, core_ids=list(range(NCORES)))
    out = np.stack([res.results[ci]["out"] for ci in range(NCORES)], axis=0)
    return out.reshape(FULL_BATCH, FULL_SEQ, D).astype(np.float32)
```
